# Optimizing a Trainium2 kernel written in Bass

```python
import math
import jax, jax.numpy as jnp
from jax import lax
import numpy as np

D_MODEL = 1024
BATCH = 16
SEQ = 2048
DEPTH = 2

HEAD_DIM = 64
DIL_GROUPS = ((128, 1), (512, 4), (2048, 16))
DIL_HEADS_PER_GROUP = 4
DIL_HEADS = DIL_HEADS_PER_GROUP * len(DIL_GROUPS)
DIL_WIDTH = DIL_HEADS * HEAD_DIM
DIL_OUT = DIL_HEADS_PER_GROUP * HEAD_DIM
HYENA_WIDTH = 512
HYENA_ORDER = 2
HYENA_BANDS = 16
HYENA_EMB = 2 * HYENA_BANDS + 1
HYENA_HIDDEN = 64
HYENA_DECAY_MIN = -math.log(1e-2) / 1.5
HYENA_DECAY_MAX = -math.log(1e-2) / 0.3
SHORT_CONV = 3
HY_IN = (HYENA_ORDER + 1) * HYENA_WIDTH
SWA_Q_HEADS = 8
SWA_KV_HEADS = 2
SWA_RADIUS = 128
SWA_BLOCK = 128
SWA_WIDTH = SWA_Q_HEADS * HEAD_DIM
SWA_IN = (SWA_Q_HEADS + 2 * SWA_KV_HEADS) * HEAD_DIM
N_BRANCHES = 3
GATE_IN = N_BRANCHES * D_MODEL
IN_SPLITS = (3 * DIL_WIDTH, 3 * DIL_WIDTH + HY_IN, 3 * DIL_WIDTH + HY_IN + SWA_IN)
IN_WIDTH = 3 * DIL_WIDTH + HY_IN + SWA_IN + GATE_IN
ROPE_THETA = 10000.0
PEER_HEADS = 8
PEER_KEYS = 128
PEER_EXPERTS = PEER_KEYS * PEER_KEYS
PEER_QUERY = 256
PEER_TOPK = 16
PEER_CHUNK = 128
ALPHA = (2 * DEPTH) ** 0.25
BETA = (8 * DEPTH) ** -0.25
LN_EPS = 1e-5
NEG_INF = -1e30

kernel_name = "hybrid_dilated_hyena_swa_peer_encoder"


def layer_norm(x, g, b):
    xf = x.astype(jnp.float32)
    mu = xf.mean(-1, keepdims=True)
    var = jnp.square(xf - mu).mean(-1, keepdims=True)
    return ((xf - mu) * lax.rsqrt(var + LN_EPS) * g.astype(jnp.float32) + b.astype(jnp.float32)).astype(x.dtype)


def rope_tables(seq):
    pos = jnp.arange(seq, dtype=jnp.float32)
    inv = ROPE_THETA ** (-jnp.arange(0, HEAD_DIM, 2, dtype=jnp.float32) / HEAD_DIM)
    ang = pos[:, None] * inv[None, :]
    return jnp.cos(ang), jnp.sin(ang)


def apply_rope(t, cos, sin):
    t1, t2 = jnp.split(t.astype(jnp.float32), 2, axis=-1)
    return jnp.concatenate([t1 * cos - t2 * sin, t2 * cos + t1 * sin], axis=-1).astype(t.dtype)


def to_heads(t, n):
    b, s, _ = t.shape
    return t.reshape(b, s, n, HEAD_DIM).transpose(0, 2, 1, 3)


def banded_attention(q, k, v, radius, block, sink=None):
    b, hk, g, L, hd = q.shape
    nb = -(-L // block)
    lp = nb * block
    q = jnp.pad(q, ((0, 0), (0, 0), (0, 0), (0, lp - L), (0, 0)))
    kv_pad = ((0, 0), (0, 0), (block, block + lp - L), (0, 0))
    kb = jnp.pad(k, kv_pad).reshape(b, hk, nb + 2, block, hd)
    vb = jnp.pad(v, kv_pad).reshape(b, hk, nb + 2, block, hd)
    kw = jnp.concatenate([kb[:, :, :-2], kb[:, :, 1:-1], kb[:, :, 2:]], axis=3)
    vw = jnp.concatenate([vb[:, :, :-2], vb[:, :, 1:-1], vb[:, :, 2:]], axis=3)
    qb = q.reshape(b, hk, g, nb, block, hd)
    qpos = jnp.arange(lp).reshape(nb, block)
    kpos = jnp.arange(nb)[:, None] * block + jnp.arange(-block, 2 * block)[None, :]
    mask = ((jnp.abs(qpos[:, :, None] - kpos[:, None, :]) <= radius)
            & (kpos >= 0)[:, None, :] & (kpos < L)[:, None, :])
    s = jnp.einsum('bhgnqd,bhnkd->bhgnqk', qb, kw).astype(jnp.float32) * (hd ** -0.5)
    s = jnp.where(mask, s, NEG_INF)
    m = s.max(-1, keepdims=True)
    if sink is not None:
        sk = sink.astype(jnp.float32)[None, :, :, None, None, None]
        m = jnp.maximum(m, sk)
    p = jnp.exp(s - m)
    denom = p.sum(-1, keepdims=True)
    if sink is not None:
        denom = denom + jnp.exp(sk - m)
    out = jnp.einsum('bhgnqk,bhnkd->bhgnqd', (p / denom).astype(v.dtype), vw)
    lse = (m + jnp.log(denom))[..., 0]
    return out.reshape(b, hk, g, lp, hd)[:, :, :, :L], lse.reshape(b, hk, g, lp)[..., :L]


def dilated_attention(q, k, v, dilation, radius):
    b, h, s, hd = q.shape
    ls = s // dilation

    def split(t):
        return t.reshape(b, h, ls, dilation, hd).transpose(0, 1, 3, 2, 4).reshape(b, h * dilation, ls, hd)

    out, lse = banded_attention(split(q)[:, :, None], split(k), split(v), radius, radius)
    out = out[:, :, 0].reshape(b, h, dilation, ls, hd).transpose(0, 1, 3, 2, 4).reshape(b, h, s, hd)
    lse = lse[:, :, 0].reshape(b, h, dilation, ls).transpose(0, 1, 3, 2).reshape(b, h, s)
    return out, lse


def short_conv(t, w, bias):
    half = SHORT_CONV // 2
    s = t.shape[1]
    tp = jnp.pad(t, ((0, 0), (half, half), (0, 0)))
    out = bias
    for i in range(SHORT_CONV):
        out = out + tp[:, i:i + s] * w[i]
    return out


def hyena_filter_spectrum(L, w1, b1, w2, b2, w3, freq, log_decay):
    f32 = jnp.float32
    idx = jnp.arange(L, dtype=f32)
    t = idx / max(L - 1, 1)
    w = 2.0 * math.pi * idx / L
    bands = jnp.linspace(1e-4, HYENA_BANDS - 1, HYENA_BANDS, dtype=f32)
    ang = w[:, None] * bands[None, :]
    z = jnp.concatenate([t[:, None], jnp.cos(ang), -jnp.sin(ang)], axis=-1)
    freq = freq.astype(f32)
    h = jnp.sin(freq[0] * (z @ w1.astype(f32) + b1.astype(f32)))
    h = jnp.sin(freq[1] * (h @ w2.astype(f32) + b2.astype(f32)))
    h = (h @ w3.astype(f32)) * jnp.exp(-t[:, None] * jnp.exp(log_decay.astype(f32))[None, :])
    h = h.reshape(L, 2, HYENA_ORDER, HYENA_WIDTH)
    filt = jnp.concatenate([h[:, 0], jnp.zeros((1, HYENA_ORDER, HYENA_WIDTH), f32), h[1:, 1][::-1]], axis=0)
    filt = filt * lax.rsqrt(jnp.sum(jnp.square(filt), axis=0, keepdims=True) + 1e-12)
    return jnp.fft.rfft(filt, axis=0)


def fft_conv(z, filt_f):
    L = z.shape[1]
    zf = jnp.fft.rfft(z.astype(jnp.float32), n=2 * L, axis=1)
    return jnp.fft.irfft(zf * filt_f[None], n=2 * L, axis=1)[:, :L].astype(z.dtype)


def hybrid_mixer(u, cos, sin, w_in, conv_w, conv_b, hy_w1, hy_b1, hy_w2, hy_b2, hy_w3, hy_freq,
                 hy_log_decay, hy_bias, attn_sink, w_branch_a, w_branch_b, w_branch_c, w_out):
    b, s, _ = u.shape
    proj = u @ w_in
    qkv_a, hy_in, qkv_c, gate_logits = jnp.split(proj, IN_SPLITS, axis=-1)

    qa, ka, va = [to_heads(t, DIL_HEADS) for t in jnp.split(qkv_a, 3, axis=-1)]
    qa, ka = apply_rope(qa, cos, sin), apply_rope(ka, cos, sin)
    outs, lses = [], []
    for gi, (window, dil) in enumerate(DIL_GROUPS):
        hs = slice(gi * DIL_HEADS_PER_GROUP, (gi + 1) * DIL_HEADS_PER_GROUP)
        o, lse = dilated_attention(qa[:, hs], ka[:, hs], va[:, hs], dil, window // (2 * dil))
        outs.append(o)
        lses.append(lse)
    wts = jax.nn.softmax(jnp.stack(lses, axis=0), axis=0)
    ya = jnp.einsum('gbhs,gbhsd->bshd', wts.astype(u.dtype), jnp.stack(outs, axis=0)).reshape(b, s, DIL_OUT)

    hy = short_conv(hy_in, conv_w, conv_b)
    hy_parts = jnp.split(hy, HYENA_ORDER + 1, axis=-1)
    filt_f = hyena_filter_spectrum(s, hy_w1, hy_b1, hy_w2, hy_b2, hy_w3, hy_freq, hy_log_decay)
    z = hy_parts[0]
    for o in range(HYENA_ORDER):
        z = hy_parts[o + 1] * (fft_conv(z, filt_f[:, o]) + hy_bias[o] * z)
    yb = z

    qc, kc, vc = jnp.split(qkv_c, (SWA_WIDTH, SWA_WIDTH + SWA_KV_HEADS * HEAD_DIM), axis=-1)
    qc = apply_rope(to_heads(qc, SWA_Q_HEADS), cos, sin)
    kc = apply_rope(to_heads(kc, SWA_KV_HEADS), cos, sin)
    vc = to_heads(vc, SWA_KV_HEADS)
    grp = SWA_Q_HEADS // SWA_KV_HEADS
    qc = qc.reshape(b, SWA_KV_HEADS, grp, s, HEAD_DIM)
    oc, _ = banded_attention(qc, kc, vc, SWA_RADIUS, SWA_BLOCK, attn_sink.reshape(SWA_KV_HEADS, grp))
    yc = oc.reshape(b, SWA_Q_HEADS, s, HEAD_DIM).transpose(0, 2, 1, 3).reshape(b, s, SWA_WIDTH)

    ga, gb, gc = jnp.split(jax.nn.sigmoid(gate_logits.astype(jnp.float32)).astype(u.dtype), N_BRANCHES, axis=-1)
    merged = ga * (ya @ w_branch_a) + gb * (yb @ w_branch_b) + gc * (yc @ w_branch_c)
    return merged @ w_out


def peer_ffn(x, w_query, sub_keys, expert_u, expert_v):
    b, s, d = x.shape
    xt = x.reshape(-1, PEER_CHUNK, d)

    def chunk(xc):
        q = (xc @ w_query).reshape(PEER_CHUNK, PEER_HEADS, 2, PEER_QUERY // 2)
        sc = jnp.einsum('thpc,hpnc->thpn', q, sub_keys).astype(jnp.float32)
        s1, i1 = lax.top_k(sc[:, :, 0], PEER_TOPK)
        s2, i2 = lax.top_k(sc[:, :, 1], PEER_TOPK)
        cand = (s1[..., :, None] + s2[..., None, :]).reshape(PEER_CHUNK, PEER_HEADS, PEER_TOPK * PEER_TOPK)
        best, ci = lax.top_k(cand, PEER_TOPK)
        e = (jnp.take_along_axis(i1, ci // PEER_TOPK, axis=-1) * PEER_KEYS
             + jnp.take_along_axis(i2, ci % PEER_TOPK, axis=-1))
        gate = jax.nn.softmax(best, axis=-1)
        act = jax.nn.gelu(jnp.einsum('thkd,td->thk', expert_u[e], xc), approximate=False)
        wgt = (gate * act.astype(jnp.float32)).astype(xc.dtype)
        return jnp.einsum('thk,thkd->td', wgt, expert_v[e])

    return lax.map(chunk, xt).reshape(b, s, d)


def setup_inputs(seed: int = 0) -> dict:
    key = jax.random.key(seed)
    ks = jax.random.split(key, 32)
    f32 = jnp.float32

    def nrm(k, shape, scale):
        return jax.random.normal(k, shape, f32) * scale

    hy_out = 2 * HYENA_ORDER * HYENA_WIDTH
    return {
        "x": nrm(ks[0], (BATCH, SEQ, D_MODEL), 1.0),
        "c": nrm(ks[1], (BATCH, D_MODEL), 1.0),
        "w_ada": nrm(ks[2], (DEPTH, D_MODEL, 6 * D_MODEL), 0.5 * D_MODEL ** -0.5),
        "b_ada": nrm(ks[3], (DEPTH, 6 * D_MODEL), 0.02),
        "w_in": nrm(ks[4], (DEPTH, D_MODEL, IN_WIDTH), D_MODEL ** -0.5),
        "conv_w": nrm(ks[5], (DEPTH, SHORT_CONV, HY_IN), SHORT_CONV ** -0.5),
        "conv_b": nrm(ks[6], (DEPTH, HY_IN), 0.02),
        "hy_w1": nrm(ks[7], (DEPTH, HYENA_EMB, HYENA_HIDDEN), HYENA_EMB ** -0.5),
        "hy_b1": nrm(ks[8], (DEPTH, HYENA_HIDDEN), 0.02),
        "hy_w2": nrm(ks[9], (DEPTH, HYENA_HIDDEN, HYENA_HIDDEN), HYENA_HIDDEN ** -0.5),
        "hy_b2": nrm(ks[10], (DEPTH, HYENA_HIDDEN), 0.02),
        "hy_w3": nrm(ks[11], (DEPTH, HYENA_HIDDEN, hy_out), HYENA_HIDDEN ** -0.5),
        "hy_freq": 1.0 + nrm(ks[12], (DEPTH, 2, HYENA_HIDDEN), 0.02),
        "hy_log_decay": jax.random.uniform(ks[13], (DEPTH, hy_out), f32,
                                           math.log(HYENA_DECAY_MIN), math.log(HYENA_DECAY_MAX)),
        "hy_bias": nrm(ks[14], (DEPTH, HYENA_ORDER, HYENA_WIDTH), 0.5),
        "attn_sink": nrm(ks[15], (DEPTH, SWA_Q_HEADS), 0.5),
        "w_branch_a": nrm(ks[16], (DEPTH, DIL_OUT, D_MODEL), BETA * DIL_OUT ** -0.5),
        "w_branch_b": nrm(ks[17], (DEPTH, HYENA_WIDTH, D_MODEL), BETA * HYENA_WIDTH ** -0.5),
        "w_branch_c": nrm(ks[18], (DEPTH, SWA_WIDTH, D_MODEL), BETA * SWA_WIDTH ** -0.5),
        "w_out": nrm(ks[19], (DEPTH, D_MODEL, D_MODEL), BETA * D_MODEL ** -0.5),
        "ln_g": 1.0 + nrm(ks[20], (DEPTH, 2, D_MODEL), 0.02),
        "ln_b": nrm(ks[21], (DEPTH, 2, D_MODEL), 0.02),
        "peer_wq": nrm(ks[22], (DEPTH, D_MODEL, PEER_HEADS * PEER_QUERY), D_MODEL ** -0.5),
        "peer_keys": nrm(ks[23], (DEPTH, PEER_HEADS, 2, PEER_KEYS, PEER_QUERY // 2), (PEER_QUERY // 2) ** -0.5),
        "peer_u": nrm(ks[24], (DEPTH, PEER_EXPERTS, D_MODEL), D_MODEL ** -0.5),
        "peer_v": nrm(ks[25], (DEPTH, PEER_EXPERTS, D_MODEL), BETA),
    }


def reference(x, c, w_ada, b_ada, w_in, conv_w, conv_b, hy_w1, hy_b1, hy_w2, hy_b2, hy_w3, hy_freq,
              hy_log_decay, hy_bias, attn_sink, w_branch_a, w_branch_b, w_branch_c, w_out, ln_g, ln_b,
              peer_wq, peer_keys, peer_u, peer_v):
    s = x.shape[1]
    cos, sin = rope_tables(s)
    cond = jax.nn.silu(c)
    for l in range(DEPTH):
        ada = cond @ w_ada[l] + b_ada[l]
        sh1, sc1, g1, sh2, sc2, g2 = [a[:, None, :] for a in jnp.split(ada, 6, axis=-1)]
        u = x * (1.0 + sc1) + sh1
        mix = hybrid_mixer(u, cos, sin, w_in[l], conv_w[l], conv_b[l], hy_w1[l], hy_b1[l], hy_w2[l],
                           hy_b2[l], hy_w3[l], hy_freq[l], hy_log_decay[l], hy_bias[l], attn_sink[l],
                           w_branch_a[l], w_branch_b[l], w_branch_c[l], w_out[l])
        x = layer_norm(ALPHA * x + g1 * mix, ln_g[l, 0], ln_b[l, 0])
        u = x * (1.0 + sc2) + sh2
        ffn = peer_ffn(u, peer_wq[l], peer_keys[l], peer_u[l], peer_v[l])
        x = layer_norm(ALPHA * x + g2 * ffn, ln_g[l, 1], ln_b[l, 1])
    return x
```

```python
import numpy as np
import concourse.bass as bass
import concourse.mybir as mybir
from contextlib import ExitStack

F32 = mybir.dt.float32
BF16 = mybir.dt.bfloat16
I32 = mybir.dt.int32
U32 = mybir.dt.uint32
ALU = mybir.AluOpType
AF = mybir.ActivationFunctionType
AX = mybir.AxisListType

ENGS = ("pe", "act", "dve", "pool", "sp")
EPOCH = 12000
NDMASEM = 24
SELF_SKIP = 1 << 30


def _region(ap):
    t = ap.tensor
    name = t.name
    pairs = ap.ap
    off = int(ap.offset)
    sp = str(ap.space)
    if "SB" in sp or "PSUM" in sp:
        pstride = pairs[0][0]
        if pstride == 0:
            pstride = 1 << 40
        p0 = off // pstride if pstride < (1 << 40) else 0
        f0 = off - p0 * pstride if pstride < (1 << 40) else off
        p1 = p0 + pairs[0][1]
        ext = 0
        for st, cn in pairs[1:]:
            ext += abs(st) * (cn - 1)
        if "PSUM" in sp:
            bank = 2048 // mybir.dt.size(ap.dtype)
            f1 = f0 + ext + 1
            return (name, 0, 128, (f0 // bank) * bank, ((f1 + bank - 1) // bank) * bank)
        return (name, p0, p1, f0, f0 + ext + 1)
    ext = 0
    for st, cn in pairs:
        ext += abs(st) * (cn - 1)
    return (name, 0, 1, off, off + ext + 1)


def _ovl(a, b):
    return a[1] < b[2] and b[1] < a[2] and a[3] < b[4] and b[3] < a[4]


def _covers(a, b):
    return a[1] <= b[1] and a[2] >= b[2] and a[3] <= b[3] and a[4] >= b[4]


class Prog:
    def __init__(self, nc):
        self.nc = nc
        self.es = ExitStack()
        self.streams = {e: [] for e in ENGS}
        self.cnt = {e: 0 for e in ENGS}
        self.cur = {}
        self.allsems = []
        for e in ENGS:
            self.cur[e] = self._newsem("pg_" + e)
        self.dsems = [self._newsem("dma%d" % i) for i in range(NDMASEM)]
        self.dcum = [0] * NDMASEM
        self.dnext = 0
        self.waited = {e: {} for e in ENGS}
        self.hist = {}
        self.floor = {}
        self.semobj = {}
        self.ninstr = 0

    def _newsem(self, name):
        s = self.es.enter_context(self.nc.semaphore(name + "_%d" % len(self.allsems)))
        self.allsems.append(s)
        return s

    def _deps(self, reads, writes, eng=None):
        deps = []
        for ap in reads:
            r = _region(ap)
            psum = "PSUM" in str(ap.space)
            for (reg, isw, ev) in self.hist.get(r[0], ()):
                if _ovl(reg, r) and (isw or (psum and ev[0] != eng)):
                    deps.append(ev)
            deps.extend(self.floor.get(r[0], ()))
        for ap in writes:
            r = _region(ap)
            for (reg, isw, ev) in self.hist.get(r[0], ()):
                if _ovl(reg, r):
                    deps.append(ev)
            deps.extend(self.floor.get(r[0], ()))
        return deps

    def _record(self, reads, writes, ev):
        for ap in writes:
            r = _region(ap)
            h = self.hist.setdefault(r[0], [])
            h[:] = [x for x in h if not _covers(r, x[0])]
            h.append((r, True, ev))
            self._trim(r[0])
        for ap in reads:
            r = _region(ap)
            h = self.hist.setdefault(r[0], [])
            rep = False
            for i, (reg, isw, oev) in enumerate(h):
                if (not isw) and reg == r and oev[0] == ev[0] and oev[3] is False:
                    h[i] = (r, False, ev)
                    rep = True
                    break
            if not rep:
                h.append((r, False, ev))
                self._trim(r[0])

    def _trim(self, name):
        h = self.hist[name]
        if len(h) > 96:
            drop = h[:32]
            del h[:32]
            fl = self.floor.setdefault(name, [])
            fl.extend(x[2] for x in drop)
            best = {}
            for ev in fl:
                k = id(ev[1])
                if k not in best or best[k][2] < ev[2]:
                    best[k] = ev
            self.floor[name] = list(best.values())

    def _waits(self, eng, deps, skip_self_pe=True):
        need = {}
        for ev in deps:
            src, sem, val, isdma = ev
            if src == eng and not isdma and eng == "pe":
                continue
            if src == eng and not isdma and sem is self.cur[eng] and self.cnt[eng] - val >= SELF_SKIP:
                continue
            k = id(sem)
            if self.waited[eng].get(k, 0) >= val:
                continue
            if k not in need or need[k][1] < val:
                need[k] = (sem, val)
        out = []
        for k, (sem, val) in need.items():
            self.waited[eng][k] = val
            out.append((sem, val))
        return out

    def op(self, eng, fn, reads=(), writes=()):
        reads = [a for a in reads if a is not None and not isinstance(a, (int, float))]
        deps = self._deps(reads, writes, eng)
        waits = self._waits(eng, deps)
        if self.cnt[eng] >= EPOCH:
            self.cur[eng] = self._newsem("pg_" + eng)
            self.cnt[eng] = 0
        sem = self.cur[eng]
        self.cnt[eng] += 1
        ev = (eng, sem, self.cnt[eng], False)
        self._record(reads, writes, ev)
        self.streams[eng].append((waits, fn, sem, 1))
        self.ninstr += 1

    def dma(self, q, out, in_, **kw):
        deps = self._deps([in_], [out], q)
        waits = self._waits(q, deps)
        k = self.dnext
        self.dnext = (self.dnext + 1) % NDMASEM
        sem = self.dsems[k]
        self.dcum[k] += 16
        ev = (q, sem, self.dcum[k], True)
        self._record([in_], [out], ev)
        self.streams[q].append((waits, lambda e, o=out, i=in_, kw=kw: e.dma_start(out=o, in_=i, **kw), sem, 16))
        self.ninstr += 1

    def barrier(self):
        evs = []
        for e in ENGS:
            if self.cnt[e] > 0:
                evs.append((e, self.cur[e], self.cnt[e], False))
        for k in range(NDMASEM):
            if self.dcum[k] > 0:
                evs.append(("dma", self.dsems[k], self.dcum[k], True))
        for e in ENGS:
            ws = []
            for (src, sem, val, isdma) in evs:
                if src == e and not isdma:
                    continue
                kk = id(sem)
                if self.waited[e].get(kk, 0) >= val:
                    continue
                self.waited[e][kk] = val
                ws.append((sem, val))
            if ws:
                self.streams[e].append((ws, None, None, 0))
        self.hist = {}
        self.floor = {}

    def emit(self):
        nc = self.nc
        streams = self.streams
        self.streams = {e: [] for e in ENGS}

        def run(engobj, lst):
            for (waits, fn, sem, inc) in lst:
                for (s, v) in waits:
                    engobj.wait_ge(s, v)
                if fn is not None:
                    ins = fn(engobj)
                    ins.then_inc(sem, inc)

        with nc.Block() as block:
            @block.tensor
            def _(e):
                run(e, streams["pe"])

            @block.scalar
            def _(e):
                run(e, streams["act"])

            @block.vector
            def _(e):
                run(e, streams["dve"])

            @block.gpsimd
            def _(e):
                run(e, streams["pool"])

            @block.sync
            def _(e):
                run(e, streams["sp"])

    def mm(self, out, lhsT, rhs, start=True, stop=True):
        self.op("pe", lambda e: e.matmul(out, lhsT, rhs, start=start, stop=stop),
                reads=[lhsT, rhs], writes=[out])

    def tr(self, out, in_, ident):
        self.op("pe", lambda e: e.transpose(out, in_, ident), reads=[in_, ident], writes=[out])

    def act(self, out, in_, func, bias=0.0, scale=1.0, accum_out=None, eng="act"):
        rd = [in_]
        if not isinstance(bias, (int, float)):
            rd.append(bias)
        if not isinstance(scale, (int, float)):
            rd.append(scale)
        wr = [out] + ([accum_out] if accum_out is not None else [])
        kw = {}
        if accum_out is not None:
            kw["accum_out"] = accum_out
        self.op("act", lambda e: e.activation(out, in_, func, bias=bias, scale=scale, **kw),
                reads=rd, writes=wr)

    def tt(self, eng, out, in0, in1, op):
        self.op(eng, lambda e: e.tensor_tensor(out, in0, in1, op), reads=[in0, in1], writes=[out])

    def ts(self, eng, out, in0, s1, s2=None, op0=ALU.mult, op1=None, accum_out=None):
        rd = [in0] + [s for s in (s1, s2) if s is not None and not isinstance(s, (int, float))]
        wr = [out] + ([accum_out] if accum_out is not None else [])
        kw = {}
        if op1 is not None:
            kw["op1"] = op1
        if accum_out is not None:
            kw["accum_out"] = accum_out
        self.op(eng, lambda e: e.tensor_scalar(out, in0, s1, s2, op0, **kw), reads=rd, writes=wr)

    def stt(self, eng, out, in0, scalar, in1, op0, op1):
        rd = [in0, in1] + ([scalar] if not isinstance(scalar, (int, float)) else [])
        self.op(eng, lambda e: e.scalar_tensor_tensor(out, in0, scalar, in1, op0, op1), reads=rd, writes=[out])

    def copy(self, eng, out, in_):
        if eng == "act":
            self.op("act", lambda e: e.copy(out, in_), reads=[in_], writes=[out])
        else:
            self.op(eng, lambda e: e.tensor_copy(out, in_), reads=[in_], writes=[out])

    def memset(self, eng, ap, val):
        self.op(eng, lambda e: e.memset(ap, val), reads=[], writes=[ap])

    def reduce(self, eng, out, in_, op, axis=AX.X):
        self.op(eng, lambda e: e.tensor_reduce(out, in_, axis, op), reads=[in_], writes=[out])

import math
import os
import ml_dtypes
from contextlib import contextmanager

NPBF = ml_dtypes.bfloat16
T = 2048
DM = 1024
ALPHA_C = 4.0 ** 0.25
LN_EPS_C = 1e-5
PI = math.pi


def host_consts():
    c = {}
    L = T
    N = 2 * T
    pos = np.arange(L, dtype=np.float32)
    inv = np.power(np.float32(10000.0), -(np.arange(0, 64, 2, dtype=np.float32) / np.float32(64))).astype(np.float32)
    ang = (pos[:, None] * inv[None, :]).astype(np.float32)
    cs, sn = np.cos(ang).astype(np.float32), np.sin(ang).astype(np.float32)
    p = np.arange(128)
    ropec = cs[:, p % 32].T.copy()
    sgn = np.where((p % 64) < 32, 1.0, -1.0).astype(np.float32)
    ropes = (sn[:, p % 32].T * sgn[:, None]).astype(np.float32)
    c["ropec"] = np.ascontiguousarray(ropec)
    c["ropes"] = np.ascontiguousarray(ropes)
    t = np.arange(L, dtype=np.int64)
    idx = (t[:, None] * t[None, :]) % N
    th = 2.0 * np.pi * idx.astype(np.float64) / N
    C = np.cos(th)
    S = -np.sin(th)
    alt = np.where(t % 2 == 0, 1.0, -1.0)
    Sf = S.copy()
    Sf[:, 0] = alt
    def blk_f(M):
        return np.ascontiguousarray(M.reshape(16, 128, 16, 128).transpose(2, 1, 0, 3)).astype(NPBF)
    c["cf"] = blk_f(C)
    c["sf"] = blk_f(Sf)
    Ci = (2.0 / N) * C
    Ci[0, :] = 1.0 / N
    Si = (2.0 / N) * S
    Si[0, :] = alt / N
    def blk_i(M):
        return np.ascontiguousarray(M.reshape(16, 128, 8, 256).transpose(2, 1, 0, 3)).astype(NPBF)
    c["ci"] = blk_i(Ci)
    c["si"] = blk_i(Si)
    idxf = np.arange(L, dtype=np.float32)
    tt_ = (idxf / np.float32(L - 1)).astype(np.float32)
    w = (np.float32(2.0 * math.pi) * idxf / np.float32(L)).astype(np.float32)
    bands = np.linspace(1e-4, 15, 16, dtype=np.float32)
    ang2 = (w[:, None] * bands[None, :]).astype(np.float32)
    z = np.concatenate([tt_[:, None], np.cos(ang2), -np.sin(ang2)], axis=-1).astype(np.float32)
    c["zembT"] = np.ascontiguousarray(z.T)
    c["negt"] = np.ascontiguousarray((-tt_).reshape(16, 128).T)
    a = np.arange(128)[:, None]
    b = np.arange(128)[None, :]
    mA = np.concatenate([(np.abs(128 * (sg - 1) + a - b) <= 64) for sg in range(3)], axis=1)
    c["mA"] = mA.astype(NPBF)
    mC = np.stack([(b <= a), np.ones((128, 128), bool), (a <= b)], axis=1)
    c["mC"] = np.ascontiguousarray(mC).astype(NPBF)
    sel = np.zeros((65, 64), np.float32)
    sel[64, :] = 1.0
    c["sel"] = sel
    c["ones"] = np.ones((128, 128), np.float32)
    c["identf"] = np.eye(128, dtype=np.float32)
    c["identb"] = np.eye(128).astype(NPBF)
    sw = np.zeros((128, 128), np.float32)
    sw[np.arange(128), np.arange(128) ^ 32] = 1.0
    c["swapm"] = sw.astype(NPBF)
    c["iota16"] = np.tile(np.arange(16, dtype=np.float32)[None, :], (128, 1))
    c["iota128"] = np.tile(np.arange(128)[None, :], (128, 1)).astype(NPBF)
    return c


CONST_DT = {"ropec": F32, "ropes": F32, "cf": BF16, "sf": BF16, "ci": BF16, "si": BF16, "zembT": F32,
            "negt": F32, "mA": BF16, "mC": BF16, "sel": F32, "ones": F32, "identf": F32, "identb": BF16,
            "iota16": F32, "iota128": BF16, "swapm": BF16}

WSHAPES = {
    "w_ada": [2, 1024, 6144], "b_ada": [2, 6144], "w_in": [2, 1024, 7680], "conv_w": [2, 3, 1536],
    "conv_b": [2, 1536], "hy_w1": [2, 33, 64], "hy_b1": [2, 64], "hy_w2": [2, 64, 64], "hy_b2": [2, 64],
    "hy_w3": [2, 64, 2048], "hy_freq": [2, 2, 64], "hy_log_decay": [2, 2048], "hy_bias": [2, 2, 512],
    "attn_sink": [2, 8], "w_branch_a": [2, 256, 1024], "w_branch_b": [2, 512, 1024],
    "w_branch_c": [2, 512, 1024], "w_out": [2, 1024, 1024], "ln_g": [2, 2, 1024], "ln_b": [2, 2, 1024],
    "peer_wq": [2, 1024, 2048], "peer_keys": [2, 8, 2, 128, 128], "peer_u": [2, 16384, 1024],
    "peer_v": [2, 16384, 1024],
}


def cap(ap, offset, pairs):
    return bass.AP(ap.tensor, offset, [list(p) for p in pairs])


class Scope:
    def __init__(self, B):
        self.B = B
        self.es = ExitStack()

    def sb(self, shape, dt, name="t"):
        self.B.uid += 1
        t = self.es.enter_context(self.B.nc.sbuf_tensor("%s_%d" % (name, self.B.uid), list(shape), dt))
        return t

    def ps(self, shape, dt=F32, name="ps"):
        self.B.uid += 1
        return self.es.enter_context(self.B.nc.psum_tensor("%s_%d" % (name, self.B.uid), list(shape), dt))


class Builder:
    def __init__(self, dbg=()):
        self.nc = nc = bass.Bass("TRN2", target_bir_lowering=False)
        self.P = Prog(nc)
        self.uid = 0
        self.dbg = set(dbg)
        self.I = {}
        self.I["x"] = nc.dram_tensor("x", [2, T, DM], F32, kind="ExternalInput").ap()
        self.I["c"] = nc.dram_tensor("c", [2, DM], F32, kind="ExternalInput").ap()
        for k, shp in WSHAPES.items():
            self.I[k] = nc.dram_tensor(k, shp, F32, kind="ExternalInput").ap()
        self.C = {}
        hc = host_consts()
        self.hc = hc
        for k, v in hc.items():
            self.C[k] = nc.dram_tensor("k_" + k, list(v.shape), CONST_DT[k], kind="ExternalInput").ap()
        self.out = nc.dram_tensor("out", [2, T, DM], F32, kind="ExternalOutput").ap()
        S = self.S = {}

        def scr(name, shape, dt):
            kind = "ExternalOutput" if name in self.dbg else "Internal"
            S[name] = nc.dram_tensor("s_" + name, list(shape), dt, kind=kind).ap()
        for l in range(2):
            scr("ada%d" % l, [2, 6144], F32)
            scr("hspec%d" % l, [3, 2048, 1024], BF16)
            scr("ut%d" % l, [128, 128, 8, 128], BF16)
            scr("vb%d" % l, [128, 128, 1024], BF16)
        scr("xres", [2, T, DM], F32)
        scr("qTa", [768, T], BF16)
        scr("kTa", [768, T], BF16)
        scr("va", [3, T, 256], BF16)
        scr("hyT", [1536, T], F32)
        scr("hcT", [1536, T], F32)
        scr("z1T", [512, T], F32)
        scr("qTc", [512, T], BF16)
        scr("kTc", [128, T], BF16)
        scr("vc", [T, 128], BF16)
        scr("gateT", [3072, T], BF16)
        scr("yaT", [256, T], BF16)
        scr("ybT", [512, T], BF16)
        scr("ycT", [512, T], BF16)
        scr("u2T", [128, 8, T], BF16)
        scr("pa", [128, T], BF16)
        scr("pb", [128, T], BF16)
        scr("pg", [128, T], BF16)

    @contextmanager
    def stage(self):
        sc = Scope(self)
        try:
            yield sc
        finally:
            self.P.barrier()
            self.P.emit()
            sc.es.close()

    def finish(self):
        self.P.es.close()

    def bcast_row(self, sc, src_ap_1d, n, name="bc"):
        t = sc.sb([128, n], F32, name)
        src = cap(src_ap_1d, int(src_ap_1d.offset), [[0, 128], [1, n]])
        self.P.dma("sp", t[:], src)
        return t

    def load_const(self, sc, name):
        v = self.hc[name]
        t = sc.sb(list(v.shape), CONST_DT[name], "c_" + name)
        self.P.dma("sp", t[:], self.C[name])
        return t

    def st_ada(self, l):
        P = self.P
        with self.stage() as sc:
            cT = sc.sb([128, 8, 2], F32, "cT")
            for b in range(2):
                P.dma("sp", cT[:, :, b], self.I["c"][b].rearrange("(k p) -> p k", p=128),
                      allow_slow_non_contiguous=True)
            P.act(cT[:], cT[:], AF.Silu)
            ada = sc.sb([2, 6144], F32, "ada")
            bb = sc.sb([2, 6144], F32, "bb")
            ba = self.I["b_ada"][l]
            P.dma("sp", bb[:], cap(ba, int(ba.offset), [[0, 2], [1, 6144]]))
            ws = [sc.sb([128, 8, 512], F32, "w%d" % i) for i in range(2)]
            pss = [sc.ps([128, 512], F32) for i in range(2)]
            for cb in range(12):
                w = ws[cb % 2]
                P.dma("sp", w[:], self.I["w_ada"][l][:, cb * 512:(cb + 1) * 512].rearrange("(k p) n -> p k n", p=128))
                ps = pss[cb % 2]
                for k in range(8):
                    P.mm(ps[0:2, :], cT[:, k, :], w[:, k, :], start=(k == 0), stop=(k == 7))
                P.tt("dve", ada[:, cb * 512:(cb + 1) * 512], ps[0:2, :], bb[:, cb * 512:(cb + 1) * 512], ALU.add)
            for o in (1024, 4096):
                P.ts("dve", ada[:, o:o + 1024], ada[:, o:o + 1024], 1.0, None, op0=ALU.add)
            P.dma("sp", self.S["ada%d" % l], ada[:])

    def make_uT(self, sc, xsrc, scb, shb, uT, identb, pst, ntiles=16, tok0=0, xkeep=None):
        P = self.P
        xts = [sc.sb([128, DM], F32, "xt%d" % i) for i in range(2)]
        ubs = [sc.sb([128, DM], BF16, "ub%d" % i) for i in range(2)]
        uf = sc.sb([128, DM], F32, "uf")
        for i in range(ntiles):
            xt = xts[i % 2] if xkeep is None else xkeep[i]
            ub = ubs[i % 2]
            P.dma("sp", xt[:], xsrc[tok0 + i * 128: tok0 + (i + 1) * 128, :])
            P.tt("dve", uf[:], xt[:], scb[:], ALU.mult)
            P.tt("pool", ub[:], uf[:], shb[:], ALU.add)
            for k in range(8):
                P.tr(pst[:, k * 128:(k + 1) * 128], ub[:, k * 128:(k + 1) * 128], identb[:])
            P.copy("act", uT[:, :, i * 128:(i + 1) * 128], pst[:].rearrange("p (k t) -> p k t", k=8))

    def st_inproj(self, s, l):
        P = self.P
        xsrc = self.I["x"][s] if l == 0 else self.S["xres"][s]
        ada = self.S["ada%d" % l]
        with self.stage() as sc:
            identb = self.load_const(sc, "identb")
            ropec = self.load_const(sc, "ropec")
            ropes = self.load_const(sc, "ropes")
            shb = self.bcast_row(sc, ada[s, 0:1024], 1024, "shb")
            scb = self.bcast_row(sc, ada[s, 1024:2048], 1024, "scb")
            uT = sc.sb([128, 8, T], BF16, "uT")
            pst = sc.ps([128, 1024], BF16, "pst")
            self.make_uT(sc, xsrc, scb, shb, uT, identb, pst)
            wfs = [sc.sb([128, 8, 256], F32, "wf%d" % i) for i in range(2)]
            wbs = [sc.sb([128, 8, 256], BF16, "wb%d" % i) for i in range(2)]
            pss = [sc.ps([128, 512], F32, "pf%d" % i) for i in range(4)]
            stg = [sc.sb([128, T], BF16, "stg%d" % i) for i in range(3)]
            stf = [sc.sb([128, T], F32, "stf%d" % i) for i in range(2)]
            tbs = [sc.sb([128, 512], BF16, "tbs%d" % i) for i in range(2)]
            ps2 = [sc.ps([128, 512], F32, "ps2_%d" % i) for i in range(2)]
            swapm = self.load_const(sc, "swapm")
            tA = [sc.sb([128, 512], F32, "tA%d" % i) for i in range(2)]
            tB = [sc.sb([128, 512], F32, "tB%d" % i) for i in range(2)]
            vst = [sc.sb([128, 16, 256], BF16, "vst%d" % i) for i in range(2)]
            cnt = {"ps": 0, "stg": 0, "stf": 0, "tmp": 0, "v": 0}

            def tokview(ap2, tt, D):
                if D == 1:
                    return ap2[:, tt * 512:(tt + 1) * 512]
                if D == 4:
                    return ap2.rearrange("p (j r) -> p r j", r=4)[:, tt, :]
                return ap2.rearrange("p (j r) -> p r j", r=16)[:, 4 * tt:4 * tt + 4, :]

            def shp(ap2, D):
                if D == 16:
                    return ap2.rearrange("p (r j) -> p r j", r=4)
                return ap2

            def tok128(ap2, i, D):
                if D == 1:
                    return ap2[:, i * 128:(i + 1) * 128]
                if D == 4:
                    return ap2.rearrange("p (j r) -> p r j", r=4)[:, i // 4, (i % 4) * 128:(i % 4 + 1) * 128]
                return ap2.rearrange("p (j r) -> p r j", r=16)[:, i, :]

            def fm_chunk(wb, cc, kind, D, dest):
                if kind == "hy":
                    st = stf[cnt["stf"] % 2]
                    cnt["stf"] += 1
                else:
                    st = stg[cnt["stg"] % 3]
                    cnt["stg"] += 1
                for tt in range(4):
                    ps = pss[cnt["ps"] % 4]
                    cnt["ps"] += 1
                    for k in range(8):
                        P.mm(ps[:, :], wb[:, k, cc * 128:(cc + 1) * 128], uT[:, k, tt * 512:(tt + 1) * 512],
                             start=(k == 0), stop=(k == 7))
                    o = st[:, tt * 512:(tt + 1) * 512]
                    if kind == "hy":
                        P.copy("act", o, ps[:, :])
                    elif kind == "gate":
                        P.act(o, ps[:, :], AF.Sigmoid)
                    else:
                        j = cnt["tmp"] % 2
                        cnt["tmp"] += 1
                        tb_, a_, b_, p2 = tbs[j], tA[j], tB[j], ps2[j]
                        P.copy("act", tb_[:], ps[:, :])
                        P.mm(p2[:, :], swapm[:], tb_[:])
                        ts_ = slice(tt * 512, (tt + 1) * 512)
                        P.tt("dve", a_[:], ps[:, :], ropec[:, ts_], ALU.mult)
                        P.tt("dve", b_[:], p2[:, :], ropes[:, ts_], ALU.mult)
                        if D == 1:
                            P.tt("pool", o, a_[:], b_[:], ALU.subtract)
                        else:
                            n_ = 512 // D
                            ov = st[:, :].rearrange("p (r j) -> p j r", r=D)[:, tt * n_:(tt + 1) * n_, :]
                            P.tt("pool", ov, a_[:].rearrange("p (j r) -> p j r", r=D),
                                 b_[:].rearrange("p (j r) -> p j r", r=D), ALU.subtract)
                P.dma("pool", dest, st[:])

            def tm_block(wb, c0, ncols, D, dest):
                st = vst[cnt["v"] % 2]
                cnt["v"] += 1
                for i in range(16):
                    ps = pss[cnt["ps"] % 4]
                    cnt["ps"] += 1
                    for k in range(8):
                        P.mm(ps[:, 0:ncols], tok128(uT[:, k, :], i, D), wb[:, k, c0:c0 + ncols],
                             start=(k == 0), stop=(k == 7))
                    P.copy("act", st[:, i, 0:ncols], ps[:, 0:ncols])
                P.dma("pool", dest.rearrange("(i p) c -> p i c", p=128), st[:, :, 0:ncols])

            DG = (1, 4, 16)
            for blk in range(30):
                wf, wb = wfs[blk % 2], wbs[blk % 2]
                col = blk * 256
                P.dma("sp", wf[:], self.I["w_in"][l][:, col:col + 256].rearrange("(k p) n -> p k n", p=128))
                P.copy("pool" if blk % 2 else "dve", wb[:], wf[:])
                if col < 1536:
                    nm = "qTa" if col < 768 else "kTa"
                    base = 0 if col < 768 else 768
                    for cc in range(2):
                        j = (col - base) // 128 + cc
                        fm_chunk(wb, cc, "rope", DG[j // 2], self.S[nm][j * 128:(j + 1) * 128, :])
                elif col < 2304:
                    g = (col - 1536) // 256
                    tm_block(wb, 0, 256, DG[g], self.S["va"][g])
                elif col < 3840:
                    for cc in range(2):
                        j = (col - 2304) // 128 + cc
                        fm_chunk(wb, cc, "hy", 1, self.S["hyT"][j * 128:(j + 1) * 128, :])
                elif col < 4352:
                    for cc in range(2):
                        j = (col - 3840) // 128 + cc
                        fm_chunk(wb, cc, "rope", 1, self.S["qTc"][j * 128:(j + 1) * 128, :])
                elif col < 4608:
                    fm_chunk(wb, 0, "rope", 1, self.S["kTc"][:, :])
                    tm_block(wb, 128, 128, 1, self.S["vc"])
                else:
                    for cc in range(2):
                        j = (col - 4608) // 128 + cc
                        fm_chunk(wb, cc, "gate", 1, self.S["gateT"][j * 128:(j + 1) * 128, :])


def _attn_finalize(B, sc, numer, nheads, sel, psB, dest):
    P = B.P
    recs = [sc.sb([64, 512], F32, "rec%d" % i) for i in range(2)]
    ysts = [sc.sb([64, T], BF16, "yst%d" % i) for i in range(2)]
    n = 0
    for h in range(nheads):
        yst = ysts[h % 2]
        for tt in range(4):
            ps = psB[n % 2]
            rec = recs[n % 2]
            n += 1
            P.mm(ps[0:64, :], sel[0:65, 0:64], numer[0:65, h, tt * 512:(tt + 1) * 512])
            P.op("dve", lambda e, o=rec[:], i=ps[0:64, :]: e.reciprocal(o, i), reads=[ps[0:64, :]], writes=[rec[:]])
            P.tt("pool", yst[:, tt * 512:(tt + 1) * 512], numer[0:64, h, tt * 512:(tt + 1) * 512], rec[:], ALU.mult)
        P.dma("pool", dest[h * 64:(h + 1) * 64, :], yst[:])


def st_attn_a(self, s, l):
    P = self.P
    with self.stage() as sc:
        mA = self.load_const(sc, "mA")
        sel = self.load_const(sc, "sel")
        numer = sc.sb([65, 4, T], F32, "numer")
        q = sc.sb([64, 4, T], BF16, "q")
        k = sc.sb([64, 4, T], BF16, "k")
        va = sc.sb([128, 16, 4, 65], BF16, "va")
        psS = [sc.ps([128, 512], F32, "psS%d" % i) for i in range(3)]
        psO = [sc.ps([128, 512], F32, "psO%d" % i) for i in range(2)]
        psB = [sc.ps([128, 512], F32, "psB%d" % i) for i in range(2)]
        pT = [sc.sb([128, 384], BF16, "pT%d" % i) for i in range(2)]
        pT2 = [sc.sb([128, 384], BF16, "pTm%d" % i) for i in range(3)]
        P.memset("pool", va[:], 1.0)
        n = 0
        for g, D in enumerate((1, 4, 16)):
            Ls = T // D
            nb = Ls // 128
            for h in range(4):
                hh = g * 4 + h
                P.dma("sp", q[:, h, :], self.S["qTa"][hh * 64:(hh + 1) * 64, :])
                P.dma("sp", k[:, h, :], self.S["kTa"][hh * 64:(hh + 1) * 64, :])
                P.dma("sp", va[:, :, h, 0:64],
                      self.S["va"][g][:, h * 64:(h + 1) * 64].rearrange("(i p) d -> p i d", p=128))
            jobs = [(h, rho, i) for h in range(4) for rho in range(D) for i in range(nb)]

            def s_phase(n, job):
                h, rho, i = job
                base = rho * Ls
                js = [j for j in (i - 1, i, i + 1) if 0 <= j < nb]
                ps = psS[n % 3]
                p1 = pT[n % 2]
                p2 = pT2[n % 3]
                q0 = base + i * 128
                for j in js:
                    sg = j - i + 1
                    P.mm(ps[:, sg * 128:(sg + 1) * 128], k[0:64, h, base + j * 128: base + (j + 1) * 128],
                         q[0:64, h, q0:q0 + 128])
                c0 = (js[0] - i + 1) * 128
                c1 = (js[-1] - i + 2) * 128
                P.act(p1[:, c0:c1], ps[:, c0:c1], AF.Exp, scale=0.125)
                P.tt("pool" if n % 2 else "dve", p2[:, c0:c1], p1[:, c0:c1], mA[:, c0:c1], ALU.mult)

            def pv_phase(n, job):
                h, rho, i = job
                js = [j for j in (i - 1, i, i + 1) if 0 <= j < nb]
                po = psO[n % 2]
                p2 = pT2[n % 3]
                for jj, j in enumerate(js):
                    sg = j - i + 1
                    P.mm(po[0:65, 0:128], va[:, rho * nb + j, h, :], p2[:, sg * 128:(sg + 1) * 128],
                         start=(jj == 0), stop=(jj == len(js) - 1))
                if D == 1:
                    nv = numer[0:65, h, i * 128:(i + 1) * 128]
                else:
                    nv = numer[0:65, h, :].rearrange("p (j r) -> p r j", r=D)[:, rho, i * 128:(i + 1) * 128]
                if g == 0:
                    P.copy("act", nv, po[0:65, 0:128])
                else:
                    P.tt("dve", nv, po[0:65, 0:128], nv, ALU.add)
            for n_, job in enumerate(jobs):
                s_phase(n + n_, job)
                if n_ > 0:
                    pv_phase(n + n_ - 1, jobs[n_ - 1])
            pv_phase(n + len(jobs) - 1, jobs[-1])
            n += len(jobs)
        _attn_finalize(self, sc, numer, 4, sel, psB, self.S["yaT"])


def st_attn_c(self, s, l):
    P = self.P
    with self.stage() as sc:
        mC = self.load_const(sc, "mC")
        sel = self.load_const(sc, "sel")
        numer = sc.sb([65, 8, T], F32, "numerc")
        q = sc.sb([64, 8, T], BF16, "qc")
        k = sc.sb([64, 2, T], BF16, "kc")
        va = sc.sb([128, 16, 2, 65], BF16, "vca")
        sk = sc.sb([65, 8], F32, "sk")
        psS = [sc.ps([128, 512], F32, "psS%d" % i) for i in range(3)]
        psO = [sc.ps([128, 512], F32, "psO%d" % i) for i in range(2)]
        psB = [sc.ps([128, 512], F32, "psB%d" % i) for i in range(2)]
        pT = [sc.sb([128, 3, 512], BF16, "pTc%d" % i) for i in range(2)]
        P.memset("pool", va[:], 1.0)
        for h in range(8):
            P.dma("sp", q[:, h, :], self.S["qTc"][h * 64:(h + 1) * 64, :])
        for h in range(2):
            P.dma("sp", k[:, h, :], self.S["kTc"][h * 64:(h + 1) * 64, :])
            P.dma("sp", va[:, :, h, 0:64], self.S["vc"][:, h * 64:(h + 1) * 64].rearrange("(i p) d -> p i d", p=128))
        asink = self.I["attn_sink"][l]
        P.dma("sp", sk[64:65, :], cap(asink, int(asink.offset), [[0, 1], [1, 8]]))
        P.act(sk[64:65, :], sk[64:65, :], AF.Exp)
        n = 0
        mcb = mC[:]
        pst = mcb.ap[0][0]
        jobs = [(kv, i) for kv in range(2) for i in range(16)]

        def s_phase(n, job):
            kv, i = job
            js = [j for j in (i - 1, i, i + 1) if 0 <= j < 16]
            p1 = pT[n % 2]
            for j in js:
                sg = j - i + 1
                ps = psS[sg]
                P.mm(ps[:, :].rearrange("p (h t) -> p h t", h=4), k[0:64, kv, j * 128:(j + 1) * 128],
                     q[0:64, 4 * kv:4 * kv + 4, i * 128:(i + 1) * 128])
                P.act(p1[:, sg, :], ps[:, :], AF.Exp, scale=0.125)
                if sg != 1:
                    mv = cap(mcb, int(mcb.offset) + sg * 128, [[pst, 128], [0, 4], [1, 128]])
                    pv = p1[:, sg, :].rearrange("p (h t) -> p h t", h=4)
                    P.tt("pool" if sg == 0 else "dve", pv, pv, mv, ALU.mult)

        def pv_phase(n, job):
            kv, i = job
            js = [j for j in (i - 1, i, i + 1) if 0 <= j < 16]
            p1 = pT[n % 2]
            po = psO[n % 2]
            for jj, j in enumerate(js):
                sg = j - i + 1
                P.mm(po[0:65, :], va[:, j, kv, :], p1[:, sg, :], start=(jj == 0), stop=(jj == len(js) - 1))
            P.copy("act", numer[0:65, 4 * kv:4 * kv + 4, i * 128:(i + 1) * 128],
                   po[0:65, :].rearrange("p (h t) -> p h t", h=4))
        for n_, job in enumerate(jobs):
            s_phase(n_, job)
            if n_ > 0:
                pv_phase(n_ - 1, jobs[n_ - 1])
        pv_phase(len(jobs) - 1, jobs[-1])
        for h in range(8):
            P.ts("dve", numer[64:65, h, :], numer[64:65, h, :], sk[64:65, h:h + 1], None, op0=ALU.add)
        _attn_finalize(self, sc, numer, 8, sel, psB, self.S["ycT"])


Builder.st_attn_a = st_attn_a
Builder.st_attn_c = st_attn_c


def _filter_body(self, sc, l):
    P = self.P
    I = self.I
    if True:
        zembT = self.load_const(sc, "zembT")
        negt = self.load_const(sc, "negt")
        ones = self.load_const(sc, "ones")
        w1 = sc.sb([33, 64], F32, "w1")
        w2 = sc.sb([64, 64], F32, "w2")
        w3 = sc.sb([64, 2048], F32, "w3")
        P.dma("sp", w1[:], I["hy_w1"][l])
        P.dma("sp", w2[:], I["hy_w2"][l])
        P.dma("sp", w3[:], I["hy_w3"][l])
        prm = sc.sb([64, 4], F32, "prm")
        for j, ap1 in enumerate((I["hy_b1"][l], I["hy_b2"][l], I["hy_freq"][l][0], I["hy_freq"][l][1])):
            P.dma("sp", prm[:, j:j + 1], cap(ap1, int(ap1.offset), [[1, 64], [1, 1]]))
        fb = sc.sb([64, 2], F32, "fb")
        P.tt("dve", fb[:, 0:1], prm[:, 0:1], prm[:, 2:3], ALU.mult)
        P.tt("dve", fb[:, 1:2], prm[:, 1:2], prm[:, 3:4], ALU.mult)
        psF = sc.ps([128, 2048], F32, "psF")
        psN = sc.ps([128, 1024], F32, "psN")
        h1T = sc.sb([64, T], F32, "h1T")
        h2T = sc.sb([64, T], F32, "h2T")
        arg = sc.sb([64, 512], F32, "arg")
        ki = sc.sb([64, 512], I32, "ki")
        kf = sc.sb([64, 512], F32, "kf")
        mk = sc.sb([64, 512], F32, "mk")

        def sin_layer(dst, lhsT, src, kk, fcol, fbcol):
            for tt in range(4):
                ps = psF[0:64, tt * 512:(tt + 1) * 512]
                P.mm(ps, lhsT, src[0:kk, tt * 512:(tt + 1) * 512])
                P.ts("dve", arg[:], ps, prm[:, fcol:fcol + 1], fb[:, fbcol:fbcol + 1], op0=ALU.mult, op1=ALU.add)
                P.ts("dve", arg[:], arg[:], 1.0 / (2.0 * PI), None, op0=ALU.mult)
                P.copy("dve", ki[:], arg[:])
                P.copy("dve", kf[:], ki[:])
                P.tt("dve", arg[:], arg[:], kf[:], ALU.subtract)
                P.ts("dve", mk[:], arg[:], 0.5, None, op0=ALU.is_gt)
                P.tt("dve", arg[:], arg[:], mk[:], ALU.subtract)
                P.ts("dve", mk[:], arg[:], -0.5, None, op0=ALU.is_lt)
                P.tt("dve", arg[:], arg[:], mk[:], ALU.add)
                P.act(dst[:, tt * 512:(tt + 1) * 512], arg[:], AF.Sin, scale=6.28318)
                yield
        yield from sin_layer(h1T, w1[0:33, :], zembT, 33, 2, 0)
        yield from sin_layer(h2T, w2[0:64, :], h1T, 64, 3, 1)
        ld = self.bcast_row(sc, I["hy_log_decay"][l], 2048, "ld")
        P.act(ld[:], ld[:], AF.Exp)
        gsum = sc.sb([128, 16, 1024], BF16, "gsum")
        gdiff = sc.sb([128, 16, 1024], BF16, "gdiff")
        dec = sc.sb([128, 2048], F32, "dec")
        filt = sc.sb([128, 2048], F32, "filt")
        sq = dec
        for mc in range(16):
            for cb in range(4):
                P.mm(psF[:, cb * 512:(cb + 1) * 512], h2T[0:64, mc * 128:(mc + 1) * 128], w3[0:64, cb * 512:(cb + 1) * 512])
            P.act(dec[:], ld[:], AF.Exp, scale=negt[:, mc:mc + 1])
            P.tt("dve", filt[:], psF[:, :], dec[:], ALU.mult)
            if mc == 0:
                P.memset("dve", filt[0:1, 1024:2048], 0.0)
            P.act(sq[:], filt[:], AF.Square)
            for half in range(2):
                for d in range(2):
                    c0 = d * 1024 + half * 512
                    P.mm(psN[:, half * 512:(half + 1) * 512], ones[:, :], sq[:, c0:c0 + 512],
                         start=(mc == 0 and d == 0), stop=(mc == 15 and d == 1))
            P.tt("pool", gsum[:, mc, :], filt[:, 0:1024], filt[:, 1024:2048], ALU.add)
            P.tt("pool", gdiff[:, mc, :], filt[:, 0:1024], filt[:, 1024:2048], ALU.subtract)
            yield
        rs = sc.sb([128, 1024], F32, "rs")
        P.ts("dve", rs[:], psN[:, :], 1e-12, None, op0=ALU.add)
        P.act(rs[:], rs[:], AF.Sqrt)
        P.op("dve", lambda e: e.reciprocal(rs[:], rs[:]), reads=[rs[:]], writes=[rs[:]])
        cfs = [sc.sb([128, 16, 128], BF16, "cf%d" % i) for i in range(2)]
        sfs = [sc.sb([128, 16, 128], BF16, "sf%d" % i) for i in range(2)]
        ho = [[sc.sb([128, 512], BF16, "ho%d_%d" % (i, j)) for j in range(3)] for i in range(2)]
        hs = self.S["hspec%d" % l]
        n = 0
        for fc in range(16):
            cf, sf = cfs[fc % 2], sfs[fc % 2]
            P.dma("sp", cf[:], self.C["cf"][fc])
            P.dma("sp", sf[:], self.C["sf"][fc])
            for half in range(2):
                cs = slice(half * 512, (half + 1) * 512)
                pre, pim, px = psF[:, 0:512], psF[:, 512:1024], psF[:, 1024:1536]
                for mc in range(16):
                    P.mm(pre, cf[:, mc, :], gsum[:, mc, cs], start=(mc == 0), stop=(mc == 15))
                for mc in range(16):
                    P.mm(pim, sf[:, mc, :], gdiff[:, mc, cs], start=(mc == 0), stop=(mc == 15))
                if fc == 0:
                    for mc in range(16):
                        P.mm(px, sf[:, mc, :], gsum[:, mc, cs], start=(mc == 0), stop=(mc == 15))
                hre, him, hrb = ho[n % 2]
                n += 1
                P.tt("dve", hre[:], pre, rs[:, cs], ALU.mult)
                P.tt("dve", him[:], pim, rs[:, cs], ALU.mult)
                P.copy("pool", hrb[:], hre[:])
                if fc == 0:
                    P.memset("pool", him[0:1, :], 0.0)
                    P.tt("dve", hrb[0:1, :], px[0:1, :], rs[0:1, cs], ALU.mult)
                for j, t_ in enumerate((hre, him, hrb)):
                    P.dma("pool", hs[j, fc * 128:(fc + 1) * 128, cs], t_[:])
                yield


def st_hconv(self, s, l):
    P = self.P
    with self.stage() as sc:
        cw = sc.sb([128, 12, 3], F32, "cw")
        cb = sc.sb([128, 12], F32, "cb")
        for i in range(3):
            P.dma("sp", cw[:, :, i], self.I["conv_w"][l][i].rearrange("(k p) -> p k", p=128), allow_slow_non_contiguous=True)
        P.dma("sp", cb[:], self.I["conv_b"][l].rearrange("(k p) -> p k", p=128), allow_slow_non_contiguous=True)
        xs = [sc.sb([128, T], F32, "hx%d" % i) for i in range(2)]
        os_ = [sc.sb([128, T], F32, "ho%d" % i) for i in range(2)]
        for cc in range(12):
            x, o = xs[cc % 2], os_[cc % 2]
            P.dma("sp", x[:], self.S["hyT"][cc * 128:(cc + 1) * 128, :])
            P.act(o[:], x[:], AF.Identity, bias=cb[:, cc:cc + 1], scale=cw[:, cc, 1:2])
            P.stt("dve", o[:, 1:T], x[:, 0:T - 1], cw[:, cc, 0:1], o[:, 1:T], ALU.mult, ALU.add)
            P.stt("dve", o[:, 0:T - 1], x[:, 1:T], cw[:, cc, 2:3], o[:, 0:T - 1], ALU.mult, ALU.add)
            P.dma("pool", self.S["hcT"][cc * 128:(cc + 1) * 128, :], o[:])


def st_hyena(self, s, l, o):
    P = self.P
    zsrc = self.S["hcT"][0:512, :] if o == 0 else self.S["z1T"]
    gsrc = self.S["hcT"][(o + 1) * 512:(o + 2) * 512, :]
    hs = self.S["hspec%d" % l]
    with self.stage() as sc:
        identb = self.load_const(sc, "identb")
        hb = sc.sb([128, 4], F32, "hb")
        P.dma("sp", hb[:], self.I["hy_bias"][l][o].rearrange("(k p) -> p k", p=128), allow_slow_non_contiguous=True)
        zb = sc.sb([128, 4, T], BF16, "zb")
        zfs = [sc.sb([128, T], F32, "zf%d" % i) for i in range(2)]
        for cc in range(4):
            zf = zfs[cc % 2]
            P.dma("sp", zf[:], zsrc[cc * 128:(cc + 1) * 128, :])
            P.copy("pool" if cc % 2 else "dve", zb[:, cc, :], zf[:])
        zTM = sc.sb([128, 16, 512], BF16, "zTM")
        psT = [sc.ps([128, 1024], BF16, "psT%d" % i) for i in range(2)]
        for tc in range(16):
            pt = psT[tc % 2]
            for cc in range(4):
                P.tr(pt[:, cc * 128:(cc + 1) * 128], zb[:, cc, tc * 128:(tc + 1) * 128], identb[:])
            P.copy("act", zTM[:, tc, :], pt[:, 0:512])
        Yre = sc.sb([128, 16, 512], BF16, "Yre")
        Yim = sc.sb([128, 16, 512], BF16, "Yim")
        cfs = [sc.sb([128, 16, 128], BF16, "cf%d" % i) for i in range(2)]
        sfs = [sc.sb([128, 16, 128], BF16, "sf%d" % i) for i in range(2)]
        hts = [[sc.sb([128, 512], BF16, "h%d_%d" % (i, j)) for j in range(3)] for i in range(2)]
        tmps = [[sc.sb([128, 512], F32, "tm%d_%d" % (i, j)) for j in range(4)] for i in range(2)]
        psR = [sc.ps([128, 512], F32, "psR%d" % i) for i in range(2)]
        psI = [sc.ps([128, 512], F32, "psI%d" % i) for i in range(2)]
        cs = slice(o * 512, (o + 1) * 512)
        for fc in range(16):
            cf, sf = cfs[fc % 2], sfs[fc % 2]
            P.dma("sp", cf[:], self.C["cf"][fc])
            P.dma("sp", sf[:], self.C["sf"][fc])
            hre, him, hrb = hts[fc % 2]
            for j, t_ in enumerate((hre, him, hrb)):
                P.dma("sp", t_[:], hs[j, fc * 128:(fc + 1) * 128, cs])
            pr, pi = psR[fc % 2], psI[fc % 2]
            for tc in range(16):
                P.mm(pr[:, :], cf[:, tc, :], zTM[:, tc, :], start=(tc == 0), stop=(tc == 15))
            for tc in range(16):
                P.mm(pi[:, :], sf[:, tc, :], zTM[:, tc, :], start=(tc == 0), stop=(tc == 15))
            t1, t2, t3, t4 = tmps[fc % 2]
            P.tt("dve", t1[:], pr[:, :], hre[:], ALU.mult)
            P.tt("dve", t2[:], pi[:, :], him[:], ALU.mult)
            P.tt("dve", t3[:], pr[:, :], him[:], ALU.mult)
            P.tt("dve", t4[:], pi[:, :], hrb[:], ALU.mult)
            P.tt("pool", Yre[:, fc, :], t1[:], t2[:], ALU.subtract)
            P.tt("pool", Yim[:, fc, :], t3[:], t4[:], ALU.add)
        cis = [sc.sb([128, 16, 256], BF16, "ci%d" % i) for i in range(2)]
        sis = [sc.sb([128, 16, 256], BF16, "si%d" % i) for i in range(2)]
        psY = [sc.ps([128, 512], F32, "psY%d" % i) for i in range(2)]
        zps = [sc.sb([128, 256], F32, "zp%d" % i) for i in range(2)]
        gps = [sc.sb([128, 256], F32, "gp%d" % i) for i in range(2)]
        tms = [sc.sb([128, 256], F32, "tq%d" % i) for i in range(2)]
        odt = F32 if o == 0 else BF16
        ors = [sc.sb([128, 256], odt, "or%d" % i) for i in range(2)]
        dest = self.S["z1T"] if o == 0 else self.S["ybT"]
        n = 0
        for tt in range(8):
            ci, si = cis[tt % 2], sis[tt % 2]
            P.dma("sp", ci[:], self.C["ci"][tt])
            P.dma("sp", si[:], self.C["si"][tt])
            ts_ = slice(tt * 256, (tt + 1) * 256)
            for cc in range(4):
                py = psY[n % 2]
                zp, gp, tm, orr = zps[n % 2], gps[n % 2], tms[n % 2], ors[n % 2]
                n += 1
                P.dma("sp", zp[:], zsrc[cc * 128:(cc + 1) * 128, ts_])
                P.dma("sp", gp[:], gsrc[cc * 128:(cc + 1) * 128, ts_])
                for fc in range(16):
                    P.mm(py[:, 0:256], Yre[:, fc, cc * 128:(cc + 1) * 128], ci[:, fc, :], start=(fc == 0), stop=False)
                for fc in range(16):
                    P.mm(py[:, 0:256], Yim[:, fc, cc * 128:(cc + 1) * 128], si[:, fc, :], start=False, stop=(fc == 15))
                P.stt("dve", tm[:], zp[:], hb[:, cc:cc + 1], py[:, 0:256], ALU.mult, ALU.add)
                P.tt("pool", orr[:], tm[:], gp[:], ALU.mult)
                P.dma("pool", dest[cc * 128:(cc + 1) * 128, ts_], orr[:])


def st_filter(self, l):
    with self.stage() as sc:
        for _ in _filter_body(self, sc, l):
            pass


Builder.st_filter = st_filter
Builder.st_hconv = st_hconv
Builder.st_hyena = st_hyena


class LNBufs:
    def __init__(self, sc):
        self.s1 = sc.sb([128, 1], F32, "ln_s1")
        self.nm = sc.sb([128, 1], F32, "ln_nm")
        self.ss = sc.sb([128, 1], F32, "ln_ss")
        self.rstd = sc.sb([128, 1], F32, "ln_rstd")
        self.sq = sc.sb([128, DM], F32, "ln_sq")
        self.y = [sc.sb([128, DM], F32, "ln_y%d" % i) for i in range(2)]
        self.n = 0


def ln_tile(P, lb, r, lng, lnb, dest):
    y = lb.y[lb.n % 2]
    lb.n += 1
    P.reduce("dve", lb.s1[:], r[:], ALU.add)
    P.ts("dve", lb.nm[:], lb.s1[:], -1.0 / DM, None, op0=ALU.mult)
    P.act(lb.sq[:], r[:], AF.Square, bias=lb.nm[:, 0:1])
    P.reduce("dve", lb.ss[:], lb.sq[:], ALU.add)
    P.ts("dve", lb.rstd[:], lb.ss[:], 1.0 / DM, LN_EPS_C, op0=ALU.mult, op1=ALU.add)
    P.act(lb.rstd[:], lb.rstd[:], AF.Sqrt)
    P.op("dve", lambda e: e.reciprocal(lb.rstd[:], lb.rstd[:]), reads=[lb.rstd[:]], writes=[lb.rstd[:]])
    P.ts("dve", y[:], r[:], lb.nm[:, 0:1], lb.rstd[:, 0:1], op0=ALU.add, op1=ALU.mult)
    P.tt("pool", y[:], y[:], lng[:], ALU.mult)
    P.tt("pool", y[:], y[:], lnb[:], ALU.add)
    P.dma("pool", dest, y[:])


def st_merge(self, s, l):
    P = self.P
    I = self.I
    xsrc = I["x"][s] if l == 0 else self.S["xres"][s]
    ada = self.S["ada%d" % l]
    with self.stage() as sc:
        ys = {}
        for nm, nk in (("yaT", 2), ("ybT", 4), ("ycT", 4)):
            t_ = sc.sb([128, nk, T], BF16, nm)
            for k in range(nk):
                P.dma("sp", t_[:, k, :], self.S[nm][k * 128:(k + 1) * 128, :])
            ys[nm] = t_
        wst = [sc.sb([128, DM], F32, "wst%d" % i) for i in range(2)]
        ws = {}
        n = 0
        for nm, nk in (("w_branch_a", 2), ("w_branch_b", 4), ("w_branch_c", 4), ("w_out", 8)):
            t_ = sc.sb([128, nk, DM], BF16, nm)
            for k in range(nk):
                st = wst[n % 2]
                P.dma("sp", st[:], I[nm][l][k * 128:(k + 1) * 128, :])
                P.copy("pool" if n % 2 else "dve", t_[:, k, :], st[:])
                n += 1
            ws[nm] = t_
        mergedT = sc.sb([128, 8, T], BF16, "mergedT")
        psb = [sc.ps([128, 512], F32, "psbr%d" % i) for i in range(3)]
        gts = [[sc.sb([128, 512], BF16, "g%d_%d" % (i, j)) for j in range(3)] for i in range(2)]
        ms = [[sc.sb([128, 512], F32, "m%d_%d" % (i, j)) for j in range(3)] for i in range(2)]
        n = 0
        for fc in range(8):
            for tt in range(4):
                ts_ = slice(tt * 512, (tt + 1) * 512)
                g3 = gts[n % 2]
                m3 = ms[n % 2]
                n += 1
                for j, (yn, wn, nk) in enumerate((("yaT", "w_branch_a", 2), ("ybT", "w_branch_b", 4), ("ycT", "w_branch_c", 4))):
                    for k in range(nk):
                        P.mm(psb[j][:, :], ws[wn][:, k, fc * 128:(fc + 1) * 128], ys[yn][:, k, ts_],
                             start=(k == 0), stop=(k == nk - 1))
                    P.dma("sp", g3[j][:], self.S["gateT"][j * 1024 + fc * 128: j * 1024 + (fc + 1) * 128, ts_])
                    P.tt("dve", m3[j][:], psb[j][:, :], g3[j][:], ALU.mult)
                P.tt("pool", m3[0][:], m3[0][:], m3[1][:], ALU.add)
                P.tt("pool", mergedT[:, fc, ts_], m3[0][:], m3[2][:], ALU.add)
        g1b = self.bcast_row(sc, ada[s, 2048:3072], 1024, "g1b")
        lng = self.bcast_row(sc, I["ln_g"][l, 0], 1024, "lng")
        lnb = self.bcast_row(sc, I["ln_b"][l, 0], 1024, "lnb")
        lb = LNBufs(sc)
        psM = [sc.ps([128, 1024], F32, "psM%d" % i) for i in range(2)]
        xts = [sc.sb([128, DM], F32, "xt%d" % i) for i in range(2)]
        tms = [sc.sb([128, DM], F32, "tm%d" % i) for i in range(2)]
        for i in range(16):
            pm = psM[i % 2]
            xt = xts[i % 2]
            tm = tms[i % 2]
            P.dma("sp", xt[:], xsrc[i * 128:(i + 1) * 128, :])
            for hf in range(2):
                for k in range(8):
                    P.mm(pm[:, hf * 512:(hf + 1) * 512], mergedT[:, k, i * 128:(i + 1) * 128],
                         ws["w_out"][:, k, hf * 512:(hf + 1) * 512], start=(k == 0), stop=(k == 7))
            P.tt("dve", tm[:], pm[:, :], g1b[:], ALU.mult)
            P.stt("dve", tm[:], xt[:], ALPHA_C, tm[:], ALU.mult, ALU.add)
            ln_tile(P, lb, tm, lng, lnb, self.S["xres"][s][i * 128:(i + 1) * 128, :])


def _uv_body(self, sc, l):
    P = self.P
    if True:
        identb = self.load_const(sc, "identb")
        ufs = [sc.sb([128, DM], F32, "uf%d" % i) for i in range(2)]
        vfs = [sc.sb([128, DM], F32, "vf%d" % i) for i in range(2)]
        ubs = [sc.sb([128, DM], BF16, "ub%d" % i) for i in range(2)]
        vbs = [sc.sb([128, DM], BF16, "vb%d" % i) for i in range(2)]
        uts = [sc.sb([128, 8, 128], BF16, "ut%d" % i) for i in range(2)]
        pst = [sc.ps([128, 1024], BF16, "pst%d" % i) for i in range(2)]
        U = self.I["peer_u"][l]
        V = self.I["peer_v"][l]
        for b in range(128):
            uf, vf, ub, vb, ut, pt = ufs[b % 2], vfs[b % 2], ubs[b % 2], vbs[b % 2], uts[b % 2], pst[b % 2]
            P.dma("sp", uf[:], cap(U, int(U.offset) + b * DM, [[128 * DM, 128], [1, DM]]))
            P.dma("sp", vf[:], cap(V, int(V.offset) + b * DM, [[128 * DM, 128], [1, DM]]))
            P.copy("dve", ub[:], uf[:])
            P.copy("pool", vb[:], vf[:])
            for k in range(8):
                P.tr(pt[:, k * 128:(k + 1) * 128], ub[:, k * 128:(k + 1) * 128], identb[:])
            P.copy("act", ut[:], pt[:, :].rearrange("p (k a) -> p k a", k=8))
            P.dma("pool", self.S["ut%d" % l][b], ut[:])
            P.dma("pool", self.S["vb%d" % l][b], vb[:])
            yield


def st_peer_a(self, s, l):
    P = self.P
    I = self.I
    ada = self.S["ada%d" % l]
    with self.stage() as sc:
        identb = self.load_const(sc, "identb")
        identf = self.load_const(sc, "identf")
        iota16 = self.load_const(sc, "iota16")
        shb = self.bcast_row(sc, ada[s, 3072:4096], 1024, "sh2b")
        scb = self.bcast_row(sc, ada[s, 4096:5120], 1024, "sc2b")
        u2T = sc.sb([128, 8, T], BF16, "u2T")
        pst = sc.ps([128, 1024], BF16, "pst")
        self.make_uT(sc, self.S["xres"][s], scb, shb, u2T, identb, pst)
        P.dma("pool", self.S["u2T"], u2T[:])
        wq = sc.sb([128, 8, 2048], BF16, "wq")
        wst = [sc.sb([128, 2048], F32, "wqs%d" % i) for i in range(2)]
        for k in range(8):
            P.dma("sp", wst[k % 2][:], I["peer_wq"][l][k * 128:(k + 1) * 128, :])
            P.copy("pool" if k % 2 else "dve", wq[:, k, :], wst[k % 2][:])
        psK = sc.ps([128, 512], F32, "psK")
        keysT = sc.sb([128, 16, 128], BF16, "keysT")
        kst = [sc.sb([128, 128], F32, "kst%d" % i) for i in range(2)]
        for hp in range(16):
            P.dma("sp", kst[hp % 2][:], I["peer_keys"][l][hp // 2, hp % 2])
            P.tr(psK[:, 0:128], kst[hp % 2][:], identf[:])
            P.copy("act", keysT[:, hp, :], psK[:, 0:128])
        psQ = [sc.ps([128, 512], F32, "psQ%d" % i) for i in range(2)]
        psSc = sc.ps([128, 2048], F32, "psSc")
        qT = sc.sb([128, 16, 128], BF16, "qT")
        scs = sc.sb([128, 16, 128], F32, "scs")
        vals = sc.sb([128, 16, 16], F32, "vals")
        idxu = sc.sb([128, 16, 16], U32, "idxu")
        idxf = sc.sb([128, 16, 16], F32, "idxf")
        wk = sc.sb([128, 16, 128], F32, "wk")
        cand = sc.sb([128, 8, 256], F32, "cand")
        wk2 = sc.sb([128, 8, 256], F32, "wk2")
        best = sc.sb([128, 8, 16], F32, "best")
        cidx = sc.sb([128, 8, 16], U32, "cidx")
        iku = sc.sb([128, 128], U32, "iku")
        jku = sc.sb([128, 128], U32, "jku")
        ikf = sc.sb([128, 128], F32, "ikf")
        jkf = sc.sb([128, 128], F32, "jkf")
        eq = sc.sb([128, 8, 16, 16], F32, "eq")
        eq2 = sc.sb([128, 8, 16, 16], F32, "eq2")
        abg = sc.sb([128, 3, 128], F32, "abg")
        eg = sc.sb([128, 8, 16], F32, "eg")
        zz = sc.sb([128, 8], F32, "zz")
        abgT = [sc.sb([128, 3, 128], BF16, "abgT%d" % i) for i in range(2)]

        def top16b(items):
            for (vout, iout, src, scratch) in items:
                P.op("dve", lambda e, vout=vout, src=src: e.max(out=vout[:, 0:8], in_=src), reads=[src], writes=[vout[:, 0:8]])
            for (vout, iout, src, scratch) in items:
                P.op("dve", lambda e, vout=vout, iout=iout, src=src: e.max_index(out=iout[:, 0:8], in_max=vout[:, 0:8], in_values=src),
                     reads=[src, vout[:, 0:8]], writes=[iout[:, 0:8]])
            for (vout, iout, src, scratch) in items:
                P.op("dve", lambda e, vout=vout, src=src, scratch=scratch: e.match_replace(out=scratch, in_to_replace=vout[:, 0:8], in_values=src, imm_value=-1e30),
                     reads=[src, vout[:, 0:8]], writes=[scratch])
            for (vout, iout, src, scratch) in items:
                P.op("dve", lambda e, vout=vout, scratch=scratch: e.max(out=vout[:, 8:16], in_=scratch), reads=[scratch], writes=[vout[:, 8:16]])
            for (vout, iout, src, scratch) in items:
                P.op("dve", lambda e, vout=vout, iout=iout, scratch=scratch: e.max_index(out=iout[:, 8:16], in_max=vout[:, 8:16], in_values=scratch),
                     reads=[scratch, vout[:, 8:16]], writes=[iout[:, 8:16]])

        def pstr(t_):
            return t_[:].ap[0][0]

        for i in range(16):
            tsl = slice(i * 128, (i + 1) * 128)
            for hp in range(16):
                pq = psQ[(hp // 4) % 2]
                for k in range(8):
                    P.mm(pq[:, (hp % 4) * 128:(hp % 4 + 1) * 128], wq[:, k, hp * 128:(hp + 1) * 128], u2T[:, k, tsl],
                         start=(k == 0), stop=(k == 7))
                if hp % 4 == 3:
                    P.copy("act", qT[:, hp - 3:hp + 1, :], pq[:, :].rearrange("p (h t) -> p h t", h=4))
            for hp in range(16):
                P.mm(psSc[:, hp * 128:(hp + 1) * 128], qT[:, hp, :], keysT[:, hp, :])
            P.copy("act", scs[:], psSc[:, :].rearrange("p (h n) -> p h n", h=16))
            top16b([(vals[:, hp, :], idxu[:, hp, :], scs[:, hp, :], wk[:, hp, :]) for hp in range(16)])
            P.copy("dve", idxf[:], idxu[:])
            vb_ = vals[:]
            P.tt("dve", cand[:].rearrange("p h (i j) -> p h i j", i=16),
                 cap(vb_, int(vb_.offset), [[pstr(vals), 128], [32, 8], [1, 16], [0, 16]]),
                 cap(vb_, int(vb_.offset) + 16, [[pstr(vals), 128], [32, 8], [0, 16], [1, 16]]), ALU.add)
            top16b([(best[:, h, :], cidx[:, h, :], cand[:, h, :], wk2[:, h, :]) for h in range(8)])
            cflat = cidx[:].rearrange("p h k -> p (h k)")
            P.ts("dve", iku[:], cflat, 4, None, op0=ALU.logical_shift_right)
            P.ts("dve", jku[:], cflat, 15, None, op0=ALU.bitwise_and)
            P.copy("dve", ikf[:], iku[:])
            P.copy("dve", jkf[:], jku[:])
            io = iota16[:]
            iob = cap(io, int(io.offset), [[pstr(iota16), 128], [0, 8], [0, 16], [1, 16]])
            fb_ = idxf[:]
            for (kf_, off, col, e_) in ((ikf, 0, 0, eq), (jkf, 16, 1, eq2)):
                kb_ = kf_[:]
                P.tt("dve", e_[:], cap(kb_, int(kb_.offset), [[pstr(kf_), 128], [16, 8], [1, 16], [0, 16]]), iob, ALU.is_equal)
                P.tt("dve" if col == 0 else "pool", e_[:], e_[:],
                     cap(fb_, int(fb_.offset) + off, [[pstr(idxf), 128], [32, 8], [0, 16], [1, 16]]), ALU.mult)
                P.reduce("dve", abg[:, col, :].rearrange("p (h k) -> p h k", h=8), e_[:], ALU.add)
            bb_ = best[:]
            P.tt("dve", eg[:], best[:], cap(bb_, int(bb_.offset), [[pstr(best), 128], [16, 8], [0, 16]]), ALU.subtract)
            P.act(eg[:], eg[:], AF.Exp)
            P.reduce("dve", zz[:], eg[:], ALU.add)
            P.op("dve", lambda e: e.reciprocal(zz[:], zz[:]), reads=[zz[:]], writes=[zz[:]])
            zb_ = zz[:]
            P.tt("dve", abg[:, 2, :].rearrange("p (h k) -> p h k", h=8), eg[:],
                 cap(zb_, int(zb_.offset), [[pstr(zz), 128], [1, 8], [0, 16]]), ALU.mult)
            at = abgT[i % 2]
            for j in range(3):
                P.tr(psK[:, j * 128:(j + 1) * 128], abg[:, j, :], identf[:])
            P.copy("act", at[:], psK[:, 0:384].rearrange("p (j t) -> p j t", j=3))
            for j, nm in enumerate(("pa", "pb", "pg")):
                P.dma("pool", self.S[nm][:, tsl], at[:, j, :])


def st_peer_b(self, s, l):
    P = self.P
    I = self.I
    ada = self.S["ada%d" % l]
    dest = self.out[s] if l == 1 else self.S["xres"][s]
    with self.stage() as sc:
        iota = self.load_const(sc, "iota128")
        g2b = self.bcast_row(sc, ada[s, 5120:6144], 1024, "g2b")
        lng = self.bcast_row(sc, I["ln_g"][l, 1], 1024, "lng")
        lnb = self.bcast_row(sc, I["ln_b"][l, 1], 1024, "lnb")
        lb = LNBufs(sc)
        GTs = [sc.sb([128, 256, 64], BF16, "GT%d" % i) for i in range(2)]
        psBig = sc.ps([128, 2048], F32, "psBig")
        psA = [sc.ps([128, 512], F32, "psA%d" % i) for i in range(2)]
        psG = [sc.ps([128, 512], F32, "psG%d" % i) for i in range(2)]
        u2s = [sc.sb([128, 8, 256], BF16, "u2_%d" % i) for i in range(2)]
        abTs = [sc.sb([128, 3, 256], BF16, "abT%d" % i) for i in range(2)]
        OAs = [sc.sb([128, 16, 128], BF16, "OA%d" % i) for i in range(2)]
        OBs = [sc.sb([128, 16, 64], BF16, "OB%d" % i) for i in range(2)]
        uts = [sc.sb([128, 8, 128], BF16, "utb%d" % i) for i in range(6)]
        vbs = [sc.sb([128, DM], BF16, "vbb%d" % i) for i in range(6)]
        gls = [sc.sb([128, 256], F32, "gl%d" % i) for i in range(4)]
        Ws = [sc.sb([128, 256], BF16, "W%d" % i) for i in range(4)]
        xts = [sc.sb([128, DM], F32, "xt%d" % i) for i in range(2)]
        tms = [sc.sb([128, DM], F32, "tm%d" % i) for i in range(2)]
        io = iota[:]
        pio = io.ap[0][0]
        iob = cap(io, int(io.offset), [[pio, 128], [0, 16], [1, 128]])
        st = {"ne": 0, "nb": 0}

        def load_group(g):
            t0 = g * 256
            P.dma("sp", u2s[g % 2][:], self.S["u2T"][:, :, t0:t0 + 256])
            for j, nm in enumerate(("pa", "pb", "pg")):
                P.dma("sp", abTs[g % 2][:, j, :], self.S[nm][:, t0:t0 + 256])

        def gen_sub(g, half, sub):
            ab = abTs[g % 2][:]
            pab = ab.ap[0][0]
            OA, OB = OAs[sub % 2], OBs[sub % 2]
            av = cap(ab, int(ab.offset) + sub * 16, [[pab, 128], [1, 16], [0, 128]])
            bv = cap(ab, int(ab.offset) + 256 + sub * 16, [[pab, 128], [1, 16], [0, 64]])
            gv = cap(ab, int(ab.offset) + 512 + sub * 16, [[pab, 128], [1, 16], [0, 64]])
            iobh = cap(io, int(io.offset) + half * 64, [[pio, 128], [0, 16], [1, 64]])
            P.tt("dve", OB[:], iobh, bv, ALU.is_equal)
            P.tt("pool", OB[:], OB[:], gv, ALU.mult)
            P.tt("dve", OA[:], iob, av, ALU.is_equal)

        def mm_sub(g, half, sub):
            GT = GTs[half]
            OA, OB = OAs[sub % 2], OBs[sub % 2]
            for q in range(4):
                pg = psG[st["ne"] % 2]
                for t4 in range(4):
                    tk = q * 4 + t4
                    P.mm(pg[:, t4 * 64:(t4 + 1) * 64], OA[:, tk, :], OB[:, tk, :])
                tok = sub * 16 + q * 4
                P.copy("act", GT[:, tok:tok + 4, :],
                       pg[:, 0:256].rearrange("p (t b) -> p t b", t=4))
                st["ne"] += 1

        def build_sub(g, half, sub):
            gen_sub(g, half, sub)
            mm_sub(g, half, sub)

        def a_phase(g, b):
            u2 = u2s[g % 2]
            ut, vb = uts[b % 6], vbs[b % 6]
            P.dma("sp", ut[:], self.S["ut%d" % l][b])
            P.dma("sp", vb[:], self.S["vb%d" % l][b])
            pa_ = psA[b % 2][:, 0:256]
            for k in range(8):
                P.mm(pa_, ut[:, k, :], u2[:, k, :], start=(k == 0), stop=(k == 7))
            gl, W = gls[b % 4], Ws[b % 4]
            P.act(gl[:], pa_, AF.Gelu)
            P.tt("pool" if b % 2 else "dve", W[:], gl[:], GTs[b // 64][:, :, b % 64], ALU.mult)

        def v_phase(b):
            vb = vbs[b % 6]
            W = Ws[b % 4]
            for ti in range(2):
                for hf in range(2):
                    o0 = ti * 1024 + hf * 512
                    P.mm(psBig[:, o0:o0 + 512], W[:, ti * 128:(ti + 1) * 128], vb[:, hf * 512:(hf + 1) * 512],
                         start=(b == 0), stop=(b == 127))

        load_group(0)
        for sub in range(16):
            build_sub(0, 0, sub)
        for g in range(8):
            t0 = g * 256
            if g + 1 < 8:
                load_group(g + 1)
            for b in range(128):
                a_phase(g, b)
                if b >= 2:
                    v_phase(b - 2)
                tgt = (g, 1) if b < 64 else ((g + 1, 0) if g + 1 < 8 else None)
                if tgt is not None:
                    if b % 4 == 1:
                        sub = (b % 64) // 4
                        if sub > 0:
                            mm_sub(tgt[0], tgt[1], sub - 1)
                        gen_sub(tgt[0], tgt[1], sub)
                    elif b % 64 == 63:
                        mm_sub(tgt[0], tgt[1], 15)
            v_phase(126)
            v_phase(127)
            for ti in range(2):
                xt, tm = xts[ti], tms[ti]
                rows = slice(t0 + ti * 128, t0 + (ti + 1) * 128)
                P.dma("sp", xt[:], self.S["xres"][s][rows, :])
                P.tt("dve", tm[:], psBig[:, ti * 1024:(ti + 1) * 1024], g2b[:], ALU.mult)
                P.stt("dve", tm[:], xt[:], ALPHA_C, tm[:], ALU.mult, ALU.add)
                ln_tile(P, lb, tm, lng, lnb, dest[rows, :])


Builder.st_merge = st_merge


def st_uv(self, l):
    with self.stage() as sc:
        for _ in _uv_body(self, sc, l):
            pass


def st_pre(self, l):
    with self.stage() as sc:
        ga = _filter_body(self, sc, l)
        gb = _uv_body(self, sc, l)
        da = db = False
        while not (da and db):
            if not da:
                try:
                    next(ga)
                except StopIteration:
                    da = True
            for _ in range(2):
                if not db:
                    try:
                        next(gb)
                    except StopIteration:
                        db = True


Builder.st_uv = st_uv
Builder.st_pre = st_pre
Builder.st_peer_a = st_peer_a
Builder.st_peer_b = st_peer_b


def build_all(B):
    for l in range(2):
        B.st_pre(l)
        B.st_ada(l)
    for s in range(2):
        for l in range(2):
            B.st_inproj(s, l)
            B.st_attn_a(s, l)
            B.st_attn_c(s, l)
            B.st_hconv(s, l)
            B.st_hyena(s, l, 0)
            B.st_hyena(s, l, 1)
            B.st_merge(s, l)
            B.st_peer_a(s, l)
            B.st_peer_b(s, l)


def kernel(**inputs):
    from concourse.bass_utils import run_bass_kernel_spmd
    inp = {k: np.ascontiguousarray(np.asarray(v, dtype=np.float32)) for k, v in inputs.items()}
    B = Builder()
    build_all(B)
    B.finish()
    ncores = 8
    maps = []
    for c in range(ncores):
        m = {"x": np.ascontiguousarray(inp["x"][2 * c:2 * c + 2]),
             "c": np.ascontiguousarray(inp["c"][2 * c:2 * c + 2])}
        for k in WSHAPES:
            m[k] = inp[k]
        for k, v in B.hc.items():
            m["k_" + k] = v
        maps.append(m)
    res = run_bass_kernel_spmd(B.nc, maps, core_ids=list(range(ncores)))
    out = np.concatenate([np.asarray(r["out"], dtype=np.float32) for r in res.results], axis=0)
    return out
```

```python
import numpy as np
import concourse.bass as bass
import concourse.mybir as mybir
from contextlib import ExitStack

F32 = mybir.dt.float32
BF16 = mybir.dt.bfloat16
I32 = mybir.dt.int32
U32 = mybir.dt.uint32
ALU = mybir.AluOpType
AF = mybir.ActivationFunctionType
AX = mybir.AxisListType

ENGS = ("pe", "act", "dve", "pool", "sp")
EPOCH = 12000
NDMASEM = 24
SELF_SKIP = 1 << 30


def _region(ap):
    t = ap.tensor
    name = t.name
    pairs = ap.ap
    off = int(ap.offset)
    sp = str(ap.space)
    if "SB" in sp or "PSUM" in sp:
        pstride = pairs[0][0]
        if pstride == 0:
            pstride = 1 << 40
        p0 = off // pstride if pstride < (1 << 40) else 0
        f0 = off - p0 * pstride if pstride < (1 << 40) else off
        p1 = p0 + pairs[0][1]
        ext = 0
        for st, cn in pairs[1:]:
            ext += abs(st) * (cn - 1)
        if "PSUM" in sp:
            bank = 2048 // mybir.dt.size(ap.dtype)
            f1 = f0 + ext + 1
            return (name, 0, 128, (f0 // bank) * bank, ((f1 + bank - 1) // bank) * bank)
        return (name, p0, p1, f0, f0 + ext + 1)
    ext = 0
    for st, cn in pairs:
        ext += abs(st) * (cn - 1)
    return (name, 0, 1, off, off + ext + 1)


def _ovl(a, b):
    return a[1] < b[2] and b[1] < a[2] and a[3] < b[4] and b[3] < a[4]


def _covers(a, b):
    return a[1] <= b[1] and a[2] >= b[2] and a[3] <= b[3] and a[4] >= b[4]


class Prog:
    def __init__(self, nc):
        self.nc = nc
        self.es = ExitStack()
        self.streams = {e: [] for e in ENGS}
        self.cnt = {e: 0 for e in ENGS}
        self.cur = {}
        self.allsems = []
        for e in ENGS:
            self.cur[e] = self._newsem("pg_" + e)
        self.dsems = [self._newsem("dma%d" % i) for i in range(NDMASEM)]
        self.dcum = [0] * NDMASEM
        self.dnext = 0
        self.waited = {e: {} for e in ENGS}
        self.hist = {}
        self.floor = {}
        self.semobj = {}
        self.ninstr = 0

    def _newsem(self, name):
        s = self.es.enter_context(self.nc.semaphore(name + "_%d" % len(self.allsems)))
        self.allsems.append(s)
        return s

    def _deps(self, reads, writes, eng=None):
        deps = []
        for ap in reads:
            r = _region(ap)
            psum = "PSUM" in str(ap.space)
            for (reg, isw, ev) in self.hist.get(r[0], ()):
                if _ovl(reg, r) and (isw or (psum and ev[0] != eng)):
                    deps.append(ev)
            deps.extend(self.floor.get(r[0], ()))
        for ap in writes:
            r = _region(ap)
            for (reg, isw, ev) in self.hist.get(r[0], ()):
                if _ovl(reg, r):
                    deps.append(ev)
            deps.extend(self.floor.get(r[0], ()))
        return deps

    def _record(self, reads, writes, ev):
        for ap in writes:
            r = _region(ap)
            h = self.hist.setdefault(r[0], [])
            h[:] = [x for x in h if not _covers(r, x[0])]
            h.append((r, True, ev))
            self._trim(r[0])
        for ap in reads:
            r = _region(ap)
            h = self.hist.setdefault(r[0], [])
            rep = False
            for i, (reg, isw, oev) in enumerate(h):
                if (not isw) and reg == r and oev[0] == ev[0] and oev[3] is False:
                    h[i] = (r, False, ev)
                    rep = True
                    break
            if not rep:
                h.append((r, False, ev))
                self._trim(r[0])

    def _trim(self, name):
        h = self.hist[name]
        if len(h) > 96:
            drop = h[:32]
            del h[:32]
            fl = self.floor.setdefault(name, [])
            fl.extend(x[2] for x in drop)
            best = {}
            for ev in fl:
                k = id(ev[1])
                if k not in best or best[k][2] < ev[2]:
                    best[k] = ev
            self.floor[name] = list(best.values())

    def _waits(self, eng, deps, skip_self_pe=True):
        need = {}
        for ev in deps:
            src, sem, val, isdma = ev
            if src == eng and not isdma and eng == "pe":
                continue
            if src == eng and not isdma and sem is self.cur[eng] and self.cnt[eng] - val >= SELF_SKIP:
                continue
            k = id(sem)
            if self.waited[eng].get(k, 0) >= val:
                continue
            if k not in need or need[k][1] < val:
                need[k] = (sem, val)
        out = []
        for k, (sem, val) in need.items():
            self.waited[eng][k] = val
            out.append((sem, val))
        return out

    def op(self, eng, fn, reads=(), writes=()):
        reads = [a for a in reads if a is not None and not isinstance(a, (int, float))]
        deps = self._deps(reads, writes, eng)
        waits = self._waits(eng, deps)
        if self.cnt[eng] >= EPOCH:
            self.cur[eng] = self._newsem("pg_" + eng)
            self.cnt[eng] = 0
        sem = self.cur[eng]
        self.cnt[eng] += 1
        ev = (eng, sem, self.cnt[eng], False)
        self._record(reads, writes, ev)
        self.streams[eng].append((waits, fn, sem, 1))
        self.ninstr += 1

    def dma(self, q, out, in_, **kw):
        deps = self._deps([in_], [out], q)
        waits = self._waits(q, deps)
        k = self.dnext
        self.dnext = (self.dnext + 1) % NDMASEM
        sem = self.dsems[k]
        self.dcum[k] += 16
        ev = (q, sem, self.dcum[k], True)
        self._record([in_], [out], ev)
        self.streams[q].append((waits, lambda e, o=out, i=in_, kw=kw: e.dma_start(out=o, in_=i, **kw), sem, 16))
        self.ninstr += 1

    def barrier(self):
        evs = []
        for e in ENGS:
            if self.cnt[e] > 0:
                evs.append((e, self.cur[e], self.cnt[e], False))
        for k in range(NDMASEM):
            if self.dcum[k] > 0:
                evs.append(("dma", self.dsems[k], self.dcum[k], True))
        for e in ENGS:
            ws = []
            for (src, sem, val, isdma) in evs:
                if src == e and not isdma:
                    continue
                kk = id(sem)
                if self.waited[e].get(kk, 0) >= val:
                    continue
                self.waited[e][kk] = val
                ws.append((sem, val))
            if ws:
                self.streams[e].append((ws, None, None, 0))
        self.hist = {}
        self.floor = {}

    def emit(self):
        nc = self.nc
        streams = self.streams
        self.streams = {e: [] for e in ENGS}

        def run(engobj, lst):
            for (waits, fn, sem, inc) in lst:
                for (s, v) in waits:
                    engobj.wait_ge(s, v)
                if fn is not None:
                    ins = fn(engobj)
                    ins.then_inc(sem, inc)

        with nc.Block() as block:
            @block.tensor
            def _(e):
                run(e, streams["pe"])

            @block.scalar
            def _(e):
                run(e, streams["act"])

            @block.vector
            def _(e):
                run(e, streams["dve"])

            @block.gpsimd
            def _(e):
                run(e, streams["pool"])

            @block.sync
            def _(e):
                run(e, streams["sp"])

    def mm(self, out, lhsT, rhs, start=True, stop=True):
        self.op("pe", lambda e: e.matmul(out, lhsT, rhs, start=start, stop=stop),
                reads=[lhsT, rhs], writes=[out])

    def tr(self, out, in_, ident):
        self.op("pe", lambda e: e.transpose(out, in_, ident), reads=[in_, ident], writes=[out])

    def act(self, out, in_, func, bias=0.0, scale=1.0, accum_out=None, eng="act"):
        rd = [in_]
        if not isinstance(bias, (int, float)):
            rd.append(bias)
        if not isinstance(scale, (int, float)):
            rd.append(scale)
        wr = [out] + ([accum_out] if accum_out is not None else [])
        kw = {}
        if accum_out is not None:
            kw["accum_out"] = accum_out
        self.op("act", lambda e: e.activation(out, in_, func, bias=bias, scale=scale, **kw),
                reads=rd, writes=wr)

    def tt(self, eng, out, in0, in1, op):
        self.op(eng, lambda e: e.tensor_tensor(out, in0, in1, op), reads=[in0, in1], writes=[out])

    def ts(self, eng, out, in0, s1, s2=None, op0=ALU.mult, op1=None, accum_out=None):
        rd = [in0] + [s for s in (s1, s2) if s is not None and not isinstance(s, (int, float))]
        wr = [out] + ([accum_out] if accum_out is not None else [])
        kw = {}
        if op1 is not None:
            kw["op1"] = op1
        if accum_out is not None:
            kw["accum_out"] = accum_out
        self.op(eng, lambda e: e.tensor_scalar(out, in0, s1, s2, op0, **kw), reads=rd, writes=wr)

    def stt(self, eng, out, in0, scalar, in1, op0, op1):
        rd = [in0, in1] + ([scalar] if not isinstance(scalar, (int, float)) else [])
        self.op(eng, lambda e: e.scalar_tensor_tensor(out, in0, scalar, in1, op0, op1), reads=rd, writes=[out])

    def copy(self, eng, out, in_):
        if eng == "act":
            self.op("act", lambda e: e.copy(out, in_), reads=[in_], writes=[out])
        else:
            self.op(eng, lambda e: e.tensor_copy(out, in_), reads=[in_], writes=[out])

    def memset(self, eng, ap, val):
        self.op(eng, lambda e: e.memset(ap, val), reads=[], writes=[ap])

    def reduce(self, eng, out, in_, op, axis=AX.X):
        self.op(eng, lambda e: e.tensor_reduce(out, in_, axis, op), reads=[in_], writes=[out])

import math
import os
import ml_dtypes
from contextlib import contextmanager

NPBF = ml_dtypes.bfloat16
T = 2048
DM = 1024
ALPHA_C = 4.0 ** 0.25
LN_EPS_C = 1e-5
PI = math.pi


def host_consts():
    c = {}
    L = T
    N = 2 * T
    pos = np.arange(L, dtype=np.float32)
    inv = np.power(np.float32(10000.0), -(np.arange(0, 64, 2, dtype=np.float32) / np.float32(64))).astype(np.float32)
    ang = (pos[:, None] * inv[None, :]).astype(np.float32)
    cs, sn = np.cos(ang).astype(np.float32), np.sin(ang).astype(np.float32)
    p = np.arange(128)
    ropec = cs[:, p % 32].T.copy()
    sgn = np.where((p % 64) < 32, 1.0, -1.0).astype(np.float32)
    ropes = (sn[:, p % 32].T * sgn[:, None]).astype(np.float32)
    c["ropec"] = np.ascontiguousarray(ropec)
    c["ropes"] = np.ascontiguousarray(ropes)
    t = np.arange(L, dtype=np.int64)
    idx = (t[:, None] * t[None, :]) % N
    th = 2.0 * np.pi * idx.astype(np.float64) / N
    C = np.cos(th)
    S = -np.sin(th)
    alt = np.where(t % 2 == 0, 1.0, -1.0)
    Sf = S.copy()
    Sf[:, 0] = alt
    def blk_f(M):
        return np.ascontiguousarray(M.reshape(16, 128, 16, 128).transpose(2, 1, 0, 3)).astype(NPBF)
    c["cf"] = blk_f(C)
    c["sf"] = blk_f(Sf)
    Ci = (2.0 / N) * C
    Ci[0, :] = 1.0 / N
    Si = (2.0 / N) * S
    Si[0, :] = alt / N
    def blk_i(M):
        return np.ascontiguousarray(M.reshape(16, 128, 8, 256).transpose(2, 1, 0, 3)).astype(NPBF)
    c["ci"] = blk_i(Ci)
    c["si"] = blk_i(Si)
    idxf = np.arange(L, dtype=np.float32)
    tt_ = (idxf / np.float32(L - 1)).astype(np.float32)
    w = (np.float32(2.0 * math.pi) * idxf / np.float32(L)).astype(np.float32)
    bands = np.linspace(1e-4, 15, 16, dtype=np.float32)
    ang2 = (w[:, None] * bands[None, :]).astype(np.float32)
    z = np.concatenate([tt_[:, None], np.cos(ang2), -np.sin(ang2)], axis=-1).astype(np.float32)
    c["zembT"] = np.ascontiguousarray(z.T)
    c["negt"] = np.ascontiguousarray((-tt_).reshape(16, 128).T)
    a = np.arange(128)[:, None]
    b = np.arange(128)[None, :]
    mA = np.concatenate([(np.abs(128 * (sg - 1) + a - b) <= 64) for sg in range(3)], axis=1)
    c["mA"] = mA.astype(NPBF)
    mC = np.stack([(b <= a), np.ones((128, 128), bool), (a <= b)], axis=1)
    c["mC"] = np.ascontiguousarray(mC).astype(NPBF)
    sel = np.zeros((65, 64), np.float32)
    sel[64, :] = 1.0
    c["sel"] = sel
    c["ones"] = np.ones((128, 128), np.float32)
    c["identf"] = np.eye(128, dtype=np.float32)
    c["identb"] = np.eye(128).astype(NPBF)
    sw = np.zeros((128, 128), np.float32)
    sw[np.arange(128), np.arange(128) ^ 32] = 1.0
    c["swapm"] = sw.astype(NPBF)
    c["iota16"] = np.tile(np.arange(16, dtype=np.float32)[None, :], (128, 1))
    c["iota128"] = np.tile(np.arange(128)[None, :], (128, 1)).astype(NPBF)
    return c


CONST_DT = {"ropec": F32, "ropes": F32, "cf": BF16, "sf": BF16, "ci": BF16, "si": BF16, "zembT": F32,
            "negt": F32, "mA": BF16, "mC": BF16, "sel": F32, "ones": F32, "identf": F32, "identb": BF16,
            "iota16": F32, "iota128": BF16, "swapm": BF16}

WSHAPES = {
    "w_ada": [2, 1024, 6144], "b_ada": [2, 6144], "w_in": [2, 1024, 7680], "conv_w": [2, 3, 1536],
    "conv_b": [2, 1536], "hy_w1": [2, 33, 64], "hy_b1": [2, 64], "hy_w2": [2, 64, 64], "hy_b2": [2, 64],
    "hy_w3": [2, 64, 2048], "hy_freq": [2, 2, 64], "hy_log_decay": [2, 2048], "hy_bias": [2, 2, 512],
    "attn_sink": [2, 8], "w_branch_a": [2, 256, 1024], "w_branch_b": [2, 512, 1024],
    "w_branch_c": [2, 512, 1024], "w_out": [2, 1024, 1024], "ln_g": [2, 2, 1024], "ln_b": [2, 2, 1024],
    "peer_wq": [2, 1024, 2048], "peer_keys": [2, 8, 2, 128, 128], "peer_u": [2, 16384, 1024],
    "peer_v": [2, 16384, 1024],
}


def cap(ap, offset, pairs):
    return bass.AP(ap.tensor, offset, [list(p) for p in pairs])


class Scope:
    def __init__(self, B):
        self.B = B
        self.es = ExitStack()

    def sb(self, shape, dt, name="t"):
        self.B.uid += 1
        t = self.es.enter_context(self.B.nc.sbuf_tensor("%s_%d" % (name, self.B.uid), list(shape), dt))
        return t

    def ps(self, shape, dt=F32, name="ps"):
        self.B.uid += 1
        return self.es.enter_context(self.B.nc.psum_tensor("%s_%d" % (name, self.B.uid), list(shape), dt))


class Builder:
    def __init__(self, dbg=()):
        self.nc = nc = bass.Bass("TRN2", target_bir_lowering=False)
        self.P = Prog(nc)
        self.uid = 0
        self.dbg = set(dbg)
        self.I = {}
        self.I["x"] = nc.dram_tensor("x", [2, T, DM], F32, kind="ExternalInput").ap()
        self.I["c"] = nc.dram_tensor("c", [2, DM], F32, kind="ExternalInput").ap()
        for k, shp in WSHAPES.items():
            self.I[k] = nc.dram_tensor(k, shp, F32, kind="ExternalInput").ap()
        self.C = {}
        hc = host_consts()
        self.hc = hc
        for k, v in hc.items():
            self.C[k] = nc.dram_tensor("k_" + k, list(v.shape), CONST_DT[k], kind="ExternalInput").ap()
        self.out = nc.dram_tensor("out", [2, T, DM], F32, kind="ExternalOutput").ap()
        S = self.S = {}

        def scr(name, shape, dt):
            kind = "ExternalOutput" if name in self.dbg else "Internal"
            S[name] = nc.dram_tensor("s_" + name, list(shape), dt, kind=kind).ap()
        for l in range(2):
            scr("ada%d" % l, [2, 6144], F32)
            scr("hspec%d" % l, [3, 2048, 1024], BF16)
            scr("ut%d" % l, [128, 128, 8, 128], BF16)
            scr("vb%d" % l, [128, 128, 1024], BF16)
        scr("xres", [2, T, DM], F32)
        scr("qTa", [768, T], BF16)
        scr("kTa", [768, T], BF16)
        scr("va", [3, T, 256], BF16)
        scr("hyT", [1536, T], F32)
        scr("hcT", [1536, T], F32)
        scr("z1T", [512, T], F32)
        scr("qTc", [512, T], BF16)
        scr("kTc", [128, T], BF16)
        scr("vc", [T, 128], BF16)
        scr("gateT", [3072, T], BF16)
        scr("yaT", [256, T], BF16)
        scr("ybT", [512, T], BF16)
        scr("ycT", [512, T], BF16)
        scr("u2T", [128, 8, T], BF16)
        scr("scs", [T, 2048], F32)
        scr("pa", [128, T], BF16)
        scr("pb", [128, T], BF16)
        scr("pg", [128, T], BF16)

    @contextmanager
    def stage(self):
        sc = Scope(self)
        try:
            yield sc
        finally:
            self.P.barrier()
            self.P.emit()
            sc.es.close()

    def finish(self):
        self.P.es.close()

    def bcast_row(self, sc, src_ap_1d, n, name="bc"):
        t = sc.sb([128, n], F32, name)
        src = cap(src_ap_1d, int(src_ap_1d.offset), [[0, 128], [1, n]])
        self.P.dma("sp", t[:], src)
        return t

    def load_const(self, sc, name):
        v = self.hc[name]
        t = sc.sb(list(v.shape), CONST_DT[name], "c_" + name)
        self.P.dma("sp", t[:], self.C[name])
        return t

    def st_ada(self, l):
        P = self.P
        with self.stage() as sc:
            cT = sc.sb([128, 8, 2], F32, "cT")
            for b in range(2):
                P.dma("sp", cT[:, :, b], self.I["c"][b].rearrange("(k p) -> p k", p=128),
                      allow_slow_non_contiguous=True)
            P.act(cT[:], cT[:], AF.Silu)
            ada = sc.sb([2, 6144], F32, "ada")
            bb = sc.sb([2, 6144], F32, "bb")
            ba = self.I["b_ada"][l]
            P.dma("sp", bb[:], cap(ba, int(ba.offset), [[0, 2], [1, 6144]]))
            ws = [sc.sb([128, 8, 512], F32, "w%d" % i) for i in range(2)]
            pss = [sc.ps([128, 512], F32) for i in range(2)]
            for cb in range(12):
                w = ws[cb % 2]
                P.dma("sp", w[:], self.I["w_ada"][l][:, cb * 512:(cb + 1) * 512].rearrange("(k p) n -> p k n", p=128))
                ps = pss[cb % 2]
                for k in range(8):
                    P.mm(ps[0:2, :], cT[:, k, :], w[:, k, :], start=(k == 0), stop=(k == 7))
                P.tt("dve", ada[:, cb * 512:(cb + 1) * 512], ps[0:2, :], bb[:, cb * 512:(cb + 1) * 512], ALU.add)
            for o in (1024, 4096):
                P.ts("dve", ada[:, o:o + 1024], ada[:, o:o + 1024], 1.0, None, op0=ALU.add)
            P.dma("sp", self.S["ada%d" % l], ada[:])

    def make_uT(self, sc, xsrc, scb, shb, uT, identb, pst, ntiles=16, tok0=0, xkeep=None):
        P = self.P
        xts = [sc.sb([128, DM], F32, "xt%d" % i) for i in range(2)]
        ubs = [sc.sb([128, DM], BF16, "ub%d" % i) for i in range(2)]
        uf = sc.sb([128, DM], F32, "uf")
        for i in range(ntiles):
            xt = xts[i % 2] if xkeep is None else xkeep[i]
            ub = ubs[i % 2]
            P.dma("sp", xt[:], xsrc[tok0 + i * 128: tok0 + (i + 1) * 128, :])
            P.tt("dve", uf[:], xt[:], scb[:], ALU.mult)
            P.tt("pool", ub[:], uf[:], shb[:], ALU.add)
            for k in range(8):
                P.tr(pst[:, k * 128:(k + 1) * 128], ub[:, k * 128:(k + 1) * 128], identb[:])
            P.copy("act", uT[:, :, i * 128:(i + 1) * 128], pst[:].rearrange("p (k t) -> p k t", k=8))

    def st_inproj(self, s, l):
        P = self.P
        xsrc = self.I["x"][s] if l == 0 else self.S["xres"][s]
        ada = self.S["ada%d" % l]
        with self.stage() as sc:
            identb = self.load_const(sc, "identb")
            ropec = self.load_const(sc, "ropec")
            ropes = self.load_const(sc, "ropes")
            shb = self.bcast_row(sc, ada[s, 0:1024], 1024, "shb")
            scb = self.bcast_row(sc, ada[s, 1024:2048], 1024, "scb")
            uT = sc.sb([128, 8, T], BF16, "uT")
            pst = sc.ps([128, 1024], BF16, "pst")
            self.make_uT(sc, xsrc, scb, shb, uT, identb, pst)
            wfs = [sc.sb([128, 8, 256], F32, "wf%d" % i) for i in range(2)]
            wbs = [sc.sb([128, 8, 256], BF16, "wb%d" % i) for i in range(2)]
            pss = [sc.ps([128, 512], F32, "pf%d" % i) for i in range(4)]
            stg = [sc.sb([128, T], BF16, "stg%d" % i) for i in range(3)]
            stf = [sc.sb([128, T], F32, "stf%d" % i) for i in range(2)]
            tbs = [sc.sb([128, 512], BF16, "tbs%d" % i) for i in range(2)]
            ps2 = [sc.ps([128, 512], F32, "ps2_%d" % i) for i in range(2)]
            swapm = self.load_const(sc, "swapm")
            tA = [sc.sb([128, 512], F32, "tA%d" % i) for i in range(2)]
            tB = [sc.sb([128, 512], F32, "tB%d" % i) for i in range(2)]
            vst = [sc.sb([128, 16, 256], BF16, "vst%d" % i) for i in range(2)]
            cnt = {"ps": 0, "stg": 0, "stf": 0, "tmp": 0, "v": 0}

            def tokview(ap2, tt, D):
                if D == 1:
                    return ap2[:, tt * 512:(tt + 1) * 512]
                if D == 4:
                    return ap2.rearrange("p (j r) -> p r j", r=4)[:, tt, :]
                return ap2.rearrange("p (j r) -> p r j", r=16)[:, 4 * tt:4 * tt + 4, :]

            def shp(ap2, D):
                if D == 16:
                    return ap2.rearrange("p (r j) -> p r j", r=4)
                return ap2

            def tok128(ap2, i, D):
                if D == 1:
                    return ap2[:, i * 128:(i + 1) * 128]
                if D == 4:
                    return ap2.rearrange("p (j r) -> p r j", r=4)[:, i // 4, (i % 4) * 128:(i % 4 + 1) * 128]
                return ap2.rearrange("p (j r) -> p r j", r=16)[:, i, :]

            def fm_chunk(wb, cc, kind, D, dest):
                if kind == "hy":
                    st = stf[cnt["stf"] % 2]
                    cnt["stf"] += 1
                else:
                    st = stg[cnt["stg"] % 3]
                    cnt["stg"] += 1
                for tt in range(4):
                    ps = pss[cnt["ps"] % 4]
                    cnt["ps"] += 1
                    for k in range(8):
                        P.mm(ps[:, :], wb[:, k, cc * 128:(cc + 1) * 128], uT[:, k, tt * 512:(tt + 1) * 512],
                             start=(k == 0), stop=(k == 7))
                    o = st[:, tt * 512:(tt + 1) * 512]
                    if kind == "hy":
                        P.copy("act", o, ps[:, :])
                    elif kind == "gate":
                        P.act(o, ps[:, :], AF.Sigmoid)
                    else:
                        j = cnt["tmp"] % 2
                        cnt["tmp"] += 1
                        tb_, a_, b_, p2 = tbs[j], tA[j], tB[j], ps2[j]
                        P.copy("act", tb_[:], ps[:, :])
                        P.mm(p2[:, :], swapm[:], tb_[:])
                        ts_ = slice(tt * 512, (tt + 1) * 512)
                        P.tt("dve", a_[:], ps[:, :], ropec[:, ts_], ALU.mult)
                        P.tt("dve", b_[:], p2[:, :], ropes[:, ts_], ALU.mult)
                        if D == 1:
                            P.tt("pool", o, a_[:], b_[:], ALU.subtract)
                        else:
                            n_ = 512 // D
                            ov = st[:, :].rearrange("p (r j) -> p j r", r=D)[:, tt * n_:(tt + 1) * n_, :]
                            P.tt("pool", ov, a_[:].rearrange("p (j r) -> p j r", r=D),
                                 b_[:].rearrange("p (j r) -> p j r", r=D), ALU.subtract)
                P.dma("pool", dest, st[:])

            def tm_block(wb, c0, ncols, D, dest):
                st = vst[cnt["v"] % 2]
                cnt["v"] += 1
                for i in range(16):
                    ps = pss[cnt["ps"] % 4]
                    cnt["ps"] += 1
                    for k in range(8):
                        P.mm(ps[:, 0:ncols], tok128(uT[:, k, :], i, D), wb[:, k, c0:c0 + ncols],
                             start=(k == 0), stop=(k == 7))
                    P.copy("act", st[:, i, 0:ncols], ps[:, 0:ncols])
                P.dma("pool", dest.rearrange("(i p) c -> p i c", p=128), st[:, :, 0:ncols])

            DG = (1, 4, 16)
            for blk in range(30):
                wf, wb = wfs[blk % 2], wbs[blk % 2]
                col = blk * 256
                P.dma("sp", wf[:], self.I["w_in"][l][:, col:col + 256].rearrange("(k p) n -> p k n", p=128))
                P.copy("pool" if blk % 2 else "dve", wb[:], wf[:])
                if col < 1536:
                    nm = "qTa" if col < 768 else "kTa"
                    base = 0 if col < 768 else 768
                    for cc in range(2):
                        j = (col - base) // 128 + cc
                        fm_chunk(wb, cc, "rope", DG[j // 2], self.S[nm][j * 128:(j + 1) * 128, :])
                elif col < 2304:
                    g = (col - 1536) // 256
                    tm_block(wb, 0, 256, DG[g], self.S["va"][g])
                elif col < 3840:
                    for cc in range(2):
                        j = (col - 2304) // 128 + cc
                        fm_chunk(wb, cc, "hy", 1, self.S["hyT"][j * 128:(j + 1) * 128, :])
                elif col < 4352:
                    for cc in range(2):
                        j = (col - 3840) // 128 + cc
                        fm_chunk(wb, cc, "rope", 1, self.S["qTc"][j * 128:(j + 1) * 128, :])
                elif col < 4608:
                    fm_chunk(wb, 0, "rope", 1, self.S["kTc"][:, :])
                    tm_block(wb, 128, 128, 1, self.S["vc"])
                else:
                    for cc in range(2):
                        j = (col - 4608) // 128 + cc
                        fm_chunk(wb, cc, "gate", 1, self.S["gateT"][j * 128:(j + 1) * 128, :])


def _attn_finalize(B, sc, numer, nheads, sel, psB, dest):
    P = B.P
    recs = [sc.sb([64, 512], F32, "rec%d" % i) for i in range(2)]
    ysts = [sc.sb([64, T], BF16, "yst%d" % i) for i in range(2)]
    n = 0
    for h in range(nheads):
        yst = ysts[h % 2]
        for tt in range(4):
            ps = psB[n % 2]
            rec = recs[n % 2]
            n += 1
            P.mm(ps[0:64, :], sel[0:65, 0:64], numer[0:65, h, tt * 512:(tt + 1) * 512])
            P.op("dve", lambda e, o=rec[:], i=ps[0:64, :]: e.reciprocal(o, i), reads=[ps[0:64, :]], writes=[rec[:]])
            P.tt("pool", yst[:, tt * 512:(tt + 1) * 512], numer[0:64, h, tt * 512:(tt + 1) * 512], rec[:], ALU.mult)
        P.dma("pool", dest[h * 64:(h + 1) * 64, :], yst[:])


def st_attn_a(self, s, l):
    P = self.P
    with self.stage() as sc:
        mA = self.load_const(sc, "mA")
        sel = self.load_const(sc, "sel")
        numer = sc.sb([65, 4, T], F32, "numer")
        q = sc.sb([64, 4, T], BF16, "q")
        k = sc.sb([64, 4, T], BF16, "k")
        va = sc.sb([128, 16, 4, 65], BF16, "va")
        psS = [sc.ps([128, 512], F32, "psS%d" % i) for i in range(3)]
        psO = [sc.ps([128, 512], F32, "psO%d" % i) for i in range(2)]
        psB = [sc.ps([128, 512], F32, "psB%d" % i) for i in range(2)]
        pT = [sc.sb([128, 384], BF16, "pT%d" % i) for i in range(2)]
        pT2 = [sc.sb([128, 384], BF16, "pTm%d" % i) for i in range(3)]
        P.memset("pool", va[:], 1.0)
        n = 0
        for g, D in enumerate((1, 4, 16)):
            Ls = T // D
            nb = Ls // 128
            for h in range(4):
                hh = g * 4 + h
                P.dma("sp", q[:, h, :], self.S["qTa"][hh * 64:(hh + 1) * 64, :])
                P.dma("sp", k[:, h, :], self.S["kTa"][hh * 64:(hh + 1) * 64, :])
                P.dma("sp", va[:, :, h, 0:64],
                      self.S["va"][g][:, h * 64:(h + 1) * 64].rearrange("(i p) d -> p i d", p=128))
            jobs = [(h, rho, i) for h in range(4) for rho in range(D) for i in range(nb)]

            def s_phase(n, job):
                h, rho, i = job
                base = rho * Ls
                js = [j for j in (i - 1, i, i + 1) if 0 <= j < nb]
                ps = psS[n % 3]
                p1 = pT[n % 2]
                p2 = pT2[n % 3]
                q0 = base + i * 128
                for j in js:
                    sg = j - i + 1
                    P.mm(ps[:, sg * 128:(sg + 1) * 128], k[0:64, h, base + j * 128: base + (j + 1) * 128],
                         q[0:64, h, q0:q0 + 128])
                c0 = (js[0] - i + 1) * 128
                c1 = (js[-1] - i + 2) * 128
                P.act(p1[:, c0:c1], ps[:, c0:c1], AF.Exp, scale=0.125)
                P.tt("pool" if n % 2 else "dve", p2[:, c0:c1], p1[:, c0:c1], mA[:, c0:c1], ALU.mult)

            def pv_phase(n, job):
                h, rho, i = job
                js = [j for j in (i - 1, i, i + 1) if 0 <= j < nb]
                po = psO[n % 2]
                p2 = pT2[n % 3]
                for jj, j in enumerate(js):
                    sg = j - i + 1
                    P.mm(po[0:65, 0:128], va[:, rho * nb + j, h, :], p2[:, sg * 128:(sg + 1) * 128],
                         start=(jj == 0), stop=(jj == len(js) - 1))
                if D == 1:
                    nv = numer[0:65, h, i * 128:(i + 1) * 128]
                else:
                    nv = numer[0:65, h, :].rearrange("p (j r) -> p r j", r=D)[:, rho, i * 128:(i + 1) * 128]
                if g == 0:
                    P.copy("act", nv, po[0:65, 0:128])
                else:
                    P.tt("dve", nv, po[0:65, 0:128], nv, ALU.add)
            for n_, job in enumerate(jobs):
                s_phase(n + n_, job)
                if n_ > 0:
                    pv_phase(n + n_ - 1, jobs[n_ - 1])
            pv_phase(n + len(jobs) - 1, jobs[-1])
            n += len(jobs)
        _attn_finalize(self, sc, numer, 4, sel, psB, self.S["yaT"])


def st_attn_c(self, s, l):
    P = self.P
    with self.stage() as sc:
        mC = self.load_const(sc, "mC")
        sel = self.load_const(sc, "sel")
        numer = sc.sb([65, 8, T], F32, "numerc")
        q = sc.sb([64, 8, T], BF16, "qc")
        k = sc.sb([64, 2, T], BF16, "kc")
        va = sc.sb([128, 16, 2, 65], BF16, "vca")
        sk = sc.sb([65, 8], F32, "sk")
        psS = [sc.ps([128, 512], F32, "psS%d" % i) for i in range(3)]
        psO = [sc.ps([128, 512], F32, "psO%d" % i) for i in range(2)]
        psB = [sc.ps([128, 512], F32, "psB%d" % i) for i in range(2)]
        pT = [sc.sb([128, 3, 512], BF16, "pTc%d" % i) for i in range(2)]
        P.memset("pool", va[:], 1.0)
        for h in range(8):
            P.dma("sp", q[:, h, :], self.S["qTc"][h * 64:(h + 1) * 64, :])
        for h in range(2):
            P.dma("sp", k[:, h, :], self.S["kTc"][h * 64:(h + 1) * 64, :])
            P.dma("sp", va[:, :, h, 0:64], self.S["vc"][:, h * 64:(h + 1) * 64].rearrange("(i p) d -> p i d", p=128))
        asink = self.I["attn_sink"][l]
        P.dma("sp", sk[64:65, :], cap(asink, int(asink.offset), [[0, 1], [1, 8]]))
        P.act(sk[64:65, :], sk[64:65, :], AF.Exp)
        n = 0
        mcb = mC[:]
        pst = mcb.ap[0][0]
        jobs = [(kv, i) for kv in range(2) for i in range(16)]

        def s_phase(n, job):
            kv, i = job
            js = [j for j in (i - 1, i, i + 1) if 0 <= j < 16]
            p1 = pT[n % 2]
            for j in js:
                sg = j - i + 1
                ps = psS[sg]
                P.mm(ps[:, :].rearrange("p (h t) -> p h t", h=4), k[0:64, kv, j * 128:(j + 1) * 128],
                     q[0:64, 4 * kv:4 * kv + 4, i * 128:(i + 1) * 128])
                P.act(p1[:, sg, :], ps[:, :], AF.Exp, scale=0.125)
                if sg != 1:
                    mv = cap(mcb, int(mcb.offset) + sg * 128, [[pst, 128], [0, 4], [1, 128]])
                    pv = p1[:, sg, :].rearrange("p (h t) -> p h t", h=4)
                    P.tt("pool" if sg == 0 else "dve", pv, pv, mv, ALU.mult)

        def pv_phase(n, job):
            kv, i = job
            js = [j for j in (i - 1, i, i + 1) if 0 <= j < 16]
            p1 = pT[n % 2]
            po = psO[n % 2]
            for jj, j in enumerate(js):
                sg = j - i + 1
                P.mm(po[0:65, :], va[:, j, kv, :], p1[:, sg, :], start=(jj == 0), stop=(jj == len(js) - 1))
            P.copy("act", numer[0:65, 4 * kv:4 * kv + 4, i * 128:(i + 1) * 128],
                   po[0:65, :].rearrange("p (h t) -> p h t", h=4))
        for n_, job in enumerate(jobs):
            s_phase(n_, job)
            if n_ > 0:
                pv_phase(n_ - 1, jobs[n_ - 1])
        pv_phase(len(jobs) - 1, jobs[-1])
        for h in range(8):
            P.ts("dve", numer[64:65, h, :], numer[64:65, h, :], sk[64:65, h:h + 1], None, op0=ALU.add)
        _attn_finalize(self, sc, numer, 8, sel, psB, self.S["ycT"])


Builder.st_attn_a = st_attn_a
Builder.st_attn_c = st_attn_c


def _filter_body(self, sc, l):
    P = self.P
    I = self.I
    if True:
        zembT = self.load_const(sc, "zembT")
        negt = self.load_const(sc, "negt")
        ones = self.load_const(sc, "ones")
        w1 = sc.sb([33, 64], F32, "w1")
        w2 = sc.sb([64, 64], F32, "w2")
        w3 = sc.sb([64, 2048], F32, "w3")
        P.dma("sp", w1[:], I["hy_w1"][l])
        P.dma("sp", w2[:], I["hy_w2"][l])
        P.dma("sp", w3[:], I["hy_w3"][l])
        prm = sc.sb([64, 4], F32, "prm")
        for j, ap1 in enumerate((I["hy_b1"][l], I["hy_b2"][l], I["hy_freq"][l][0], I["hy_freq"][l][1])):
            P.dma("sp", prm[:, j:j + 1], cap(ap1, int(ap1.offset), [[1, 64], [1, 1]]))
        fb = sc.sb([64, 2], F32, "fb")
        P.tt("dve", fb[:, 0:1], prm[:, 0:1], prm[:, 2:3], ALU.mult)
        P.tt("dve", fb[:, 1:2], prm[:, 1:2], prm[:, 3:4], ALU.mult)
        psF = sc.ps([128, 2048], F32, "psF")
        psN = sc.ps([128, 1024], F32, "psN")
        h1T = sc.sb([64, T], F32, "h1T")
        h2T = sc.sb([64, T], F32, "h2T")
        arg = sc.sb([64, 512], F32, "arg")
        ki = sc.sb([64, 512], I32, "ki")
        kf = sc.sb([64, 512], F32, "kf")
        mk = sc.sb([64, 512], F32, "mk")

        def sin_layer(dst, lhsT, src, kk, fcol, fbcol):
            for tt in range(4):
                ps = psF[0:64, tt * 512:(tt + 1) * 512]
                P.mm(ps, lhsT, src[0:kk, tt * 512:(tt + 1) * 512])
                P.ts("dve", arg[:], ps, prm[:, fcol:fcol + 1], fb[:, fbcol:fbcol + 1], op0=ALU.mult, op1=ALU.add)
                P.ts("dve", arg[:], arg[:], 1.0 / (2.0 * PI), None, op0=ALU.mult)
                P.copy("dve", ki[:], arg[:])
                P.copy("dve", kf[:], ki[:])
                P.tt("dve", arg[:], arg[:], kf[:], ALU.subtract)
                P.ts("dve", mk[:], arg[:], 0.5, None, op0=ALU.is_gt)
                P.tt("dve", arg[:], arg[:], mk[:], ALU.subtract)
                P.ts("dve", mk[:], arg[:], -0.5, None, op0=ALU.is_lt)
                P.tt("dve", arg[:], arg[:], mk[:], ALU.add)
                P.act(dst[:, tt * 512:(tt + 1) * 512], arg[:], AF.Sin, scale=6.28318)
                yield
        yield from sin_layer(h1T, w1[0:33, :], zembT, 33, 2, 0)
        yield from sin_layer(h2T, w2[0:64, :], h1T, 64, 3, 1)
        ld = self.bcast_row(sc, I["hy_log_decay"][l], 2048, "ld")
        P.act(ld[:], ld[:], AF.Exp)
        gsum = sc.sb([128, 16, 1024], BF16, "gsum")
        gdiff = sc.sb([128, 16, 1024], BF16, "gdiff")
        dec = sc.sb([128, 2048], F32, "dec")
        filt = sc.sb([128, 2048], F32, "filt")
        sq = dec
        for mc in range(16):
            for cb in range(4):
                P.mm(psF[:, cb * 512:(cb + 1) * 512], h2T[0:64, mc * 128:(mc + 1) * 128], w3[0:64, cb * 512:(cb + 1) * 512])
            P.act(dec[:], ld[:], AF.Exp, scale=negt[:, mc:mc + 1])
            P.tt("dve", filt[:], psF[:, :], dec[:], ALU.mult)
            if mc == 0:
                P.memset("dve", filt[0:1, 1024:2048], 0.0)
            P.act(sq[:], filt[:], AF.Square)
            for half in range(2):
                for d in range(2):
                    c0 = d * 1024 + half * 512
                    P.mm(psN[:, half * 512:(half + 1) * 512], ones[:, :], sq[:, c0:c0 + 512],
                         start=(mc == 0 and d == 0), stop=(mc == 15 and d == 1))
            P.tt("pool", gsum[:, mc, :], filt[:, 0:1024], filt[:, 1024:2048], ALU.add)
            P.tt("pool", gdiff[:, mc, :], filt[:, 0:1024], filt[:, 1024:2048], ALU.subtract)
            yield
        rs = sc.sb([128, 1024], F32, "rs")
        P.ts("dve", rs[:], psN[:, :], 1e-12, None, op0=ALU.add)
        P.act(rs[:], rs[:], AF.Sqrt)
        P.op("dve", lambda e: e.reciprocal(rs[:], rs[:]), reads=[rs[:]], writes=[rs[:]])
        cfs = [sc.sb([128, 16, 128], BF16, "cf%d" % i) for i in range(2)]
        sfs = [sc.sb([128, 16, 128], BF16, "sf%d" % i) for i in range(2)]
        ho = [[sc.sb([128, 512], BF16, "ho%d_%d" % (i, j)) for j in range(3)] for i in range(2)]
        hs = self.S["hspec%d" % l]
        n = 0
        for fc in range(16):
            cf, sf = cfs[fc % 2], sfs[fc % 2]
            P.dma("sp", cf[:], self.C["cf"][fc])
            P.dma("sp", sf[:], self.C["sf"][fc])
            for half in range(2):
                cs = slice(half * 512, (half + 1) * 512)
                pre, pim, px = psF[:, 0:512], psF[:, 512:1024], psF[:, 1024:1536]
                for mc in range(16):
                    P.mm(pre, cf[:, mc, :], gsum[:, mc, cs], start=(mc == 0), stop=(mc == 15))
                for mc in range(16):
                    P.mm(pim, sf[:, mc, :], gdiff[:, mc, cs], start=(mc == 0), stop=(mc == 15))
                if fc == 0:
                    for mc in range(16):
                        P.mm(px, sf[:, mc, :], gsum[:, mc, cs], start=(mc == 0), stop=(mc == 15))
                hre, him, hrb = ho[n % 2]
                n += 1
                P.tt("dve", hre[:], pre, rs[:, cs], ALU.mult)
                P.tt("dve", him[:], pim, rs[:, cs], ALU.mult)
                P.copy("pool", hrb[:], hre[:])
                if fc == 0:
                    P.memset("pool", him[0:1, :], 0.0)
                    P.tt("dve", hrb[0:1, :], px[0:1, :], rs[0:1, cs], ALU.mult)
                for j, t_ in enumerate((hre, him, hrb)):
                    P.dma("pool", hs[j, fc * 128:(fc + 1) * 128, cs], t_[:])
                yield


def st_hconv(self, s, l):
    P = self.P
    with self.stage() as sc:
        cw = sc.sb([128, 12, 3], F32, "cw")
        cb = sc.sb([128, 12], F32, "cb")
        for i in range(3):
            P.dma("sp", cw[:, :, i], self.I["conv_w"][l][i].rearrange("(k p) -> p k", p=128), allow_slow_non_contiguous=True)
        P.dma("sp", cb[:], self.I["conv_b"][l].rearrange("(k p) -> p k", p=128), allow_slow_non_contiguous=True)
        xs = [sc.sb([128, T], F32, "hx%d" % i) for i in range(2)]
        os_ = [sc.sb([128, T], F32, "ho%d" % i) for i in range(2)]
        for cc in range(12):
            x, o = xs[cc % 2], os_[cc % 2]
            P.dma("sp", x[:], self.S["hyT"][cc * 128:(cc + 1) * 128, :])
            P.act(o[:], x[:], AF.Identity, bias=cb[:, cc:cc + 1], scale=cw[:, cc, 1:2])
            P.stt("dve", o[:, 1:T], x[:, 0:T - 1], cw[:, cc, 0:1], o[:, 1:T], ALU.mult, ALU.add)
            P.stt("dve", o[:, 0:T - 1], x[:, 1:T], cw[:, cc, 2:3], o[:, 0:T - 1], ALU.mult, ALU.add)
            P.dma("pool", self.S["hcT"][cc * 128:(cc + 1) * 128, :], o[:])


def st_hyena(self, s, l, o):
    P = self.P
    zsrc = self.S["hcT"][0:512, :] if o == 0 else self.S["z1T"]
    gsrc = self.S["hcT"][(o + 1) * 512:(o + 2) * 512, :]
    hs = self.S["hspec%d" % l]
    with self.stage() as sc:
        identb = self.load_const(sc, "identb")
        hb = sc.sb([128, 4], F32, "hb")
        P.dma("sp", hb[:], self.I["hy_bias"][l][o].rearrange("(k p) -> p k", p=128), allow_slow_non_contiguous=True)
        zb = sc.sb([128, 4, T], BF16, "zb")
        zfs = [sc.sb([128, T], F32, "zf%d" % i) for i in range(2)]
        for cc in range(4):
            zf = zfs[cc % 2]
            P.dma("sp", zf[:], zsrc[cc * 128:(cc + 1) * 128, :])
            P.copy("pool" if cc % 2 else "dve", zb[:, cc, :], zf[:])
        zTM = sc.sb([128, 16, 512], BF16, "zTM")
        psT = [sc.ps([128, 1024], BF16, "psT%d" % i) for i in range(2)]
        for tc in range(16):
            pt = psT[tc % 2]
            for cc in range(4):
                P.tr(pt[:, cc * 128:(cc + 1) * 128], zb[:, cc, tc * 128:(tc + 1) * 128], identb[:])
            P.copy("act", zTM[:, tc, :], pt[:, 0:512])
        Yre = sc.sb([128, 16, 512], BF16, "Yre")
        Yim = sc.sb([128, 16, 512], BF16, "Yim")
        cfs = [sc.sb([128, 16, 128], BF16, "cf%d" % i) for i in range(2)]
        sfs = [sc.sb([128, 16, 128], BF16, "sf%d" % i) for i in range(2)]
        hts = [[sc.sb([128, 512], BF16, "h%d_%d" % (i, j)) for j in range(3)] for i in range(2)]
        tmps = [[sc.sb([128, 512], F32, "tm%d_%d" % (i, j)) for j in range(4)] for i in range(2)]
        psR = [sc.ps([128, 512], F32, "psR%d" % i) for i in range(2)]
        psI = [sc.ps([128, 512], F32, "psI%d" % i) for i in range(2)]
        cs = slice(o * 512, (o + 1) * 512)
        for fc in range(16):
            cf, sf = cfs[fc % 2], sfs[fc % 2]
            P.dma("sp", cf[:], self.C["cf"][fc])
            P.dma("sp", sf[:], self.C["sf"][fc])
            hre, him, hrb = hts[fc % 2]
            for j, t_ in enumerate((hre, him, hrb)):
                P.dma("sp", t_[:], hs[j, fc * 128:(fc + 1) * 128, cs])
            pr, pi = psR[fc % 2], psI[fc % 2]
            for tc in range(16):
                P.mm(pr[:, :], cf[:, tc, :], zTM[:, tc, :], start=(tc == 0), stop=(tc == 15))
            for tc in range(16):
                P.mm(pi[:, :], sf[:, tc, :], zTM[:, tc, :], start=(tc == 0), stop=(tc == 15))
            t1, t2, t3, t4 = tmps[fc % 2]
            P.tt("dve", t1[:], pr[:, :], hre[:], ALU.mult)
            P.tt("dve", t2[:], pi[:, :], him[:], ALU.mult)
            P.tt("dve", t3[:], pr[:, :], him[:], ALU.mult)
            P.tt("dve", t4[:], pi[:, :], hrb[:], ALU.mult)
            P.tt("pool", Yre[:, fc, :], t1[:], t2[:], ALU.subtract)
            P.tt("pool", Yim[:, fc, :], t3[:], t4[:], ALU.add)
        cis = [sc.sb([128, 16, 256], BF16, "ci%d" % i) for i in range(2)]
        sis = [sc.sb([128, 16, 256], BF16, "si%d" % i) for i in range(2)]
        psY = [sc.ps([128, 512], F32, "psY%d" % i) for i in range(2)]
        zps = [sc.sb([128, 256], F32, "zp%d" % i) for i in range(2)]
        gps = [sc.sb([128, 256], F32, "gp%d" % i) for i in range(2)]
        tms = [sc.sb([128, 256], F32, "tq%d" % i) for i in range(2)]
        odt = F32 if o == 0 else BF16
        ors = [sc.sb([128, 256], odt, "or%d" % i) for i in range(2)]
        dest = self.S["z1T"] if o == 0 else self.S["ybT"]
        n = 0
        for tt in range(8):
            ci, si = cis[tt % 2], sis[tt % 2]
            P.dma("sp", ci[:], self.C["ci"][tt])
            P.dma("sp", si[:], self.C["si"][tt])
            ts_ = slice(tt * 256, (tt + 1) * 256)
            for cc in range(4):
                py = psY[n % 2]
                zp, gp, tm, orr = zps[n % 2], gps[n % 2], tms[n % 2], ors[n % 2]
                n += 1
                P.dma("sp", zp[:], zsrc[cc * 128:(cc + 1) * 128, ts_])
                P.dma("sp", gp[:], gsrc[cc * 128:(cc + 1) * 128, ts_])
                for fc in range(16):
                    P.mm(py[:, 0:256], Yre[:, fc, cc * 128:(cc + 1) * 128], ci[:, fc, :], start=(fc == 0), stop=False)
                for fc in range(16):
                    P.mm(py[:, 0:256], Yim[:, fc, cc * 128:(cc + 1) * 128], si[:, fc, :], start=False, stop=(fc == 15))
                P.stt("dve", tm[:], zp[:], hb[:, cc:cc + 1], py[:, 0:256], ALU.mult, ALU.add)
                P.tt("pool", orr[:], tm[:], gp[:], ALU.mult)
                P.dma("pool", dest[cc * 128:(cc + 1) * 128, ts_], orr[:])


def st_filter(self, l):
    with self.stage() as sc:
        for _ in _filter_body(self, sc, l):
            pass


Builder.st_filter = st_filter
Builder.st_hconv = st_hconv
Builder.st_hyena = st_hyena

import os


class LNBufs:
    def __init__(self, sc):
        self.s1 = sc.sb([128, 1], F32, "ln_s1")
        self.nm = sc.sb([128, 1], F32, "ln_nm")
        self.ss = sc.sb([128, 1], F32, "ln_ss")
        self.rstd = sc.sb([128, 1], F32, "ln_rstd")
        self.sq = sc.sb([128, DM], F32, "ln_sq")
        self.y = [sc.sb([128, DM], F32, "ln_y%d" % i) for i in range(2)]
        self.n = 0


def ln_tile(P, lb, r, lng, lnb, dest):
    y = lb.y[lb.n % 2]
    lb.n += 1
    P.reduce("dve", lb.s1[:], r[:], ALU.add)
    P.ts("dve", lb.nm[:], lb.s1[:], -1.0 / DM, None, op0=ALU.mult)
    P.act(lb.sq[:], r[:], AF.Square, bias=lb.nm[:, 0:1])
    P.reduce("dve", lb.ss[:], lb.sq[:], ALU.add)
    P.ts("dve", lb.rstd[:], lb.ss[:], 1.0 / DM, LN_EPS_C, op0=ALU.mult, op1=ALU.add)
    P.act(lb.rstd[:], lb.rstd[:], AF.Sqrt)
    P.op("dve", lambda e: e.reciprocal(lb.rstd[:], lb.rstd[:]), reads=[lb.rstd[:]], writes=[lb.rstd[:]])
    P.ts("dve", y[:], r[:], lb.nm[:, 0:1], lb.rstd[:, 0:1], op0=ALU.add, op1=ALU.mult)
    P.tt("pool", y[:], y[:], lng[:], ALU.mult)
    P.tt("pool", y[:], y[:], lnb[:], ALU.add)
    P.dma("pool", dest, y[:])


def st_merge(self, s, l):
    P = self.P
    I = self.I
    xsrc = I["x"][s] if l == 0 else self.S["xres"][s]
    ada = self.S["ada%d" % l]
    with self.stage() as sc:
        ys = {}
        for nm, nk in (("yaT", 2), ("ybT", 4), ("ycT", 4)):
            t_ = sc.sb([128, nk, T], BF16, nm)
            for k in range(nk):
                P.dma("sp", t_[:, k, :], self.S[nm][k * 128:(k + 1) * 128, :])
            ys[nm] = t_
        wst = [sc.sb([128, DM], F32, "wst%d" % i) for i in range(2)]
        ws = {}
        n = 0
        for nm, nk in (("w_branch_a", 2), ("w_branch_b", 4), ("w_branch_c", 4), ("w_out", 8)):
            t_ = sc.sb([128, nk, DM], BF16, nm)
            for k in range(nk):
                st = wst[n % 2]
                P.dma("sp", st[:], I[nm][l][k * 128:(k + 1) * 128, :])
                P.copy("pool" if n % 2 else "dve", t_[:, k, :], st[:])
                n += 1
            ws[nm] = t_
        mergedT = sc.sb([128, 8, T], BF16, "mergedT")
        psb = [sc.ps([128, 512], F32, "psbr%d" % i) for i in range(3)]
        gts = [[sc.sb([128, 512], BF16, "g%d_%d" % (i, j)) for j in range(3)] for i in range(2)]
        ms = [[sc.sb([128, 512], F32, "m%d_%d" % (i, j)) for j in range(3)] for i in range(2)]
        n = 0
        for fc in range(8):
            for tt in range(4):
                ts_ = slice(tt * 512, (tt + 1) * 512)
                g3 = gts[n % 2]
                m3 = ms[n % 2]
                n += 1
                for j, (yn, wn, nk) in enumerate((("yaT", "w_branch_a", 2), ("ybT", "w_branch_b", 4), ("ycT", "w_branch_c", 4))):
                    for k in range(nk):
                        P.mm(psb[j][:, :], ws[wn][:, k, fc * 128:(fc + 1) * 128], ys[yn][:, k, ts_],
                             start=(k == 0), stop=(k == nk - 1))
                    P.dma("sp", g3[j][:], self.S["gateT"][j * 1024 + fc * 128: j * 1024 + (fc + 1) * 128, ts_])
                    P.tt("dve", m3[j][:], psb[j][:, :], g3[j][:], ALU.mult)
                P.tt("pool", m3[0][:], m3[0][:], m3[1][:], ALU.add)
                P.tt("pool", mergedT[:, fc, ts_], m3[0][:], m3[2][:], ALU.add)
        g1b = self.bcast_row(sc, ada[s, 2048:3072], 1024, "g1b")
        lng = self.bcast_row(sc, I["ln_g"][l, 0], 1024, "lng")
        lnb = self.bcast_row(sc, I["ln_b"][l, 0], 1024, "lnb")
        lb = LNBufs(sc)
        psM = [sc.ps([128, 1024], F32, "psM%d" % i) for i in range(2)]
        xts = [sc.sb([128, DM], F32, "xt%d" % i) for i in range(2)]
        tms = [sc.sb([128, DM], F32, "tm%d" % i) for i in range(2)]
        for i in range(16):
            pm = psM[i % 2]
            xt = xts[i % 2]
            tm = tms[i % 2]
            P.dma("sp", xt[:], xsrc[i * 128:(i + 1) * 128, :])
            for hf in range(2):
                for k in range(8):
                    P.mm(pm[:, hf * 512:(hf + 1) * 512], mergedT[:, k, i * 128:(i + 1) * 128],
                         ws["w_out"][:, k, hf * 512:(hf + 1) * 512], start=(k == 0), stop=(k == 7))
            P.tt("dve", tm[:], pm[:, :], g1b[:], ALU.mult)
            P.stt("dve", tm[:], xt[:], ALPHA_C, tm[:], ALU.mult, ALU.add)
            ln_tile(P, lb, tm, lng, lnb, self.S["xres"][s][i * 128:(i + 1) * 128, :])


def _uv_body(self, sc, l):
    P = self.P
    if True:
        identb = self.load_const(sc, "identb")
        ufs = [sc.sb([128, DM], F32, "uf%d" % i) for i in range(2)]
        vfs = [sc.sb([128, DM], F32, "vf%d" % i) for i in range(2)]
        ubs = [sc.sb([128, DM], BF16, "ub%d" % i) for i in range(2)]
        vbs = [sc.sb([128, DM], BF16, "vb%d" % i) for i in range(2)]
        uts = [sc.sb([128, 8, 128], BF16, "ut%d" % i) for i in range(2)]
        pst = [sc.ps([128, 1024], BF16, "pst%d" % i) for i in range(2)]
        U = self.I["peer_u"][l]
        V = self.I["peer_v"][l]
        for b in range(128):
            uf, vf, ub, vb, ut, pt = ufs[b % 2], vfs[b % 2], ubs[b % 2], vbs[b % 2], uts[b % 2], pst[b % 2]
            P.dma("sp", uf[:], cap(U, int(U.offset) + b * DM, [[128 * DM, 128], [1, DM]]))
            P.dma("sp", vf[:], cap(V, int(V.offset) + b * DM, [[128 * DM, 128], [1, DM]]))
            P.copy("dve", ub[:], uf[:])
            P.copy("pool", vb[:], vf[:])
            for k in range(8):
                P.tr(pt[:, k * 128:(k + 1) * 128], ub[:, k * 128:(k + 1) * 128], identb[:])
            P.copy("act", ut[:], pt[:, :].rearrange("p (k a) -> p k a", k=8))
            P.dma("pool", self.S["ut%d" % l][b], ut[:])
            P.dma("pool", self.S["vb%d" % l][b], vb[:])
            yield


def st_peer_a(self, s, l):
    P = self.P
    I = self.I
    ada = self.S["ada%d" % l]
    with self.stage() as sc:
        identb = self.load_const(sc, "identb")
        identf = self.load_const(sc, "identf")
        iota16 = self.load_const(sc, "iota16")
        shb = self.bcast_row(sc, ada[s, 3072:4096], 1024, "sh2b")
        scb = self.bcast_row(sc, ada[s, 4096:5120], 1024, "sc2b")
        u2T = sc.sb([128, 8, T], BF16, "u2T")
        pst = sc.ps([128, 1024], BF16, "pst")
        self.make_uT(sc, self.S["xres"][s], scb, shb, u2T, identb, pst)
        P.dma("pool", self.S["u2T"], u2T[:])
        wq = sc.sb([128, 8, 2048], BF16, "wq")
        wst = [sc.sb([128, 2048], F32, "wqs%d" % i) for i in range(2)]
        for k in range(8):
            P.dma("sp", wst[k % 2][:], I["peer_wq"][l][k * 128:(k + 1) * 128, :])
            P.copy("pool" if k % 2 else "dve", wq[:, k, :], wst[k % 2][:])
        psK = sc.ps([128, 512], F32, "psK")
        keysT = sc.sb([128, 16, 128], BF16, "keysT")
        kst = [sc.sb([128, 128], F32, "kst%d" % i) for i in range(2)]
        for hp in range(16):
            P.dma("sp", kst[hp % 2][:], I["peer_keys"][l][hp // 2, hp % 2])
            P.tr(psK[:, 0:128], kst[hp % 2][:], identf[:])
            P.copy("act", keysT[:, hp, :], psK[:, 0:128])
        psQ = [sc.ps([128, 512], F32, "psQ%d" % i) for i in range(2)]
        psSc = sc.ps([128, 2048], F32, "psSc")
        qT = sc.sb([128, 16, 128], BF16, "qT")
        scs = sc.sb([128, 16, 128], F32, "scs")
        vals = sc.sb([128, 16, 16], F32, "vals")
        idxu = sc.sb([128, 16, 16], U32, "idxu")
        idxf = sc.sb([128, 16, 16], F32, "idxf")
        wk = sc.sb([128, 16, 128], F32, "wk")
        cand = sc.sb([128, 8, 256], F32, "cand")
        wk2 = sc.sb([128, 8, 256], F32, "wk2")
        best = sc.sb([128, 8, 16], F32, "best")
        cidx = sc.sb([128, 8, 16], U32, "cidx")
        iku = sc.sb([128, 128], U32, "iku")
        jku = sc.sb([128, 128], U32, "jku")
        ikf = sc.sb([128, 128], F32, "ikf")
        jkf = sc.sb([128, 128], F32, "jkf")
        eq = sc.sb([128, 8, 16, 16], F32, "eq")
        eq2 = sc.sb([128, 8, 16, 16], F32, "eq2")
        abg = sc.sb([128, 3, 128], F32, "abg")
        eg = sc.sb([128, 8, 16], F32, "eg")
        zz = sc.sb([128, 8], F32, "zz")
        abgT = [sc.sb([128, 3, 128], BF16, "abgT%d" % i) for i in range(2)]

        def top16b(items):
            for (vout, iout, src, scratch) in items:
                P.op("dve", lambda e, vout=vout, src=src: e.max(out=vout[:, 0:8], in_=src), reads=[src], writes=[vout[:, 0:8]])
            for (vout, iout, src, scratch) in items:
                P.op("dve", lambda e, vout=vout, iout=iout, src=src: e.max_index(out=iout[:, 0:8], in_max=vout[:, 0:8], in_values=src),
                     reads=[src, vout[:, 0:8]], writes=[iout[:, 0:8]])
            for (vout, iout, src, scratch) in items:
                P.op("dve", lambda e, vout=vout, src=src, scratch=scratch: e.match_replace(out=scratch, in_to_replace=vout[:, 0:8], in_values=src, imm_value=-1e30),
                     reads=[src, vout[:, 0:8]], writes=[scratch])
            for (vout, iout, src, scratch) in items:
                P.op("dve", lambda e, vout=vout, scratch=scratch: e.max(out=vout[:, 8:16], in_=scratch), reads=[scratch], writes=[vout[:, 8:16]])
            for (vout, iout, src, scratch) in items:
                P.op("dve", lambda e, vout=vout, iout=iout, scratch=scratch: e.max_index(out=iout[:, 8:16], in_max=vout[:, 8:16], in_values=scratch),
                     reads=[scratch, vout[:, 8:16]], writes=[iout[:, 8:16]])

        def pstr(t_):
            return t_[:].ap[0][0]

        for i in range(16):
            tsl = slice(i * 128, (i + 1) * 128)
            for hp in range(16):
                pq = psQ[(hp // 4) % 2]
                for k in range(8):
                    P.mm(pq[:, (hp % 4) * 128:(hp % 4 + 1) * 128], wq[:, k, hp * 128:(hp + 1) * 128], u2T[:, k, tsl],
                         start=(k == 0), stop=(k == 7))
                if hp % 4 == 3:
                    P.copy("act", qT[:, hp - 3:hp + 1, :], pq[:, :].rearrange("p (h t) -> p h t", h=4))
            for hp in range(16):
                P.mm(psSc[:, hp * 128:(hp + 1) * 128], qT[:, hp, :], keysT[:, hp, :])
            P.copy("act", scs[:], psSc[:, :].rearrange("p (h n) -> p h n", h=16))
            if os.environ.get('PA_SKIP'):
                continue
            top16b([(vals[:, hp, :], idxu[:, hp, :], scs[:, hp, :], wk[:, hp, :]) for hp in range(16)])
            P.copy("act", idxf[:], idxu[:])
            vb_ = vals[:]
            P.tt("pool", cand[:].rearrange("p h (i j) -> p h i j", i=16),
                 cap(vb_, int(vb_.offset), [[pstr(vals), 128], [32, 8], [1, 16], [0, 16]]),
                 cap(vb_, int(vb_.offset) + 16, [[pstr(vals), 128], [32, 8], [0, 16], [1, 16]]), ALU.add)
            top16b([(best[:, h, :], cidx[:, h, :], cand[:, h, :], wk2[:, h, :]) for h in range(8)])
            cflat = cidx[:].rearrange("p h k -> p (h k)")
            P.ts("dve", iku[:], cflat, 4, None, op0=ALU.logical_shift_right)
            P.ts("dve", jku[:], cflat, 15, None, op0=ALU.bitwise_and)
            P.copy("act", ikf[:], iku[:])
            P.copy("act", jkf[:], jku[:])
            io = iota16[:]
            iob = cap(io, int(io.offset), [[pstr(iota16), 128], [0, 8], [0, 16], [1, 16]])
            fb_ = idxf[:]
            for (kf_, off, col, e_) in ((ikf, 0, 0, eq), (jkf, 16, 1, eq2)):
                kb_ = kf_[:]
                P.tt("dve", e_[:], cap(kb_, int(kb_.offset), [[pstr(kf_), 128], [16, 8], [1, 16], [0, 16]]), iob, ALU.is_equal)
                P.tt("pool", e_[:], e_[:],
                     cap(fb_, int(fb_.offset) + off, [[pstr(idxf), 128], [32, 8], [0, 16], [1, 16]]), ALU.mult)
                P.reduce("dve", abg[:, col, :].rearrange("p (h k) -> p h k", h=8), e_[:], ALU.add)
            bb_ = best[:]
            P.tt("pool", eg[:], best[:], cap(bb_, int(bb_.offset), [[pstr(best), 128], [16, 8], [0, 16]]), ALU.subtract)
            P.act(eg[:], eg[:], AF.Exp)
            P.reduce("dve", zz[:], eg[:], ALU.add)
            P.op("dve", lambda e: e.reciprocal(zz[:], zz[:]), reads=[zz[:]], writes=[zz[:]])
            zb_ = zz[:]
            P.tt("pool", abg[:, 2, :].rearrange("p (h k) -> p h k", h=8), eg[:],
                 cap(zb_, int(zb_.offset), [[pstr(zz), 128], [1, 8], [0, 16]]), ALU.mult)
            at = abgT[i % 2]
            for j in range(3):
                P.tr(psK[:, j * 128:(j + 1) * 128], abg[:, j, :], identf[:])
            P.copy("act", at[:], psK[:, 0:384].rearrange("p (j t) -> p j t", j=3))
            for j, nm in enumerate(("pa", "pb", "pg")):
                P.dma("pool", self.S[nm][:, tsl], at[:, j, :])


def st_peer_b(self, s, l, a2=False):
    P = self.P
    I = self.I
    ada = self.S["ada%d" % l]
    dest = self.out[s] if l == 1 else self.S["xres"][s]
    with self.stage() as sc:
        iota = self.load_const(sc, "iota128")
        g2b = self.bcast_row(sc, ada[s, 5120:6144], 1024, "g2b")
        lng = self.bcast_row(sc, I["ln_g"][l, 1], 1024, "lng")
        lnb = self.bcast_row(sc, I["ln_b"][l, 1], 1024, "lnb")
        lb = LNBufs(sc)
        GTs = [sc.sb([128, 256, 64], BF16, "GT%d" % i) for i in range(2)]
        psBig = sc.ps([128, 2048], F32, "psBig")
        psA = [sc.ps([128, 512], F32, "psA%d" % i) for i in range(2)]
        psG = [sc.ps([128, 512], F32, "psG%d" % i) for i in range(2)]
        u2s = [sc.sb([128, 8, 256], BF16, "u2_%d" % i) for i in range(2)]
        abTs = [sc.sb([128, 3, 256], BF16, "abT%d" % i) for i in range(2)]
        OAs = [sc.sb([128, 16, 128], BF16, "OA%d" % i) for i in range(2)]
        OBs = [sc.sb([128, 16, 64], BF16, "OB%d" % i) for i in range(2)]
        uts = [sc.sb([128, 8, 128], BF16, "utb%d" % i) for i in range(5)]
        vbs = [sc.sb([128, DM], BF16, "vbb%d" % i) for i in range(5)]
        gls = [sc.sb([128, 256], F32, "gl%d" % i) for i in range(4)]
        Ws = [sc.sb([128, 256], BF16, "W%d" % i) for i in range(4)]
        xts = [sc.sb([128, DM], F32, "xt%d" % i) for i in range(1)] * 2
        tms = [sc.sb([128, DM], F32, "tm%d" % i) for i in range(1)] * 2
        io = iota[:]
        pio = io.ap[0][0]
        iob = cap(io, int(io.offset), [[pio, 128], [0, 16], [1, 128]])
        st = {"ne": 0, "nb": 0}

        def load_group(g):
            t0 = g * 256
            P.dma("sp", u2s[g % 2][:], self.S["u2T"][:, :, t0:t0 + 256])
            for j, nm in enumerate(("pa", "pb", "pg")):
                P.dma("sp", abTs[g % 2][:, j, :], self.S[nm][:, t0:t0 + 256])

        def gen_sub(g, half, sub):
            ab = abTs[g % 2][:]
            pab = ab.ap[0][0]
            OA, OB = OAs[sub % 2], OBs[sub % 2]
            av = cap(ab, int(ab.offset) + sub * 16, [[pab, 128], [1, 16], [0, 128]])
            bv = cap(ab, int(ab.offset) + 256 + sub * 16, [[pab, 128], [1, 16], [0, 64]])
            gv = cap(ab, int(ab.offset) + 512 + sub * 16, [[pab, 128], [1, 16], [0, 64]])
            iobh = cap(io, int(io.offset) + half * 64, [[pio, 128], [0, 16], [1, 64]])
            P.tt("dve", OB[:], iobh, bv, ALU.is_equal)
            P.tt("pool", OB[:], OB[:], gv, ALU.mult)
            P.tt("dve", OA[:], iob, av, ALU.is_equal)

        def mm_sub(g, half, sub):
            GT = GTs[half]
            OA, OB = OAs[sub % 2], OBs[sub % 2]
            for q in range(4):
                pg = psG[st["ne"] % 2]
                for t4 in range(4):
                    tk = q * 4 + t4
                    P.mm(pg[:, t4 * 64:(t4 + 1) * 64], OA[:, tk, :], OB[:, tk, :])
                tok = sub * 16 + q * 4
                P.copy("act", GT[:, tok:tok + 4, :],
                       pg[:, 0:256].rearrange("p (t b) -> p t b", t=4))
                st["ne"] += 1

        def build_sub(g, half, sub):
            gen_sub(g, half, sub)
            mm_sub(g, half, sub)

        def a_phase(g, b):
            u2 = u2s[g % 2]
            ut, vb = uts[b % 5], vbs[b % 5]
            P.dma("sp", ut[:], self.S["ut%d" % l][b])
            P.dma("sp", vb[:], self.S["vb%d" % l][b])
            pa_ = psA[b % 2][:, 0:256]
            for k in range(8):
                P.mm(pa_, ut[:, k, :], u2[:, k, :], start=(k == 0), stop=(k == 7))
            gl, W = gls[b % 4], Ws[b % 4]
            P.act(gl[:], pa_, AF.Gelu)
            P.tt("pool", W[:], gl[:], GTs[b // 64][:, :, b % 64], ALU.mult)

        def v_phase(b):
            vb = vbs[b % 5]
            W = Ws[b % 4]
            for ti in range(2):
                for hf in range(2):
                    o0 = ti * 1024 + hf * 512
                    P.mm(psBig[:, o0:o0 + 512], W[:, ti * 128:(ti + 1) * 128], vb[:, hf * 512:(hf + 1) * 512],
                         start=(b == 0), stop=(b == 127))

        a2gen = None
        if a2:
            bf = A2Bufs(self, sc)
            trp_ab = psG[0][:, 256:512]
            trp_g = psG[1][:, 256:384]

            def a2_pair(g2):
                for ti_ in (2 * g2, 2 * g2 + 1):
                    yield from _a2_tile(self, bf, ti_, trp_ab, trp_g)
        load_group(0)
        for sub in range(16):
            build_sub(0, 0, sub)
        for g in range(8):
            t0 = g * 256
            if a2 and a2gen is not None:
                for _ in a2gen:
                    pass
                a2gen = None
            if g + 1 < 8:
                load_group(g + 1)
            if a2 and g + 2 < 8:
                a2gen = a2_pair(g + 2)
            for b in range(128):
                if a2gen is not None and b % 2 == 0:
                    try:
                        next(a2gen)
                    except StopIteration:
                        a2gen = None
                a_phase(g, b)
                if b >= 2:
                    v_phase(b - 2)
                tgt = (g, 1) if b < 64 else ((g + 1, 0) if g + 1 < 8 else None)
                if tgt is not None:
                    if b % 4 == 1:
                        sub = (b % 64) // 4
                        if sub > 0:
                            mm_sub(tgt[0], tgt[1], sub - 1)
                        gen_sub(tgt[0], tgt[1], sub)
                    elif b % 64 == 63:
                        mm_sub(tgt[0], tgt[1], 15)
            v_phase(126)
            v_phase(127)
            for ti in range(2):
                xt, tm = xts[ti], tms[ti]
                rows = slice(t0 + ti * 128, t0 + (ti + 1) * 128)
                P.dma("sp", xt[:], self.S["xres"][s][rows, :])
                P.tt("dve", tm[:], psBig[:, ti * 1024:(ti + 1) * 1024], g2b[:], ALU.mult)
                P.stt("dve", tm[:], xt[:], ALPHA_C, tm[:], ALU.mult, ALU.add)
                ln_tile(P, lb, tm, lng, lnb, dest[rows, :])


Builder.st_merge = st_merge


def st_uv(self, l):
    with self.stage() as sc:
        for _ in _uv_body(self, sc, l):
            pass


def st_pre(self, l):
    with self.stage() as sc:
        ga = _filter_body(self, sc, l)
        gb = _uv_body(self, sc, l)
        da = db = False
        while not (da and db):
            if not da:
                try:
                    next(ga)
                except StopIteration:
                    da = True
            for _ in range(2):
                if not db:
                    try:
                        next(gb)
                    except StopIteration:
                        db = True


Builder.st_uv = st_uv
Builder.st_pre = st_pre
Builder.st_peer_a = st_peer_a
Builder.st_peer_b = st_peer_b


class A2Bufs:
    def __init__(self, B, sc):
        self.identf = B.load_const(sc, "identf")
        self.iota16 = B.load_const(sc, "iota16")
        self.scs = sc.sb([128, 16, 128], F32, "a2scs")
        self.wk = sc.sb([128, 16, 128], F32, "a2wk")
        self.vals = sc.sb([128, 16, 16], F32, "a2vals")
        self.idxu = sc.sb([128, 16, 16], U32, "a2idxu")
        self.idxf = sc.sb([128, 16, 16], F32, "a2idxf")
        self.best = sc.sb([128, 8, 16], F32, "a2best")
        self.cidx = sc.sb([128, 8, 16], U32, "a2cidx")
        self.iku = sc.sb([128, 128], U32, "a2iku")
        self.jku = sc.sb([128, 128], U32, "a2jku")
        self.ikf = sc.sb([128, 128], F32, "a2ikf")
        self.jkf = sc.sb([128, 128], F32, "a2jkf")
        self.eq = sc.sb([128, 8, 16, 16], F32, "a2eq")
        self.abg = sc.sb([128, 3, 128], F32, "a2abg")
        self.eg = sc.sb([128, 8, 16], F32, "a2eg")
        self.zz = sc.sb([128, 8], F32, "a2zz")
        self.abgT = [sc.sb([128, 3, 128], BF16, "a2abgT%d" % i) for i in range(2)]


def _a2_tile(B, bf, i, trp_ab, trp_g):
    P = B.P
    scs, wk, vals, idxu, idxf = bf.scs, bf.wk, bf.vals, bf.idxu, bf.idxf
    best, cidx, eq, abg, eg, zz = bf.best, bf.cidx, bf.eq, bf.abg, bf.eg, bf.zz
    cand = scs[:].rearrange("p a b -> p (a b)").rearrange("p (h n) -> p h n", h=8)
    wk2 = wk[:].rearrange("p a b -> p (a b)").rearrange("p (h n) -> p h n", h=8)
    tsl = slice(i * 128, (i + 1) * 128)
    P.dma("sp", scs[:], B.S["scs"][tsl, :].rearrange("p (h n) -> p h n", h=16))

    def pstr(t_):
        return t_[:].ap[0][0]

    def top16b(items, nsplit):
        steps = []
        for (vout, iout, src, scratch) in items:
            steps.append((lambda e, vout=vout, src=src: e.max(out=vout[:, 0:8], in_=src), [src], [vout[:, 0:8]]))
        for (vout, iout, src, scratch) in items:
            steps.append((lambda e, vout=vout, iout=iout, src=src: e.max_index(out=iout[:, 0:8], in_max=vout[:, 0:8], in_values=src),
                          [src, vout[:, 0:8]], [iout[:, 0:8]]))
        for (vout, iout, src, scratch) in items:
            steps.append((lambda e, vout=vout, src=src, scratch=scratch: e.match_replace(out=scratch, in_to_replace=vout[:, 0:8], in_values=src, imm_value=-1e30),
                          [src, vout[:, 0:8]], [scratch]))
        for (vout, iout, src, scratch) in items:
            steps.append((lambda e, vout=vout, scratch=scratch: e.max(out=vout[:, 8:16], in_=scratch), [scratch], [vout[:, 8:16]]))
        for (vout, iout, src, scratch) in items:
            steps.append((lambda e, vout=vout, iout=iout, scratch=scratch: e.max_index(out=iout[:, 8:16], in_max=vout[:, 8:16], in_values=scratch),
                          [scratch, vout[:, 8:16]], [iout[:, 8:16]]))
        for n_, (fn, rd, wr) in enumerate(steps):
            P.op("dve", fn, reads=rd, writes=wr)
            if n_ % nsplit == nsplit - 1:
                yield

    yield
    yield from top16b([(vals[:, hp, :], idxu[:, hp, :], scs[:, hp, :], wk[:, hp, :]) for hp in range(16)], 8)
    P.copy("act", idxf[:], idxu[:])
    vb_ = vals[:]
    P.tt("pool", cand.rearrange("p h (i j) -> p h i j", i=16),
         cap(vb_, int(vb_.offset), [[pstr(vals), 128], [32, 8], [1, 16], [0, 16]]),
         cap(vb_, int(vb_.offset) + 16, [[pstr(vals), 128], [32, 8], [0, 16], [1, 16]]), ALU.add)
    yield
    yield from top16b([(best[:, h, :], cidx[:, h, :], cand[:, h, :], wk2[:, h, :]) for h in range(8)], 8)
    cflat = cidx[:].rearrange("p h k -> p (h k)")
    P.ts("dve", bf.iku[:], cflat, 4, None, op0=ALU.logical_shift_right)
    P.ts("dve", bf.jku[:], cflat, 15, None, op0=ALU.bitwise_and)
    P.copy("act", bf.ikf[:], bf.iku[:])
    P.copy("act", bf.jkf[:], bf.jku[:])
    yield
    io = bf.iota16[:]
    iob = cap(io, int(io.offset), [[pstr(bf.iota16), 128], [0, 8], [0, 16], [1, 16]])
    fb_ = idxf[:]
    for (kf_, off, col) in ((bf.ikf, 0, 0), (bf.jkf, 16, 1)):
        kb_ = kf_[:]
        P.tt("dve", eq[:], cap(kb_, int(kb_.offset), [[pstr(kf_), 128], [16, 8], [1, 16], [0, 16]]), iob, ALU.is_equal)
        P.tt("pool", eq[:], eq[:],
             cap(fb_, int(fb_.offset) + off, [[pstr(idxf), 128], [32, 8], [0, 16], [1, 16]]), ALU.mult)
        P.reduce("dve", abg[:, col, :].rearrange("p (h k) -> p h k", h=8), eq[:], ALU.add)
        yield
    bb_ = best[:]
    P.tt("pool", eg[:], best[:], cap(bb_, int(bb_.offset), [[pstr(best), 128], [16, 8], [0, 16]]), ALU.subtract)
    P.act(eg[:], eg[:], AF.Exp)
    P.reduce("dve", zz[:], eg[:], ALU.add)
    P.op("dve", lambda e: e.reciprocal(zz[:], zz[:]), reads=[zz[:]], writes=[zz[:]])
    zb_ = zz[:]
    P.tt("pool", abg[:, 2, :].rearrange("p (h k) -> p h k", h=8), eg[:],
         cap(zb_, int(zb_.offset), [[pstr(zz), 128], [1, 8], [0, 16]]), ALU.mult)
    yield
    at = bf.abgT[i % 2]
    P.tr(trp_ab[:, 0:128], abg[:, 0, :], bf.identf[:])
    P.tr(trp_ab[:, 128:256], abg[:, 1, :], bf.identf[:])
    P.copy("act", at[:, 0:2, :], trp_ab.rearrange("p (j t) -> p j t", j=2))
    P.tr(trp_g, abg[:, 2, :], bf.identf[:])
    P.copy("act", at[:, 2, :], trp_g)
    for j, nm in enumerate(("pa", "pb", "pg")):
        P.dma("pool", B.S[nm][:, tsl], at[:, j, :])
    yield


def st_peer_a1(self, s, l):
    P = self.P
    I = self.I
    ada = self.S["ada%d" % l]
    with self.stage() as sc:
        identb = self.load_const(sc, "identb")
        identf = self.load_const(sc, "identf")
        shb = self.bcast_row(sc, ada[s, 3072:4096], 1024, "sh2b")
        scb = self.bcast_row(sc, ada[s, 4096:5120], 1024, "sc2b")
        u2T = sc.sb([128, 8, T], BF16, "u2T")
        pst = sc.ps([128, 1024], BF16, "pst")
        self.make_uT(sc, self.S["xres"][s], scb, shb, u2T, identb, pst)
        P.dma("pool", self.S["u2T"], u2T[:])
        wq = sc.sb([128, 8, 2048], BF16, "wq")
        wst = [sc.sb([128, 2048], F32, "wqs%d" % i) for i in range(2)]
        for k in range(8):
            P.dma("sp", wst[k % 2][:], I["peer_wq"][l][k * 128:(k + 1) * 128, :])
            P.copy("pool" if k % 2 else "dve", wq[:, k, :], wst[k % 2][:])
        psK = sc.ps([128, 512], F32, "psK")
        keysT = sc.sb([128, 16, 128], BF16, "keysT")
        kst = [sc.sb([128, 128], F32, "kst%d" % i) for i in range(2)]
        for hp in range(16):
            P.dma("sp", kst[hp % 2][:], I["peer_keys"][l][hp // 2, hp % 2])
            P.tr(psK[:, 0:128], kst[hp % 2][:], identf[:])
            P.copy("act", keysT[:, hp, :], psK[:, 0:128])
        psQ = [sc.ps([128, 512], F32, "psQ%d" % i) for i in range(2)]
        psSc = sc.ps([128, 2048], F32, "psSc")
        qTs = [sc.sb([128, 16, 128], BF16, "qT%d" % i) for i in range(2)]
        scss = [sc.sb([128, 16, 128], F32, "scs%d" % i) for i in range(2)]
        bf = A2Bufs(self, sc)
        trp_ab = psK[:, 0:256]
        trp_g = psK[:, 256:384]
        a2state = {"gen": None, "next": 0}

        def a2_hook(done_tiles):
            if a2state["gen"] is None and a2state["next"] < min(4, done_tiles):
                a2state["gen"] = _a2_tile(self, bf, a2state["next"], trp_ab, trp_g)
                a2state["next"] += 1
            if a2state["gen"] is not None:
                try:
                    next(a2state["gen"])
                except StopIteration:
                    a2state["gen"] = None
        for i in range(16):
            tsl = slice(i * 128, (i + 1) * 128)
            qT, scs = qTs[i % 2], scss[i % 2]
            for hp in range(16):
                a2_hook(i)
                pq = psQ[(hp // 4) % 2]
                for k in range(8):
                    P.mm(pq[:, (hp % 4) * 128:(hp % 4 + 1) * 128], wq[:, k, hp * 128:(hp + 1) * 128], u2T[:, k, tsl],
                         start=(k == 0), stop=(k == 7))
                if hp % 4 == 3:
                    P.copy("act" if (hp // 4) % 2 else "dve", qT[:, hp - 3:hp + 1, :], pq[:, :].rearrange("p (h t) -> p h t", h=4))
            for hp in range(16):
                P.mm(psSc[:, hp * 128:(hp + 1) * 128], qT[:, hp, :], keysT[:, hp, :])
            P.copy("act", scs[:, 0:8, :], psSc[:, 0:1024].rearrange("p (h n) -> p h n", h=8))
            P.copy("dve", scs[:, 8:16, :], psSc[:, 1024:2048].rearrange("p (h n) -> p h n", h=8))
            P.dma("pool", self.S["scs"][tsl, :].rearrange("p (h n) -> p h n", h=16), scs[:])
        while a2state["gen"] is not None or a2state["next"] < 4:
            a2_hook(16)


Builder.st_peer_a1 = st_peer_a1


def build_all(B):
    for l in range(2):
        B.st_pre(l)
        B.st_ada(l)
    for s in range(2):
        for l in range(2):
            B.st_inproj(s, l)
            B.st_attn_a(s, l)
            B.st_attn_c(s, l)
            B.st_hconv(s, l)
            B.st_hyena(s, l, 0)
            B.st_hyena(s, l, 1)
            B.st_merge(s, l)
            B.st_peer_a1(s, l)
            B.st_peer_b(s, l, a2=True)


def kernel(**inputs):
    from concourse.bass_utils import run_bass_kernel_spmd
    inp = {k: np.ascontiguousarray(np.asarray(v, dtype=np.float32)) for k, v in inputs.items()}
    B = Builder()
    build_all(B)
    B.finish()
    ncores = 8
    maps = []
    for c in range(ncores):
        m = {"x": np.ascontiguousarray(inp["x"][2 * c:2 * c + 2]),
             "c": np.ascontiguousarray(inp["c"][2 * c:2 * c + 2])}
        for k in WSHAPES:
            m[k] = inp[k]
        for k, v in B.hc.items():
            m["k_" + k] = v
        maps.append(m)
    res = run_bass_kernel_spmd(B.nc, maps, core_ids=list(range(ncores)))
    out = np.concatenate([np.asarray(r["out"], dtype=np.float32) for r in res.results], axis=0)
    return out
```

```python
import numpy as np
import concourse.bass as bass
import concourse.mybir as mybir
from contextlib import ExitStack

F32 = mybir.dt.float32
BF16 = mybir.dt.bfloat16
I32 = mybir.dt.int32
U32 = mybir.dt.uint32
ALU = mybir.AluOpType
AF = mybir.ActivationFunctionType
AX = mybir.AxisListType

ENGS = ("pe", "act", "dve", "pool", "sp")
EPOCH = 12000
NDMASEM = 24
SELF_SKIP = 1 << 30


def _region(ap):
    t = ap.tensor
    name = t.name
    pairs = ap.ap
    off = int(ap.offset)
    sp = str(ap.space)
    if "SB" in sp or "PSUM" in sp:
        pstride = pairs[0][0]
        if pstride == 0:
            pstride = 1 << 40
        p0 = off // pstride if pstride < (1 << 40) else 0
        f0 = off - p0 * pstride if pstride < (1 << 40) else off
        p1 = p0 + pairs[0][1]
        ext = 0
        for st, cn in pairs[1:]:
            ext += abs(st) * (cn - 1)
        if "PSUM" in sp:
            bank = 2048 // mybir.dt.size(ap.dtype)
            f1 = f0 + ext + 1
            return (name, 0, 128, (f0 // bank) * bank, ((f1 + bank - 1) // bank) * bank)
        return (name, p0, p1, f0, f0 + ext + 1)
    ext = 0
    for st, cn in pairs:
        ext += abs(st) * (cn - 1)
    return (name, 0, 1, off, off + ext + 1)


def _ovl(a, b):
    return a[1] < b[2] and b[1] < a[2] and a[3] < b[4] and b[3] < a[4]


def _covers(a, b):
    return a[1] <= b[1] and a[2] >= b[2] and a[3] <= b[3] and a[4] >= b[4]


class Prog:
    def __init__(self, nc):
        self.nc = nc
        self.es = ExitStack()
        self.streams = {e: [] for e in ENGS}
        self.cnt = {e: 0 for e in ENGS}
        self.cur = {}
        self.allsems = []
        for e in ENGS:
            self.cur[e] = self._newsem("pg_" + e)
        self.dsems = [self._newsem("dma%d" % i) for i in range(NDMASEM)]
        self.dcum = [0] * NDMASEM
        self.dnext = 0
        self.waited = {e: {} for e in ENGS}
        self.hist = {}
        self.floor = {}
        self.semobj = {}
        self.ninstr = 0

    def _newsem(self, name):
        s = self.es.enter_context(self.nc.semaphore(name + "_%d" % len(self.allsems)))
        self.allsems.append(s)
        return s

    def _deps(self, reads, writes, eng=None):
        deps = []
        for ap in reads:
            r = _region(ap)
            psum = "PSUM" in str(ap.space)
            for (reg, isw, ev) in self.hist.get(r[0], ()):
                if _ovl(reg, r) and (isw or (psum and ev[0] != eng)):
                    deps.append(ev)
            deps.extend(self.floor.get(r[0], ()))
        for ap in writes:
            r = _region(ap)
            for (reg, isw, ev) in self.hist.get(r[0], ()):
                if _ovl(reg, r):
                    deps.append(ev)
            deps.extend(self.floor.get(r[0], ()))
        return deps

    def _record(self, reads, writes, ev):
        for ap in writes:
            r = _region(ap)
            h = self.hist.setdefault(r[0], [])
            h[:] = [x for x in h if not _covers(r, x[0])]
            h.append((r, True, ev))
            self._trim(r[0])
        for ap in reads:
            r = _region(ap)
            h = self.hist.setdefault(r[0], [])
            rep = False
            for i, (reg, isw, oev) in enumerate(h):
                if (not isw) and reg == r and oev[0] == ev[0] and oev[3] is False:
                    h[i] = (r, False, ev)
                    rep = True
                    break
            if not rep:
                h.append((r, False, ev))
                self._trim(r[0])

    def _trim(self, name):
        h = self.hist[name]
        if len(h) > 96:
            drop = h[:32]
            del h[:32]
            fl = self.floor.setdefault(name, [])
            fl.extend(x[2] for x in drop)
            best = {}
            for ev in fl:
                k = id(ev[1])
                if k not in best or best[k][2] < ev[2]:
                    best[k] = ev
            self.floor[name] = list(best.values())

    def _waits(self, eng, deps, skip_self_pe=True):
        need = {}
        for ev in deps:
            src, sem, val, isdma = ev
            if src == eng and not isdma and eng == "pe":
                continue
            if src == eng and not isdma and sem is self.cur[eng] and self.cnt[eng] - val >= SELF_SKIP:
                continue
            k = id(sem)
            if self.waited[eng].get(k, 0) >= val:
                continue
            if k not in need or need[k][1] < val:
                need[k] = (sem, val)
        out = []
        for k, (sem, val) in need.items():
            self.waited[eng][k] = val
            out.append((sem, val))
        return out

    def op(self, eng, fn, reads=(), writes=()):
        reads = [a for a in reads if a is not None and not isinstance(a, (int, float))]
        deps = self._deps(reads, writes, eng)
        waits = self._waits(eng, deps)
        if self.cnt[eng] >= EPOCH:
            self.cur[eng] = self._newsem("pg_" + eng)
            self.cnt[eng] = 0
        sem = self.cur[eng]
        self.cnt[eng] += 1
        ev = (eng, sem, self.cnt[eng], False)
        self._record(reads, writes, ev)
        self.streams[eng].append((waits, fn, sem, 1))
        self.ninstr += 1

    def dma(self, q, out, in_, **kw):
        deps = self._deps([in_], [out], q)
        waits = self._waits(q, deps)
        k = self.dnext
        self.dnext = (self.dnext + 1) % NDMASEM
        sem = self.dsems[k]
        self.dcum[k] += 16
        ev = (q, sem, self.dcum[k], True)
        self._record([in_], [out], ev)
        self.streams[q].append((waits, lambda e, o=out, i=in_, kw=kw: e.dma_start(out=o, in_=i, **kw), sem, 16))
        self.ninstr += 1

    def barrier(self):
        evs = []
        for e in ENGS:
            if self.cnt[e] > 0:
                evs.append((e, self.cur[e], self.cnt[e], False))
        for k in range(NDMASEM):
            if self.dcum[k] > 0:
                evs.append(("dma", self.dsems[k], self.dcum[k], True))
        for e in ENGS:
            ws = []
            for (src, sem, val, isdma) in evs:
                if src == e and not isdma:
                    continue
                kk = id(sem)
                if self.waited[e].get(kk, 0) >= val:
                    continue
                self.waited[e][kk] = val
                ws.append((sem, val))
            if ws:
                self.streams[e].append((ws, None, None, 0))
        self.hist = {}
        self.floor = {}

    def emit(self):
        nc = self.nc
        streams = self.streams
        self.streams = {e: [] for e in ENGS}

        def run(engobj, lst):
            for (waits, fn, sem, inc) in lst:
                for (s, v) in waits:
                    engobj.wait_ge(s, v)
                if fn is not None:
                    ins = fn(engobj)
                    ins.then_inc(sem, inc)

        with nc.Block() as block:
            @block.tensor
            def _(e):
                run(e, streams["pe"])

            @block.scalar
            def _(e):
                run(e, streams["act"])

            @block.vector
            def _(e):
                run(e, streams["dve"])

            @block.gpsimd
            def _(e):
                run(e, streams["pool"])

            @block.sync
            def _(e):
                run(e, streams["sp"])

    def mm(self, out, lhsT, rhs, start=True, stop=True):
        self.op("pe", lambda e: e.matmul(out, lhsT, rhs, start=start, stop=stop),
                reads=[lhsT, rhs], writes=[out])

    def tr(self, out, in_, ident):
        self.op("pe", lambda e: e.transpose(out, in_, ident), reads=[in_, ident], writes=[out])

    def act(self, out, in_, func, bias=0.0, scale=1.0, accum_out=None, eng="act"):
        rd = [in_]
        if not isinstance(bias, (int, float)):
            rd.append(bias)
        if not isinstance(scale, (int, float)):
            rd.append(scale)
        wr = [out] + ([accum_out] if accum_out is not None else [])
        kw = {}
        if accum_out is not None:
            kw["accum_out"] = accum_out
        self.op("act", lambda e: e.activation(out, in_, func, bias=bias, scale=scale, **kw),
                reads=rd, writes=wr)

    def tt(self, eng, out, in0, in1, op):
        self.op(eng, lambda e: e.tensor_tensor(out, in0, in1, op), reads=[in0, in1], writes=[out])

    def ts(self, eng, out, in0, s1, s2=None, op0=ALU.mult, op1=None, accum_out=None):
        rd = [in0] + [s for s in (s1, s2) if s is not None and not isinstance(s, (int, float))]
        wr = [out] + ([accum_out] if accum_out is not None else [])
        kw = {}
        if op1 is not None:
            kw["op1"] = op1
        if accum_out is not None:
            kw["accum_out"] = accum_out
        self.op(eng, lambda e: e.tensor_scalar(out, in0, s1, s2, op0, **kw), reads=rd, writes=wr)

    def stt(self, eng, out, in0, scalar, in1, op0, op1):
        rd = [in0, in1] + ([scalar] if not isinstance(scalar, (int, float)) else [])
        self.op(eng, lambda e: e.scalar_tensor_tensor(out, in0, scalar, in1, op0, op1), reads=rd, writes=[out])

    def copy(self, eng, out, in_):
        if eng == "act":
            self.op("act", lambda e: e.copy(out, in_), reads=[in_], writes=[out])
        else:
            self.op(eng, lambda e: e.tensor_copy(out, in_), reads=[in_], writes=[out])

    def memset(self, eng, ap, val):
        self.op(eng, lambda e: e.memset(ap, val), reads=[], writes=[ap])

    def reduce(self, eng, out, in_, op, axis=AX.X):
        self.op(eng, lambda e: e.tensor_reduce(out, in_, axis, op), reads=[in_], writes=[out])

import math
import os
import ml_dtypes
from contextlib import contextmanager

NPBF = ml_dtypes.bfloat16
T = 2048
DM = 1024
ALPHA_C = 4.0 ** 0.25
LN_EPS_C = 1e-5
PI = math.pi


def host_consts():
    c = {}
    L = T
    N = 2 * T
    pos = np.arange(L, dtype=np.float32)
    inv = np.power(np.float32(10000.0), -(np.arange(0, 64, 2, dtype=np.float32) / np.float32(64))).astype(np.float32)
    ang = (pos[:, None] * inv[None, :]).astype(np.float32)
    cs, sn = np.cos(ang).astype(np.float32), np.sin(ang).astype(np.float32)
    p = np.arange(128)
    ropec = cs[:, p % 32].T.copy()
    sgn = np.where((p % 64) < 32, 1.0, -1.0).astype(np.float32)
    ropes = (sn[:, p % 32].T * sgn[:, None]).astype(np.float32)
    c["ropec"] = np.ascontiguousarray(ropec)
    c["ropes"] = np.ascontiguousarray(ropes)
    t = np.arange(L, dtype=np.int64)
    idx = (t[:, None] * t[None, :]) % N
    th = 2.0 * np.pi * idx.astype(np.float64) / N
    C = np.cos(th)
    S = -np.sin(th)
    alt = np.where(t % 2 == 0, 1.0, -1.0)
    Sf = S.copy()
    Sf[:, 0] = alt
    def blk_f(M):
        return np.ascontiguousarray(M.reshape(16, 128, 16, 128).transpose(2, 1, 0, 3)).astype(NPBF)
    c["cf"] = blk_f(C)
    c["sf"] = blk_f(Sf)
    Ci = (2.0 / N) * C
    Ci[0, :] = 1.0 / N
    Si = (2.0 / N) * S
    Si[0, :] = alt / N
    def blk_i(M):
        return np.ascontiguousarray(M.reshape(16, 128, 8, 256).transpose(2, 1, 0, 3)).astype(NPBF)
    c["ci"] = blk_i(Ci)
    c["si"] = blk_i(Si)
    idxf = np.arange(L, dtype=np.float32)
    tt_ = (idxf / np.float32(L - 1)).astype(np.float32)
    w = (np.float32(2.0 * math.pi) * idxf / np.float32(L)).astype(np.float32)
    bands = np.linspace(1e-4, 15, 16, dtype=np.float32)
    ang2 = (w[:, None] * bands[None, :]).astype(np.float32)
    z = np.concatenate([tt_[:, None], np.cos(ang2), -np.sin(ang2)], axis=-1).astype(np.float32)
    c["zembT"] = np.ascontiguousarray(z.T)
    c["negt"] = np.ascontiguousarray((-tt_).reshape(16, 128).T)
    a = np.arange(128)[:, None]
    b = np.arange(128)[None, :]
    mA = np.concatenate([(np.abs(128 * (sg - 1) + a - b) <= 64) for sg in range(3)], axis=1)
    c["mA"] = mA.astype(NPBF)
    mC = np.stack([(b <= a), np.ones((128, 128), bool), (a <= b)], axis=1)
    c["mC"] = np.ascontiguousarray(mC).astype(NPBF)
    sel = np.zeros((65, 64), np.float32)
    sel[64, :] = 1.0
    c["sel"] = sel
    c["ones"] = np.ones((128, 128), np.float32)
    c["identf"] = np.eye(128, dtype=np.float32)
    c["identb"] = np.eye(128).astype(NPBF)
    sw = np.zeros((128, 128), np.float32)
    sw[np.arange(128), np.arange(128) ^ 32] = 1.0
    c["swapm"] = sw.astype(NPBF)
    c["iota16"] = np.tile(np.arange(16, dtype=np.float32)[None, :], (128, 1))
    c["iota128"] = np.tile(np.arange(128)[None, :], (128, 1)).astype(NPBF)
    return c


CONST_DT = {"ropec": F32, "ropes": F32, "cf": BF16, "sf": BF16, "ci": BF16, "si": BF16, "zembT": F32,
            "negt": F32, "mA": BF16, "mC": BF16, "sel": F32, "ones": F32, "identf": F32, "identb": BF16,
            "iota16": F32, "iota128": BF16, "swapm": BF16}

WSHAPES = {
    "w_ada": [2, 1024, 6144], "b_ada": [2, 6144], "w_in": [2, 1024, 7680], "conv_w": [2, 3, 1536],
    "conv_b": [2, 1536], "hy_w1": [2, 33, 64], "hy_b1": [2, 64], "hy_w2": [2, 64, 64], "hy_b2": [2, 64],
    "hy_w3": [2, 64, 2048], "hy_freq": [2, 2, 64], "hy_log_decay": [2, 2048], "hy_bias": [2, 2, 512],
    "attn_sink": [2, 8], "w_branch_a": [2, 256, 1024], "w_branch_b": [2, 512, 1024],
    "w_branch_c": [2, 512, 1024], "w_out": [2, 1024, 1024], "ln_g": [2, 2, 1024], "ln_b": [2, 2, 1024],
    "peer_wq": [2, 1024, 2048], "peer_keys": [2, 8, 2, 128, 128], "peer_u": [2, 16384, 1024],
    "peer_v": [2, 16384, 1024],
}


def cap(ap, offset, pairs):
    return bass.AP(ap.tensor, offset, [list(p) for p in pairs])


class Scope:
    def __init__(self, B):
        self.B = B
        self.es = ExitStack()

    def sb(self, shape, dt, name="t"):
        self.B.uid += 1
        t = self.es.enter_context(self.B.nc.sbuf_tensor("%s_%d" % (name, self.B.uid), list(shape), dt))
        return t

    def ps(self, shape, dt=F32, name="ps"):
        self.B.uid += 1
        return self.es.enter_context(self.B.nc.psum_tensor("%s_%d" % (name, self.B.uid), list(shape), dt))


class Builder:
    def __init__(self, dbg=()):
        self.nc = nc = bass.Bass("TRN2", target_bir_lowering=False)
        self.P = Prog(nc)
        self.uid = 0
        self.dbg = set(dbg)
        self.I = {}
        self.I["x"] = nc.dram_tensor("x", [2, T, DM], F32, kind="ExternalInput").ap()
        self.I["c"] = nc.dram_tensor("c", [2, DM], F32, kind="ExternalInput").ap()
        for k, shp in WSHAPES.items():
            self.I[k] = nc.dram_tensor(k, shp, F32, kind="ExternalInput").ap()
        self.C = {}
        hc = host_consts()
        self.hc = hc
        for k, v in hc.items():
            self.C[k] = nc.dram_tensor("k_" + k, list(v.shape), CONST_DT[k], kind="ExternalInput").ap()
        self.out = nc.dram_tensor("out", [2, T, DM], F32, kind="ExternalOutput").ap()
        S = self.S = {}

        def scr(name, shape, dt):
            kind = "ExternalOutput" if name in self.dbg else "Internal"
            S[name] = nc.dram_tensor("s_" + name, list(shape), dt, kind=kind).ap()
        for l in range(2):
            scr("ada%d" % l, [2, 6144], F32)
            scr("hspec%d" % l, [3, 2048, 1024], BF16)
            scr("ut%d" % l, [128, 128, 8, 128], BF16)
            scr("vb%d" % l, [128, 128, 1024], BF16)
        scr("xres", [2, T, DM], F32)
        scr("qTa", [768, T], BF16)
        scr("kTa", [768, T], BF16)
        scr("va", [3, T, 256], BF16)
        scr("hyT", [1536, T], F32)
        scr("hcT", [1536, T], F32)
        scr("z1T", [512, T], F32)
        scr("qTc", [512, T], BF16)
        scr("kTc", [128, T], BF16)
        scr("vc", [T, 128], BF16)
        scr("gateT", [3072, T], BF16)
        scr("yaT", [256, T], BF16)
        scr("ybT", [512, T], BF16)
        scr("ycT", [512, T], BF16)
        scr("u2T", [128, 8, T], BF16)
        scr("scs", [T, 2048], F32)
        scr("pa", [128, T], BF16)
        scr("pb", [128, T], BF16)
        scr("pg", [128, T], BF16)

    @contextmanager
    def stage(self):
        sc = Scope(self)
        try:
            yield sc
        finally:
            self.P.barrier()
            self.P.emit()
            sc.es.close()

    def finish(self):
        self.P.es.close()

    def bcast_row(self, sc, src_ap_1d, n, name="bc"):
        t = sc.sb([128, n], F32, name)
        src = cap(src_ap_1d, int(src_ap_1d.offset), [[0, 128], [1, n]])
        self.P.dma("sp", t[:], src)
        return t

    def load_const(self, sc, name):
        v = self.hc[name]
        t = sc.sb(list(v.shape), CONST_DT[name], "c_" + name)
        self.P.dma("sp", t[:], self.C[name])
        return t

    def st_ada(self, l):
        P = self.P
        with self.stage() as sc:
            cT = sc.sb([128, 8, 2], F32, "cT")
            for b in range(2):
                P.dma("sp", cT[:, :, b], self.I["c"][b].rearrange("(k p) -> p k", p=128),
                      allow_slow_non_contiguous=True)
            P.act(cT[:], cT[:], AF.Silu)
            ada = sc.sb([2, 6144], F32, "ada")
            bb = sc.sb([2, 6144], F32, "bb")
            ba = self.I["b_ada"][l]
            P.dma("sp", bb[:], cap(ba, int(ba.offset), [[0, 2], [1, 6144]]))
            ws = [sc.sb([128, 8, 512], F32, "w%d" % i) for i in range(2)]
            pss = [sc.ps([128, 512], F32) for i in range(2)]
            for cb in range(12):
                w = ws[cb % 2]
                P.dma("sp", w[:], self.I["w_ada"][l][:, cb * 512:(cb + 1) * 512].rearrange("(k p) n -> p k n", p=128))
                ps = pss[cb % 2]
                for k in range(8):
                    P.mm(ps[0:2, :], cT[:, k, :], w[:, k, :], start=(k == 0), stop=(k == 7))
                P.tt("dve", ada[:, cb * 512:(cb + 1) * 512], ps[0:2, :], bb[:, cb * 512:(cb + 1) * 512], ALU.add)
            for o in (1024, 4096):
                P.ts("dve", ada[:, o:o + 1024], ada[:, o:o + 1024], 1.0, None, op0=ALU.add)
            P.dma("sp", self.S["ada%d" % l], ada[:])

    def make_uT(self, sc, xsrc, scb, shb, uT, identb, pst, ntiles=16, tok0=0, xkeep=None):
        P = self.P
        xts = [sc.sb([128, DM], F32, "xt%d" % i) for i in range(2)]
        ubs = [sc.sb([128, DM], BF16, "ub%d" % i) for i in range(2)]
        uf = sc.sb([128, DM], F32, "uf")
        for i in range(ntiles):
            xt = xts[i % 2] if xkeep is None else xkeep[i]
            ub = ubs[i % 2]
            P.dma("sp", xt[:], xsrc[tok0 + i * 128: tok0 + (i + 1) * 128, :])
            P.tt("dve", uf[:], xt[:], scb[:], ALU.mult)
            P.tt("pool", ub[:], uf[:], shb[:], ALU.add)
            for k in range(8):
                P.tr(pst[:, k * 128:(k + 1) * 128], ub[:, k * 128:(k + 1) * 128], identb[:])
            P.copy("act", uT[:, :, i * 128:(i + 1) * 128], pst[:].rearrange("p (k t) -> p k t", k=8))

    def st_inproj(self, s, l):
        P = self.P
        xsrc = self.I["x"][s] if l == 0 else self.S["xres"][s]
        ada = self.S["ada%d" % l]
        with self.stage() as sc:
            identb = self.load_const(sc, "identb")
            ropec = self.load_const(sc, "ropec")
            ropes = self.load_const(sc, "ropes")
            shb = self.bcast_row(sc, ada[s, 0:1024], 1024, "shb")
            scb = self.bcast_row(sc, ada[s, 1024:2048], 1024, "scb")
            uT = sc.sb([128, 8, T], BF16, "uT")
            pst = sc.ps([128, 1024], BF16, "pst")
            self.make_uT(sc, xsrc, scb, shb, uT, identb, pst)
            wfs = [sc.sb([128, 8, 256], F32, "wf%d" % i) for i in range(2)]
            wbs = [sc.sb([128, 8, 256], BF16, "wb%d" % i) for i in range(2)]
            pss = [sc.ps([128, 512], F32, "pf%d" % i) for i in range(4)]
            stg = [sc.sb([128, T], BF16, "stg%d" % i) for i in range(3)]
            stf = [sc.sb([128, T], F32, "stf%d" % i) for i in range(2)]
            tbs = [sc.sb([128, 512], BF16, "tbs%d" % i) for i in range(2)]
            ps2 = [sc.ps([128, 512], F32, "ps2_%d" % i) for i in range(2)]
            swapm = self.load_const(sc, "swapm")
            tA = [sc.sb([128, 512], F32, "tA%d" % i) for i in range(2)]
            tB = [sc.sb([128, 512], F32, "tB%d" % i) for i in range(2)]
            vst = [sc.sb([128, 16, 256], BF16, "vst%d" % i) for i in range(2)]
            cnt = {"ps": 0, "stg": 0, "stf": 0, "tmp": 0, "v": 0}

            def tokview(ap2, tt, D):
                if D == 1:
                    return ap2[:, tt * 512:(tt + 1) * 512]
                if D == 4:
                    return ap2.rearrange("p (j r) -> p r j", r=4)[:, tt, :]
                return ap2.rearrange("p (j r) -> p r j", r=16)[:, 4 * tt:4 * tt + 4, :]

            def shp(ap2, D):
                if D == 16:
                    return ap2.rearrange("p (r j) -> p r j", r=4)
                return ap2

            def tok128(ap2, i, D):
                if D == 1:
                    return ap2[:, i * 128:(i + 1) * 128]
                if D == 4:
                    return ap2.rearrange("p (j r) -> p r j", r=4)[:, i // 4, (i % 4) * 128:(i % 4 + 1) * 128]
                return ap2.rearrange("p (j r) -> p r j", r=16)[:, i, :]

            def fm_chunk(wb, cc, kind, D, dest):
                if kind == "hy":
                    st = stf[cnt["stf"] % 2]
                    cnt["stf"] += 1
                else:
                    st = stg[cnt["stg"] % 3]
                    cnt["stg"] += 1
                for tt in range(4):
                    ps = pss[cnt["ps"] % 4]
                    cnt["ps"] += 1
                    for k in range(8):
                        P.mm(ps[:, :], wb[:, k, cc * 128:(cc + 1) * 128], uT[:, k, tt * 512:(tt + 1) * 512],
                             start=(k == 0), stop=(k == 7))
                    o = st[:, tt * 512:(tt + 1) * 512]
                    if kind == "hy":
                        P.copy("act", o, ps[:, :])
                    elif kind == "gate":
                        P.act(o, ps[:, :], AF.Sigmoid)
                    else:
                        j = cnt["tmp"] % 2
                        cnt["tmp"] += 1
                        tb_, a_, b_, p2 = tbs[j], tA[j], tB[j], ps2[j]
                        P.copy("act", tb_[:], ps[:, :])
                        P.mm(p2[:, :], swapm[:], tb_[:])
                        ts_ = slice(tt * 512, (tt + 1) * 512)
                        P.tt("dve", a_[:], ps[:, :], ropec[:, ts_], ALU.mult)
                        P.tt("dve", b_[:], p2[:, :], ropes[:, ts_], ALU.mult)
                        if D == 1:
                            P.tt("pool", o, a_[:], b_[:], ALU.subtract)
                        else:
                            n_ = 512 // D
                            ov = st[:, :].rearrange("p (r j) -> p j r", r=D)[:, tt * n_:(tt + 1) * n_, :]
                            P.tt("pool", ov, a_[:].rearrange("p (j r) -> p j r", r=D),
                                 b_[:].rearrange("p (j r) -> p j r", r=D), ALU.subtract)
                P.dma("pool", dest, st[:])

            def tm_block(wb, c0, ncols, D, dest):
                st = vst[cnt["v"] % 2]
                cnt["v"] += 1
                for i in range(16):
                    ps = pss[cnt["ps"] % 4]
                    cnt["ps"] += 1
                    for k in range(8):
                        P.mm(ps[:, 0:ncols], tok128(uT[:, k, :], i, D), wb[:, k, c0:c0 + ncols],
                             start=(k == 0), stop=(k == 7))
                    P.copy("act", st[:, i, 0:ncols], ps[:, 0:ncols])
                P.dma("pool", dest.rearrange("(i p) c -> p i c", p=128), st[:, :, 0:ncols])

            DG = (1, 4, 16)
            for blk in range(30):
                wf, wb = wfs[blk % 2], wbs[blk % 2]
                col = blk * 256
                P.dma("sp", wf[:], self.I["w_in"][l][:, col:col + 256].rearrange("(k p) n -> p k n", p=128))
                P.copy("pool" if blk % 2 else "dve", wb[:], wf[:])
                if col < 1536:
                    nm = "qTa" if col < 768 else "kTa"
                    base = 0 if col < 768 else 768
                    for cc in range(2):
                        j = (col - base) // 128 + cc
                        fm_chunk(wb, cc, "rope", DG[j // 2], self.S[nm][j * 128:(j + 1) * 128, :])
                elif col < 2304:
                    g = (col - 1536) // 256
                    tm_block(wb, 0, 256, DG[g], self.S["va"][g])
                elif col < 3840:
                    for cc in range(2):
                        j = (col - 2304) // 128 + cc
                        fm_chunk(wb, cc, "hy", 1, self.S["hyT"][j * 128:(j + 1) * 128, :])
                elif col < 4352:
                    for cc in range(2):
                        j = (col - 3840) // 128 + cc
                        fm_chunk(wb, cc, "rope", 1, self.S["qTc"][j * 128:(j + 1) * 128, :])
                elif col < 4608:
                    fm_chunk(wb, 0, "rope", 1, self.S["kTc"][:, :])
                    tm_block(wb, 128, 128, 1, self.S["vc"])
                else:
                    for cc in range(2):
                        j = (col - 4608) // 128 + cc
                        fm_chunk(wb, cc, "gate", 1, self.S["gateT"][j * 128:(j + 1) * 128, :])


def _attn_finalize(B, sc, numer, nheads, sel, psB, dest):
    P = B.P
    recs = [sc.sb([64, 512], F32, "rec%d" % i) for i in range(2)]
    ysts = [sc.sb([64, T], BF16, "yst%d" % i) for i in range(2)]
    n = 0
    for h in range(nheads):
        yst = ysts[h % 2]
        for tt in range(4):
            ps = psB[n % 2]
            rec = recs[n % 2]
            n += 1
            P.mm(ps[0:64, :], sel[0:65, 0:64], numer[0:65, h, tt * 512:(tt + 1) * 512])
            P.op("dve", lambda e, o=rec[:], i=ps[0:64, :]: e.reciprocal(o, i), reads=[ps[0:64, :]], writes=[rec[:]])
            P.tt("pool", yst[:, tt * 512:(tt + 1) * 512], numer[0:64, h, tt * 512:(tt + 1) * 512], rec[:], ALU.mult)
        P.dma("pool", dest[h * 64:(h + 1) * 64, :], yst[:])


def st_attn_a(self, s, l):
    P = self.P
    with self.stage() as sc:
        mA = self.load_const(sc, "mA")
        sel = self.load_const(sc, "sel")
        numer = sc.sb([65, 4, T], F32, "numer")
        q = sc.sb([64, 4, T], BF16, "q")
        k = sc.sb([64, 4, T], BF16, "k")
        va = sc.sb([128, 16, 4, 65], BF16, "va")
        psS = [sc.ps([128, 512], F32, "psS%d" % i) for i in range(3)]
        psO = [sc.ps([128, 512], F32, "psO%d" % i) for i in range(2)]
        psB = [sc.ps([128, 512], F32, "psB%d" % i) for i in range(2)]
        pT = [sc.sb([128, 384], BF16, "pT%d" % i) for i in range(2)]
        pT2 = [sc.sb([128, 384], BF16, "pTm%d" % i) for i in range(3)]
        P.memset("pool", va[:], 1.0)
        n = 0
        for g, D in enumerate((1, 4, 16)):
            Ls = T // D
            nb = Ls // 128
            for h in range(4):
                hh = g * 4 + h
                P.dma("sp", q[:, h, :], self.S["qTa"][hh * 64:(hh + 1) * 64, :])
                P.dma("sp", k[:, h, :], self.S["kTa"][hh * 64:(hh + 1) * 64, :])
                P.dma("sp", va[:, :, h, 0:64],
                      self.S["va"][g][:, h * 64:(h + 1) * 64].rearrange("(i p) d -> p i d", p=128))
            jobs = [(h, rho, i) for h in range(4) for rho in range(D) for i in range(nb)]

            def s_phase(n, job):
                h, rho, i = job
                base = rho * Ls
                js = [j for j in (i - 1, i, i + 1) if 0 <= j < nb]
                ps = psS[n % 3]
                p1 = pT[n % 2]
                p2 = pT2[n % 3]
                q0 = base + i * 128
                for j in js:
                    sg = j - i + 1
                    P.mm(ps[:, sg * 128:(sg + 1) * 128], k[0:64, h, base + j * 128: base + (j + 1) * 128],
                         q[0:64, h, q0:q0 + 128])
                c0 = (js[0] - i + 1) * 128
                c1 = (js[-1] - i + 2) * 128
                P.act(p1[:, c0:c1], ps[:, c0:c1], AF.Exp, scale=0.125)
                P.tt("dve", p2[:, c0:c1], p1[:, c0:c1], mA[:, c0:c1], ALU.mult)

            def pv_phase(n, job):
                h, rho, i = job
                js = [j for j in (i - 1, i, i + 1) if 0 <= j < nb]
                po = psO[n % 2]
                p2 = pT2[n % 3]
                for jj, j in enumerate(js):
                    sg = j - i + 1
                    P.mm(po[0:65, 0:128], va[:, rho * nb + j, h, :], p2[:, sg * 128:(sg + 1) * 128],
                         start=(jj == 0), stop=(jj == len(js) - 1))
                if D == 1:
                    nv = numer[0:65, h, i * 128:(i + 1) * 128]
                else:
                    nv = numer[0:65, h, :].rearrange("p (j r) -> p r j", r=D)[:, rho, i * 128:(i + 1) * 128]
                if g == 0:
                    P.copy("act", nv, po[0:65, 0:128])
                else:
                    P.tt("dve", nv, po[0:65, 0:128], nv, ALU.add)
            for n_, job in enumerate(jobs):
                s_phase(n + n_, job)
                if n_ > 0:
                    pv_phase(n + n_ - 1, jobs[n_ - 1])
            pv_phase(n + len(jobs) - 1, jobs[-1])
            n += len(jobs)
        _attn_finalize(self, sc, numer, 4, sel, psB, self.S["yaT"])


def st_attn_c(self, s, l, with_hconv=False):
    P = self.P
    with self.stage() as sc:
        hgen = _hconv_body(self, sc, l) if with_hconv else None
        mC = self.load_const(sc, "mC")
        sel = self.load_const(sc, "sel")
        numer = sc.sb([65, 8, T], F32, "numerc")
        q = sc.sb([64, 8, T], BF16, "qc")
        k = sc.sb([64, 2, T], BF16, "kc")
        va = sc.sb([128, 16, 2, 65], BF16, "vca")
        sk = sc.sb([65, 8], F32, "sk")
        psS = [sc.ps([128, 512], F32, "psS%d" % i) for i in range(3)]
        psO = [sc.ps([128, 512], F32, "psO%d" % i) for i in range(2)]
        psB = [sc.ps([128, 512], F32, "psB%d" % i) for i in range(2)]
        pT = [sc.sb([128, 3, 512], BF16, "pTc%d" % i) for i in range(2)]
        P.memset("pool", va[:], 1.0)
        for h in range(8):
            P.dma("sp", q[:, h, :], self.S["qTc"][h * 64:(h + 1) * 64, :])
        for h in range(2):
            P.dma("sp", k[:, h, :], self.S["kTc"][h * 64:(h + 1) * 64, :])
            P.dma("sp", va[:, :, h, 0:64], self.S["vc"][:, h * 64:(h + 1) * 64].rearrange("(i p) d -> p i d", p=128))
        asink = self.I["attn_sink"][l]
        P.dma("sp", sk[64:65, :], cap(asink, int(asink.offset), [[0, 1], [1, 8]]))
        P.act(sk[64:65, :], sk[64:65, :], AF.Exp)
        n = 0
        mcb = mC[:]
        pst = mcb.ap[0][0]
        jobs = [(kv, i) for kv in range(2) for i in range(16)]

        def s_phase(n, job):
            kv, i = job
            js = [j for j in (i - 1, i, i + 1) if 0 <= j < 16]
            p1 = pT[n % 2]
            for j in js:
                sg = j - i + 1
                ps = psS[sg]
                P.mm(ps[:, :].rearrange("p (h t) -> p h t", h=4), k[0:64, kv, j * 128:(j + 1) * 128],
                     q[0:64, 4 * kv:4 * kv + 4, i * 128:(i + 1) * 128])
                P.act(p1[:, sg, :], ps[:, :], AF.Exp, scale=0.125)
                if sg != 1:
                    mv = cap(mcb, int(mcb.offset) + sg * 128, [[pst, 128], [0, 4], [1, 128]])
                    pv = p1[:, sg, :].rearrange("p (h t) -> p h t", h=4)
                    P.tt("pool" if sg == 0 else "dve", pv, pv, mv, ALU.mult)

        def pv_phase(n, job):
            kv, i = job
            js = [j for j in (i - 1, i, i + 1) if 0 <= j < 16]
            p1 = pT[n % 2]
            po = psO[n % 2]
            for jj, j in enumerate(js):
                sg = j - i + 1
                P.mm(po[0:65, :], va[:, j, kv, :], p1[:, sg, :], start=(jj == 0), stop=(jj == len(js) - 1))
            P.copy("act", numer[0:65, 4 * kv:4 * kv + 4, i * 128:(i + 1) * 128],
                   po[0:65, :].rearrange("p (h t) -> p h t", h=4))
        for n_, job in enumerate(jobs):
            s_phase(n_, job)
            if n_ > 0:
                pv_phase(n_ - 1, jobs[n_ - 1])
            if hgen is not None:
                try:
                    next(hgen)
                except StopIteration:
                    hgen = None
        pv_phase(len(jobs) - 1, jobs[-1])
        if hgen is not None:
            for _ in hgen:
                pass
        for h in range(8):
            P.ts("dve", numer[64:65, h, :], numer[64:65, h, :], sk[64:65, h:h + 1], None, op0=ALU.add)
        _attn_finalize(self, sc, numer, 8, sel, psB, self.S["ycT"])


Builder.st_attn_a = st_attn_a
Builder.st_attn_c = st_attn_c


def _filter_body(self, sc, l):
    P = self.P
    I = self.I
    if True:
        zembT = self.load_const(sc, "zembT")
        negt = self.load_const(sc, "negt")
        ones = self.load_const(sc, "ones")
        w1 = sc.sb([33, 64], F32, "w1")
        w2 = sc.sb([64, 64], F32, "w2")
        w3 = sc.sb([64, 2048], F32, "w3")
        P.dma("sp", w1[:], I["hy_w1"][l])
        P.dma("sp", w2[:], I["hy_w2"][l])
        P.dma("sp", w3[:], I["hy_w3"][l])
        prm = sc.sb([64, 4], F32, "prm")
        for j, ap1 in enumerate((I["hy_b1"][l], I["hy_b2"][l], I["hy_freq"][l][0], I["hy_freq"][l][1])):
            P.dma("sp", prm[:, j:j + 1], cap(ap1, int(ap1.offset), [[1, 64], [1, 1]]))
        fb = sc.sb([64, 2], F32, "fb")
        P.tt("dve", fb[:, 0:1], prm[:, 0:1], prm[:, 2:3], ALU.mult)
        P.tt("dve", fb[:, 1:2], prm[:, 1:2], prm[:, 3:4], ALU.mult)
        psF = sc.ps([128, 2048], F32, "psF")
        psN = sc.ps([128, 1024], F32, "psN")
        h1T = sc.sb([64, T], F32, "h1T")
        h2T = sc.sb([64, T], F32, "h2T")
        arg = sc.sb([64, 512], F32, "arg")
        ki = sc.sb([64, 512], I32, "ki")
        kf = sc.sb([64, 512], F32, "kf")
        mk = sc.sb([64, 512], F32, "mk")

        def sin_layer(dst, lhsT, src, kk, fcol, fbcol):
            for tt in range(4):
                ps = psF[0:64, tt * 512:(tt + 1) * 512]
                P.mm(ps, lhsT, src[0:kk, tt * 512:(tt + 1) * 512])
                P.ts("dve", arg[:], ps, prm[:, fcol:fcol + 1], fb[:, fbcol:fbcol + 1], op0=ALU.mult, op1=ALU.add)
                P.ts("dve", arg[:], arg[:], 1.0 / (2.0 * PI), None, op0=ALU.mult)
                P.copy("dve", ki[:], arg[:])
                P.copy("dve", kf[:], ki[:])
                P.tt("dve", arg[:], arg[:], kf[:], ALU.subtract)
                P.ts("dve", mk[:], arg[:], 0.5, None, op0=ALU.is_gt)
                P.tt("dve", arg[:], arg[:], mk[:], ALU.subtract)
                P.ts("dve", mk[:], arg[:], -0.5, None, op0=ALU.is_lt)
                P.tt("dve", arg[:], arg[:], mk[:], ALU.add)
                P.act(dst[:, tt * 512:(tt + 1) * 512], arg[:], AF.Sin, scale=6.28318)
                yield
        yield from sin_layer(h1T, w1[0:33, :], zembT, 33, 2, 0)
        yield from sin_layer(h2T, w2[0:64, :], h1T, 64, 3, 1)
        ld = self.bcast_row(sc, I["hy_log_decay"][l], 2048, "ld")
        P.act(ld[:], ld[:], AF.Exp)
        gsum = sc.sb([128, 16, 1024], BF16, "gsum")
        gdiff = sc.sb([128, 16, 1024], BF16, "gdiff")
        dec = sc.sb([128, 2048], F32, "dec")
        filt = sc.sb([128, 2048], F32, "filt")
        sq = dec
        for mc in range(16):
            for cb in range(4):
                P.mm(psF[:, cb * 512:(cb + 1) * 512], h2T[0:64, mc * 128:(mc + 1) * 128], w3[0:64, cb * 512:(cb + 1) * 512])
            P.act(dec[:], ld[:], AF.Exp, scale=negt[:, mc:mc + 1])
            P.tt("dve", filt[:], psF[:, :], dec[:], ALU.mult)
            if mc == 0:
                P.memset("dve", filt[0:1, 1024:2048], 0.0)
            P.act(sq[:], filt[:], AF.Square)
            for half in range(2):
                for d in range(2):
                    c0 = d * 1024 + half * 512
                    P.mm(psN[:, half * 512:(half + 1) * 512], ones[:, :], sq[:, c0:c0 + 512],
                         start=(mc == 0 and d == 0), stop=(mc == 15 and d == 1))
            P.tt("pool", gsum[:, mc, :], filt[:, 0:1024], filt[:, 1024:2048], ALU.add)
            P.tt("pool", gdiff[:, mc, :], filt[:, 0:1024], filt[:, 1024:2048], ALU.subtract)
            yield
        rs = sc.sb([128, 1024], F32, "rs")
        P.ts("dve", rs[:], psN[:, :], 1e-12, None, op0=ALU.add)
        P.act(rs[:], rs[:], AF.Sqrt)
        P.op("dve", lambda e: e.reciprocal(rs[:], rs[:]), reads=[rs[:]], writes=[rs[:]])
        cfs = [sc.sb([128, 16, 128], BF16, "cf%d" % i) for i in range(2)]
        sfs = [sc.sb([128, 16, 128], BF16, "sf%d" % i) for i in range(2)]
        ho = [[sc.sb([128, 512], BF16, "ho%d_%d" % (i, j)) for j in range(3)] for i in range(2)]
        hs = self.S["hspec%d" % l]
        n = 0
        for fc in range(16):
            cf, sf = cfs[fc % 2], sfs[fc % 2]
            P.dma("sp", cf[:], self.C["cf"][fc])
            P.dma("sp", sf[:], self.C["sf"][fc])
            for half in range(2):
                cs = slice(half * 512, (half + 1) * 512)
                pre, pim, px = psF[:, 0:512], psF[:, 512:1024], psF[:, 1024:1536]
                for mc in range(16):
                    P.mm(pre, cf[:, mc, :], gsum[:, mc, cs], start=(mc == 0), stop=(mc == 15))
                for mc in range(16):
                    P.mm(pim, sf[:, mc, :], gdiff[:, mc, cs], start=(mc == 0), stop=(mc == 15))
                if fc == 0:
                    for mc in range(16):
                        P.mm(px, sf[:, mc, :], gsum[:, mc, cs], start=(mc == 0), stop=(mc == 15))
                hre, him, hrb = ho[n % 2]
                n += 1
                P.tt("dve", hre[:], pre, rs[:, cs], ALU.mult)
                P.tt("dve", him[:], pim, rs[:, cs], ALU.mult)
                P.copy("pool", hrb[:], hre[:])
                if fc == 0:
                    P.memset("pool", him[0:1, :], 0.0)
                    P.tt("dve", hrb[0:1, :], px[0:1, :], rs[0:1, cs], ALU.mult)
                for j, t_ in enumerate((hre, him, hrb)):
                    P.dma("pool", hs[j, fc * 128:(fc + 1) * 128, cs], t_[:])
                yield


def _hconv_body(self, sc, l):
    P = self.P
    cw = sc.sb([128, 12, 3], F32, "cw")
    cb = sc.sb([128, 12], F32, "cb")
    for i in range(3):
        P.dma("sp", cw[:, :, i], self.I["conv_w"][l][i].rearrange("(k p) -> p k", p=128), allow_slow_non_contiguous=True)
    P.dma("sp", cb[:], self.I["conv_b"][l].rearrange("(k p) -> p k", p=128), allow_slow_non_contiguous=True)
    xs = [sc.sb([128, T], F32, "hx%d" % i) for i in range(2)]
    os_ = [sc.sb([128, T], F32, "ho%d" % i) for i in range(2)]
    for cc in range(12):
        x, o = xs[cc % 2], os_[cc % 2]
        P.dma("sp", x[:], self.S["hyT"][cc * 128:(cc + 1) * 128, :])
        yield
        P.act(o[:], x[:], AF.Identity, bias=cb[:, cc:cc + 1], scale=cw[:, cc, 1:2])
        P.stt("dve", o[:, 1:T], x[:, 0:T - 1], cw[:, cc, 0:1], o[:, 1:T], ALU.mult, ALU.add)
        P.stt("dve", o[:, 0:T - 1], x[:, 1:T], cw[:, cc, 2:3], o[:, 0:T - 1], ALU.mult, ALU.add)
        P.dma("pool", self.S["hcT"][cc * 128:(cc + 1) * 128, :], o[:])
        yield


def st_hconv(self, s, l):
    with self.stage() as sc:
        for _ in _hconv_body(self, sc, l):
            pass


def st_hyena(self, s, l, o):
    P = self.P
    zsrc = self.S["hcT"][0:512, :] if o == 0 else self.S["z1T"]
    gsrc = self.S["hcT"][(o + 1) * 512:(o + 2) * 512, :]
    hs = self.S["hspec%d" % l]
    with self.stage() as sc:
        identb = self.load_const(sc, "identb")
        hb = sc.sb([128, 4], F32, "hb")
        P.dma("sp", hb[:], self.I["hy_bias"][l][o].rearrange("(k p) -> p k", p=128), allow_slow_non_contiguous=True)
        zb = sc.sb([128, 4, T], BF16, "zb")
        zfs = [sc.sb([128, T], F32, "zf%d" % i) for i in range(2)]
        for cc in range(4):
            zf = zfs[cc % 2]
            P.dma("sp", zf[:], zsrc[cc * 128:(cc + 1) * 128, :])
            P.copy("pool" if cc % 2 else "dve", zb[:, cc, :], zf[:])
        zTM = sc.sb([128, 16, 512], BF16, "zTM")
        psT = [sc.ps([128, 1024], BF16, "psT%d" % i) for i in range(2)]
        for tc in range(16):
            pt = psT[tc % 2]
            for cc in range(4):
                P.tr(pt[:, cc * 128:(cc + 1) * 128], zb[:, cc, tc * 128:(tc + 1) * 128], identb[:])
            P.copy("act", zTM[:, tc, :], pt[:, 0:512])
        Yre = sc.sb([128, 16, 512], BF16, "Yre")
        Yim = sc.sb([128, 16, 512], BF16, "Yim")
        cfs = [sc.sb([128, 16, 128], BF16, "cf%d" % i) for i in range(2)]
        sfs = [sc.sb([128, 16, 128], BF16, "sf%d" % i) for i in range(2)]
        hts = [[sc.sb([128, 512], BF16, "h%d_%d" % (i, j)) for j in range(3)] for i in range(2)]
        tmps = [[sc.sb([128, 512], F32, "tm%d_%d" % (i, j)) for j in range(4)] for i in range(2)]
        psR = [sc.ps([128, 512], F32, "psR%d" % i) for i in range(2)]
        psI = [sc.ps([128, 512], F32, "psI%d" % i) for i in range(2)]
        cs = slice(o * 512, (o + 1) * 512)
        for fc in range(16):
            cf, sf = cfs[fc % 2], sfs[fc % 2]
            P.dma("sp", cf[:], self.C["cf"][fc])
            P.dma("sp", sf[:], self.C["sf"][fc])
            hre, him, hrb = hts[fc % 2]
            for j, t_ in enumerate((hre, him, hrb)):
                P.dma("sp", t_[:], hs[j, fc * 128:(fc + 1) * 128, cs])
            pr, pi = psR[fc % 2], psI[fc % 2]
            for tc in range(16):
                P.mm(pr[:, :], cf[:, tc, :], zTM[:, tc, :], start=(tc == 0), stop=(tc == 15))
            for tc in range(16):
                P.mm(pi[:, :], sf[:, tc, :], zTM[:, tc, :], start=(tc == 0), stop=(tc == 15))
            t1, t2, t3, t4 = tmps[fc % 2]
            P.tt("dve", t1[:], pr[:, :], hre[:], ALU.mult)
            P.tt("dve", t2[:], pi[:, :], him[:], ALU.mult)
            P.tt("dve", t3[:], pr[:, :], him[:], ALU.mult)
            P.tt("dve", t4[:], pi[:, :], hrb[:], ALU.mult)
            P.tt("pool", Yre[:, fc, :], t1[:], t2[:], ALU.subtract)
            P.tt("pool", Yim[:, fc, :], t3[:], t4[:], ALU.add)
        cis = [sc.sb([128, 16, 256], BF16, "ci%d" % i) for i in range(2)]
        sis = [sc.sb([128, 16, 256], BF16, "si%d" % i) for i in range(2)]
        psY = [sc.ps([128, 512], F32, "psY%d" % i) for i in range(2)]
        zps = [sc.sb([128, 256], F32, "zp%d" % i) for i in range(2)]
        gps = [sc.sb([128, 256], F32, "gp%d" % i) for i in range(2)]
        tms = [sc.sb([128, 256], F32, "tq%d" % i) for i in range(2)]
        odt = F32 if o == 0 else BF16
        ors = [sc.sb([128, 256], odt, "or%d" % i) for i in range(2)]
        dest = self.S["z1T"] if o == 0 else self.S["ybT"]
        n = 0
        for tt in range(8):
            ci, si = cis[tt % 2], sis[tt % 2]
            P.dma("sp", ci[:], self.C["ci"][tt])
            P.dma("sp", si[:], self.C["si"][tt])
            ts_ = slice(tt * 256, (tt + 1) * 256)
            for cc in range(4):
                py = psY[n % 2]
                zp, gp, tm, orr = zps[n % 2], gps[n % 2], tms[n % 2], ors[n % 2]
                n += 1
                P.dma("sp", zp[:], zsrc[cc * 128:(cc + 1) * 128, ts_])
                P.dma("sp", gp[:], gsrc[cc * 128:(cc + 1) * 128, ts_])
                for fc in range(16):
                    P.mm(py[:, 0:256], Yre[:, fc, cc * 128:(cc + 1) * 128], ci[:, fc, :], start=(fc == 0), stop=False)
                for fc in range(16):
                    P.mm(py[:, 0:256], Yim[:, fc, cc * 128:(cc + 1) * 128], si[:, fc, :], start=False, stop=(fc == 15))
                P.stt("dve", tm[:], zp[:], hb[:, cc:cc + 1], py[:, 0:256], ALU.mult, ALU.add)
                P.tt("pool", orr[:], tm[:], gp[:], ALU.mult)
                P.dma("pool", dest[cc * 128:(cc + 1) * 128, ts_], orr[:])


def st_filter(self, l):
    with self.stage() as sc:
        for _ in _filter_body(self, sc, l):
            pass


Builder.st_filter = st_filter
Builder.st_hconv = st_hconv
Builder.st_hyena = st_hyena

import os


class LNBufs:
    def __init__(self, sc):
        self.s1 = sc.sb([128, 1], F32, "ln_s1")
        self.nm = sc.sb([128, 1], F32, "ln_nm")
        self.ss = sc.sb([128, 1], F32, "ln_ss")
        self.rstd = sc.sb([128, 1], F32, "ln_rstd")
        self.sq = sc.sb([128, DM], F32, "ln_sq")
        self.y = [sc.sb([128, DM], F32, "ln_y%d" % i) for i in range(2)]
        self.n = 0


def ln_tile(P, lb, r, lng, lnb, dest):
    y = lb.y[lb.n % 2]
    lb.n += 1
    P.reduce("dve", lb.s1[:], r[:], ALU.add)
    P.ts("dve", lb.nm[:], lb.s1[:], -1.0 / DM, None, op0=ALU.mult)
    P.act(lb.sq[:], r[:], AF.Square, bias=lb.nm[:, 0:1])
    P.reduce("dve", lb.ss[:], lb.sq[:], ALU.add)
    P.ts("dve", lb.rstd[:], lb.ss[:], 1.0 / DM, LN_EPS_C, op0=ALU.mult, op1=ALU.add)
    P.act(lb.rstd[:], lb.rstd[:], AF.Sqrt)
    P.op("dve", lambda e: e.reciprocal(lb.rstd[:], lb.rstd[:]), reads=[lb.rstd[:]], writes=[lb.rstd[:]])
    P.ts("dve", y[:], r[:], lb.nm[:, 0:1], lb.rstd[:, 0:1], op0=ALU.add, op1=ALU.mult)
    P.tt("pool", y[:], y[:], lng[:], ALU.mult)
    P.tt("pool", y[:], y[:], lnb[:], ALU.add)
    P.dma("pool", dest, y[:])


def st_merge(self, s, l):
    P = self.P
    I = self.I
    xsrc = I["x"][s] if l == 0 else self.S["xres"][s]
    ada = self.S["ada%d" % l]
    with self.stage() as sc:
        ys = {}
        for nm, nk in (("yaT", 2), ("ybT", 4), ("ycT", 4)):
            t_ = sc.sb([128, nk, T], BF16, nm)
            for k in range(nk):
                P.dma("sp", t_[:, k, :], self.S[nm][k * 128:(k + 1) * 128, :])
            ys[nm] = t_
        wst = [sc.sb([128, DM], F32, "wst%d" % i) for i in range(2)]
        ws = {}
        n = 0
        for nm, nk in (("w_branch_a", 2), ("w_branch_b", 4), ("w_branch_c", 4), ("w_out", 8)):
            t_ = sc.sb([128, nk, DM], BF16, nm)
            for k in range(nk):
                st = wst[n % 2]
                P.dma("sp", st[:], I[nm][l][k * 128:(k + 1) * 128, :])
                P.copy("pool" if n % 2 else "dve", t_[:, k, :], st[:])
                n += 1
            ws[nm] = t_
        mergedT = sc.sb([128, 8, T], BF16, "mergedT")
        psb = [sc.ps([128, 512], F32, "psbr%d" % i) for i in range(3)]
        gts = [[sc.sb([128, 512], BF16, "g%d_%d" % (i, j)) for j in range(3)] for i in range(2)]
        ms = [[sc.sb([128, 512], F32, "m%d_%d" % (i, j)) for j in range(3)] for i in range(2)]
        n = 0
        for fc in range(8):
            for tt in range(4):
                ts_ = slice(tt * 512, (tt + 1) * 512)
                g3 = gts[n % 2]
                m3 = ms[n % 2]
                n += 1
                for j, (yn, wn, nk) in enumerate((("yaT", "w_branch_a", 2), ("ybT", "w_branch_b", 4), ("ycT", "w_branch_c", 4))):
                    for k in range(nk):
                        P.mm(psb[j][:, :], ws[wn][:, k, fc * 128:(fc + 1) * 128], ys[yn][:, k, ts_],
                             start=(k == 0), stop=(k == nk - 1))
                    P.dma("sp", g3[j][:], self.S["gateT"][j * 1024 + fc * 128: j * 1024 + (fc + 1) * 128, ts_])
                    P.tt("dve", m3[j][:], psb[j][:, :], g3[j][:], ALU.mult)
                P.tt("pool", m3[0][:], m3[0][:], m3[1][:], ALU.add)
                P.tt("pool", mergedT[:, fc, ts_], m3[0][:], m3[2][:], ALU.add)
        g1b = self.bcast_row(sc, ada[s, 2048:3072], 1024, "g1b")
        lng = self.bcast_row(sc, I["ln_g"][l, 0], 1024, "lng")
        lnb = self.bcast_row(sc, I["ln_b"][l, 0], 1024, "lnb")
        lb = LNBufs(sc)
        psM = [sc.ps([128, 1024], F32, "psM%d" % i) for i in range(2)]
        xts = [sc.sb([128, DM], F32, "xt%d" % i) for i in range(2)]
        tms = [sc.sb([128, DM], F32, "tm%d" % i) for i in range(2)]
        for i in range(16):
            pm = psM[i % 2]
            xt = xts[i % 2]
            tm = tms[i % 2]
            P.dma("sp", xt[:], xsrc[i * 128:(i + 1) * 128, :])
            for hf in range(2):
                for k in range(8):
                    P.mm(pm[:, hf * 512:(hf + 1) * 512], mergedT[:, k, i * 128:(i + 1) * 128],
                         ws["w_out"][:, k, hf * 512:(hf + 1) * 512], start=(k == 0), stop=(k == 7))
            P.tt("dve", tm[:], pm[:, :], g1b[:], ALU.mult)
            P.stt("dve", tm[:], xt[:], ALPHA_C, tm[:], ALU.mult, ALU.add)
            ln_tile(P, lb, tm, lng, lnb, self.S["xres"][s][i * 128:(i + 1) * 128, :])


def _uv_body(self, sc, l):
    P = self.P
    if True:
        identb = self.load_const(sc, "identb")
        ufs = [sc.sb([128, DM], F32, "uf%d" % i) for i in range(2)]
        vfs = [sc.sb([128, DM], F32, "vf%d" % i) for i in range(2)]
        ubs = [sc.sb([128, DM], BF16, "ub%d" % i) for i in range(2)]
        vbs = [sc.sb([128, DM], BF16, "vb%d" % i) for i in range(2)]
        uts = [sc.sb([128, 8, 128], BF16, "ut%d" % i) for i in range(2)]
        pst = [sc.ps([128, 1024], BF16, "pst%d" % i) for i in range(2)]
        U = self.I["peer_u"][l]
        V = self.I["peer_v"][l]
        for b in range(128):
            uf, vf, ub, vb, ut, pt = ufs[b % 2], vfs[b % 2], ubs[b % 2], vbs[b % 2], uts[b % 2], pst[b % 2]
            P.dma("sp", uf[:], cap(U, int(U.offset) + b * DM, [[128 * DM, 128], [1, DM]]))
            P.dma("sp", vf[:], cap(V, int(V.offset) + b * DM, [[128 * DM, 128], [1, DM]]))
            P.copy("dve", ub[:], uf[:])
            P.copy("pool", vb[:], vf[:])
            for k in range(8):
                P.tr(pt[:, k * 128:(k + 1) * 128], ub[:, k * 128:(k + 1) * 128], identb[:])
            P.copy("act", ut[:], pt[:, :].rearrange("p (k a) -> p k a", k=8))
            P.dma("pool", self.S["ut%d" % l][b], ut[:])
            P.dma("pool", self.S["vb%d" % l][b], vb[:])
            yield


def st_peer_a(self, s, l):
    P = self.P
    I = self.I
    ada = self.S["ada%d" % l]
    with self.stage() as sc:
        identb = self.load_const(sc, "identb")
        identf = self.load_const(sc, "identf")
        iota16 = self.load_const(sc, "iota16")
        shb = self.bcast_row(sc, ada[s, 3072:4096], 1024, "sh2b")
        scb = self.bcast_row(sc, ada[s, 4096:5120], 1024, "sc2b")
        u2T = sc.sb([128, 8, T], BF16, "u2T")
        pst = sc.ps([128, 1024], BF16, "pst")
        self.make_uT(sc, self.S["xres"][s], scb, shb, u2T, identb, pst)
        P.dma("pool", self.S["u2T"], u2T[:])
        wq = sc.sb([128, 8, 2048], BF16, "wq")
        wst = [sc.sb([128, 2048], F32, "wqs%d" % i) for i in range(2)]
        for k in range(8):
            P.dma("sp", wst[k % 2][:], I["peer_wq"][l][k * 128:(k + 1) * 128, :])
            P.copy("pool" if k % 2 else "dve", wq[:, k, :], wst[k % 2][:])
        psK = sc.ps([128, 512], F32, "psK")
        keysT = sc.sb([128, 16, 128], BF16, "keysT")
        kst = [sc.sb([128, 128], F32, "kst%d" % i) for i in range(2)]
        for hp in range(16):
            P.dma("sp", kst[hp % 2][:], I["peer_keys"][l][hp // 2, hp % 2])
            P.tr(psK[:, 0:128], kst[hp % 2][:], identf[:])
            P.copy("act", keysT[:, hp, :], psK[:, 0:128])
        psQ = [sc.ps([128, 512], F32, "psQ%d" % i) for i in range(2)]
        psSc = sc.ps([128, 2048], F32, "psSc")
        qT = sc.sb([128, 16, 128], BF16, "qT")
        scs = sc.sb([128, 16, 128], F32, "scs")
        vals = sc.sb([128, 16, 16], F32, "vals")
        idxu = sc.sb([128, 16, 16], U32, "idxu")
        idxf = sc.sb([128, 16, 16], F32, "idxf")
        wk = sc.sb([128, 16, 128], F32, "wk")
        cand = sc.sb([128, 8, 256], F32, "cand")
        wk2 = sc.sb([128, 8, 256], F32, "wk2")
        best = sc.sb([128, 8, 16], F32, "best")
        cidx = sc.sb([128, 8, 16], U32, "cidx")
        iku = sc.sb([128, 128], U32, "iku")
        jku = sc.sb([128, 128], U32, "jku")
        ikf = sc.sb([128, 128], F32, "ikf")
        jkf = sc.sb([128, 128], F32, "jkf")
        eq = sc.sb([128, 8, 16, 16], F32, "eq")
        eq2 = sc.sb([128, 8, 16, 16], F32, "eq2")
        abg = sc.sb([128, 3, 128], F32, "abg")
        eg = sc.sb([128, 8, 16], F32, "eg")
        zz = sc.sb([128, 8], F32, "zz")
        abgT = [sc.sb([128, 3, 128], BF16, "abgT%d" % i) for i in range(2)]

        def top16b(items):
            for (vout, iout, src, scratch) in items:
                P.op("dve", lambda e, vout=vout, src=src: e.max(out=vout[:, 0:8], in_=src), reads=[src], writes=[vout[:, 0:8]])
            for (vout, iout, src, scratch) in items:
                P.op("dve", lambda e, vout=vout, iout=iout, src=src: e.max_index(out=iout[:, 0:8], in_max=vout[:, 0:8], in_values=src),
                     reads=[src, vout[:, 0:8]], writes=[iout[:, 0:8]])
            for (vout, iout, src, scratch) in items:
                P.op("dve", lambda e, vout=vout, src=src, scratch=scratch: e.match_replace(out=scratch, in_to_replace=vout[:, 0:8], in_values=src, imm_value=-1e30),
                     reads=[src, vout[:, 0:8]], writes=[scratch])
            for (vout, iout, src, scratch) in items:
                P.op("dve", lambda e, vout=vout, scratch=scratch: e.max(out=vout[:, 8:16], in_=scratch), reads=[scratch], writes=[vout[:, 8:16]])
            for (vout, iout, src, scratch) in items:
                P.op("dve", lambda e, vout=vout, iout=iout, scratch=scratch: e.max_index(out=iout[:, 8:16], in_max=vout[:, 8:16], in_values=scratch),
                     reads=[scratch, vout[:, 8:16]], writes=[iout[:, 8:16]])

        def pstr(t_):
            return t_[:].ap[0][0]

        for i in range(16):
            tsl = slice(i * 128, (i + 1) * 128)
            for hp in range(16):
                pq = psQ[(hp // 4) % 2]
                for k in range(8):
                    P.mm(pq[:, (hp % 4) * 128:(hp % 4 + 1) * 128], wq[:, k, hp * 128:(hp + 1) * 128], u2T[:, k, tsl],
                         start=(k == 0), stop=(k == 7))
                if hp % 4 == 3:
                    P.copy("act", qT[:, hp - 3:hp + 1, :], pq[:, :].rearrange("p (h t) -> p h t", h=4))
            for hp in range(16):
                P.mm(psSc[:, hp * 128:(hp + 1) * 128], qT[:, hp, :], keysT[:, hp, :])
            P.copy("act", scs[:], psSc[:, :].rearrange("p (h n) -> p h n", h=16))
            if os.environ.get('PA_SKIP'):
                continue
            top16b([(vals[:, hp, :], idxu[:, hp, :], scs[:, hp, :], wk[:, hp, :]) for hp in range(16)])
            P.copy("act", idxf[:], idxu[:])
            vb_ = vals[:]
            P.tt("pool", cand[:].rearrange("p h (i j) -> p h i j", i=16),
                 cap(vb_, int(vb_.offset), [[pstr(vals), 128], [32, 8], [1, 16], [0, 16]]),
                 cap(vb_, int(vb_.offset) + 16, [[pstr(vals), 128], [32, 8], [0, 16], [1, 16]]), ALU.add)
            top16b([(best[:, h, :], cidx[:, h, :], cand[:, h, :], wk2[:, h, :]) for h in range(8)])
            cflat = cidx[:].rearrange("p h k -> p (h k)")
            P.ts("dve", iku[:], cflat, 4, None, op0=ALU.logical_shift_right)
            P.ts("dve", jku[:], cflat, 15, None, op0=ALU.bitwise_and)
            P.copy("act", ikf[:], iku[:])
            P.copy("act", jkf[:], jku[:])
            io = iota16[:]
            iob = cap(io, int(io.offset), [[pstr(iota16), 128], [0, 8], [0, 16], [1, 16]])
            fb_ = idxf[:]
            for (kf_, off, col, e_) in ((ikf, 0, 0, eq), (jkf, 16, 1, eq2)):
                kb_ = kf_[:]
                P.tt("dve", e_[:], cap(kb_, int(kb_.offset), [[pstr(kf_), 128], [16, 8], [1, 16], [0, 16]]), iob, ALU.is_equal)
                P.tt("pool", e_[:], e_[:],
                     cap(fb_, int(fb_.offset) + off, [[pstr(idxf), 128], [32, 8], [0, 16], [1, 16]]), ALU.mult)
                P.reduce("dve", abg[:, col, :].rearrange("p (h k) -> p h k", h=8), e_[:], ALU.add)
            bb_ = best[:]
            P.tt("pool", eg[:], best[:], cap(bb_, int(bb_.offset), [[pstr(best), 128], [16, 8], [0, 16]]), ALU.subtract)
            P.act(eg[:], eg[:], AF.Exp)
            P.reduce("dve", zz[:], eg[:], ALU.add)
            P.op("dve", lambda e: e.reciprocal(zz[:], zz[:]), reads=[zz[:]], writes=[zz[:]])
            zb_ = zz[:]
            P.tt("pool", abg[:, 2, :].rearrange("p (h k) -> p h k", h=8), eg[:],
                 cap(zb_, int(zb_.offset), [[pstr(zz), 128], [1, 8], [0, 16]]), ALU.mult)
            at = abgT[i % 2]
            for j in range(3):
                P.tr(psK[:, j * 128:(j + 1) * 128], abg[:, j, :], identf[:])
            P.copy("act", at[:], psK[:, 0:384].rearrange("p (j t) -> p j t", j=3))
            for j, nm in enumerate(("pa", "pb", "pg")):
                P.dma("pool", self.S[nm][:, tsl], at[:, j, :])


def st_peer_b(self, s, l, a2=False):
    P = self.P
    I = self.I
    ada = self.S["ada%d" % l]
    dest = self.out[s] if l == 1 else self.S["xres"][s]
    with self.stage() as sc:
        iota = self.load_const(sc, "iota128")
        g2b = self.bcast_row(sc, ada[s, 5120:6144], 1024, "g2b")
        lng = self.bcast_row(sc, I["ln_g"][l, 1], 1024, "lng")
        lnb = self.bcast_row(sc, I["ln_b"][l, 1], 1024, "lnb")
        lb = LNBufs(sc)
        GTs = [sc.sb([128, 256, 64], BF16, "GT%d" % i) for i in range(2)]
        psBig = sc.ps([128, 2048], F32, "psBig")
        psA = [sc.ps([128, 512], F32, "psA%d" % i) for i in range(2)]
        psG = [sc.ps([128, 512], F32, "psG%d" % i) for i in range(2)]
        u2s = [sc.sb([128, 8, 256], BF16, "u2_%d" % i) for i in range(2)]
        abTs = [sc.sb([128, 3, 256], BF16, "abT%d" % i) for i in range(2)]
        OAs = [sc.sb([128, 16, 128], BF16, "OA%d" % i) for i in range(3)]
        OBs = [sc.sb([128, 16, 64], BF16, "OB%d" % i) for i in range(3)]
        uts = [sc.sb([128, 8, 128], BF16, "utb%d" % i) for i in range(5)]
        vbs = [sc.sb([128, DM], BF16, "vbb%d" % i) for i in range(5)]
        gls = [sc.sb([128, 256], F32, "gl%d" % i) for i in range(4)]
        Ws = [sc.sb([128, 256], BF16, "W%d" % i) for i in range(4)]
        xts = [sc.sb([128, DM], F32, "xt%d" % i) for i in range(1)] * 2
        tms = [sc.sb([128, DM], F32, "tm%d" % i) for i in range(1)] * 2
        io = iota[:]
        pio = io.ap[0][0]
        iob = cap(io, int(io.offset), [[pio, 128], [0, 16], [1, 128]])
        st = {"ne": 0, "nb": 0}

        def load_group(g):
            t0 = g * 256
            P.dma("sp", u2s[g % 2][:], self.S["u2T"][:, :, t0:t0 + 256])
            for j, nm in enumerate(("pa", "pb", "pg")):
                P.dma("sp", abTs[g % 2][:, j, :], self.S[nm][:, t0:t0 + 256])

        def gen_sub(g, half, sub):
            ab = abTs[g % 2][:]
            pab = ab.ap[0][0]
            OA, OB = OAs[sub % 3], OBs[sub % 3]
            av = cap(ab, int(ab.offset) + sub * 16, [[pab, 128], [1, 16], [0, 128]])
            bv = cap(ab, int(ab.offset) + 256 + sub * 16, [[pab, 128], [1, 16], [0, 64]])
            gv = cap(ab, int(ab.offset) + 512 + sub * 16, [[pab, 128], [1, 16], [0, 64]])
            iobh = cap(io, int(io.offset) + half * 64, [[pio, 128], [0, 16], [1, 64]])
            P.tt("dve", OB[:], iobh, bv, ALU.is_equal)
            P.tt("pool", OB[:], OB[:], gv, ALU.mult)
            P.tt("dve", OA[:], iob, av, ALU.is_equal)

        def mm_sub(g, half, sub):
            GT = GTs[half]
            OA, OB = OAs[sub % 3], OBs[sub % 3]
            for q in range(4):
                pg = psG[st["ne"] % 2]
                for t4 in range(4):
                    tk = q * 4 + t4
                    P.mm(pg[:, t4 * 64:(t4 + 1) * 64], OA[:, tk, :], OB[:, tk, :])
                tok = sub * 16 + q * 4
                P.copy("act", GT[:, tok:tok + 4, :],
                       pg[:, 0:256].rearrange("p (t b) -> p t b", t=4))
                st["ne"] += 1

        def build_sub(g, half, sub):
            gen_sub(g, half, sub)
            mm_sub(g, half, sub)

        def a_phase(g, b):
            u2 = u2s[g % 2]
            ut, vb = uts[b % 5], vbs[b % 5]
            P.dma("sp", ut[:], self.S["ut%d" % l][b])
            P.dma("sp", vb[:], self.S["vb%d" % l][b])
            pa_ = psA[b % 2][:, 0:256]
            for k in range(8):
                P.mm(pa_, ut[:, k, :], u2[:, k, :], start=(k == 0), stop=(k == 7))
            gl, W = gls[b % 4], Ws[b % 4]
            P.act(gl[:], pa_, AF.Gelu)
            P.tt("pool", W[:], gl[:], GTs[b // 64][:, :, b % 64], ALU.mult)

        def v_phase(b):
            vb = vbs[b % 5]
            W = Ws[b % 4]
            for ti in range(2):
                for hf in range(2):
                    o0 = ti * 1024 + hf * 512
                    P.mm(psBig[:, o0:o0 + 512], W[:, ti * 128:(ti + 1) * 128], vb[:, hf * 512:(hf + 1) * 512],
                         start=(b == 0), stop=(b == 127))

        a2gen = None
        if a2:
            bf = A2Bufs(self, sc)
            trp_ab = psG[0][:, 256:512]
            trp_g = psG[1][:, 256:384]

            def a2_pair(g2):
                for ti_ in (2 * g2, 2 * g2 + 1):
                    yield from _a2_tile(self, bf, ti_, trp_ab, trp_g)
        load_group(0)
        for sub in range(16):
            build_sub(0, 0, sub)
        for g in range(8):
            t0 = g * 256
            if a2 and a2gen is not None:
                for _ in a2gen:
                    pass
                a2gen = None
            if g + 1 < 8:
                load_group(g + 1)
            if a2 and g + 2 < 8:
                a2gen = a2_pair(g + 2)
            for b in range(128):
                if a2gen is not None and b % 2 == 0:
                    try:
                        next(a2gen)
                    except StopIteration:
                        a2gen = None
                a_phase(g, b)
                if b >= 2:
                    v_phase(b - 2)
                tgt = (g, 1) if b < 64 else ((g + 1, 0) if g + 1 < 8 else None)
                if tgt is not None:
                    if b % 4 == 1:
                        sub = (b % 64) // 4
                        if sub > 1:
                            mm_sub(tgt[0], tgt[1], sub - 2)
                        gen_sub(tgt[0], tgt[1], sub)
                    elif b % 64 == 62:
                        mm_sub(tgt[0], tgt[1], 14)
                    elif b % 64 == 63:
                        mm_sub(tgt[0], tgt[1], 15)
            v_phase(126)
            v_phase(127)
            for ti in range(2):
                xt, tm = xts[ti], tms[ti]
                rows = slice(t0 + ti * 128, t0 + (ti + 1) * 128)
                P.dma("sp", xt[:], self.S["xres"][s][rows, :])
                P.tt("dve", tm[:], psBig[:, ti * 1024:(ti + 1) * 1024], g2b[:], ALU.mult)
                P.stt("dve", tm[:], xt[:], ALPHA_C, tm[:], ALU.mult, ALU.add)
                ln_tile(P, lb, tm, lng, lnb, dest[rows, :])


Builder.st_merge = st_merge


def st_uv(self, l):
    with self.stage() as sc:
        for _ in _uv_body(self, sc, l):
            pass


def st_pre(self, l):
    with self.stage() as sc:
        ga = _filter_body(self, sc, l)
        gb = _uv_body(self, sc, l)
        da = db = False
        while not (da and db):
            if not da:
                try:
                    next(ga)
                except StopIteration:
                    da = True
            for _ in range(2):
                if not db:
                    try:
                        next(gb)
                    except StopIteration:
                        db = True


Builder.st_uv = st_uv
Builder.st_pre = st_pre
Builder.st_peer_a = st_peer_a
Builder.st_peer_b = st_peer_b


class A2Bufs:
    def __init__(self, B, sc):
        self.identf = B.load_const(sc, "identf")
        self.iota16 = B.load_const(sc, "iota16")
        self.scs = sc.sb([128, 16, 128], F32, "a2scs")
        self.wk = sc.sb([128, 16, 128], F32, "a2wk")
        self.vals = sc.sb([128, 16, 16], F32, "a2vals")
        self.idxu = sc.sb([128, 16, 16], U32, "a2idxu")
        self.idxf = sc.sb([128, 16, 16], F32, "a2idxf")
        self.best = sc.sb([128, 8, 16], F32, "a2best")
        self.cidx = sc.sb([128, 8, 16], U32, "a2cidx")
        self.iku = sc.sb([128, 128], U32, "a2iku")
        self.jku = sc.sb([128, 128], U32, "a2jku")
        self.ikf = sc.sb([128, 128], F32, "a2ikf")
        self.jkf = sc.sb([128, 128], F32, "a2jkf")
        self.eq = sc.sb([128, 8, 16, 16], F32, "a2eq")
        self.abg = sc.sb([128, 3, 128], F32, "a2abg")
        self.eg = sc.sb([128, 8, 16], F32, "a2eg")
        self.zz = sc.sb([128, 8], F32, "a2zz")
        self.abgT = [sc.sb([128, 3, 128], BF16, "a2abgT%d" % i) for i in range(2)]


def _a2_tile(B, bf, i, trp_ab, trp_g):
    P = B.P
    scs, wk, vals, idxu, idxf = bf.scs, bf.wk, bf.vals, bf.idxu, bf.idxf
    best, cidx, eq, abg, eg, zz = bf.best, bf.cidx, bf.eq, bf.abg, bf.eg, bf.zz
    cand = scs[:].rearrange("p a b -> p (a b)").rearrange("p (h n) -> p h n", h=8)
    wk2 = wk[:].rearrange("p a b -> p (a b)").rearrange("p (h n) -> p h n", h=8)
    tsl = slice(i * 128, (i + 1) * 128)
    P.dma("sp", scs[:], B.S["scs"][tsl, :].rearrange("p (h n) -> p h n", h=16))

    def pstr(t_):
        return t_[:].ap[0][0]

    def top16b(items, nsplit):
        steps = []
        for (vout, iout, src, scratch) in items:
            steps.append((lambda e, vout=vout, src=src: e.max(out=vout[:, 0:8], in_=src), [src], [vout[:, 0:8]]))
        for (vout, iout, src, scratch) in items:
            steps.append((lambda e, vout=vout, iout=iout, src=src: e.max_index(out=iout[:, 0:8], in_max=vout[:, 0:8], in_values=src),
                          [src, vout[:, 0:8]], [iout[:, 0:8]]))
        for (vout, iout, src, scratch) in items:
            steps.append((lambda e, vout=vout, src=src, scratch=scratch: e.match_replace(out=scratch, in_to_replace=vout[:, 0:8], in_values=src, imm_value=-1e30),
                          [src, vout[:, 0:8]], [scratch]))
        for (vout, iout, src, scratch) in items:
            steps.append((lambda e, vout=vout, scratch=scratch: e.max(out=vout[:, 8:16], in_=scratch), [scratch], [vout[:, 8:16]]))
        for (vout, iout, src, scratch) in items:
            steps.append((lambda e, vout=vout, iout=iout, scratch=scratch: e.max_index(out=iout[:, 8:16], in_max=vout[:, 8:16], in_values=scratch),
                          [scratch, vout[:, 8:16]], [iout[:, 8:16]]))
        for n_, (fn, rd, wr) in enumerate(steps):
            P.op("dve", fn, reads=rd, writes=wr)
            if n_ % nsplit == nsplit - 1:
                yield

    yield
    yield from top16b([(vals[:, hp, :], idxu[:, hp, :], scs[:, hp, :], wk[:, hp, :]) for hp in range(16)], 8)
    P.copy("act", idxf[:], idxu[:])
    vb_ = vals[:]
    c4 = cand.rearrange("p h (i j) -> p h i j", i=16)
    for hq in range(4):
        P.tt("pool", c4[:, 2 * hq:2 * hq + 2],
             cap(vb_, int(vb_.offset) + 64 * hq, [[pstr(vals), 128], [32, 2], [1, 16], [0, 16]]),
             cap(vb_, int(vb_.offset) + 64 * hq + 16, [[pstr(vals), 128], [32, 2], [0, 16], [1, 16]]), ALU.add)
        yield
    yield from top16b([(best[:, h, :], cidx[:, h, :], cand[:, h, :], wk2[:, h, :]) for h in range(8)], 8)
    cflat = cidx[:].rearrange("p h k -> p (h k)")
    P.ts("dve", bf.iku[:], cflat, 4, None, op0=ALU.logical_shift_right)
    P.ts("dve", bf.jku[:], cflat, 15, None, op0=ALU.bitwise_and)
    P.copy("act", bf.ikf[:], bf.iku[:])
    P.copy("act", bf.jkf[:], bf.jku[:])
    yield
    io = bf.iota16[:]
    iob = cap(io, int(io.offset), [[pstr(bf.iota16), 128], [0, 8], [0, 16], [1, 16]])
    fb_ = idxf[:]
    for (kf_, off, col) in ((bf.ikf, 0, 0), (bf.jkf, 16, 1)):
        kb_ = kf_[:]
        iob2 = cap(io, int(io.offset), [[pstr(bf.iota16), 128], [0, 2], [0, 16], [1, 16]])
        for hq in range(4):
            e_ = eq[:, 2 * hq:2 * hq + 2]
            P.tt("dve", e_, cap(kb_, int(kb_.offset) + 32 * hq, [[pstr(kf_), 128], [16, 2], [1, 16], [0, 16]]), iob2, ALU.is_equal)
            P.tt("pool", e_, e_,
                 cap(fb_, int(fb_.offset) + off + 64 * hq, [[pstr(idxf), 128], [32, 2], [0, 16], [1, 16]]), ALU.mult)
            yield
        P.reduce("dve", abg[:, col, :].rearrange("p (h k) -> p h k", h=8), eq[:], ALU.add)
        yield
    bb_ = best[:]
    P.tt("pool", eg[:], best[:], cap(bb_, int(bb_.offset), [[pstr(best), 128], [16, 8], [0, 16]]), ALU.subtract)
    P.act(eg[:], eg[:], AF.Exp)
    P.reduce("dve", zz[:], eg[:], ALU.add)
    P.op("dve", lambda e: e.reciprocal(zz[:], zz[:]), reads=[zz[:]], writes=[zz[:]])
    zb_ = zz[:]
    P.tt("pool", abg[:, 2, :].rearrange("p (h k) -> p h k", h=8), eg[:],
         cap(zb_, int(zb_.offset), [[pstr(zz), 128], [1, 8], [0, 16]]), ALU.mult)
    yield
    at = bf.abgT[i % 2]
    P.tr(trp_ab[:, 0:128], abg[:, 0, :], bf.identf[:])
    P.tr(trp_ab[:, 128:256], abg[:, 1, :], bf.identf[:])
    P.copy("act", at[:, 0:2, :], trp_ab.rearrange("p (j t) -> p j t", j=2))
    P.tr(trp_g, abg[:, 2, :], bf.identf[:])
    P.copy("act", at[:, 2, :], trp_g)
    for j, nm in enumerate(("pa", "pb", "pg")):
        P.dma("pool", B.S[nm][:, tsl], at[:, j, :])
    yield


def st_peer_a1(self, s, l):
    P = self.P
    I = self.I
    ada = self.S["ada%d" % l]
    with self.stage() as sc:
        identb = self.load_const(sc, "identb")
        identf = self.load_const(sc, "identf")
        shb = self.bcast_row(sc, ada[s, 3072:4096], 1024, "sh2b")
        scb = self.bcast_row(sc, ada[s, 4096:5120], 1024, "sc2b")
        u2T = sc.sb([128, 8, T], BF16, "u2T")
        pst = sc.ps([128, 1024], BF16, "pst")
        self.make_uT(sc, self.S["xres"][s], scb, shb, u2T, identb, pst)
        P.dma("pool", self.S["u2T"], u2T[:])
        wq = sc.sb([128, 8, 2048], BF16, "wq")
        wst = [sc.sb([128, 2048], F32, "wqs%d" % i) for i in range(2)]
        for k in range(8):
            P.dma("sp", wst[k % 2][:], I["peer_wq"][l][k * 128:(k + 1) * 128, :])
            P.copy("pool" if k % 2 else "dve", wq[:, k, :], wst[k % 2][:])
        psK = sc.ps([128, 512], F32, "psK")
        keysT = sc.sb([128, 16, 128], BF16, "keysT")
        kst = [sc.sb([128, 128], F32, "kst%d" % i) for i in range(2)]
        for hp in range(16):
            P.dma("sp", kst[hp % 2][:], I["peer_keys"][l][hp // 2, hp % 2])
            P.tr(psK[:, 0:128], kst[hp % 2][:], identf[:])
            P.copy("act", keysT[:, hp, :], psK[:, 0:128])
        psQ = [sc.ps([128, 512], F32, "psQ%d" % i) for i in range(2)]
        psSc = sc.ps([128, 2048], F32, "psSc")
        qTs = [sc.sb([128, 16, 128], BF16, "qT%d" % i) for i in range(2)]
        scss = [sc.sb([128, 16, 128], F32, "scs%d" % i) for i in range(2)]
        bf = A2Bufs(self, sc)
        trp_ab = psK[:, 0:256]
        trp_g = psK[:, 256:384]
        a2state = {"gen": None, "next": 0}

        def a2_hook(done_tiles):
            if a2state["gen"] is None and a2state["next"] < min(4, done_tiles):
                a2state["gen"] = _a2_tile(self, bf, a2state["next"], trp_ab, trp_g)
                a2state["next"] += 1
            if a2state["gen"] is not None:
                try:
                    next(a2state["gen"])
                except StopIteration:
                    a2state["gen"] = None
        for i in range(16):
            tsl = slice(i * 128, (i + 1) * 128)
            qT, scs = qTs[i % 2], scss[i % 2]
            for hp in range(16):
                a2_hook(i)
                pq = psQ[(hp // 4) % 2]
                for k in range(8):
                    P.mm(pq[:, (hp % 4) * 128:(hp % 4 + 1) * 128], wq[:, k, hp * 128:(hp + 1) * 128], u2T[:, k, tsl],
                         start=(k == 0), stop=(k == 7))
                if hp % 4 == 3:
                    P.copy("act" if (hp // 4) % 2 else "dve", qT[:, hp - 3:hp + 1, :], pq[:, :].rearrange("p (h t) -> p h t", h=4))
            for hp in range(16):
                P.mm(psSc[:, hp * 128:(hp + 1) * 128], qT[:, hp, :], keysT[:, hp, :])
            P.copy("act", scs[:, 0:8, :], psSc[:, 0:1024].rearrange("p (h n) -> p h n", h=8))
            P.copy("dve", scs[:, 8:16, :], psSc[:, 1024:2048].rearrange("p (h n) -> p h n", h=8))
            P.dma("pool", self.S["scs"][tsl, :].rearrange("p (h n) -> p h n", h=16), scs[:])
        while a2state["gen"] is not None or a2state["next"] < 4:
            a2_hook(16)


Builder.st_peer_a1 = st_peer_a1


def build_all(B):
    for l in range(2):
        B.st_pre(l)
        B.st_ada(l)
    for s in range(2):
        for l in range(2):
            B.st_inproj(s, l)
            B.st_attn_a(s, l)
            B.st_attn_c(s, l, with_hconv=True)
            B.st_hyena(s, l, 0)
            B.st_hyena(s, l, 1)
            B.st_merge(s, l)
            B.st_peer_a1(s, l)
            B.st_peer_b(s, l, a2=True)


def kernel(**inputs):
    from concourse.bass_utils import run_bass_kernel_spmd
    inp = {k: np.ascontiguousarray(np.asarray(v, dtype=np.float32)) for k, v in inputs.items()}
    B = Builder()
    build_all(B)
    B.finish()
    ncores = 8
    maps = []
    for c in range(ncores):
        m = {"x": np.ascontiguousarray(inp["x"][2 * c:2 * c + 2]),
             "c": np.ascontiguousarray(inp["c"][2 * c:2 * c + 2])}
        for k in WSHAPES:
            m[k] = inp[k]
        for k, v in B.hc.items():
            m["k_" + k] = v
        maps.append(m)
    res = run_bass_kernel_spmd(B.nc, maps, core_ids=list(range(ncores)))
    out = np.concatenate([np.asarray(r["out"], dtype=np.float32) for r in res.results], axis=0)
    return out
```

```python
import numpy as np
import concourse.bass as bass
import concourse.mybir as mybir
from contextlib import ExitStack

F32 = mybir.dt.float32
BF16 = mybir.dt.bfloat16
I32 = mybir.dt.int32
U32 = mybir.dt.uint32
ALU = mybir.AluOpType
AF = mybir.ActivationFunctionType
AX = mybir.AxisListType

ENGS = ("pe", "act", "dve", "pool", "sp")
EPOCH = 12000
NDMASEM = 24
SELF_SKIP = 1 << 30


def _region(ap):
    t = ap.tensor
    name = t.name
    pairs = ap.ap
    off = int(ap.offset)
    sp = str(ap.space)
    if "SB" in sp or "PSUM" in sp:
        pstride = pairs[0][0]
        if pstride == 0:
            pstride = 1 << 40
        p0 = off // pstride if pstride < (1 << 40) else 0
        f0 = off - p0 * pstride if pstride < (1 << 40) else off
        p1 = p0 + pairs[0][1]
        ext = 0
        for st, cn in pairs[1:]:
            ext += abs(st) * (cn - 1)
        if "PSUM" in sp:
            bank = 2048 // mybir.dt.size(ap.dtype)
            f1 = f0 + ext + 1
            return (name, 0, 128, (f0 // bank) * bank, ((f1 + bank - 1) // bank) * bank)
        return (name, p0, p1, f0, f0 + ext + 1)
    ext = 0
    for st, cn in pairs:
        ext += abs(st) * (cn - 1)
    return (name, 0, 1, off, off + ext + 1)


def _ovl(a, b):
    return a[1] < b[2] and b[1] < a[2] and a[3] < b[4] and b[3] < a[4]


def _covers(a, b):
    return a[1] <= b[1] and a[2] >= b[2] and a[3] <= b[3] and a[4] >= b[4]


class Prog:
    def __init__(self, nc):
        self.nc = nc
        self.es = ExitStack()
        self.streams = {e: [] for e in ENGS}
        self.cnt = {e: 0 for e in ENGS}
        self.cur = {}
        self.allsems = []
        for e in ENGS:
            self.cur[e] = self._newsem("pg_" + e)
        self.dsems = [self._newsem("dma%d" % i) for i in range(NDMASEM)]
        self.dcum = [0] * NDMASEM
        self.dnext = 0
        self.waited = {e: {} for e in ENGS}
        self.hist = {}
        self.floor = {}
        self.semobj = {}
        self.ninstr = 0

    def _newsem(self, name):
        s = self.es.enter_context(self.nc.semaphore(name + "_%d" % len(self.allsems)))
        self.allsems.append(s)
        return s

    def _deps(self, reads, writes, eng=None):
        deps = []
        for ap in reads:
            r = _region(ap)
            psum = "PSUM" in str(ap.space)
            for (reg, isw, ev) in self.hist.get(r[0], ()):
                if _ovl(reg, r) and (isw or (psum and ev[0] != eng)):
                    deps.append(ev)
            deps.extend(self.floor.get(r[0], ()))
        for ap in writes:
            r = _region(ap)
            for (reg, isw, ev) in self.hist.get(r[0], ()):
                if _ovl(reg, r):
                    deps.append(ev)
            deps.extend(self.floor.get(r[0], ()))
        return deps

    def _record(self, reads, writes, ev):
        for ap in writes:
            r = _region(ap)
            h = self.hist.setdefault(r[0], [])
            h[:] = [x for x in h if not _covers(r, x[0])]
            h.append((r, True, ev))
            self._trim(r[0])
        for ap in reads:
            r = _region(ap)
            h = self.hist.setdefault(r[0], [])
            rep = False
            for i, (reg, isw, oev) in enumerate(h):
                if (not isw) and reg == r and oev[0] == ev[0] and oev[3] is False:
                    h[i] = (r, False, ev)
                    rep = True
                    break
            if not rep:
                h.append((r, False, ev))
                self._trim(r[0])

    def _trim(self, name):
        h = self.hist[name]
        if len(h) > 96:
            drop = h[:32]
            del h[:32]
            fl = self.floor.setdefault(name, [])
            fl.extend(x[2] for x in drop)
            best = {}
            for ev in fl:
                k = id(ev[1])
                if k not in best or best[k][2] < ev[2]:
                    best[k] = ev
            self.floor[name] = list(best.values())

    def _waits(self, eng, deps, skip_self_pe=True):
        need = {}
        for ev in deps:
            src, sem, val, isdma = ev
            if src == eng and not isdma and eng == "pe":
                continue
            if src == eng and not isdma and sem is self.cur[eng] and self.cnt[eng] - val >= SELF_SKIP:
                continue
            k = id(sem)
            if self.waited[eng].get(k, 0) >= val:
                continue
            if k not in need or need[k][1] < val:
                need[k] = (sem, val)
        out = []
        for k, (sem, val) in need.items():
            self.waited[eng][k] = val
            out.append((sem, val))
        return out

    def op(self, eng, fn, reads=(), writes=()):
        reads = [a for a in reads if a is not None and not isinstance(a, (int, float))]
        deps = self._deps(reads, writes, eng)
        waits = self._waits(eng, deps)
        if self.cnt[eng] >= EPOCH:
            self.cur[eng] = self._newsem("pg_" + eng)
            self.cnt[eng] = 0
        sem = self.cur[eng]
        self.cnt[eng] += 1
        ev = (eng, sem, self.cnt[eng], False)
        self._record(reads, writes, ev)
        self.streams[eng].append((waits, fn, sem, 1))
        self.ninstr += 1

    def dma(self, q, out, in_, **kw):
        deps = self._deps([in_], [out], q)
        waits = self._waits(q, deps)
        k = self.dnext
        self.dnext = (self.dnext + 1) % NDMASEM
        sem = self.dsems[k]
        self.dcum[k] += 16
        ev = (q, sem, self.dcum[k], True)
        self._record([in_], [out], ev)
        self.streams[q].append((waits, lambda e, o=out, i=in_, kw=kw: e.dma_start(out=o, in_=i, **kw), sem, 16))
        self.ninstr += 1

    def barrier(self):
        evs = []
        for e in ENGS:
            if self.cnt[e] > 0:
                evs.append((e, self.cur[e], self.cnt[e], False))
        for k in range(NDMASEM):
            if self.dcum[k] > 0:
                evs.append(("dma", self.dsems[k], self.dcum[k], True))
        for e in ENGS:
            ws = []
            for (src, sem, val, isdma) in evs:
                if src == e and not isdma:
                    continue
                kk = id(sem)
                if self.waited[e].get(kk, 0) >= val:
                    continue
                self.waited[e][kk] = val
                ws.append((sem, val))
            if ws:
                self.streams[e].append((ws, None, None, 0))
        self.hist = {}
        self.floor = {}

    def emit(self):
        nc = self.nc
        streams = self.streams
        self.streams = {e: [] for e in ENGS}

        def run(engobj, lst):
            for (waits, fn, sem, inc) in lst:
                for (s, v) in waits:
                    engobj.wait_ge(s, v)
                if fn is not None:
                    ins = fn(engobj)
                    ins.then_inc(sem, inc)

        with nc.Block() as block:
            @block.tensor
            def _(e):
                run(e, streams["pe"])

            @block.scalar
            def _(e):
                run(e, streams["act"])

            @block.vector
            def _(e):
                run(e, streams["dve"])

            @block.gpsimd
            def _(e):
                run(e, streams["pool"])

            @block.sync
            def _(e):
                run(e, streams["sp"])

    def mm(self, out, lhsT, rhs, start=True, stop=True):
        self.op("pe", lambda e: e.matmul(out, lhsT, rhs, start=start, stop=stop),
                reads=[lhsT, rhs], writes=[out])

    def tr(self, out, in_, ident):
        self.op("pe", lambda e: e.transpose(out, in_, ident), reads=[in_, ident], writes=[out])

    def act(self, out, in_, func, bias=0.0, scale=1.0, accum_out=None, eng="act"):
        rd = [in_]
        if not isinstance(bias, (int, float)):
            rd.append(bias)
        if not isinstance(scale, (int, float)):
            rd.append(scale)
        wr = [out] + ([accum_out] if accum_out is not None else [])
        kw = {}
        if accum_out is not None:
            kw["accum_out"] = accum_out
        self.op("act", lambda e: e.activation(out, in_, func, bias=bias, scale=scale, **kw),
                reads=rd, writes=wr)

    def tt(self, eng, out, in0, in1, op):
        self.op(eng, lambda e: e.tensor_tensor(out, in0, in1, op), reads=[in0, in1], writes=[out])

    def ts(self, eng, out, in0, s1, s2=None, op0=ALU.mult, op1=None, accum_out=None):
        rd = [in0] + [s for s in (s1, s2) if s is not None and not isinstance(s, (int, float))]
        wr = [out] + ([accum_out] if accum_out is not None else [])
        kw = {}
        if op1 is not None:
            kw["op1"] = op1
        if accum_out is not None:
            kw["accum_out"] = accum_out
        self.op(eng, lambda e: e.tensor_scalar(out, in0, s1, s2, op0, **kw), reads=rd, writes=wr)

    def stt(self, eng, out, in0, scalar, in1, op0, op1):
        rd = [in0, in1] + ([scalar] if not isinstance(scalar, (int, float)) else [])
        self.op(eng, lambda e: e.scalar_tensor_tensor(out, in0, scalar, in1, op0, op1), reads=rd, writes=[out])

    def copy(self, eng, out, in_):
        if eng == "act":
            self.op("act", lambda e: e.copy(out, in_), reads=[in_], writes=[out])
        else:
            self.op(eng, lambda e: e.tensor_copy(out, in_), reads=[in_], writes=[out])

    def memset(self, eng, ap, val):
        self.op(eng, lambda e: e.memset(ap, val), reads=[], writes=[ap])

    def reduce(self, eng, out, in_, op, axis=AX.X):
        self.op(eng, lambda e: e.tensor_reduce(out, in_, axis, op), reads=[in_], writes=[out])

import math
import os
import ml_dtypes
from contextlib import contextmanager

NPBF = ml_dtypes.bfloat16
T = 2048
DM = 1024
ALPHA_C = 4.0 ** 0.25
LN_EPS_C = 1e-5
PI = math.pi


def host_consts():
    c = {}
    L = T
    N = 2 * T
    pos = np.arange(L, dtype=np.float32)
    inv = np.power(np.float32(10000.0), -(np.arange(0, 64, 2, dtype=np.float32) / np.float32(64))).astype(np.float32)
    ang = (pos[:, None] * inv[None, :]).astype(np.float32)
    cs, sn = np.cos(ang).astype(np.float32), np.sin(ang).astype(np.float32)
    p = np.arange(128)
    ropec = cs[:, p % 32].T.copy()
    sgn = np.where((p % 64) < 32, 1.0, -1.0).astype(np.float32)
    ropes = (sn[:, p % 32].T * sgn[:, None]).astype(np.float32)
    c["ropec"] = np.ascontiguousarray(ropec)
    c["ropes"] = np.ascontiguousarray(ropes)
    t = np.arange(L, dtype=np.int64)
    idx = (t[:, None] * t[None, :]) % N
    th = 2.0 * np.pi * idx.astype(np.float64) / N
    C = np.cos(th)
    S = -np.sin(th)
    alt = np.where(t % 2 == 0, 1.0, -1.0)
    Sf = S.copy()
    Sf[:, 0] = alt
    def blk_f(M):
        return np.ascontiguousarray(M.reshape(16, 128, 16, 128).transpose(2, 1, 0, 3)).astype(NPBF)
    c["cf"] = blk_f(C)
    c["sf"] = blk_f(Sf)
    Ci = (2.0 / N) * C
    Ci[0, :] = 1.0 / N
    Si = (2.0 / N) * S
    Si[0, :] = alt / N
    def blk_i(M):
        return np.ascontiguousarray(M.reshape(16, 128, 8, 256).transpose(2, 1, 0, 3)).astype(NPBF)
    c["ci"] = blk_i(Ci)
    c["si"] = blk_i(Si)
    idxf = np.arange(L, dtype=np.float32)
    tt_ = (idxf / np.float32(L - 1)).astype(np.float32)
    w = (np.float32(2.0 * math.pi) * idxf / np.float32(L)).astype(np.float32)
    bands = np.linspace(1e-4, 15, 16, dtype=np.float32)
    ang2 = (w[:, None] * bands[None, :]).astype(np.float32)
    z = np.concatenate([tt_[:, None], np.cos(ang2), -np.sin(ang2)], axis=-1).astype(np.float32)
    c["zembT"] = np.ascontiguousarray(z.T)
    c["negt"] = np.ascontiguousarray((-tt_).reshape(16, 128).T)
    a = np.arange(128)[:, None]
    b = np.arange(128)[None, :]
    mA = np.concatenate([(np.abs(128 * (sg - 1) + a - b) <= 64) for sg in range(3)], axis=1)
    c["mA"] = mA.astype(NPBF)
    mC = np.stack([(b <= a), np.ones((128, 128), bool), (a <= b)], axis=1)
    c["mC"] = np.ascontiguousarray(mC).astype(NPBF)
    sel = np.zeros((65, 64), np.float32)
    sel[64, :] = 1.0
    c["sel"] = sel
    c["ones"] = np.ones((128, 128), np.float32)
    c["identf"] = np.eye(128, dtype=np.float32)
    c["identb"] = np.eye(128).astype(NPBF)
    sw = np.zeros((128, 128), np.float32)
    sw[np.arange(128), np.arange(128) ^ 32] = 1.0
    c["swapm"] = sw.astype(NPBF)
    c["iota16"] = np.tile(np.arange(16, dtype=np.float32)[None, :], (128, 1))
    c["iota128"] = np.tile(np.arange(128)[None, :], (128, 1)).astype(NPBF)
    return c


CONST_DT = {"ropec": F32, "ropes": F32, "cf": BF16, "sf": BF16, "ci": BF16, "si": BF16, "zembT": F32,
            "negt": F32, "mA": BF16, "mC": BF16, "sel": F32, "ones": F32, "identf": F32, "identb": BF16,
            "iota16": F32, "iota128": BF16, "swapm": BF16}

WSHAPES = {
    "w_ada": [2, 1024, 6144], "b_ada": [2, 6144], "w_in": [2, 1024, 7680], "conv_w": [2, 3, 1536],
    "conv_b": [2, 1536], "hy_w1": [2, 33, 64], "hy_b1": [2, 64], "hy_w2": [2, 64, 64], "hy_b2": [2, 64],
    "hy_w3": [2, 64, 2048], "hy_freq": [2, 2, 64], "hy_log_decay": [2, 2048], "hy_bias": [2, 2, 512],
    "attn_sink": [2, 8], "w_branch_a": [2, 256, 1024], "w_branch_b": [2, 512, 1024],
    "w_branch_c": [2, 512, 1024], "w_out": [2, 1024, 1024], "ln_g": [2, 2, 1024], "ln_b": [2, 2, 1024],
    "peer_wq": [2, 1024, 2048], "peer_keys": [2, 8, 2, 128, 128], "peer_u": [2, 16384, 1024],
    "peer_v": [2, 16384, 1024],
}


def cap(ap, offset, pairs):
    return bass.AP(ap.tensor, offset, [list(p) for p in pairs])


class Scope:
    def __init__(self, B):
        self.B = B
        self.es = ExitStack()

    def sb(self, shape, dt, name="t"):
        self.B.uid += 1
        t = self.es.enter_context(self.B.nc.sbuf_tensor("%s_%d" % (name, self.B.uid), list(shape), dt))
        return t

    def ps(self, shape, dt=F32, name="ps"):
        self.B.uid += 1
        return self.es.enter_context(self.B.nc.psum_tensor("%s_%d" % (name, self.B.uid), list(shape), dt))


class Builder:
    def __init__(self, dbg=()):
        self.nc = nc = bass.Bass("TRN2", target_bir_lowering=False)
        self.P = Prog(nc)
        self.uid = 0
        self.dbg = set(dbg)
        self.I = {}
        self.I["x"] = nc.dram_tensor("x", [2, T, DM], F32, kind="ExternalInput").ap()
        self.I["c"] = nc.dram_tensor("c", [2, DM], F32, kind="ExternalInput").ap()
        for k, shp in WSHAPES.items():
            self.I[k] = nc.dram_tensor(k, shp, F32, kind="ExternalInput").ap()
        self.C = {}
        hc = host_consts()
        self.hc = hc
        for k, v in hc.items():
            self.C[k] = nc.dram_tensor("k_" + k, list(v.shape), CONST_DT[k], kind="ExternalInput").ap()
        self.out = nc.dram_tensor("out", [2, T, DM], F32, kind="ExternalOutput").ap()
        S = self.S = {}

        def scr(name, shape, dt):
            kind = "ExternalOutput" if name in self.dbg else "Internal"
            S[name] = nc.dram_tensor("s_" + name, list(shape), dt, kind=kind).ap()
        for l in range(2):
            scr("ada%d" % l, [2, 6144], F32)
            scr("hspec%d" % l, [3, 2048, 1024], BF16)
            scr("ut%d" % l, [128, 128, 8, 128], BF16)
            scr("vb%d" % l, [128, 128, 1024], BF16)
        scr("xres", [2, T, DM], F32)
        scr("qTa", [768, T], BF16)
        scr("kTa", [768, T], BF16)
        scr("va", [3, T, 256], BF16)
        scr("hyT", [1536, T], F32)
        scr("hcT", [1536, T], F32)
        scr("z1T", [512, T], F32)
        scr("qTc", [512, T], BF16)
        scr("kTc", [128, T], BF16)
        scr("vc", [T, 128], BF16)
        scr("gateT", [3072, T], BF16)
        scr("yaT", [256, T], BF16)
        scr("ybT", [512, T], BF16)
        scr("ycT", [512, T], BF16)
        scr("u2T", [128, 8, T], BF16)
        scr("scs", [T, 2048], F32)
        scr("pa", [128, T], BF16)
        scr("pb", [128, T], BF16)
        scr("pg", [128, T], BF16)

    @contextmanager
    def stage(self):
        sc = Scope(self)
        try:
            yield sc
        finally:
            self.P.barrier()
            self.P.emit()
            sc.es.close()

    def finish(self):
        self.P.es.close()

    def bcast_row(self, sc, src_ap_1d, n, name="bc"):
        t = sc.sb([128, n], F32, name)
        src = cap(src_ap_1d, int(src_ap_1d.offset), [[0, 128], [1, n]])
        self.P.dma("sp", t[:], src)
        return t

    def load_const(self, sc, name):
        v = self.hc[name]
        t = sc.sb(list(v.shape), CONST_DT[name], "c_" + name)
        self.P.dma("sp", t[:], self.C[name])
        return t

    def st_ada(self, l):
        P = self.P
        with self.stage() as sc:
            cT = sc.sb([128, 8, 2], F32, "cT")
            for b in range(2):
                P.dma("sp", cT[:, :, b], self.I["c"][b].rearrange("(k p) -> p k", p=128),
                      allow_slow_non_contiguous=True)
            P.act(cT[:], cT[:], AF.Silu)
            ada = sc.sb([2, 6144], F32, "ada")
            bb = sc.sb([2, 6144], F32, "bb")
            ba = self.I["b_ada"][l]
            P.dma("sp", bb[:], cap(ba, int(ba.offset), [[0, 2], [1, 6144]]))
            ws = [sc.sb([128, 8, 512], F32, "w%d" % i) for i in range(2)]
            pss = [sc.ps([128, 512], F32) for i in range(2)]
            for cb in range(12):
                w = ws[cb % 2]
                P.dma("sp", w[:], self.I["w_ada"][l][:, cb * 512:(cb + 1) * 512].rearrange("(k p) n -> p k n", p=128))
                ps = pss[cb % 2]
                for k in range(8):
                    P.mm(ps[0:2, :], cT[:, k, :], w[:, k, :], start=(k == 0), stop=(k == 7))
                P.tt("dve", ada[:, cb * 512:(cb + 1) * 512], ps[0:2, :], bb[:, cb * 512:(cb + 1) * 512], ALU.add)
            for o in (1024, 4096):
                P.ts("dve", ada[:, o:o + 1024], ada[:, o:o + 1024], 1.0, None, op0=ALU.add)
            P.dma("sp", self.S["ada%d" % l], ada[:])

    def make_uT(self, sc, xsrc, scb, shb, uT, identb, pst, ntiles=16, tok0=0, xkeep=None):
        P = self.P
        xts = [sc.sb([128, DM], F32, "xt%d" % i) for i in range(2)]
        ubs = [sc.sb([128, DM], BF16, "ub%d" % i) for i in range(2)]
        uf = sc.sb([128, DM], F32, "uf")
        for i in range(ntiles):
            xt = xts[i % 2] if xkeep is None else xkeep[i]
            ub = ubs[i % 2]
            P.dma("sp", xt[:], xsrc[tok0 + i * 128: tok0 + (i + 1) * 128, :])
            P.tt("dve", uf[:], xt[:], scb[:], ALU.mult)
            P.tt("pool", ub[:], uf[:], shb[:], ALU.add)
            for k in range(8):
                P.tr(pst[:, k * 128:(k + 1) * 128], ub[:, k * 128:(k + 1) * 128], identb[:])
            P.copy("act", uT[:, :, i * 128:(i + 1) * 128], pst[:].rearrange("p (k t) -> p k t", k=8))

    def st_inproj(self, s, l):
        P = self.P
        xsrc = self.I["x"][s] if l == 0 else self.S["xres"][s]
        ada = self.S["ada%d" % l]
        with self.stage() as sc:
            identb = self.load_const(sc, "identb")
            ropec = self.load_const(sc, "ropec")
            ropes = self.load_const(sc, "ropes")
            shb = self.bcast_row(sc, ada[s, 0:1024], 1024, "shb")
            scb = self.bcast_row(sc, ada[s, 1024:2048], 1024, "scb")
            uT = sc.sb([128, 8, T], BF16, "uT")
            pst = sc.ps([128, 1024], BF16, "pst")
            self.make_uT(sc, xsrc, scb, shb, uT, identb, pst)
            wfs = [sc.sb([128, 8, 256], F32, "wf%d" % i) for i in range(2)]
            wbs = [sc.sb([128, 8, 256], BF16, "wb%d" % i) for i in range(2)]
            pss = [sc.ps([128, 512], F32, "pf%d" % i) for i in range(4)]
            stg = [sc.sb([128, T], BF16, "stg%d" % i) for i in range(3)]
            stf = [sc.sb([128, T], F32, "stf%d" % i) for i in range(2)]
            tbs = [sc.sb([128, 512], BF16, "tbs%d" % i) for i in range(2)]
            ps2 = [sc.ps([128, 512], F32, "ps2_%d" % i) for i in range(2)]
            swapm = self.load_const(sc, "swapm")
            tA = [sc.sb([128, 512], F32, "tA%d" % i) for i in range(2)]
            tB = [sc.sb([128, 512], F32, "tB%d" % i) for i in range(2)]
            vst = [sc.sb([128, 16, 256], BF16, "vst%d" % i) for i in range(2)]
            cnt = {"ps": 0, "stg": 0, "stf": 0, "tmp": 0, "v": 0}

            def tokview(ap2, tt, D):
                if D == 1:
                    return ap2[:, tt * 512:(tt + 1) * 512]
                if D == 4:
                    return ap2.rearrange("p (j r) -> p r j", r=4)[:, tt, :]
                return ap2.rearrange("p (j r) -> p r j", r=16)[:, 4 * tt:4 * tt + 4, :]

            def shp(ap2, D):
                if D == 16:
                    return ap2.rearrange("p (r j) -> p r j", r=4)
                return ap2

            def tok128(ap2, i, D):
                if D == 1:
                    return ap2[:, i * 128:(i + 1) * 128]
                if D == 4:
                    return ap2.rearrange("p (j r) -> p r j", r=4)[:, i // 4, (i % 4) * 128:(i % 4 + 1) * 128]
                return ap2.rearrange("p (j r) -> p r j", r=16)[:, i, :]

            def fm_chunk(wb, cc, kind, D, dest):
                if kind == "hy":
                    st = stf[cnt["stf"] % 2]
                    cnt["stf"] += 1
                else:
                    st = stg[cnt["stg"] % 3]
                    cnt["stg"] += 1
                for tt in range(4):
                    ps = pss[cnt["ps"] % 4]
                    cnt["ps"] += 1
                    for k in range(8):
                        P.mm(ps[:, :], wb[:, k, cc * 128:(cc + 1) * 128], uT[:, k, tt * 512:(tt + 1) * 512],
                             start=(k == 0), stop=(k == 7))
                    o = st[:, tt * 512:(tt + 1) * 512]
                    if kind == "hy":
                        P.copy("act", o, ps[:, :])
                    elif kind == "gate":
                        P.act(o, ps[:, :], AF.Sigmoid)
                    else:
                        j = cnt["tmp"] % 2
                        cnt["tmp"] += 1
                        tb_, a_, b_, p2 = tbs[j], tA[j], tB[j], ps2[j]
                        P.copy("act", tb_[:], ps[:, :])
                        P.mm(p2[:, :], swapm[:], tb_[:])
                        ts_ = slice(tt * 512, (tt + 1) * 512)
                        P.tt("dve", a_[:], ps[:, :], ropec[:, ts_], ALU.mult)
                        P.tt("dve", b_[:], p2[:, :], ropes[:, ts_], ALU.mult)
                        if D == 1:
                            P.tt("pool", o, a_[:], b_[:], ALU.subtract)
                        else:
                            n_ = 512 // D
                            ov = st[:, :].rearrange("p (r j) -> p j r", r=D)[:, tt * n_:(tt + 1) * n_, :]
                            P.tt("pool", ov, a_[:].rearrange("p (j r) -> p j r", r=D),
                                 b_[:].rearrange("p (j r) -> p j r", r=D), ALU.subtract)
                P.dma("pool" if kind == "rope" else "act", dest, st[:])

            def tm_block(wb, c0, ncols, D, dest):
                st = vst[cnt["v"] % 2]
                cnt["v"] += 1
                for i in range(16):
                    ps = pss[cnt["ps"] % 4]
                    cnt["ps"] += 1
                    for k in range(8):
                        P.mm(ps[:, 0:ncols], tok128(uT[:, k, :], i, D), wb[:, k, c0:c0 + ncols],
                             start=(k == 0), stop=(k == 7))
                    P.copy("act", st[:, i, 0:ncols], ps[:, 0:ncols])
                P.dma("act", dest.rearrange("(i p) c -> p i c", p=128), st[:, :, 0:ncols])

            DG = (1, 4, 16)
            for blk in range(30):
                wf, wb = wfs[blk % 2], wbs[blk % 2]
                col = blk * 256
                P.dma("sp", wf[:], self.I["w_in"][l][:, col:col + 256].rearrange("(k p) n -> p k n", p=128))
                P.copy("pool" if blk % 2 else "dve", wb[:], wf[:])
                if col < 1536:
                    nm = "qTa" if col < 768 else "kTa"
                    base = 0 if col < 768 else 768
                    for cc in range(2):
                        j = (col - base) // 128 + cc
                        fm_chunk(wb, cc, "rope", DG[j // 2], self.S[nm][j * 128:(j + 1) * 128, :])
                elif col < 2304:
                    g = (col - 1536) // 256
                    tm_block(wb, 0, 256, DG[g], self.S["va"][g])
                elif col < 3840:
                    for cc in range(2):
                        j = (col - 2304) // 128 + cc
                        fm_chunk(wb, cc, "hy", 1, self.S["hyT"][j * 128:(j + 1) * 128, :])
                elif col < 4352:
                    for cc in range(2):
                        j = (col - 3840) // 128 + cc
                        fm_chunk(wb, cc, "rope", 1, self.S["qTc"][j * 128:(j + 1) * 128, :])
                elif col < 4608:
                    fm_chunk(wb, 0, "rope", 1, self.S["kTc"][:, :])
                    tm_block(wb, 128, 128, 1, self.S["vc"])
                else:
                    for cc in range(2):
                        j = (col - 4608) // 128 + cc
                        fm_chunk(wb, cc, "gate", 1, self.S["gateT"][j * 128:(j + 1) * 128, :])


def _attn_finalize(B, sc, numer, nheads, sel, psB, dest):
    P = B.P
    recs = [sc.sb([64, 512], F32, "rec%d" % i) for i in range(2)]
    ysts = [sc.sb([64, T], BF16, "yst%d" % i) for i in range(2)]
    n = 0
    for h in range(nheads):
        yst = ysts[h % 2]
        for tt in range(4):
            ps = psB[n % 2]
            rec = recs[n % 2]
            n += 1
            P.mm(ps[0:64, :], sel[0:65, 0:64], numer[0:65, h, tt * 512:(tt + 1) * 512])
            P.op("dve", lambda e, o=rec[:], i=ps[0:64, :]: e.reciprocal(o, i), reads=[ps[0:64, :]], writes=[rec[:]])
            P.tt("pool", yst[:, tt * 512:(tt + 1) * 512], numer[0:64, h, tt * 512:(tt + 1) * 512], rec[:], ALU.mult)
        P.dma("pool", dest[h * 64:(h + 1) * 64, :], yst[:])


def st_attn_a(self, s, l):
    P = self.P
    with self.stage() as sc:
        mA = self.load_const(sc, "mA")
        sel = self.load_const(sc, "sel")
        numer = sc.sb([65, 4, T], F32, "numer")
        q = sc.sb([64, 4, T], BF16, "q")
        k = sc.sb([64, 4, T], BF16, "k")
        va = sc.sb([128, 16, 4, 65], BF16, "va")
        psS = [sc.ps([128, 512], F32, "psS%d" % i) for i in range(3)]
        psO = [sc.ps([128, 512], F32, "psO%d" % i) for i in range(2)]
        psB = [sc.ps([128, 512], F32, "psB%d" % i) for i in range(2)]
        pT = [sc.sb([128, 384], BF16, "pT%d" % i) for i in range(2)]
        pT2 = [sc.sb([128, 384], BF16, "pTm%d" % i) for i in range(3)]
        P.memset("pool", va[:], 1.0)
        n = 0
        for g, D in enumerate((1, 4, 16)):
            Ls = T // D
            nb = Ls // 128
            for h in range(4):
                hh = g * 4 + h
                P.dma("sp", q[:, h, :], self.S["qTa"][hh * 64:(hh + 1) * 64, :])
                P.dma("sp", k[:, h, :], self.S["kTa"][hh * 64:(hh + 1) * 64, :])
                P.dma("sp", va[:, :, h, 0:64],
                      self.S["va"][g][:, h * 64:(h + 1) * 64].rearrange("(i p) d -> p i d", p=128))
            jobs = [(h, rho, i) for h in range(4) for rho in range(D) for i in range(nb)]

            def s_phase(n, job):
                h, rho, i = job
                base = rho * Ls
                js = [j for j in (i - 1, i, i + 1) if 0 <= j < nb]
                ps = psS[n % 3]
                p1 = pT[n % 2]
                p2 = pT2[n % 3]
                q0 = base + i * 128
                for j in js:
                    sg = j - i + 1
                    P.mm(ps[:, sg * 128:(sg + 1) * 128], k[0:64, h, base + j * 128: base + (j + 1) * 128],
                         q[0:64, h, q0:q0 + 128])
                c0 = (js[0] - i + 1) * 128
                c1 = (js[-1] - i + 2) * 128
                P.act(p1[:, c0:c1], ps[:, c0:c1], AF.Exp, scale=0.125)
                P.tt("dve", p2[:, c0:c1], p1[:, c0:c1], mA[:, c0:c1], ALU.mult)

            def pv_phase(n, job):
                h, rho, i = job
                js = [j for j in (i - 1, i, i + 1) if 0 <= j < nb]
                po = psO[n % 2]
                p2 = pT2[n % 3]
                for jj, j in enumerate(js):
                    sg = j - i + 1
                    P.mm(po[0:65, 0:128], va[:, rho * nb + j, h, :], p2[:, sg * 128:(sg + 1) * 128],
                         start=(jj == 0), stop=(jj == len(js) - 1))
                if D == 1:
                    nv = numer[0:65, h, i * 128:(i + 1) * 128]
                else:
                    nv = numer[0:65, h, :].rearrange("p (j r) -> p r j", r=D)[:, rho, i * 128:(i + 1) * 128]
                if g == 0:
                    P.copy("act", nv, po[0:65, 0:128])
                else:
                    P.tt("dve", nv, po[0:65, 0:128], nv, ALU.add)
            for n_, job in enumerate(jobs):
                s_phase(n + n_, job)
                if n_ > 0:
                    pv_phase(n + n_ - 1, jobs[n_ - 1])
            pv_phase(n + len(jobs) - 1, jobs[-1])
            n += len(jobs)
        _attn_finalize(self, sc, numer, 4, sel, psB, self.S["yaT"])


def st_attn_c(self, s, l, with_hconv=False):
    P = self.P
    with self.stage() as sc:
        hgen = _hconv_body(self, sc, l) if with_hconv else None
        mC = self.load_const(sc, "mC")
        sel = self.load_const(sc, "sel")
        numer = sc.sb([65, 8, T], F32, "numerc")
        q = sc.sb([64, 8, T], BF16, "qc")
        k = sc.sb([64, 2, T], BF16, "kc")
        va = sc.sb([128, 16, 2, 65], BF16, "vca")
        sk = sc.sb([65, 8], F32, "sk")
        psS = [sc.ps([128, 512], F32, "psS%d" % i) for i in range(3)]
        psO = [sc.ps([128, 512], F32, "psO%d" % i) for i in range(2)]
        psB = [sc.ps([128, 512], F32, "psB%d" % i) for i in range(2)]
        pT = [sc.sb([128, 3, 512], BF16, "pTc%d" % i) for i in range(2)]
        P.memset("pool", va[:], 1.0)
        for h in range(8):
            P.dma("sp", q[:, h, :], self.S["qTc"][h * 64:(h + 1) * 64, :])
        for h in range(2):
            P.dma("sp", k[:, h, :], self.S["kTc"][h * 64:(h + 1) * 64, :])
            P.dma("sp", va[:, :, h, 0:64], self.S["vc"][:, h * 64:(h + 1) * 64].rearrange("(i p) d -> p i d", p=128))
        asink = self.I["attn_sink"][l]
        P.dma("sp", sk[64:65, :], cap(asink, int(asink.offset), [[0, 1], [1, 8]]))
        P.act(sk[64:65, :], sk[64:65, :], AF.Exp)
        n = 0
        mcb = mC[:]
        pst = mcb.ap[0][0]
        jobs = [(kv, i) for kv in range(2) for i in range(16)]

        def s_phase(n, job):
            kv, i = job
            js = [j for j in (i - 1, i, i + 1) if 0 <= j < 16]
            p1 = pT[n % 2]
            for j in js:
                sg = j - i + 1
                ps = psS[sg]
                P.mm(ps[:, :].rearrange("p (h t) -> p h t", h=4), k[0:64, kv, j * 128:(j + 1) * 128],
                     q[0:64, 4 * kv:4 * kv + 4, i * 128:(i + 1) * 128])
                P.act(p1[:, sg, :], ps[:, :], AF.Exp, scale=0.125)
                if sg != 1:
                    mv = cap(mcb, int(mcb.offset) + sg * 128, [[pst, 128], [0, 4], [1, 128]])
                    pv = p1[:, sg, :].rearrange("p (h t) -> p h t", h=4)
                    P.tt("pool" if sg == 0 else "dve", pv, pv, mv, ALU.mult)

        def pv_phase(n, job):
            kv, i = job
            js = [j for j in (i - 1, i, i + 1) if 0 <= j < 16]
            p1 = pT[n % 2]
            po = psO[n % 2]
            for jj, j in enumerate(js):
                sg = j - i + 1
                P.mm(po[0:65, :], va[:, j, kv, :], p1[:, sg, :], start=(jj == 0), stop=(jj == len(js) - 1))
            P.copy("act", numer[0:65, 4 * kv:4 * kv + 4, i * 128:(i + 1) * 128],
                   po[0:65, :].rearrange("p (h t) -> p h t", h=4))
        for n_, job in enumerate(jobs):
            s_phase(n_, job)
            if n_ > 0:
                pv_phase(n_ - 1, jobs[n_ - 1])
            if hgen is not None:
                try:
                    next(hgen)
                except StopIteration:
                    hgen = None
        pv_phase(len(jobs) - 1, jobs[-1])
        if hgen is not None:
            for _ in hgen:
                pass
        for h in range(8):
            P.ts("dve", numer[64:65, h, :], numer[64:65, h, :], sk[64:65, h:h + 1], None, op0=ALU.add)
        _attn_finalize(self, sc, numer, 8, sel, psB, self.S["ycT"])


Builder.st_attn_a = st_attn_a
Builder.st_attn_c = st_attn_c


def _filter_body(self, sc, l):
    P = self.P
    I = self.I
    if True:
        zembT = self.load_const(sc, "zembT")
        negt = self.load_const(sc, "negt")
        ones = self.load_const(sc, "ones")
        w1 = sc.sb([33, 64], F32, "w1")
        w2 = sc.sb([64, 64], F32, "w2")
        w3 = sc.sb([64, 2048], F32, "w3")
        P.dma("sp", w1[:], I["hy_w1"][l])
        P.dma("sp", w2[:], I["hy_w2"][l])
        P.dma("sp", w3[:], I["hy_w3"][l])
        prm = sc.sb([64, 4], F32, "prm")
        for j, ap1 in enumerate((I["hy_b1"][l], I["hy_b2"][l], I["hy_freq"][l][0], I["hy_freq"][l][1])):
            P.dma("sp", prm[:, j:j + 1], cap(ap1, int(ap1.offset), [[1, 64], [1, 1]]))
        fb = sc.sb([64, 2], F32, "fb")
        P.tt("dve", fb[:, 0:1], prm[:, 0:1], prm[:, 2:3], ALU.mult)
        P.tt("dve", fb[:, 1:2], prm[:, 1:2], prm[:, 3:4], ALU.mult)
        psF = sc.ps([128, 2048], F32, "psF")
        psN = sc.ps([128, 1024], F32, "psN")
        h1T = sc.sb([64, T], F32, "h1T")
        h2T = sc.sb([64, T], F32, "h2T")
        arg = sc.sb([64, 512], F32, "arg")
        ki = sc.sb([64, 512], I32, "ki")
        kf = sc.sb([64, 512], F32, "kf")
        mk = sc.sb([64, 512], F32, "mk")

        def sin_layer(dst, lhsT, src, kk, fcol, fbcol):
            for tt in range(4):
                ps = psF[0:64, tt * 512:(tt + 1) * 512]
                P.mm(ps, lhsT, src[0:kk, tt * 512:(tt + 1) * 512])
                P.ts("dve", arg[:], ps, prm[:, fcol:fcol + 1], fb[:, fbcol:fbcol + 1], op0=ALU.mult, op1=ALU.add)
                P.ts("dve", arg[:], arg[:], 1.0 / (2.0 * PI), None, op0=ALU.mult)
                P.copy("dve", ki[:], arg[:])
                P.copy("dve", kf[:], ki[:])
                P.tt("dve", arg[:], arg[:], kf[:], ALU.subtract)
                P.ts("dve", mk[:], arg[:], 0.5, None, op0=ALU.is_gt)
                P.tt("dve", arg[:], arg[:], mk[:], ALU.subtract)
                P.ts("dve", mk[:], arg[:], -0.5, None, op0=ALU.is_lt)
                P.tt("dve", arg[:], arg[:], mk[:], ALU.add)
                P.act(dst[:, tt * 512:(tt + 1) * 512], arg[:], AF.Sin, scale=6.28318)
                yield
        yield from sin_layer(h1T, w1[0:33, :], zembT, 33, 2, 0)
        yield from sin_layer(h2T, w2[0:64, :], h1T, 64, 3, 1)
        ld = self.bcast_row(sc, I["hy_log_decay"][l], 2048, "ld")
        P.act(ld[:], ld[:], AF.Exp)
        gsum = sc.sb([128, 16, 1024], BF16, "gsum")
        gdiff = sc.sb([128, 16, 1024], BF16, "gdiff")
        dec = sc.sb([128, 2048], F32, "dec")
        filt = sc.sb([128, 2048], F32, "filt")
        sq = dec
        for mc in range(16):
            for cb in range(4):
                P.mm(psF[:, cb * 512:(cb + 1) * 512], h2T[0:64, mc * 128:(mc + 1) * 128], w3[0:64, cb * 512:(cb + 1) * 512])
            P.act(dec[:], ld[:], AF.Exp, scale=negt[:, mc:mc + 1])
            P.tt("dve", filt[:], psF[:, :], dec[:], ALU.mult)
            if mc == 0:
                P.memset("dve", filt[0:1, 1024:2048], 0.0)
            P.act(sq[:], filt[:], AF.Square)
            for half in range(2):
                for d in range(2):
                    c0 = d * 1024 + half * 512
                    P.mm(psN[:, half * 512:(half + 1) * 512], ones[:, :], sq[:, c0:c0 + 512],
                         start=(mc == 0 and d == 0), stop=(mc == 15 and d == 1))
            P.tt("pool", gsum[:, mc, :], filt[:, 0:1024], filt[:, 1024:2048], ALU.add)
            P.tt("pool", gdiff[:, mc, :], filt[:, 0:1024], filt[:, 1024:2048], ALU.subtract)
            yield
        rs = sc.sb([128, 1024], F32, "rs")
        P.ts("dve", rs[:], psN[:, :], 1e-12, None, op0=ALU.add)
        P.act(rs[:], rs[:], AF.Sqrt)
        P.op("dve", lambda e: e.reciprocal(rs[:], rs[:]), reads=[rs[:]], writes=[rs[:]])
        cfs = [sc.sb([128, 16, 128], BF16, "cf%d" % i) for i in range(2)]
        sfs = [sc.sb([128, 16, 128], BF16, "sf%d" % i) for i in range(2)]
        ho = [[sc.sb([128, 512], BF16, "ho%d_%d" % (i, j)) for j in range(3)] for i in range(2)]
        hs = self.S["hspec%d" % l]
        n = 0
        for fc in range(16):
            cf, sf = cfs[fc % 2], sfs[fc % 2]
            P.dma("sp", cf[:], self.C["cf"][fc])
            P.dma("sp", sf[:], self.C["sf"][fc])
            for half in range(2):
                cs = slice(half * 512, (half + 1) * 512)
                pre, pim, px = psF[:, 0:512], psF[:, 512:1024], psF[:, 1024:1536]
                for mc in range(16):
                    P.mm(pre, cf[:, mc, :], gsum[:, mc, cs], start=(mc == 0), stop=(mc == 15))
                for mc in range(16):
                    P.mm(pim, sf[:, mc, :], gdiff[:, mc, cs], start=(mc == 0), stop=(mc == 15))
                if fc == 0:
                    for mc in range(16):
                        P.mm(px, sf[:, mc, :], gsum[:, mc, cs], start=(mc == 0), stop=(mc == 15))
                hre, him, hrb = ho[n % 2]
                n += 1
                P.tt("dve", hre[:], pre, rs[:, cs], ALU.mult)
                P.tt("dve", him[:], pim, rs[:, cs], ALU.mult)
                P.copy("pool", hrb[:], hre[:])
                if fc == 0:
                    P.memset("pool", him[0:1, :], 0.0)
                    P.tt("dve", hrb[0:1, :], px[0:1, :], rs[0:1, cs], ALU.mult)
                for j, t_ in enumerate((hre, him, hrb)):
                    P.dma("pool", hs[j, fc * 128:(fc + 1) * 128, cs], t_[:])
                yield


def _hconv_body(self, sc, l):
    P = self.P
    cw = sc.sb([128, 12, 3], F32, "cw")
    cb = sc.sb([128, 12], F32, "cb")
    for i in range(3):
        P.dma("sp", cw[:, :, i], self.I["conv_w"][l][i].rearrange("(k p) -> p k", p=128), allow_slow_non_contiguous=True)
    P.dma("sp", cb[:], self.I["conv_b"][l].rearrange("(k p) -> p k", p=128), allow_slow_non_contiguous=True)
    xs = [sc.sb([128, T], F32, "hx%d" % i) for i in range(2)]
    os_ = [sc.sb([128, T], F32, "ho%d" % i) for i in range(2)]
    for cc in range(12):
        x, o = xs[cc % 2], os_[cc % 2]
        P.dma("sp", x[:], self.S["hyT"][cc * 128:(cc + 1) * 128, :])
        yield
        P.act(o[:], x[:], AF.Identity, bias=cb[:, cc:cc + 1], scale=cw[:, cc, 1:2])
        P.stt("dve", o[:, 1:T], x[:, 0:T - 1], cw[:, cc, 0:1], o[:, 1:T], ALU.mult, ALU.add)
        P.stt("dve", o[:, 0:T - 1], x[:, 1:T], cw[:, cc, 2:3], o[:, 0:T - 1], ALU.mult, ALU.add)
        P.dma("pool", self.S["hcT"][cc * 128:(cc + 1) * 128, :], o[:])
        yield


def st_hconv(self, s, l):
    with self.stage() as sc:
        for _ in _hconv_body(self, sc, l):
            pass


def st_hyena(self, s, l, o):
    P = self.P
    zsrc = self.S["hcT"][0:512, :] if o == 0 else self.S["z1T"]
    gsrc = self.S["hcT"][(o + 1) * 512:(o + 2) * 512, :]
    hs = self.S["hspec%d" % l]
    with self.stage() as sc:
        identb = self.load_const(sc, "identb")
        hb = sc.sb([128, 4], F32, "hb")
        P.dma("sp", hb[:], self.I["hy_bias"][l][o].rearrange("(k p) -> p k", p=128), allow_slow_non_contiguous=True)
        zb = sc.sb([128, 4, T], BF16, "zb")
        zfs = [sc.sb([128, T], F32, "zf%d" % i) for i in range(2)]
        for cc in range(4):
            zf = zfs[cc % 2]
            P.dma("sp", zf[:], zsrc[cc * 128:(cc + 1) * 128, :])
            P.copy("pool" if cc % 2 else "dve", zb[:, cc, :], zf[:])
        zTM = sc.sb([128, 16, 512], BF16, "zTM")
        psT = [sc.ps([128, 1024], BF16, "psT%d" % i) for i in range(2)]
        for tc in range(16):
            pt = psT[tc % 2]
            for cc in range(4):
                P.tr(pt[:, cc * 128:(cc + 1) * 128], zb[:, cc, tc * 128:(tc + 1) * 128], identb[:])
            P.copy("act", zTM[:, tc, :], pt[:, 0:512])
        Yre = sc.sb([128, 16, 512], BF16, "Yre")
        Yim = sc.sb([128, 16, 512], BF16, "Yim")
        cfs = [sc.sb([128, 16, 128], BF16, "cf%d" % i) for i in range(2)]
        sfs = [sc.sb([128, 16, 128], BF16, "sf%d" % i) for i in range(2)]
        hts = [[sc.sb([128, 512], BF16, "h%d_%d" % (i, j)) for j in range(3)] for i in range(2)]
        tmps = [[sc.sb([128, 512], F32, "tm%d_%d" % (i, j)) for j in range(4)] for i in range(2)]
        psR = [sc.ps([128, 512], F32, "psR%d" % i) for i in range(2)]
        psI = [sc.ps([128, 512], F32, "psI%d" % i) for i in range(2)]
        cs = slice(o * 512, (o + 1) * 512)
        for fc in range(16):
            cf, sf = cfs[fc % 2], sfs[fc % 2]
            P.dma("sp", cf[:], self.C["cf"][fc])
            P.dma("sp", sf[:], self.C["sf"][fc])
            hre, him, hrb = hts[fc % 2]
            for j, t_ in enumerate((hre, him, hrb)):
                P.dma("sp", t_[:], hs[j, fc * 128:(fc + 1) * 128, cs])
            pr, pi = psR[fc % 2], psI[fc % 2]
            for tc in range(16):
                P.mm(pr[:, :], cf[:, tc, :], zTM[:, tc, :], start=(tc == 0), stop=(tc == 15))
            for tc in range(16):
                P.mm(pi[:, :], sf[:, tc, :], zTM[:, tc, :], start=(tc == 0), stop=(tc == 15))
            t1, t2, t3, t4 = tmps[fc % 2]
            P.tt("dve", t1[:], pr[:, :], hre[:], ALU.mult)
            P.tt("dve", t2[:], pi[:, :], him[:], ALU.mult)
            P.tt("dve", t3[:], pr[:, :], him[:], ALU.mult)
            P.tt("dve", t4[:], pi[:, :], hrb[:], ALU.mult)
            P.tt("pool", Yre[:, fc, :], t1[:], t2[:], ALU.subtract)
            P.tt("pool", Yim[:, fc, :], t3[:], t4[:], ALU.add)
        cis = [sc.sb([128, 16, 256], BF16, "ci%d" % i) for i in range(2)]
        sis = [sc.sb([128, 16, 256], BF16, "si%d" % i) for i in range(2)]
        psY = [sc.ps([128, 512], F32, "psY%d" % i) for i in range(2)]
        zps = [sc.sb([128, 256], F32, "zp%d" % i) for i in range(2)]
        gps = [sc.sb([128, 256], F32, "gp%d" % i) for i in range(2)]
        tms = [sc.sb([128, 256], F32, "tq%d" % i) for i in range(2)]
        odt = F32 if o == 0 else BF16
        ors = [sc.sb([128, 256], odt, "or%d" % i) for i in range(2)]
        dest = self.S["z1T"] if o == 0 else self.S["ybT"]
        n = 0
        for tt in range(8):
            ci, si = cis[tt % 2], sis[tt % 2]
            P.dma("sp", ci[:], self.C["ci"][tt])
            P.dma("sp", si[:], self.C["si"][tt])
            ts_ = slice(tt * 256, (tt + 1) * 256)
            for cc in range(4):
                py = psY[n % 2]
                zp, gp, tm, orr = zps[n % 2], gps[n % 2], tms[n % 2], ors[n % 2]
                n += 1
                P.dma("sp", zp[:], zsrc[cc * 128:(cc + 1) * 128, ts_])
                P.dma("sp", gp[:], gsrc[cc * 128:(cc + 1) * 128, ts_])
                for fc in range(16):
                    P.mm(py[:, 0:256], Yre[:, fc, cc * 128:(cc + 1) * 128], ci[:, fc, :], start=(fc == 0), stop=False)
                for fc in range(16):
                    P.mm(py[:, 0:256], Yim[:, fc, cc * 128:(cc + 1) * 128], si[:, fc, :], start=False, stop=(fc == 15))
                P.stt("dve", tm[:], zp[:], hb[:, cc:cc + 1], py[:, 0:256], ALU.mult, ALU.add)
                P.tt("pool", orr[:], tm[:], gp[:], ALU.mult)
                P.dma("pool", dest[cc * 128:(cc + 1) * 128, ts_], orr[:])


def st_filter(self, l):
    with self.stage() as sc:
        for _ in _filter_body(self, sc, l):
            pass


Builder.st_filter = st_filter
Builder.st_hconv = st_hconv
Builder.st_hyena = st_hyena

import os


class LNBufs:
    def __init__(self, sc):
        self.s1 = sc.sb([128, 1], F32, "ln_s1")
        self.nm = sc.sb([128, 1], F32, "ln_nm")
        self.ss = sc.sb([128, 1], F32, "ln_ss")
        self.rstd = sc.sb([128, 1], F32, "ln_rstd")
        self.sq = sc.sb([128, DM], F32, "ln_sq")
        self.y = [sc.sb([128, DM], F32, "ln_y%d" % i) for i in range(2)]
        self.n = 0


def ln_tile(P, lb, r, lng, lnb, dest):
    y = lb.y[lb.n % 2]
    lb.n += 1
    P.reduce("dve", lb.s1[:], r[:], ALU.add)
    P.ts("dve", lb.nm[:], lb.s1[:], -1.0 / DM, None, op0=ALU.mult)
    P.act(lb.sq[:], r[:], AF.Square, bias=lb.nm[:, 0:1])
    P.reduce("dve", lb.ss[:], lb.sq[:], ALU.add)
    P.ts("dve", lb.rstd[:], lb.ss[:], 1.0 / DM, LN_EPS_C, op0=ALU.mult, op1=ALU.add)
    P.act(lb.rstd[:], lb.rstd[:], AF.Sqrt)
    P.op("dve", lambda e: e.reciprocal(lb.rstd[:], lb.rstd[:]), reads=[lb.rstd[:]], writes=[lb.rstd[:]])
    P.ts("dve", y[:], r[:], lb.nm[:, 0:1], lb.rstd[:, 0:1], op0=ALU.add, op1=ALU.mult)
    P.tt("pool", y[:], y[:], lng[:], ALU.mult)
    P.tt("pool", y[:], y[:], lnb[:], ALU.add)
    P.dma("pool", dest, y[:])


def st_merge(self, s, l):
    P = self.P
    I = self.I
    xsrc = I["x"][s] if l == 0 else self.S["xres"][s]
    ada = self.S["ada%d" % l]
    with self.stage() as sc:
        ys = {}
        for nm, nk in (("yaT", 2), ("ybT", 4), ("ycT", 4)):
            t_ = sc.sb([128, nk, T], BF16, nm)
            for k in range(nk):
                P.dma("sp", t_[:, k, :], self.S[nm][k * 128:(k + 1) * 128, :])
            ys[nm] = t_
        wst = [sc.sb([128, DM], F32, "wst%d" % i) for i in range(2)]
        ws = {}
        n = 0
        for nm, nk in (("w_branch_a", 2), ("w_branch_b", 4), ("w_branch_c", 4), ("w_out", 8)):
            t_ = sc.sb([128, nk, DM], BF16, nm)
            for k in range(nk):
                st = wst[n % 2]
                P.dma("sp", st[:], I[nm][l][k * 128:(k + 1) * 128, :])
                P.copy("pool" if n % 2 else "dve", t_[:, k, :], st[:])
                n += 1
            ws[nm] = t_
        mergedT = sc.sb([128, 8, T], BF16, "mergedT")
        psb = [sc.ps([128, 512], F32, "psbr%d" % i) for i in range(3)]
        gts = [[sc.sb([128, 512], BF16, "g%d_%d" % (i, j)) for j in range(3)] for i in range(2)]
        ms = [[sc.sb([128, 512], F32, "m%d_%d" % (i, j)) for j in range(3)] for i in range(2)]
        n = 0
        for fc in range(8):
            for tt in range(4):
                ts_ = slice(tt * 512, (tt + 1) * 512)
                g3 = gts[n % 2]
                m3 = ms[n % 2]
                n += 1
                for j, (yn, wn, nk) in enumerate((("yaT", "w_branch_a", 2), ("ybT", "w_branch_b", 4), ("ycT", "w_branch_c", 4))):
                    for k in range(nk):
                        P.mm(psb[j][:, :], ws[wn][:, k, fc * 128:(fc + 1) * 128], ys[yn][:, k, ts_],
                             start=(k == 0), stop=(k == nk - 1))
                    P.dma("sp", g3[j][:], self.S["gateT"][j * 1024 + fc * 128: j * 1024 + (fc + 1) * 128, ts_])
                    P.tt("dve", m3[j][:], psb[j][:, :], g3[j][:], ALU.mult)
                P.tt("pool", m3[0][:], m3[0][:], m3[1][:], ALU.add)
                P.tt("pool", mergedT[:, fc, ts_], m3[0][:], m3[2][:], ALU.add)
        g1b = self.bcast_row(sc, ada[s, 2048:3072], 1024, "g1b")
        lng = self.bcast_row(sc, I["ln_g"][l, 0], 1024, "lng")
        lnb = self.bcast_row(sc, I["ln_b"][l, 0], 1024, "lnb")
        lb = LNBufs(sc)
        psM = [sc.ps([128, 1024], F32, "psM%d" % i) for i in range(2)]
        xts = [sc.sb([128, DM], F32, "xt%d" % i) for i in range(2)]
        tms = [sc.sb([128, DM], F32, "tm%d" % i) for i in range(2)]
        for i in range(16):
            pm = psM[i % 2]
            xt = xts[i % 2]
            tm = tms[i % 2]
            P.dma("sp", xt[:], xsrc[i * 128:(i + 1) * 128, :])
            for hf in range(2):
                for k in range(8):
                    P.mm(pm[:, hf * 512:(hf + 1) * 512], mergedT[:, k, i * 128:(i + 1) * 128],
                         ws["w_out"][:, k, hf * 512:(hf + 1) * 512], start=(k == 0), stop=(k == 7))
            P.tt("dve", tm[:], pm[:, :], g1b[:], ALU.mult)
            P.stt("dve", tm[:], xt[:], ALPHA_C, tm[:], ALU.mult, ALU.add)
            ln_tile(P, lb, tm, lng, lnb, self.S["xres"][s][i * 128:(i + 1) * 128, :])


def _uv_body(self, sc, l):
    P = self.P
    if True:
        identb = self.load_const(sc, "identb")
        ufs = [sc.sb([128, DM], F32, "uf%d" % i) for i in range(2)]
        vfs = [sc.sb([128, DM], F32, "vf%d" % i) for i in range(2)]
        ubs = [sc.sb([128, DM], BF16, "ub%d" % i) for i in range(2)]
        vbs = [sc.sb([128, DM], BF16, "vb%d" % i) for i in range(2)]
        uts = [sc.sb([128, 8, 128], BF16, "ut%d" % i) for i in range(2)]
        pst = [sc.ps([128, 1024], BF16, "pst%d" % i) for i in range(2)]
        U = self.I["peer_u"][l]
        V = self.I["peer_v"][l]
        for b in range(128):
            uf, vf, ub, vb, ut, pt = ufs[b % 2], vfs[b % 2], ubs[b % 2], vbs[b % 2], uts[b % 2], pst[b % 2]
            P.dma("sp", uf[:], cap(U, int(U.offset) + b * DM, [[128 * DM, 128], [1, DM]]))
            P.dma("sp", vf[:], cap(V, int(V.offset) + b * DM, [[128 * DM, 128], [1, DM]]))
            P.copy("dve", ub[:], uf[:])
            P.copy("pool", vb[:], vf[:])
            for k in range(8):
                P.tr(pt[:, k * 128:(k + 1) * 128], ub[:, k * 128:(k + 1) * 128], identb[:])
            P.copy("act", ut[:], pt[:, :].rearrange("p (k a) -> p k a", k=8))
            P.dma("act", self.S["ut%d" % l][b], ut[:])
            P.dma("pool", self.S["vb%d" % l][b], vb[:])
            yield


def st_peer_a(self, s, l):
    P = self.P
    I = self.I
    ada = self.S["ada%d" % l]
    with self.stage() as sc:
        identb = self.load_const(sc, "identb")
        identf = self.load_const(sc, "identf")
        iota16 = self.load_const(sc, "iota16")
        shb = self.bcast_row(sc, ada[s, 3072:4096], 1024, "sh2b")
        scb = self.bcast_row(sc, ada[s, 4096:5120], 1024, "sc2b")
        u2T = sc.sb([128, 8, T], BF16, "u2T")
        pst = sc.ps([128, 1024], BF16, "pst")
        self.make_uT(sc, self.S["xres"][s], scb, shb, u2T, identb, pst)
        P.dma("pool", self.S["u2T"], u2T[:])
        wq = sc.sb([128, 8, 2048], BF16, "wq")
        wst = [sc.sb([128, 2048], F32, "wqs%d" % i) for i in range(2)]
        for k in range(8):
            P.dma("sp", wst[k % 2][:], I["peer_wq"][l][k * 128:(k + 1) * 128, :])
            P.copy("pool" if k % 2 else "dve", wq[:, k, :], wst[k % 2][:])
        psK = sc.ps([128, 512], F32, "psK")
        keysT = sc.sb([128, 16, 128], BF16, "keysT")
        kst = [sc.sb([128, 128], F32, "kst%d" % i) for i in range(2)]
        for hp in range(16):
            P.dma("sp", kst[hp % 2][:], I["peer_keys"][l][hp // 2, hp % 2])
            P.tr(psK[:, 0:128], kst[hp % 2][:], identf[:])
            P.copy("act", keysT[:, hp, :], psK[:, 0:128])
        psQ = [sc.ps([128, 512], F32, "psQ%d" % i) for i in range(2)]
        psSc = sc.ps([128, 2048], F32, "psSc")
        qT = sc.sb([128, 16, 128], BF16, "qT")
        scs = sc.sb([128, 16, 128], F32, "scs")
        vals = sc.sb([128, 16, 16], F32, "vals")
        idxu = sc.sb([128, 16, 16], U32, "idxu")
        idxf = sc.sb([128, 16, 16], F32, "idxf")
        wk = sc.sb([128, 16, 128], F32, "wk")
        cand = sc.sb([128, 8, 256], F32, "cand")
        wk2 = sc.sb([128, 8, 256], F32, "wk2")
        best = sc.sb([128, 8, 16], F32, "best")
        cidx = sc.sb([128, 8, 16], U32, "cidx")
        iku = sc.sb([128, 128], U32, "iku")
        jku = sc.sb([128, 128], U32, "jku")
        ikf = sc.sb([128, 128], F32, "ikf")
        jkf = sc.sb([128, 128], F32, "jkf")
        eq = sc.sb([128, 8, 16, 16], F32, "eq")
        eq2 = sc.sb([128, 8, 16, 16], F32, "eq2")
        abg = sc.sb([128, 3, 128], F32, "abg")
        eg = sc.sb([128, 8, 16], F32, "eg")
        zz = sc.sb([128, 8], F32, "zz")
        abgT = [sc.sb([128, 3, 128], BF16, "abgT%d" % i) for i in range(2)]

        def top16b(items):
            for (vout, iout, src, scratch) in items:
                P.op("dve", lambda e, vout=vout, src=src: e.max(out=vout[:, 0:8], in_=src), reads=[src], writes=[vout[:, 0:8]])
            for (vout, iout, src, scratch) in items:
                P.op("dve", lambda e, vout=vout, iout=iout, src=src: e.max_index(out=iout[:, 0:8], in_max=vout[:, 0:8], in_values=src),
                     reads=[src, vout[:, 0:8]], writes=[iout[:, 0:8]])
            for (vout, iout, src, scratch) in items:
                P.op("dve", lambda e, vout=vout, src=src, scratch=scratch: e.match_replace(out=scratch, in_to_replace=vout[:, 0:8], in_values=src, imm_value=-1e30),
                     reads=[src, vout[:, 0:8]], writes=[scratch])
            for (vout, iout, src, scratch) in items:
                P.op("dve", lambda e, vout=vout, scratch=scratch: e.max(out=vout[:, 8:16], in_=scratch), reads=[scratch], writes=[vout[:, 8:16]])
            for (vout, iout, src, scratch) in items:
                P.op("dve", lambda e, vout=vout, iout=iout, scratch=scratch: e.max_index(out=iout[:, 8:16], in_max=vout[:, 8:16], in_values=scratch),
                     reads=[scratch, vout[:, 8:16]], writes=[iout[:, 8:16]])

        def pstr(t_):
            return t_[:].ap[0][0]

        for i in range(16):
            tsl = slice(i * 128, (i + 1) * 128)
            for hp in range(16):
                pq = psQ[(hp // 4) % 2]
                for k in range(8):
                    P.mm(pq[:, (hp % 4) * 128:(hp % 4 + 1) * 128], wq[:, k, hp * 128:(hp + 1) * 128], u2T[:, k, tsl],
                         start=(k == 0), stop=(k == 7))
                if hp % 4 == 3:
                    P.copy("act", qT[:, hp - 3:hp + 1, :], pq[:, :].rearrange("p (h t) -> p h t", h=4))
            for hp in range(16):
                P.mm(psSc[:, hp * 128:(hp + 1) * 128], qT[:, hp, :], keysT[:, hp, :])
            P.copy("act", scs[:], psSc[:, :].rearrange("p (h n) -> p h n", h=16))
            if os.environ.get('PA_SKIP'):
                continue
            top16b([(vals[:, hp, :], idxu[:, hp, :], scs[:, hp, :], wk[:, hp, :]) for hp in range(16)])
            P.copy("act", idxf[:], idxu[:])
            vb_ = vals[:]
            P.tt("pool", cand[:].rearrange("p h (i j) -> p h i j", i=16),
                 cap(vb_, int(vb_.offset), [[pstr(vals), 128], [32, 8], [1, 16], [0, 16]]),
                 cap(vb_, int(vb_.offset) + 16, [[pstr(vals), 128], [32, 8], [0, 16], [1, 16]]), ALU.add)
            top16b([(best[:, h, :], cidx[:, h, :], cand[:, h, :], wk2[:, h, :]) for h in range(8)])
            cflat = cidx[:].rearrange("p h k -> p (h k)")
            P.ts("dve", iku[:], cflat, 4, None, op0=ALU.logical_shift_right)
            P.ts("dve", jku[:], cflat, 15, None, op0=ALU.bitwise_and)
            P.copy("act", ikf[:], iku[:])
            P.copy("act", jkf[:], jku[:])
            io = iota16[:]
            iob = cap(io, int(io.offset), [[pstr(iota16), 128], [0, 8], [0, 16], [1, 16]])
            fb_ = idxf[:]
            for (kf_, off, col, e_) in ((ikf, 0, 0, eq), (jkf, 16, 1, eq2)):
                kb_ = kf_[:]
                P.tt("dve", e_[:], cap(kb_, int(kb_.offset), [[pstr(kf_), 128], [16, 8], [1, 16], [0, 16]]), iob, ALU.is_equal)
                P.tt("pool", e_[:], e_[:],
                     cap(fb_, int(fb_.offset) + off, [[pstr(idxf), 128], [32, 8], [0, 16], [1, 16]]), ALU.mult)
                P.reduce("dve", abg[:, col, :].rearrange("p (h k) -> p h k", h=8), e_[:], ALU.add)
            bb_ = best[:]
            P.tt("pool", eg[:], best[:], cap(bb_, int(bb_.offset), [[pstr(best), 128], [16, 8], [0, 16]]), ALU.subtract)
            P.act(eg[:], eg[:], AF.Exp)
            P.reduce("dve", zz[:], eg[:], ALU.add)
            P.op("dve", lambda e: e.reciprocal(zz[:], zz[:]), reads=[zz[:]], writes=[zz[:]])
            zb_ = zz[:]
            P.tt("pool", abg[:, 2, :].rearrange("p (h k) -> p h k", h=8), eg[:],
                 cap(zb_, int(zb_.offset), [[pstr(zz), 128], [1, 8], [0, 16]]), ALU.mult)
            at = abgT[i % 2]
            for j in range(3):
                P.tr(psK[:, j * 128:(j + 1) * 128], abg[:, j, :], identf[:])
            P.copy("act", at[:], psK[:, 0:384].rearrange("p (j t) -> p j t", j=3))
            for j, nm in enumerate(("pa", "pb", "pg")):
                P.dma("pool", self.S[nm][:, tsl], at[:, j, :])


def st_peer_b(self, s, l, a2=False):
    P = self.P
    I = self.I
    ada = self.S["ada%d" % l]
    dest = self.out[s] if l == 1 else self.S["xres"][s]
    with self.stage() as sc:
        iota = self.load_const(sc, "iota128")
        g2b = self.bcast_row(sc, ada[s, 5120:6144], 1024, "g2b")
        lng = self.bcast_row(sc, I["ln_g"][l, 1], 1024, "lng")
        lnb = self.bcast_row(sc, I["ln_b"][l, 1], 1024, "lnb")
        lb = LNBufs(sc)
        GTs = [sc.sb([128, 256, 64], BF16, "GT%d" % i) for i in range(2)]
        psBig = sc.ps([128, 2048], F32, "psBig")
        psA = [sc.ps([128, 512], F32, "psA%d" % i) for i in range(2)]
        psG = [sc.ps([128, 512], F32, "psG%d" % i) for i in range(2)]
        u2s = [sc.sb([128, 8, 256], BF16, "u2_%d" % i) for i in range(2)]
        abTs = [sc.sb([128, 3, 256], BF16, "abT%d" % i) for i in range(2)]
        OAs = [sc.sb([128, 16, 128], BF16, "OA%d" % i) for i in range(3)]
        OBs = [sc.sb([128, 16, 64], BF16, "OB%d" % i) for i in range(3)]
        uts = [sc.sb([128, 8, 128], BF16, "utb%d" % i) for i in range(5)]
        vbs = [sc.sb([128, DM], BF16, "vbb%d" % i) for i in range(5)]
        gls = [sc.sb([128, 256], F32, "gl%d" % i) for i in range(4)]
        Ws = [sc.sb([128, 256], BF16, "W%d" % i) for i in range(4)]
        xts = [sc.sb([128, DM], F32, "xt%d" % i) for i in range(1)] * 2
        tms = [sc.sb([128, DM], F32, "tm%d" % i) for i in range(1)] * 2
        io = iota[:]
        pio = io.ap[0][0]
        iob = cap(io, int(io.offset), [[pio, 128], [0, 16], [1, 128]])
        st = {"ne": 0, "nb": 0}

        def load_group(g):
            t0 = g * 256
            P.dma("sp", u2s[g % 2][:], self.S["u2T"][:, :, t0:t0 + 256])
            for j, nm in enumerate(("pa", "pb", "pg")):
                P.dma("sp", abTs[g % 2][:, j, :], self.S[nm][:, t0:t0 + 256])

        def gen_sub(g, half, sub):
            ab = abTs[g % 2][:]
            pab = ab.ap[0][0]
            OA, OB = OAs[sub % 3], OBs[sub % 3]
            av = cap(ab, int(ab.offset) + sub * 16, [[pab, 128], [1, 16], [0, 128]])
            bv = cap(ab, int(ab.offset) + 256 + sub * 16, [[pab, 128], [1, 16], [0, 64]])
            gv = cap(ab, int(ab.offset) + 512 + sub * 16, [[pab, 128], [1, 16], [0, 64]])
            iobh = cap(io, int(io.offset) + half * 64, [[pio, 128], [0, 16], [1, 64]])
            P.tt("dve", OB[:], iobh, bv, ALU.is_equal)
            P.tt("pool", OB[:], OB[:], gv, ALU.mult)
            P.tt("dve", OA[:], iob, av, ALU.is_equal)

        def mm_sub(g, half, sub):
            GT = GTs[half]
            OA, OB = OAs[sub % 3], OBs[sub % 3]
            for q in range(4):
                pg = psG[st["ne"] % 2]
                for t4 in range(4):
                    tk = q * 4 + t4
                    P.mm(pg[:, t4 * 64:(t4 + 1) * 64], OA[:, tk, :], OB[:, tk, :])
                tok = sub * 16 + q * 4
                P.copy("act", GT[:, tok:tok + 4, :],
                       pg[:, 0:256].rearrange("p (t b) -> p t b", t=4))
                st["ne"] += 1

        def build_sub(g, half, sub):
            gen_sub(g, half, sub)
            mm_sub(g, half, sub)

        def a_phase(g, b):
            u2 = u2s[g % 2]
            ut, vb = uts[b % 5], vbs[b % 5]
            P.dma("sp", ut[:], self.S["ut%d" % l][b])
            P.dma("sp", vb[:], self.S["vb%d" % l][b])
            pa_ = psA[b % 2][:, 0:256]
            for k in range(8):
                P.mm(pa_, ut[:, k, :], u2[:, k, :], start=(k == 0), stop=(k == 7))
            gl, W = gls[b % 4], Ws[b % 4]
            P.act(gl[:], pa_, AF.Gelu)
            P.tt("pool", W[:], gl[:], GTs[b // 64][:, :, b % 64], ALU.mult)

        def v_phase(b):
            vb = vbs[b % 5]
            W = Ws[b % 4]
            for ti in range(2):
                for hf in range(2):
                    o0 = ti * 1024 + hf * 512
                    P.mm(psBig[:, o0:o0 + 512], W[:, ti * 128:(ti + 1) * 128], vb[:, hf * 512:(hf + 1) * 512],
                         start=(b == 0), stop=(b == 127))

        a2gen = None
        if a2:
            bf = A2Bufs(self, sc)
            trp_ab = psG[0][:, 256:512]
            trp_g = psG[1][:, 256:384]

            def a2_pair(g2):
                for ti_ in (2 * g2, 2 * g2 + 1):
                    yield from _a2_tile(self, bf, ti_, trp_ab, trp_g)
        load_group(0)
        for sub in range(16):
            build_sub(0, 0, sub)
        for g in range(8):
            t0 = g * 256
            if a2 and a2gen is not None:
                for _ in a2gen:
                    pass
                a2gen = None
            if g + 1 < 8:
                load_group(g + 1)
            if a2 and g + 2 < 8:
                a2gen = a2_pair(g + 2)
            for b in range(128):
                if a2gen is not None and b % 2 == 0:
                    try:
                        next(a2gen)
                    except StopIteration:
                        a2gen = None
                a_phase(g, b)
                if b >= 2:
                    v_phase(b - 2)
                tgt = (g, 1) if b < 64 else ((g + 1, 0) if g + 1 < 8 else None)
                if tgt is not None:
                    if b % 4 == 1:
                        sub = (b % 64) // 4
                        if sub > 1:
                            mm_sub(tgt[0], tgt[1], sub - 2)
                        gen_sub(tgt[0], tgt[1], sub)
                    elif b % 64 == 62:
                        mm_sub(tgt[0], tgt[1], 14)
                    elif b % 64 == 63:
                        mm_sub(tgt[0], tgt[1], 15)
            v_phase(126)
            v_phase(127)
            for ti in range(2):
                xt, tm = xts[ti], tms[ti]
                rows = slice(t0 + ti * 128, t0 + (ti + 1) * 128)
                P.dma("sp", xt[:], self.S["xres"][s][rows, :])
                P.tt("dve", tm[:], psBig[:, ti * 1024:(ti + 1) * 1024], g2b[:], ALU.mult)
                P.stt("dve", tm[:], xt[:], ALPHA_C, tm[:], ALU.mult, ALU.add)
                ln_tile(P, lb, tm, lng, lnb, dest[rows, :])


Builder.st_merge = st_merge


def st_uv(self, l):
    with self.stage() as sc:
        for _ in _uv_body(self, sc, l):
            pass


def st_pre(self, l):
    with self.stage() as sc:
        ga = _filter_body(self, sc, l)
        gb = _uv_body(self, sc, l)
        da = db = False
        while not (da and db):
            if not da:
                try:
                    next(ga)
                except StopIteration:
                    da = True
            for _ in range(2):
                if not db:
                    try:
                        next(gb)
                    except StopIteration:
                        db = True


Builder.st_uv = st_uv
Builder.st_pre = st_pre
Builder.st_peer_a = st_peer_a
Builder.st_peer_b = st_peer_b


A2E = "dve"


class A2Bufs:
    def __init__(self, B, sc):
        self.identf = B.load_const(sc, "identf")
        self.iota16 = B.load_const(sc, "iota16")
        self.scs = sc.sb([128, 16, 128], F32, "a2scs")
        self.wk = sc.sb([128, 16, 128], F32, "a2wk")
        self.vals = sc.sb([128, 16, 16], F32, "a2vals")
        self.idxu = sc.sb([128, 16, 16], U32, "a2idxu")
        self.idxf = sc.sb([128, 16, 16], F32, "a2idxf")
        self.best = sc.sb([128, 8, 16], F32, "a2best")
        self.cidx = sc.sb([128, 8, 16], U32, "a2cidx")
        self.iku = sc.sb([128, 128], U32, "a2iku")
        self.jku = sc.sb([128, 128], U32, "a2jku")
        self.ikf = sc.sb([128, 128], F32, "a2ikf")
        self.jkf = sc.sb([128, 128], F32, "a2jkf")
        self.eq = sc.sb([128, 8, 16, 16], F32, "a2eq")
        self.abg = sc.sb([128, 3, 128], F32, "a2abg")
        self.eg = sc.sb([128, 8, 16], F32, "a2eg")
        self.zz = sc.sb([128, 8], F32, "a2zz")
        self.abgT = [sc.sb([128, 3, 128], BF16, "a2abgT%d" % i) for i in range(2)]


def _a2_tile(B, bf, i, trp_ab, trp_g):
    P = B.P
    scs, wk, vals, idxu, idxf = bf.scs, bf.wk, bf.vals, bf.idxu, bf.idxf
    best, cidx, eq, abg, eg, zz = bf.best, bf.cidx, bf.eq, bf.abg, bf.eg, bf.zz
    cand = scs[:].rearrange("p a b -> p (a b)").rearrange("p (h n) -> p h n", h=8)
    wk2 = wk[:].rearrange("p a b -> p (a b)").rearrange("p (h n) -> p h n", h=8)
    tsl = slice(i * 128, (i + 1) * 128)
    P.dma("sp", scs[:], B.S["scs"][tsl, :].rearrange("p (h n) -> p h n", h=16))

    def pstr(t_):
        return t_[:].ap[0][0]

    def top16b(items, nsplit):
        steps = []
        for (vout, iout, src, scratch) in items:
            steps.append((lambda e, vout=vout, src=src: e.max(out=vout[:, 0:8], in_=src), [src], [vout[:, 0:8]]))
        for (vout, iout, src, scratch) in items:
            steps.append((lambda e, vout=vout, iout=iout, src=src: e.max_index(out=iout[:, 0:8], in_max=vout[:, 0:8], in_values=src),
                          [src, vout[:, 0:8]], [iout[:, 0:8]]))
        for (vout, iout, src, scratch) in items:
            steps.append((lambda e, vout=vout, src=src, scratch=scratch: e.match_replace(out=scratch, in_to_replace=vout[:, 0:8], in_values=src, imm_value=-1e30),
                          [src, vout[:, 0:8]], [scratch]))
        for (vout, iout, src, scratch) in items:
            steps.append((lambda e, vout=vout, scratch=scratch: e.max(out=vout[:, 8:16], in_=scratch), [scratch], [vout[:, 8:16]]))
        for (vout, iout, src, scratch) in items:
            steps.append((lambda e, vout=vout, iout=iout, scratch=scratch: e.max_index(out=iout[:, 8:16], in_max=vout[:, 8:16], in_values=scratch),
                          [scratch, vout[:, 8:16]], [iout[:, 8:16]]))
        for n_, (fn, rd, wr) in enumerate(steps):
            P.op("dve", fn, reads=rd, writes=wr)
            if n_ % nsplit == nsplit - 1:
                yield

    yield
    yield from top16b([(vals[:, hp, :], idxu[:, hp, :], scs[:, hp, :], wk[:, hp, :]) for hp in range(16)], 8)
    P.copy("dve", idxf[:], idxu[:])
    vb_ = vals[:]
    c4 = cand.rearrange("p h (i j) -> p h i j", i=16)
    for hq in range(4):
        P.tt(A2E, c4[:, 2 * hq:2 * hq + 2],
             cap(vb_, int(vb_.offset) + 64 * hq, [[pstr(vals), 128], [32, 2], [1, 16], [0, 16]]),
             cap(vb_, int(vb_.offset) + 64 * hq + 16, [[pstr(vals), 128], [32, 2], [0, 16], [1, 16]]), ALU.add)
        yield
    yield from top16b([(best[:, h, :], cidx[:, h, :], cand[:, h, :], wk2[:, h, :]) for h in range(8)], 8)
    cflat = cidx[:].rearrange("p h k -> p (h k)")
    P.ts("dve", bf.iku[:], cflat, 4, None, op0=ALU.logical_shift_right)
    P.ts("dve", bf.jku[:], cflat, 15, None, op0=ALU.bitwise_and)
    P.copy("dve", bf.ikf[:], bf.iku[:])
    P.copy("dve", bf.jkf[:], bf.jku[:])
    yield
    io = bf.iota16[:]
    iob = cap(io, int(io.offset), [[pstr(bf.iota16), 128], [0, 8], [0, 16], [1, 16]])
    fb_ = idxf[:]
    for (kf_, off, col) in ((bf.ikf, 0, 0), (bf.jkf, 16, 1)):
        kb_ = kf_[:]
        iob2 = cap(io, int(io.offset), [[pstr(bf.iota16), 128], [0, 2], [0, 16], [1, 16]])
        for hq in range(4):
            e_ = eq[:, 2 * hq:2 * hq + 2]
            P.tt("dve", e_, cap(kb_, int(kb_.offset) + 32 * hq, [[pstr(kf_), 128], [16, 2], [1, 16], [0, 16]]), iob2, ALU.is_equal)
            P.tt(A2E, e_, e_,
                 cap(fb_, int(fb_.offset) + off + 64 * hq, [[pstr(idxf), 128], [32, 2], [0, 16], [1, 16]]), ALU.mult)
            yield
        P.reduce("dve", abg[:, col, :].rearrange("p (h k) -> p h k", h=8), eq[:], ALU.add)
        yield
    bb_ = best[:]
    P.tt(A2E, eg[:], best[:], cap(bb_, int(bb_.offset), [[pstr(best), 128], [16, 8], [0, 16]]), ALU.subtract)
    P.act(eg[:], eg[:], AF.Exp)
    P.reduce("dve", zz[:], eg[:], ALU.add)
    P.op("dve", lambda e: e.reciprocal(zz[:], zz[:]), reads=[zz[:]], writes=[zz[:]])
    zb_ = zz[:]
    P.tt(A2E, abg[:, 2, :].rearrange("p (h k) -> p h k", h=8), eg[:],
         cap(zb_, int(zb_.offset), [[pstr(zz), 128], [1, 8], [0, 16]]), ALU.mult)
    yield
    at = bf.abgT[i % 2]
    P.tr(trp_ab[:, 0:128], abg[:, 0, :], bf.identf[:])
    P.tr(trp_ab[:, 128:256], abg[:, 1, :], bf.identf[:])
    P.copy("act", at[:, 0:2, :], trp_ab.rearrange("p (j t) -> p j t", j=2))
    P.tr(trp_g, abg[:, 2, :], bf.identf[:])
    P.copy("act", at[:, 2, :], trp_g)
    for j, nm in enumerate(("pa", "pb", "pg")):
        P.dma("act", B.S[nm][:, tsl], at[:, j, :])
    yield


def st_peer_a1(self, s, l):
    P = self.P
    I = self.I
    ada = self.S["ada%d" % l]
    with self.stage() as sc:
        identb = self.load_const(sc, "identb")
        identf = self.load_const(sc, "identf")
        shb = self.bcast_row(sc, ada[s, 3072:4096], 1024, "sh2b")
        scb = self.bcast_row(sc, ada[s, 4096:5120], 1024, "sc2b")
        u2T = sc.sb([128, 8, T], BF16, "u2T")
        pst = sc.ps([128, 1024], BF16, "pst")
        self.make_uT(sc, self.S["xres"][s], scb, shb, u2T, identb, pst)
        P.dma("act", self.S["u2T"], u2T[:])
        wq = sc.sb([128, 8, 2048], BF16, "wq")
        wst = [sc.sb([128, 2048], F32, "wqs%d" % i) for i in range(2)]
        for k in range(8):
            P.dma("sp", wst[k % 2][:], I["peer_wq"][l][k * 128:(k + 1) * 128, :])
            P.copy("pool" if k % 2 else "dve", wq[:, k, :], wst[k % 2][:])
        psK = sc.ps([128, 512], F32, "psK")
        keysT = sc.sb([128, 16, 128], BF16, "keysT")
        kst = [sc.sb([128, 128], F32, "kst%d" % i) for i in range(2)]
        for hp in range(16):
            P.dma("sp", kst[hp % 2][:], I["peer_keys"][l][hp // 2, hp % 2])
            P.tr(psK[:, 0:128], kst[hp % 2][:], identf[:])
            P.copy("act", keysT[:, hp, :], psK[:, 0:128])
        psQ = [sc.ps([128, 512], F32, "psQ%d" % i) for i in range(2)]
        psSc = sc.ps([128, 2048], F32, "psSc")
        qTs = [sc.sb([128, 16, 128], BF16, "qT%d" % i) for i in range(2)]
        scss = [sc.sb([128, 16, 128], F32, "scs%d" % i) for i in range(2)]
        bf = A2Bufs(self, sc)
        trp_ab = psK[:, 0:256]
        trp_g = psK[:, 256:384]
        a2state = {"gen": None, "next": 0}

        def a2_hook(done_tiles):
            if a2state["gen"] is None and a2state["next"] < min(4, done_tiles):
                a2state["gen"] = _a2_tile(self, bf, a2state["next"], trp_ab, trp_g)
                a2state["next"] += 1
            if a2state["gen"] is not None:
                try:
                    next(a2state["gen"])
                except StopIteration:
                    a2state["gen"] = None
        for i in range(16):
            tsl = slice(i * 128, (i + 1) * 128)
            qT, scs = qTs[i % 2], scss[i % 2]
            for hp in range(16):
                a2_hook(i)
                pq = psQ[(hp // 4) % 2]
                for k in range(8):
                    P.mm(pq[:, (hp % 4) * 128:(hp % 4 + 1) * 128], wq[:, k, hp * 128:(hp + 1) * 128], u2T[:, k, tsl],
                         start=(k == 0), stop=(k == 7))
                if hp % 4 == 3:
                    P.copy("act" if (hp // 4) % 2 else "dve", qT[:, hp - 3:hp + 1, :], pq[:, :].rearrange("p (h t) -> p h t", h=4))
            for hp in range(16):
                P.mm(psSc[:, hp * 128:(hp + 1) * 128], qT[:, hp, :], keysT[:, hp, :])
            P.copy("act", scs[:, 0:8, :], psSc[:, 0:1024].rearrange("p (h n) -> p h n", h=8))
            P.copy("dve", scs[:, 8:16, :], psSc[:, 1024:2048].rearrange("p (h n) -> p h n", h=8))
            P.dma("act", self.S["scs"][tsl, :].rearrange("p (h n) -> p h n", h=16), scs[:])
        while a2state["gen"] is not None or a2state["next"] < 4:
            a2_hook(16)


Builder.st_peer_a1 = st_peer_a1


def build_all(B):
    for l in range(2):
        B.st_pre(l)
        B.st_ada(l)
    for s in range(2):
        for l in range(2):
            B.st_inproj(s, l)
            B.st_attn_a(s, l)
            B.st_attn_c(s, l, with_hconv=True)
            B.st_hyena(s, l, 0)
            B.st_hyena(s, l, 1)
            B.st_merge(s, l)
            B.st_peer_a1(s, l)
            B.st_peer_b(s, l, a2=True)


def kernel(**inputs):
    from concourse.bass_utils import run_bass_kernel_spmd
    inp = {k: np.ascontiguousarray(np.asarray(v, dtype=np.float32)) for k, v in inputs.items()}
    B = Builder()
    build_all(B)
    B.finish()
    ncores = 8
    maps = []
    for c in range(ncores):
        m = {"x": np.ascontiguousarray(inp["x"][2 * c:2 * c + 2]),
             "c": np.ascontiguousarray(inp["c"][2 * c:2 * c + 2])}
        for k in WSHAPES:
            m[k] = inp[k]
        for k, v in B.hc.items():
            m["k_" + k] = v
        maps.append(m)
    res = run_bass_kernel_spmd(B.nc, maps, core_ids=list(range(ncores)))
    out = np.concatenate([np.asarray(r["out"], dtype=np.float32) for r in res.results], axis=0)
    return out
```

```python
import numpy as np
import concourse.bass as bass
import concourse.mybir as mybir
from contextlib import ExitStack

F32 = mybir.dt.float32
BF16 = mybir.dt.bfloat16
I32 = mybir.dt.int32
U32 = mybir.dt.uint32
ALU = mybir.AluOpType
AF = mybir.ActivationFunctionType
AX = mybir.AxisListType

ENGS = ("pe", "act", "dve", "pool", "sp")
EPOCH = 12000
NDMASEM = 48
SELF_SKIP = 1 << 30


def _region(ap):
    t = ap.tensor
    name = t.name
    pairs = ap.ap
    off = int(ap.offset)
    sp = str(ap.space)
    if "SB" in sp or "PSUM" in sp:
        pstride = pairs[0][0]
        if pstride == 0:
            pstride = 1 << 40
        p0 = off // pstride if pstride < (1 << 40) else 0
        f0 = off - p0 * pstride if pstride < (1 << 40) else off
        p1 = p0 + pairs[0][1]
        ext = 0
        for st, cn in pairs[1:]:
            ext += abs(st) * (cn - 1)
        if "PSUM" in sp:
            bank = 2048 // mybir.dt.size(ap.dtype)
            f1 = f0 + ext + 1
            return (name, 0, 128, (f0 // bank) * bank, ((f1 + bank - 1) // bank) * bank)
        return (name, p0, p1, f0, f0 + ext + 1)
    ext = 0
    for st, cn in pairs:
        ext += abs(st) * (cn - 1)
    return (name, 0, 1, off, off + ext + 1)


def _ovl(a, b):
    return a[1] < b[2] and b[1] < a[2] and a[3] < b[4] and b[3] < a[4]


def _covers(a, b):
    return a[1] <= b[1] and a[2] >= b[2] and a[3] <= b[3] and a[4] >= b[4]


class Prog:
    def __init__(self, nc):
        self.nc = nc
        self.es = ExitStack()
        self.streams = {e: [] for e in ENGS}
        self.cnt = {e: 0 for e in ENGS}
        self.cur = {}
        self.allsems = []
        for e in ENGS:
            self.cur[e] = self._newsem("pg_" + e)
        self.dsems = [self._newsem("dma%d" % i) for i in range(NDMASEM)]
        self.dcum = [0] * NDMASEM
        self.dpool = {"sp": list(range(0, 24)), "act": list(range(24, 36)), "pool": list(range(36, 48))}
        self.dnext = {"sp": 0, "act": 0, "pool": 0}
        self.waited = {e: {} for e in ENGS}
        self.hist = {}
        self.floor = {}
        self.semobj = {}
        self.ninstr = 0

    def _newsem(self, name):
        s = self.es.enter_context(self.nc.semaphore(name + "_%d" % len(self.allsems)))
        self.allsems.append(s)
        return s

    def _deps(self, reads, writes, eng=None):
        deps = []
        for ap in reads:
            r = _region(ap)
            psum = "PSUM" in str(ap.space)
            for (reg, isw, ev) in self.hist.get(r[0], ()):
                if _ovl(reg, r) and (isw or (psum and ev[0] != eng)):
                    deps.append(ev)
            deps.extend(self.floor.get(r[0], ()))
        for ap in writes:
            r = _region(ap)
            for (reg, isw, ev) in self.hist.get(r[0], ()):
                if _ovl(reg, r):
                    deps.append(ev)
            deps.extend(self.floor.get(r[0], ()))
        return deps

    def _record(self, reads, writes, ev):
        for ap in writes:
            r = _region(ap)
            h = self.hist.setdefault(r[0], [])
            h[:] = [x for x in h if not _covers(r, x[0])]
            h.append((r, True, ev))
            self._trim(r[0])
        for ap in reads:
            r = _region(ap)
            h = self.hist.setdefault(r[0], [])
            rep = False
            for i, (reg, isw, oev) in enumerate(h):
                if (not isw) and reg == r and oev[0] == ev[0] and oev[3] is False:
                    h[i] = (r, False, ev)
                    rep = True
                    break
            if not rep:
                h.append((r, False, ev))
                self._trim(r[0])

    def _trim(self, name):
        h = self.hist[name]
        if len(h) > 96:
            drop = h[:32]
            del h[:32]
            fl = self.floor.setdefault(name, [])
            fl.extend(x[2] for x in drop)
            best = {}
            for ev in fl:
                k = id(ev[1])
                if k not in best or best[k][2] < ev[2]:
                    best[k] = ev
            self.floor[name] = list(best.values())

    def _waits(self, eng, deps, skip_self_pe=True):
        need = {}
        for ev in deps:
            src, sem, val, isdma = ev
            if src == eng and not isdma and eng == "pe":
                continue
            if src == eng and not isdma and sem is self.cur[eng] and self.cnt[eng] - val >= SELF_SKIP:
                continue
            k = id(sem)
            if self.waited[eng].get(k, 0) >= val:
                continue
            if k not in need or need[k][1] < val:
                need[k] = (sem, val)
        out = []
        for k, (sem, val) in need.items():
            self.waited[eng][k] = val
            out.append((sem, val))
        return out

    def op(self, eng, fn, reads=(), writes=()):
        reads = [a for a in reads if a is not None and not isinstance(a, (int, float))]
        deps = self._deps(reads, writes, eng)
        waits = self._waits(eng, deps)
        if self.cnt[eng] >= EPOCH:
            self.cur[eng] = self._newsem("pg_" + eng)
            self.cnt[eng] = 0
        sem = self.cur[eng]
        self.cnt[eng] += 1
        ev = (eng, sem, self.cnt[eng], False)
        self._record(reads, writes, ev)
        self.streams[eng].append((waits, fn, sem, 1))
        self.ninstr += 1

    def dma(self, q, out, in_, **kw):
        deps = self._deps([in_], [out], q)
        waits = self._waits(q, deps)
        pl = self.dpool[q]
        k = pl[self.dnext[q] % len(pl)]
        self.dnext[q] += 1
        sem = self.dsems[k]
        self.dcum[k] += 16
        ev = (q, sem, self.dcum[k], True)
        self._record([in_], [out], ev)
        self.streams[q].append((waits, lambda e, o=out, i=in_, kw=kw: e.dma_start(out=o, in_=i, **kw), sem, 16))
        self.ninstr += 1

    def barrier(self):
        evs = []
        for e in ENGS:
            if self.cnt[e] > 0:
                evs.append((e, self.cur[e], self.cnt[e], False))
        for k in range(NDMASEM):
            if self.dcum[k] > 0:
                evs.append(("dma", self.dsems[k], self.dcum[k], True))
        for e in ENGS:
            ws = []
            for (src, sem, val, isdma) in evs:
                if src == e and not isdma:
                    continue
                kk = id(sem)
                if self.waited[e].get(kk, 0) >= val:
                    continue
                self.waited[e][kk] = val
                ws.append((sem, val))
            if ws:
                self.streams[e].append((ws, None, None, 0))
        self.hist = {}
        self.floor = {}

    def emit(self):
        nc = self.nc
        streams = self.streams
        self.streams = {e: [] for e in ENGS}

        def run(engobj, lst):
            for (waits, fn, sem, inc) in lst:
                for (s, v) in waits:
                    engobj.wait_ge(s, v)
                if fn is not None:
                    ins = fn(engobj)
                    ins.then_inc(sem, inc)

        with nc.Block() as block:
            @block.tensor
            def _(e):
                run(e, streams["pe"])

            @block.scalar
            def _(e):
                run(e, streams["act"])

            @block.vector
            def _(e):
                run(e, streams["dve"])

            @block.gpsimd
            def _(e):
                run(e, streams["pool"])

            @block.sync
            def _(e):
                run(e, streams["sp"])

    def mm(self, out, lhsT, rhs, start=True, stop=True):
        self.op("pe", lambda e: e.matmul(out, lhsT, rhs, start=start, stop=stop),
                reads=[lhsT, rhs], writes=[out])

    def tr(self, out, in_, ident):
        self.op("pe", lambda e: e.transpose(out, in_, ident), reads=[in_, ident], writes=[out])

    def act(self, out, in_, func, bias=0.0, scale=1.0, accum_out=None, eng="act"):
        rd = [in_]
        if not isinstance(bias, (int, float)):
            rd.append(bias)
        if not isinstance(scale, (int, float)):
            rd.append(scale)
        wr = [out] + ([accum_out] if accum_out is not None else [])
        kw = {}
        if accum_out is not None:
            kw["accum_out"] = accum_out
        self.op("act", lambda e: e.activation(out, in_, func, bias=bias, scale=scale, **kw),
                reads=rd, writes=wr)

    def tt(self, eng, out, in0, in1, op):
        self.op(eng, lambda e: e.tensor_tensor(out, in0, in1, op), reads=[in0, in1], writes=[out])

    def ts(self, eng, out, in0, s1, s2=None, op0=ALU.mult, op1=None, accum_out=None):
        rd = [in0] + [s for s in (s1, s2) if s is not None and not isinstance(s, (int, float))]
        wr = [out] + ([accum_out] if accum_out is not None else [])
        kw = {}
        if op1 is not None:
            kw["op1"] = op1
        if accum_out is not None:
            kw["accum_out"] = accum_out
        self.op(eng, lambda e: e.tensor_scalar(out, in0, s1, s2, op0, **kw), reads=rd, writes=wr)

    def stt(self, eng, out, in0, scalar, in1, op0, op1):
        rd = [in0, in1] + ([scalar] if not isinstance(scalar, (int, float)) else [])
        self.op(eng, lambda e: e.scalar_tensor_tensor(out, in0, scalar, in1, op0, op1), reads=rd, writes=[out])

    def copy(self, eng, out, in_):
        if eng == "act":
            self.op("act", lambda e: e.copy(out, in_), reads=[in_], writes=[out])
        else:
            self.op(eng, lambda e: e.tensor_copy(out, in_), reads=[in_], writes=[out])

    def memset(self, eng, ap, val):
        self.op(eng, lambda e: e.memset(ap, val), reads=[], writes=[ap])

    def reduce(self, eng, out, in_, op, axis=AX.X):
        self.op(eng, lambda e: e.tensor_reduce(out, in_, axis, op), reads=[in_], writes=[out])

import math
import os
import ml_dtypes
from contextlib import contextmanager

NPBF = ml_dtypes.bfloat16
T = 2048
DM = 1024
ALPHA_C = 4.0 ** 0.25
LN_EPS_C = 1e-5
PI = math.pi


def host_consts():
    c = {}
    L = T
    N = 2 * T
    pos = np.arange(L, dtype=np.float32)
    inv = np.power(np.float32(10000.0), -(np.arange(0, 64, 2, dtype=np.float32) / np.float32(64))).astype(np.float32)
    ang = (pos[:, None] * inv[None, :]).astype(np.float32)
    cs, sn = np.cos(ang).astype(np.float32), np.sin(ang).astype(np.float32)
    p = np.arange(128)
    ropec = cs[:, p % 32].T.copy()
    sgn = np.where((p % 64) < 32, 1.0, -1.0).astype(np.float32)
    ropes = (sn[:, p % 32].T * sgn[:, None]).astype(np.float32)
    c["ropec"] = np.ascontiguousarray(ropec)
    c["ropes"] = np.ascontiguousarray(ropes)
    t = np.arange(L, dtype=np.int64)
    idx = (t[:, None] * t[None, :]) % N
    th = 2.0 * np.pi * idx.astype(np.float64) / N
    C = np.cos(th)
    S = -np.sin(th)
    alt = np.where(t % 2 == 0, 1.0, -1.0)
    Sf = S.copy()
    Sf[:, 0] = alt
    def blk_f(M):
        return np.ascontiguousarray(M.reshape(16, 128, 16, 128).transpose(2, 1, 0, 3)).astype(NPBF)
    c["cf"] = blk_f(C)
    c["sf"] = blk_f(Sf)
    Ci = (2.0 / N) * C
    Ci[0, :] = 1.0 / N
    Si = (2.0 / N) * S
    Si[0, :] = alt / N
    def blk_i(M):
        return np.ascontiguousarray(M.reshape(16, 128, 8, 256).transpose(2, 1, 0, 3)).astype(NPBF)
    c["ci"] = blk_i(Ci)
    c["si"] = blk_i(Si)
    idxf = np.arange(L, dtype=np.float32)
    tt_ = (idxf / np.float32(L - 1)).astype(np.float32)
    w = (np.float32(2.0 * math.pi) * idxf / np.float32(L)).astype(np.float32)
    bands = np.linspace(1e-4, 15, 16, dtype=np.float32)
    ang2 = (w[:, None] * bands[None, :]).astype(np.float32)
    z = np.concatenate([tt_[:, None], np.cos(ang2), -np.sin(ang2)], axis=-1).astype(np.float32)
    c["zembT"] = np.ascontiguousarray(z.T)
    c["negt"] = np.ascontiguousarray((-tt_).reshape(16, 128).T)
    a = np.arange(128)[:, None]
    b = np.arange(128)[None, :]
    mA = np.concatenate([(np.abs(128 * (sg - 1) + a - b) <= 64) for sg in range(3)], axis=1)
    c["mA"] = mA.astype(NPBF)
    mC = np.stack([(b <= a), np.ones((128, 128), bool), (a <= b)], axis=1)
    c["mC"] = np.ascontiguousarray(mC).astype(NPBF)
    sel = np.zeros((65, 64), np.float32)
    sel[64, :] = 1.0
    c["sel"] = sel
    c["ones"] = np.ones((128, 128), np.float32)
    c["identf"] = np.eye(128, dtype=np.float32)
    c["identb"] = np.eye(128).astype(NPBF)
    sw = np.zeros((128, 128), np.float32)
    sw[np.arange(128), np.arange(128) ^ 32] = 1.0
    c["swapm"] = sw.astype(NPBF)
    c["iota16"] = np.tile(np.arange(16, dtype=np.float32)[None, :], (128, 1))
    c["iota128"] = np.tile(np.arange(128)[None, :], (128, 1)).astype(NPBF)
    return c


CONST_DT = {"ropec": F32, "ropes": F32, "cf": BF16, "sf": BF16, "ci": BF16, "si": BF16, "zembT": F32,
            "negt": F32, "mA": BF16, "mC": BF16, "sel": F32, "ones": F32, "identf": F32, "identb": BF16,
            "iota16": F32, "iota128": BF16, "swapm": BF16}

WSHAPES = {
    "w_ada": [2, 1024, 6144], "b_ada": [2, 6144], "w_in": [2, 1024, 7680], "conv_w": [2, 3, 1536],
    "conv_b": [2, 1536], "hy_w1": [2, 33, 64], "hy_b1": [2, 64], "hy_w2": [2, 64, 64], "hy_b2": [2, 64],
    "hy_w3": [2, 64, 2048], "hy_freq": [2, 2, 64], "hy_log_decay": [2, 2048], "hy_bias": [2, 2, 512],
    "attn_sink": [2, 8], "w_branch_a": [2, 256, 1024], "w_branch_b": [2, 512, 1024],
    "w_branch_c": [2, 512, 1024], "w_out": [2, 1024, 1024], "ln_g": [2, 2, 1024], "ln_b": [2, 2, 1024],
    "peer_wq": [2, 1024, 2048], "peer_keys": [2, 8, 2, 128, 128], "peer_u": [2, 16384, 1024],
    "peer_v": [2, 16384, 1024],
}


def cap(ap, offset, pairs):
    return bass.AP(ap.tensor, offset, [list(p) for p in pairs])


class Scope:
    def __init__(self, B):
        self.B = B
        self.es = ExitStack()

    def sb(self, shape, dt, name="t"):
        self.B.uid += 1
        t = self.es.enter_context(self.B.nc.sbuf_tensor("%s_%d" % (name, self.B.uid), list(shape), dt))
        return t

    def ps(self, shape, dt=F32, name="ps"):
        self.B.uid += 1
        return self.es.enter_context(self.B.nc.psum_tensor("%s_%d" % (name, self.B.uid), list(shape), dt))


class Builder:
    def __init__(self, dbg=()):
        self.nc = nc = bass.Bass("TRN2", target_bir_lowering=False)
        self.P = Prog(nc)
        self.uid = 0
        self.dbg = set(dbg)
        self.I = {}
        self.I["x"] = nc.dram_tensor("x", [2, T, DM], F32, kind="ExternalInput").ap()
        self.I["c"] = nc.dram_tensor("c", [2, DM], F32, kind="ExternalInput").ap()
        for k, shp in WSHAPES.items():
            self.I[k] = nc.dram_tensor(k, shp, F32, kind="ExternalInput").ap()
        self.C = {}
        hc = host_consts()
        self.hc = hc
        for k, v in hc.items():
            self.C[k] = nc.dram_tensor("k_" + k, list(v.shape), CONST_DT[k], kind="ExternalInput").ap()
        self.out = nc.dram_tensor("out", [2, T, DM], F32, kind="ExternalOutput").ap()
        S = self.S = {}

        def scr(name, shape, dt):
            kind = "ExternalOutput" if name in self.dbg else "Internal"
            S[name] = nc.dram_tensor("s_" + name, list(shape), dt, kind=kind).ap()
        for l in range(2):
            scr("ada%d" % l, [2, 6144], F32)
            scr("hspec%d" % l, [3, 2048, 1024], BF16)
            scr("ut%d" % l, [128, 128, 8, 128], BF16)
            scr("vb%d" % l, [128, 128, 1024], BF16)
        scr("xres", [2, T, DM], F32)
        scr("qTa", [768, T], BF16)
        scr("kTa", [768, T], BF16)
        scr("va", [3, T, 256], BF16)
        scr("hyT", [1536, T], F32)
        scr("hcT", [1536, T], F32)
        scr("z1T", [512, T], F32)
        scr("qTc", [512, T], BF16)
        scr("kTc", [128, T], BF16)
        scr("vc", [T, 128], BF16)
        scr("gateT", [3072, T], BF16)
        scr("yaT", [256, T], BF16)
        scr("ybT", [512, T], BF16)
        scr("ycT", [512, T], BF16)
        scr("u2T", [128, 8, T], BF16)
        scr("scs", [T, 2048], F32)
        scr("pa", [128, T], BF16)
        scr("pb", [128, T], BF16)
        scr("pg", [128, T], BF16)

    @contextmanager
    def stage(self):
        sc = Scope(self)
        try:
            yield sc
        finally:
            self.P.barrier()
            self.P.emit()
            sc.es.close()

    def finish(self):
        self.P.es.close()

    def bcast_row(self, sc, src_ap_1d, n, name="bc"):
        t = sc.sb([128, n], F32, name)
        src = cap(src_ap_1d, int(src_ap_1d.offset), [[0, 128], [1, n]])
        self.P.dma("sp", t[:], src)
        return t

    def load_const(self, sc, name):
        v = self.hc[name]
        t = sc.sb(list(v.shape), CONST_DT[name], "c_" + name)
        self.P.dma("sp", t[:], self.C[name])
        return t

    def st_ada(self, l):
        P = self.P
        with self.stage() as sc:
            cT = sc.sb([128, 8, 2], F32, "cT")
            for b in range(2):
                P.dma("sp", cT[:, :, b], self.I["c"][b].rearrange("(k p) -> p k", p=128),
                      allow_slow_non_contiguous=True)
            P.act(cT[:], cT[:], AF.Silu)
            ada = sc.sb([2, 6144], F32, "ada")
            bb = sc.sb([2, 6144], F32, "bb")
            ba = self.I["b_ada"][l]
            P.dma("sp", bb[:], cap(ba, int(ba.offset), [[0, 2], [1, 6144]]))
            ws = [sc.sb([128, 8, 512], F32, "w%d" % i) for i in range(2)]
            pss = [sc.ps([128, 512], F32) for i in range(2)]
            for cb in range(12):
                w = ws[cb % 2]
                P.dma("sp", w[:], self.I["w_ada"][l][:, cb * 512:(cb + 1) * 512].rearrange("(k p) n -> p k n", p=128))
                ps = pss[cb % 2]
                for k in range(8):
                    P.mm(ps[0:2, :], cT[:, k, :], w[:, k, :], start=(k == 0), stop=(k == 7))
                P.tt("dve", ada[:, cb * 512:(cb + 1) * 512], ps[0:2, :], bb[:, cb * 512:(cb + 1) * 512], ALU.add)
            for o in (1024, 4096):
                P.ts("dve", ada[:, o:o + 1024], ada[:, o:o + 1024], 1.0, None, op0=ALU.add)
            P.dma("sp", self.S["ada%d" % l], ada[:])

    def make_uT(self, sc, xsrc, scb, shb, uT, identb, pst, ntiles=16, tok0=0, xkeep=None):
        P = self.P
        xts = [sc.sb([128, DM], F32, "xt%d" % i) for i in range(2)]
        ubs = [sc.sb([128, DM], BF16, "ub%d" % i) for i in range(2)]
        uf = sc.sb([128, DM], F32, "uf")
        for i in range(ntiles):
            xt = xts[i % 2] if xkeep is None else xkeep[i]
            ub = ubs[i % 2]
            P.dma("sp", xt[:], xsrc[tok0 + i * 128: tok0 + (i + 1) * 128, :])
            P.tt("dve", uf[:], xt[:], scb[:], ALU.mult)
            P.tt("pool", ub[:], uf[:], shb[:], ALU.add)
            for k in range(8):
                P.tr(pst[:, k * 128:(k + 1) * 128], ub[:, k * 128:(k + 1) * 128], identb[:])
            P.copy("act", uT[:, :, i * 128:(i + 1) * 128], pst[:].rearrange("p (k t) -> p k t", k=8))

    def st_inproj(self, s, l):
        P = self.P
        xsrc = self.I["x"][s] if l == 0 else self.S["xres"][s]
        ada = self.S["ada%d" % l]
        with self.stage() as sc:
            identb = self.load_const(sc, "identb")
            ropec = self.load_const(sc, "ropec")
            ropes = self.load_const(sc, "ropes")
            shb = self.bcast_row(sc, ada[s, 0:1024], 1024, "shb")
            scb = self.bcast_row(sc, ada[s, 1024:2048], 1024, "scb")
            uT = sc.sb([128, 8, T], BF16, "uT")
            pst = sc.ps([128, 1024], BF16, "pst")
            self.make_uT(sc, xsrc, scb, shb, uT, identb, pst)
            wfs = [sc.sb([128, 8, 256], F32, "wf%d" % i) for i in range(2)]
            wbs = [sc.sb([128, 8, 256], BF16, "wb%d" % i) for i in range(2)]
            pss = [sc.ps([128, 512], F32, "pf%d" % i) for i in range(4)]
            stg = [sc.sb([128, T], BF16, "stg%d" % i) for i in range(3)]
            stf = [sc.sb([128, T], F32, "stf%d" % i) for i in range(2)]
            tbs = [sc.sb([128, 512], BF16, "tbs%d" % i) for i in range(2)]
            ps2 = [sc.ps([128, 512], F32, "ps2_%d" % i) for i in range(2)]
            swapm = self.load_const(sc, "swapm")
            tA = [sc.sb([128, 512], F32, "tA%d" % i) for i in range(2)]
            tB = [sc.sb([128, 512], F32, "tB%d" % i) for i in range(2)]
            vst = [sc.sb([128, 16, 256], BF16, "vst%d" % i) for i in range(2)]
            cnt = {"ps": 0, "stg": 0, "stf": 0, "tmp": 0, "v": 0}

            def tokview(ap2, tt, D):
                if D == 1:
                    return ap2[:, tt * 512:(tt + 1) * 512]
                if D == 4:
                    return ap2.rearrange("p (j r) -> p r j", r=4)[:, tt, :]
                return ap2.rearrange("p (j r) -> p r j", r=16)[:, 4 * tt:4 * tt + 4, :]

            def shp(ap2, D):
                if D == 16:
                    return ap2.rearrange("p (r j) -> p r j", r=4)
                return ap2

            def tok128(ap2, i, D):
                if D == 1:
                    return ap2[:, i * 128:(i + 1) * 128]
                if D == 4:
                    return ap2.rearrange("p (j r) -> p r j", r=4)[:, i // 4, (i % 4) * 128:(i % 4 + 1) * 128]
                return ap2.rearrange("p (j r) -> p r j", r=16)[:, i, :]

            def fm_chunk(wb, cc, kind, D, dest):
                if kind == "hy":
                    st = stf[cnt["stf"] % 2]
                    cnt["stf"] += 1
                else:
                    st = stg[cnt["stg"] % 3]
                    cnt["stg"] += 1
                for tt in range(4):
                    ps = pss[cnt["ps"] % 4]
                    cnt["ps"] += 1
                    for k in range(8):
                        P.mm(ps[:, :], wb[:, k, cc * 128:(cc + 1) * 128], uT[:, k, tt * 512:(tt + 1) * 512],
                             start=(k == 0), stop=(k == 7))
                    o = st[:, tt * 512:(tt + 1) * 512]
                    if kind == "hy":
                        P.copy("act", o, ps[:, :])
                    elif kind == "gate":
                        P.act(o, ps[:, :], AF.Sigmoid)
                    else:
                        j = cnt["tmp"] % 2
                        cnt["tmp"] += 1
                        tb_, a_, b_, p2 = tbs[j], tA[j], tB[j], ps2[j]
                        P.copy("act", tb_[:], ps[:, :])
                        P.mm(p2[:, :], swapm[:], tb_[:])
                        ts_ = slice(tt * 512, (tt + 1) * 512)
                        P.tt("dve", a_[:], ps[:, :], ropec[:, ts_], ALU.mult)
                        P.tt("dve", b_[:], p2[:, :], ropes[:, ts_], ALU.mult)
                        if D == 1:
                            P.tt("pool" if j else "dve", o, a_[:], b_[:], ALU.subtract)
                        else:
                            n_ = 512 // D
                            ov = st[:, :].rearrange("p (r j) -> p j r", r=D)[:, tt * n_:(tt + 1) * n_, :]
                            P.tt("pool" if j else "dve", ov, a_[:].rearrange("p (j r) -> p j r", r=D),
                                 b_[:].rearrange("p (j r) -> p j r", r=D), ALU.subtract)
                P.dma("pool" if kind == "rope" else "act", dest, st[:])

            def tm_block(wb, c0, ncols, D, dest):
                st = vst[cnt["v"] % 2]
                cnt["v"] += 1
                for i in range(16):
                    ps = pss[cnt["ps"] % 4]
                    cnt["ps"] += 1
                    for k in range(8):
                        P.mm(ps[:, 0:ncols], tok128(uT[:, k, :], i, D), wb[:, k, c0:c0 + ncols],
                             start=(k == 0), stop=(k == 7))
                    P.copy("act", st[:, i, 0:ncols], ps[:, 0:ncols])
                P.dma("act", dest.rearrange("(i p) c -> p i c", p=128), st[:, :, 0:ncols])

            DG = (1, 4, 16)
            for blk in range(30):
                wf, wb = wfs[blk % 2], wbs[blk % 2]
                col = blk * 256
                P.dma("sp", wf[:], self.I["w_in"][l][:, col:col + 256].rearrange("(k p) n -> p k n", p=128))
                P.copy("pool" if blk % 2 else "dve", wb[:], wf[:])
                if col < 1536:
                    nm = "qTa" if col < 768 else "kTa"
                    base = 0 if col < 768 else 768
                    for cc in range(2):
                        j = (col - base) // 128 + cc
                        fm_chunk(wb, cc, "rope", DG[j // 2], self.S[nm][j * 128:(j + 1) * 128, :])
                elif col < 2304:
                    g = (col - 1536) // 256
                    tm_block(wb, 0, 256, DG[g], self.S["va"][g])
                elif col < 3840:
                    for cc in range(2):
                        j = (col - 2304) // 128 + cc
                        fm_chunk(wb, cc, "hy", 1, self.S["hyT"][j * 128:(j + 1) * 128, :])
                elif col < 4352:
                    for cc in range(2):
                        j = (col - 3840) // 128 + cc
                        fm_chunk(wb, cc, "rope", 1, self.S["qTc"][j * 128:(j + 1) * 128, :])
                elif col < 4608:
                    fm_chunk(wb, 0, "rope", 1, self.S["kTc"][:, :])
                    tm_block(wb, 128, 128, 1, self.S["vc"])
                else:
                    for cc in range(2):
                        j = (col - 4608) // 128 + cc
                        fm_chunk(wb, cc, "gate", 1, self.S["gateT"][j * 128:(j + 1) * 128, :])


def _attn_finalize(B, sc, numer, nheads, sel, psB, dest):
    P = B.P
    recs = [sc.sb([64, 512], F32, "rec%d" % i) for i in range(2)]
    ysts = [sc.sb([64, T], BF16, "yst%d" % i) for i in range(2)]
    n = 0
    for h in range(nheads):
        yst = ysts[h % 2]
        for tt in range(4):
            ps = psB[n % 2]
            rec = recs[n % 2]
            n += 1
            P.mm(ps[0:64, :], sel[0:65, 0:64], numer[0:65, h, tt * 512:(tt + 1) * 512])
            P.op("dve", lambda e, o=rec[:], i=ps[0:64, :]: e.reciprocal(o, i), reads=[ps[0:64, :]], writes=[rec[:]])
            P.tt("pool", yst[:, tt * 512:(tt + 1) * 512], numer[0:64, h, tt * 512:(tt + 1) * 512], rec[:], ALU.mult)
        P.dma("pool", dest[h * 64:(h + 1) * 64, :], yst[:])


def st_attn_a(self, s, l):
    P = self.P
    with self.stage() as sc:
        mA = self.load_const(sc, "mA")
        sel = self.load_const(sc, "sel")
        numer = sc.sb([65, 4, T], F32, "numer")
        q = sc.sb([64, 4, T], BF16, "q")
        k = sc.sb([64, 4, T], BF16, "k")
        va = sc.sb([128, 16, 4, 65], BF16, "va")
        psS = [sc.ps([128, 512], F32, "psS%d" % i) for i in range(3)]
        psO = [sc.ps([128, 512], F32, "psO%d" % i) for i in range(2)]
        psB = [sc.ps([128, 512], F32, "psB%d" % i) for i in range(2)]
        pT = [sc.sb([128, 384], BF16, "pT%d" % i) for i in range(2)]
        pT2 = [sc.sb([128, 384], BF16, "pTm%d" % i) for i in range(3)]
        P.memset("pool", va[:], 1.0)
        n = 0
        for g, D in enumerate((1, 4, 16)):
            Ls = T // D
            nb = Ls // 128
            for h in range(4):
                hh = g * 4 + h
                P.dma("sp", q[:, h, :], self.S["qTa"][hh * 64:(hh + 1) * 64, :])
                P.dma("sp", k[:, h, :], self.S["kTa"][hh * 64:(hh + 1) * 64, :])
                P.dma("sp", va[:, :, h, 0:64],
                      self.S["va"][g][:, h * 64:(h + 1) * 64].rearrange("(i p) d -> p i d", p=128))
            jobs = [(h, rho, i) for h in range(4) for rho in range(D) for i in range(nb)]

            def s_phase(n, job):
                h, rho, i = job
                base = rho * Ls
                js = [j for j in (i - 1, i, i + 1) if 0 <= j < nb]
                ps = psS[n % 3]
                p1 = pT[n % 2]
                p2 = pT2[n % 3]
                q0 = base + i * 128
                for j in js:
                    sg = j - i + 1
                    P.mm(ps[:, sg * 128:(sg + 1) * 128], k[0:64, h, base + j * 128: base + (j + 1) * 128],
                         q[0:64, h, q0:q0 + 128])
                c0 = (js[0] - i + 1) * 128
                c1 = (js[-1] - i + 2) * 128
                P.act(p1[:, c0:c1], ps[:, c0:c1], AF.Exp, scale=0.125)
                P.tt("dve", p2[:, c0:c1], p1[:, c0:c1], mA[:, c0:c1], ALU.mult)

            def pv_phase(n, job):
                h, rho, i = job
                js = [j for j in (i - 1, i, i + 1) if 0 <= j < nb]
                po = psO[n % 2]
                p2 = pT2[n % 3]
                for jj, j in enumerate(js):
                    sg = j - i + 1
                    P.mm(po[0:65, 0:128], va[:, rho * nb + j, h, :], p2[:, sg * 128:(sg + 1) * 128],
                         start=(jj == 0), stop=(jj == len(js) - 1))
                if D == 1:
                    nv = numer[0:65, h, i * 128:(i + 1) * 128]
                else:
                    nv = numer[0:65, h, :].rearrange("p (j r) -> p r j", r=D)[:, rho, i * 128:(i + 1) * 128]
                if g == 0:
                    P.copy("act", nv, po[0:65, 0:128])
                else:
                    P.tt("dve", nv, po[0:65, 0:128], nv, ALU.add)
            for n_, job in enumerate(jobs):
                s_phase(n + n_, job)
                if n_ > 0:
                    pv_phase(n + n_ - 1, jobs[n_ - 1])
            pv_phase(n + len(jobs) - 1, jobs[-1])
            n += len(jobs)
        _attn_finalize(self, sc, numer, 4, sel, psB, self.S["yaT"])


def st_attn_c(self, s, l, with_hconv=False):
    P = self.P
    with self.stage() as sc:
        hgen = _hconv_body(self, sc, l) if with_hconv else None
        mC = self.load_const(sc, "mC")
        sel = self.load_const(sc, "sel")
        numer = sc.sb([65, 8, T], F32, "numerc")
        q = sc.sb([64, 8, T], BF16, "qc")
        k = sc.sb([64, 2, T], BF16, "kc")
        va = sc.sb([128, 16, 2, 65], BF16, "vca")
        sk = sc.sb([65, 8], F32, "sk")
        psS = [sc.ps([128, 512], F32, "psS%d" % i) for i in range(3)]
        psO = [sc.ps([128, 512], F32, "psO%d" % i) for i in range(2)]
        psB = [sc.ps([128, 512], F32, "psB%d" % i) for i in range(2)]
        pT = [sc.sb([128, 3, 512], BF16, "pTc%d" % i) for i in range(2)]
        P.memset("pool", va[:], 1.0)
        for h in range(8):
            P.dma("sp", q[:, h, :], self.S["qTc"][h * 64:(h + 1) * 64, :])
        for h in range(2):
            P.dma("sp", k[:, h, :], self.S["kTc"][h * 64:(h + 1) * 64, :])
            P.dma("sp", va[:, :, h, 0:64], self.S["vc"][:, h * 64:(h + 1) * 64].rearrange("(i p) d -> p i d", p=128))
        asink = self.I["attn_sink"][l]
        P.dma("sp", sk[64:65, :], cap(asink, int(asink.offset), [[0, 1], [1, 8]]))
        P.act(sk[64:65, :], sk[64:65, :], AF.Exp)
        n = 0
        mcb = mC[:]
        pst = mcb.ap[0][0]
        jobs = [(kv, i) for kv in range(2) for i in range(16)]

        def s_phase(n, job):
            kv, i = job
            js = [j for j in (i - 1, i, i + 1) if 0 <= j < 16]
            p1 = pT[n % 2]
            for j in js:
                sg = j - i + 1
                ps = psS[sg]
                P.mm(ps[:, :].rearrange("p (h t) -> p h t", h=4), k[0:64, kv, j * 128:(j + 1) * 128],
                     q[0:64, 4 * kv:4 * kv + 4, i * 128:(i + 1) * 128])
                P.act(p1[:, sg, :], ps[:, :], AF.Exp, scale=0.125)
                if sg != 1:
                    mv = cap(mcb, int(mcb.offset) + sg * 128, [[pst, 128], [0, 4], [1, 128]])
                    pv = p1[:, sg, :].rearrange("p (h t) -> p h t", h=4)
                    P.tt("pool" if sg == 0 else "dve", pv, pv, mv, ALU.mult)

        def pv_phase(n, job):
            kv, i = job
            js = [j for j in (i - 1, i, i + 1) if 0 <= j < 16]
            p1 = pT[n % 2]
            po = psO[n % 2]
            for jj, j in enumerate(js):
                sg = j - i + 1
                P.mm(po[0:65, :], va[:, j, kv, :], p1[:, sg, :], start=(jj == 0), stop=(jj == len(js) - 1))
            P.copy("act", numer[0:65, 4 * kv:4 * kv + 4, i * 128:(i + 1) * 128],
                   po[0:65, :].rearrange("p (h t) -> p h t", h=4))
        for n_, job in enumerate(jobs):
            s_phase(n_, job)
            if n_ > 0:
                pv_phase(n_ - 1, jobs[n_ - 1])
            if hgen is not None:
                try:
                    next(hgen)
                except StopIteration:
                    hgen = None
        pv_phase(len(jobs) - 1, jobs[-1])
        if hgen is not None:
            for _ in hgen:
                pass
        for h in range(8):
            P.ts("dve", numer[64:65, h, :], numer[64:65, h, :], sk[64:65, h:h + 1], None, op0=ALU.add)
        _attn_finalize(self, sc, numer, 8, sel, psB, self.S["ycT"])


Builder.st_attn_a = st_attn_a
Builder.st_attn_c = st_attn_c


def _filter_body(self, sc, l):
    P = self.P
    I = self.I
    if True:
        zembT = self.load_const(sc, "zembT")
        negt = self.load_const(sc, "negt")
        ones = self.load_const(sc, "ones")
        w1 = sc.sb([33, 64], F32, "w1")
        w2 = sc.sb([64, 64], F32, "w2")
        w3 = sc.sb([64, 2048], F32, "w3")
        P.dma("sp", w1[:], I["hy_w1"][l])
        P.dma("sp", w2[:], I["hy_w2"][l])
        P.dma("sp", w3[:], I["hy_w3"][l])
        prm = sc.sb([64, 4], F32, "prm")
        for j, ap1 in enumerate((I["hy_b1"][l], I["hy_b2"][l], I["hy_freq"][l][0], I["hy_freq"][l][1])):
            P.dma("sp", prm[:, j:j + 1], cap(ap1, int(ap1.offset), [[1, 64], [1, 1]]))
        fb = sc.sb([64, 2], F32, "fb")
        P.tt("dve", fb[:, 0:1], prm[:, 0:1], prm[:, 2:3], ALU.mult)
        P.tt("dve", fb[:, 1:2], prm[:, 1:2], prm[:, 3:4], ALU.mult)
        psF = sc.ps([128, 2048], F32, "psF")
        psN = sc.ps([128, 1024], F32, "psN")
        h1T = sc.sb([64, T], F32, "h1T")
        h2T = sc.sb([64, T], F32, "h2T")
        arg = sc.sb([64, 512], F32, "arg")
        ki = sc.sb([64, 512], I32, "ki")
        kf = sc.sb([64, 512], F32, "kf")
        mk = sc.sb([64, 512], F32, "mk")

        def sin_layer(dst, lhsT, src, kk, fcol, fbcol):
            for tt in range(4):
                ps = psF[0:64, tt * 512:(tt + 1) * 512]
                P.mm(ps, lhsT, src[0:kk, tt * 512:(tt + 1) * 512])
                P.ts("dve", arg[:], ps, prm[:, fcol:fcol + 1], fb[:, fbcol:fbcol + 1], op0=ALU.mult, op1=ALU.add)
                P.ts("dve", arg[:], arg[:], 1.0 / (2.0 * PI), None, op0=ALU.mult)
                P.copy("dve", ki[:], arg[:])
                P.copy("dve", kf[:], ki[:])
                P.tt("dve", arg[:], arg[:], kf[:], ALU.subtract)
                P.ts("dve", mk[:], arg[:], 0.5, None, op0=ALU.is_gt)
                P.tt("dve", arg[:], arg[:], mk[:], ALU.subtract)
                P.ts("dve", mk[:], arg[:], -0.5, None, op0=ALU.is_lt)
                P.tt("dve", arg[:], arg[:], mk[:], ALU.add)
                P.act(dst[:, tt * 512:(tt + 1) * 512], arg[:], AF.Sin, scale=6.28318)
                yield
        yield from sin_layer(h1T, w1[0:33, :], zembT, 33, 2, 0)
        yield from sin_layer(h2T, w2[0:64, :], h1T, 64, 3, 1)
        ld = self.bcast_row(sc, I["hy_log_decay"][l], 2048, "ld")
        P.act(ld[:], ld[:], AF.Exp)
        gsum = sc.sb([128, 16, 1024], BF16, "gsum")
        gdiff = sc.sb([128, 16, 1024], BF16, "gdiff")
        dec = sc.sb([128, 2048], F32, "dec")
        filt = sc.sb([128, 2048], F32, "filt")
        sq = dec
        for mc in range(16):
            for cb in range(4):
                P.mm(psF[:, cb * 512:(cb + 1) * 512], h2T[0:64, mc * 128:(mc + 1) * 128], w3[0:64, cb * 512:(cb + 1) * 512])
            P.act(dec[:], ld[:], AF.Exp, scale=negt[:, mc:mc + 1])
            P.tt("dve", filt[:], psF[:, :], dec[:], ALU.mult)
            if mc == 0:
                P.memset("dve", filt[0:1, 1024:2048], 0.0)
            P.act(sq[:], filt[:], AF.Square)
            for half in range(2):
                for d in range(2):
                    c0 = d * 1024 + half * 512
                    P.mm(psN[:, half * 512:(half + 1) * 512], ones[:, :], sq[:, c0:c0 + 512],
                         start=(mc == 0 and d == 0), stop=(mc == 15 and d == 1))
            P.tt("pool", gsum[:, mc, :], filt[:, 0:1024], filt[:, 1024:2048], ALU.add)
            P.tt("dve", gdiff[:, mc, :], filt[:, 0:1024], filt[:, 1024:2048], ALU.subtract)
            yield
        rs = sc.sb([128, 1024], F32, "rs")
        P.ts("dve", rs[:], psN[:, :], 1e-12, None, op0=ALU.add)
        P.act(rs[:], rs[:], AF.Sqrt)
        P.op("dve", lambda e: e.reciprocal(rs[:], rs[:]), reads=[rs[:]], writes=[rs[:]])
        cfs = [sc.sb([128, 16, 128], BF16, "cf%d" % i) for i in range(2)]
        sfs = [sc.sb([128, 16, 128], BF16, "sf%d" % i) for i in range(2)]
        ho = [[sc.sb([128, 512], BF16, "ho%d_%d" % (i, j)) for j in range(3)] for i in range(2)]
        hs = self.S["hspec%d" % l]
        n = 0
        for fc in range(16):
            cf, sf = cfs[fc % 2], sfs[fc % 2]
            P.dma("sp", cf[:], self.C["cf"][fc])
            P.dma("sp", sf[:], self.C["sf"][fc])
            for half in range(2):
                cs = slice(half * 512, (half + 1) * 512)
                pre, pim, px = psF[:, 0:512], psF[:, 512:1024], psF[:, 1024:1536]
                for mc in range(16):
                    P.mm(pre, cf[:, mc, :], gsum[:, mc, cs], start=(mc == 0), stop=(mc == 15))
                for mc in range(16):
                    P.mm(pim, sf[:, mc, :], gdiff[:, mc, cs], start=(mc == 0), stop=(mc == 15))
                if fc == 0:
                    for mc in range(16):
                        P.mm(px, sf[:, mc, :], gsum[:, mc, cs], start=(mc == 0), stop=(mc == 15))
                hre, him, hrb = ho[n % 2]
                n += 1
                P.tt("dve", hre[:], pre, rs[:, cs], ALU.mult)
                P.tt("dve", him[:], pim, rs[:, cs], ALU.mult)
                P.copy("pool", hrb[:], hre[:])
                if fc == 0:
                    P.memset("pool", him[0:1, :], 0.0)
                    P.tt("dve", hrb[0:1, :], px[0:1, :], rs[0:1, cs], ALU.mult)
                for j, t_ in enumerate((hre, him, hrb)):
                    P.dma("pool", hs[j, fc * 128:(fc + 1) * 128, cs], t_[:])
                yield


def _hconv_body(self, sc, l):
    P = self.P
    cw = sc.sb([128, 12, 3], F32, "cw")
    cb = sc.sb([128, 12], F32, "cb")
    for i in range(3):
        P.dma("sp", cw[:, :, i], self.I["conv_w"][l][i].rearrange("(k p) -> p k", p=128), allow_slow_non_contiguous=True)
    P.dma("sp", cb[:], self.I["conv_b"][l].rearrange("(k p) -> p k", p=128), allow_slow_non_contiguous=True)
    xs = [sc.sb([128, T], F32, "hx%d" % i) for i in range(2)]
    os_ = [sc.sb([128, T], F32, "ho%d" % i) for i in range(2)]
    for cc in range(12):
        x, o = xs[cc % 2], os_[cc % 2]
        P.dma("sp", x[:], self.S["hyT"][cc * 128:(cc + 1) * 128, :])
        yield
        P.act(o[:], x[:], AF.Identity, bias=cb[:, cc:cc + 1], scale=cw[:, cc, 1:2])
        P.stt("dve", o[:, 1:T], x[:, 0:T - 1], cw[:, cc, 0:1], o[:, 1:T], ALU.mult, ALU.add)
        P.stt("dve", o[:, 0:T - 1], x[:, 1:T], cw[:, cc, 2:3], o[:, 0:T - 1], ALU.mult, ALU.add)
        P.dma("pool", self.S["hcT"][cc * 128:(cc + 1) * 128, :], o[:])
        yield


def st_hconv(self, s, l):
    with self.stage() as sc:
        for _ in _hconv_body(self, sc, l):
            pass


def st_hyena(self, s, l, o):
    P = self.P
    zsrc = self.S["hcT"][0:512, :] if o == 0 else self.S["z1T"]
    gsrc = self.S["hcT"][(o + 1) * 512:(o + 2) * 512, :]
    hs = self.S["hspec%d" % l]
    with self.stage() as sc:
        identb = self.load_const(sc, "identb")
        hb = sc.sb([128, 4], F32, "hb")
        P.dma("sp", hb[:], self.I["hy_bias"][l][o].rearrange("(k p) -> p k", p=128), allow_slow_non_contiguous=True)
        zb = sc.sb([128, 4, T], BF16, "zb")
        zfs = [sc.sb([128, T], F32, "zf%d" % i) for i in range(2)]
        for cc in range(4):
            zf = zfs[cc % 2]
            P.dma("sp", zf[:], zsrc[cc * 128:(cc + 1) * 128, :])
            P.copy("pool" if cc % 2 else "dve", zb[:, cc, :], zf[:])
        zTM = sc.sb([128, 16, 512], BF16, "zTM")
        psT = [sc.ps([128, 1024], BF16, "psT%d" % i) for i in range(2)]
        for tc in range(16):
            pt = psT[tc % 2]
            for cc in range(4):
                P.tr(pt[:, cc * 128:(cc + 1) * 128], zb[:, cc, tc * 128:(tc + 1) * 128], identb[:])
            P.copy("act", zTM[:, tc, :], pt[:, 0:512])
        Yre = sc.sb([128, 16, 512], BF16, "Yre")
        Yim = sc.sb([128, 16, 512], BF16, "Yim")
        cfs = [sc.sb([128, 16, 128], BF16, "cf%d" % i) for i in range(2)]
        sfs = [sc.sb([128, 16, 128], BF16, "sf%d" % i) for i in range(2)]
        hts = [[sc.sb([128, 512], BF16, "h%d_%d" % (i, j)) for j in range(3)] for i in range(2)]
        tmps = [[sc.sb([128, 512], F32, "tm%d_%d" % (i, j)) for j in range(4)] for i in range(2)]
        psR = [sc.ps([128, 512], F32, "psR%d" % i) for i in range(2)]
        psI = [sc.ps([128, 512], F32, "psI%d" % i) for i in range(2)]
        cs = slice(o * 512, (o + 1) * 512)
        for fc in range(16):
            cf, sf = cfs[fc % 2], sfs[fc % 2]
            P.dma("sp", cf[:], self.C["cf"][fc])
            P.dma("sp", sf[:], self.C["sf"][fc])
            hre, him, hrb = hts[fc % 2]
            for j, t_ in enumerate((hre, him, hrb)):
                P.dma("sp", t_[:], hs[j, fc * 128:(fc + 1) * 128, cs])
            pr, pi = psR[fc % 2], psI[fc % 2]
            for tc in range(16):
                P.mm(pr[:, :], cf[:, tc, :], zTM[:, tc, :], start=(tc == 0), stop=(tc == 15))
            for tc in range(16):
                P.mm(pi[:, :], sf[:, tc, :], zTM[:, tc, :], start=(tc == 0), stop=(tc == 15))
            t1, t2, t3, t4 = tmps[fc % 2]
            P.tt("dve", t1[:], pr[:, :], hre[:], ALU.mult)
            P.tt("dve", t2[:], pi[:, :], him[:], ALU.mult)
            P.tt("dve", t3[:], pr[:, :], him[:], ALU.mult)
            P.tt("dve", t4[:], pi[:, :], hrb[:], ALU.mult)
            P.tt("pool", Yre[:, fc, :], t1[:], t2[:], ALU.subtract)
            P.tt("pool", Yim[:, fc, :], t3[:], t4[:], ALU.add)
        cis = [sc.sb([128, 16, 256], BF16, "ci%d" % i) for i in range(2)]
        sis = [sc.sb([128, 16, 256], BF16, "si%d" % i) for i in range(2)]
        psY = [sc.ps([128, 512], F32, "psY%d" % i) for i in range(2)]
        zps = [sc.sb([128, 256], F32, "zp%d" % i) for i in range(2)]
        gps = [sc.sb([128, 256], F32, "gp%d" % i) for i in range(2)]
        tms = [sc.sb([128, 256], F32, "tq%d" % i) for i in range(2)]
        odt = F32 if o == 0 else BF16
        ors = [sc.sb([128, 256], odt, "or%d" % i) for i in range(2)]
        dest = self.S["z1T"] if o == 0 else self.S["ybT"]
        n = 0
        for tt in range(8):
            ci, si = cis[tt % 2], sis[tt % 2]
            P.dma("sp", ci[:], self.C["ci"][tt])
            P.dma("sp", si[:], self.C["si"][tt])
            ts_ = slice(tt * 256, (tt + 1) * 256)
            for cc in range(4):
                py = psY[n % 2]
                zp, gp, tm, orr = zps[n % 2], gps[n % 2], tms[n % 2], ors[n % 2]
                n += 1
                P.dma("sp", zp[:], zsrc[cc * 128:(cc + 1) * 128, ts_])
                P.dma("sp", gp[:], gsrc[cc * 128:(cc + 1) * 128, ts_])
                for fc in range(16):
                    P.mm(py[:, 0:256], Yre[:, fc, cc * 128:(cc + 1) * 128], ci[:, fc, :], start=(fc == 0), stop=False)
                for fc in range(16):
                    P.mm(py[:, 0:256], Yim[:, fc, cc * 128:(cc + 1) * 128], si[:, fc, :], start=False, stop=(fc == 15))
                P.stt("dve", tm[:], zp[:], hb[:, cc:cc + 1], py[:, 0:256], ALU.mult, ALU.add)
                P.tt("pool", orr[:], tm[:], gp[:], ALU.mult)
                P.dma("pool", dest[cc * 128:(cc + 1) * 128, ts_], orr[:])


def st_filter(self, l):
    with self.stage() as sc:
        for _ in _filter_body(self, sc, l):
            pass


Builder.st_filter = st_filter
Builder.st_hconv = st_hconv
Builder.st_hyena = st_hyena

import os


class LNBufs:
    def __init__(self, sc):
        self.s1 = sc.sb([128, 1], F32, "ln_s1")
        self.nm = sc.sb([128, 1], F32, "ln_nm")
        self.ss = sc.sb([128, 1], F32, "ln_ss")
        self.rstd = sc.sb([128, 1], F32, "ln_rstd")
        self.sq = sc.sb([128, DM], F32, "ln_sq")
        self.y = [sc.sb([128, DM], F32, "ln_y%d" % i) for i in range(2)]
        self.n = 0


def ln_tile(P, lb, r, lng, lnb, dest):
    y = lb.y[lb.n % 2]
    lb.n += 1
    P.reduce("dve", lb.s1[:], r[:], ALU.add)
    P.ts("dve", lb.nm[:], lb.s1[:], -1.0 / DM, None, op0=ALU.mult)
    P.act(lb.sq[:], r[:], AF.Square, bias=lb.nm[:, 0:1])
    P.reduce("dve", lb.ss[:], lb.sq[:], ALU.add)
    P.ts("dve", lb.rstd[:], lb.ss[:], 1.0 / DM, LN_EPS_C, op0=ALU.mult, op1=ALU.add)
    P.act(lb.rstd[:], lb.rstd[:], AF.Sqrt)
    P.op("dve", lambda e: e.reciprocal(lb.rstd[:], lb.rstd[:]), reads=[lb.rstd[:]], writes=[lb.rstd[:]])
    P.ts("dve", y[:], r[:], lb.nm[:, 0:1], lb.rstd[:, 0:1], op0=ALU.add, op1=ALU.mult)
    P.tt("pool", y[:], y[:], lng[:], ALU.mult)
    P.tt("pool", y[:], y[:], lnb[:], ALU.add)
    P.dma("pool", dest, y[:])


def st_merge(self, s, l):
    P = self.P
    I = self.I
    xsrc = I["x"][s] if l == 0 else self.S["xres"][s]
    ada = self.S["ada%d" % l]
    with self.stage() as sc:
        ys = {}
        for nm, nk in (("yaT", 2), ("ybT", 4), ("ycT", 4)):
            t_ = sc.sb([128, nk, T], BF16, nm)
            for k in range(nk):
                P.dma("sp", t_[:, k, :], self.S[nm][k * 128:(k + 1) * 128, :])
            ys[nm] = t_
        wst = [sc.sb([128, DM], F32, "wst%d" % i) for i in range(2)]
        ws = {}
        n = 0
        for nm, nk in (("w_branch_a", 2), ("w_branch_b", 4), ("w_branch_c", 4), ("w_out", 8)):
            t_ = sc.sb([128, nk, DM], BF16, nm)
            for k in range(nk):
                st = wst[n % 2]
                P.dma("sp", st[:], I[nm][l][k * 128:(k + 1) * 128, :])
                P.copy("pool" if n % 2 else "dve", t_[:, k, :], st[:])
                n += 1
            ws[nm] = t_
        mergedT = sc.sb([128, 8, T], BF16, "mergedT")
        psb = [sc.ps([128, 512], F32, "psbr%d" % i) for i in range(3)]
        gts = [[sc.sb([128, 512], BF16, "g%d_%d" % (i, j)) for j in range(3)] for i in range(2)]
        ms = [[sc.sb([128, 512], F32, "m%d_%d" % (i, j)) for j in range(3)] for i in range(2)]
        n = 0
        for fc in range(8):
            for tt in range(4):
                ts_ = slice(tt * 512, (tt + 1) * 512)
                g3 = gts[n % 2]
                m3 = ms[n % 2]
                n += 1
                for j, (yn, wn, nk) in enumerate((("yaT", "w_branch_a", 2), ("ybT", "w_branch_b", 4), ("ycT", "w_branch_c", 4))):
                    for k in range(nk):
                        P.mm(psb[j][:, :], ws[wn][:, k, fc * 128:(fc + 1) * 128], ys[yn][:, k, ts_],
                             start=(k == 0), stop=(k == nk - 1))
                    P.dma("sp", g3[j][:], self.S["gateT"][j * 1024 + fc * 128: j * 1024 + (fc + 1) * 128, ts_])
                    P.tt("dve", m3[j][:], psb[j][:, :], g3[j][:], ALU.mult)
                P.tt("dve", m3[0][:], m3[0][:], m3[1][:], ALU.add)
                P.tt("pool", mergedT[:, fc, ts_], m3[0][:], m3[2][:], ALU.add)
        g1b = self.bcast_row(sc, ada[s, 2048:3072], 1024, "g1b")
        lng = self.bcast_row(sc, I["ln_g"][l, 0], 1024, "lng")
        lnb = self.bcast_row(sc, I["ln_b"][l, 0], 1024, "lnb")
        lb = LNBufs(sc)
        psM = [sc.ps([128, 1024], F32, "psM%d" % i) for i in range(2)]
        xts = [sc.sb([128, DM], F32, "xt%d" % i) for i in range(2)]
        tms = [sc.sb([128, DM], F32, "tm%d" % i) for i in range(2)]
        for i in range(16):
            pm = psM[i % 2]
            xt = xts[i % 2]
            tm = tms[i % 2]
            P.dma("sp", xt[:], xsrc[i * 128:(i + 1) * 128, :])
            for hf in range(2):
                for k in range(8):
                    P.mm(pm[:, hf * 512:(hf + 1) * 512], mergedT[:, k, i * 128:(i + 1) * 128],
                         ws["w_out"][:, k, hf * 512:(hf + 1) * 512], start=(k == 0), stop=(k == 7))
            P.tt("dve", tm[:], pm[:, :], g1b[:], ALU.mult)
            P.stt("dve", tm[:], xt[:], ALPHA_C, tm[:], ALU.mult, ALU.add)
            ln_tile(P, lb, tm, lng, lnb, self.S["xres"][s][i * 128:(i + 1) * 128, :])


def _uv_body(self, sc, l):
    P = self.P
    if True:
        identb = self.load_const(sc, "identb")
        ufs = [sc.sb([128, DM], F32, "uf%d" % i) for i in range(2)]
        vfs = [sc.sb([128, DM], F32, "vf%d" % i) for i in range(2)]
        ubs = [sc.sb([128, DM], BF16, "ub%d" % i) for i in range(2)]
        vbs = [sc.sb([128, DM], BF16, "vb%d" % i) for i in range(2)]
        uts = [sc.sb([128, 8, 128], BF16, "ut%d" % i) for i in range(2)]
        pst = [sc.ps([128, 1024], BF16, "pst%d" % i) for i in range(2)]
        U = self.I["peer_u"][l]
        V = self.I["peer_v"][l]
        for b in range(128):
            uf, vf, ub, vb, ut, pt = ufs[b % 2], vfs[b % 2], ubs[b % 2], vbs[b % 2], uts[b % 2], pst[b % 2]
            P.dma("sp", uf[:], cap(U, int(U.offset) + b * DM, [[128 * DM, 128], [1, DM]]))
            P.dma("sp", vf[:], cap(V, int(V.offset) + b * DM, [[128 * DM, 128], [1, DM]]))
            P.copy("dve", ub[:], uf[:])
            P.copy("pool", vb[:], vf[:])
            for k in range(8):
                P.tr(pt[:, k * 128:(k + 1) * 128], ub[:, k * 128:(k + 1) * 128], identb[:])
            P.copy("act", ut[:], pt[:, :].rearrange("p (k a) -> p k a", k=8))
            P.dma("act", self.S["ut%d" % l][b], ut[:])
            P.dma("pool", self.S["vb%d" % l][b], vb[:])
            yield


def st_peer_a(self, s, l):
    P = self.P
    I = self.I
    ada = self.S["ada%d" % l]
    with self.stage() as sc:
        identb = self.load_const(sc, "identb")
        identf = self.load_const(sc, "identf")
        iota16 = self.load_const(sc, "iota16")
        shb = self.bcast_row(sc, ada[s, 3072:4096], 1024, "sh2b")
        scb = self.bcast_row(sc, ada[s, 4096:5120], 1024, "sc2b")
        u2T = sc.sb([128, 8, T], BF16, "u2T")
        pst = sc.ps([128, 1024], BF16, "pst")
        self.make_uT(sc, self.S["xres"][s], scb, shb, u2T, identb, pst)
        P.dma("pool", self.S["u2T"], u2T[:])
        wq = sc.sb([128, 8, 2048], BF16, "wq")
        wst = [sc.sb([128, 2048], F32, "wqs%d" % i) for i in range(2)]
        for k in range(8):
            P.dma("sp", wst[k % 2][:], I["peer_wq"][l][k * 128:(k + 1) * 128, :])
            P.copy("pool" if k % 2 else "dve", wq[:, k, :], wst[k % 2][:])
        psK = sc.ps([128, 512], F32, "psK")
        keysT = sc.sb([128, 16, 128], BF16, "keysT")
        kst = [sc.sb([128, 128], F32, "kst%d" % i) for i in range(2)]
        for hp in range(16):
            P.dma("sp", kst[hp % 2][:], I["peer_keys"][l][hp // 2, hp % 2])
            P.tr(psK[:, 0:128], kst[hp % 2][:], identf[:])
            P.copy("act", keysT[:, hp, :], psK[:, 0:128])
        psQ = [sc.ps([128, 512], F32, "psQ%d" % i) for i in range(2)]
        psSc = sc.ps([128, 2048], F32, "psSc")
        qT = sc.sb([128, 16, 128], BF16, "qT")
        scs = sc.sb([128, 16, 128], F32, "scs")
        vals = sc.sb([128, 16, 16], F32, "vals")
        idxu = sc.sb([128, 16, 16], U32, "idxu")
        idxf = sc.sb([128, 16, 16], F32, "idxf")
        wk = sc.sb([128, 16, 128], F32, "wk")
        cand = sc.sb([128, 8, 256], F32, "cand")
        wk2 = sc.sb([128, 8, 256], F32, "wk2")
        best = sc.sb([128, 8, 16], F32, "best")
        cidx = sc.sb([128, 8, 16], U32, "cidx")
        iku = sc.sb([128, 128], U32, "iku")
        jku = sc.sb([128, 128], U32, "jku")
        ikf = sc.sb([128, 128], F32, "ikf")
        jkf = sc.sb([128, 128], F32, "jkf")
        eq = sc.sb([128, 8, 16, 16], F32, "eq")
        eq2 = sc.sb([128, 8, 16, 16], F32, "eq2")
        abg = sc.sb([128, 3, 128], F32, "abg")
        eg = sc.sb([128, 8, 16], F32, "eg")
        zz = sc.sb([128, 8], F32, "zz")
        abgT = [sc.sb([128, 3, 128], BF16, "abgT%d" % i) for i in range(2)]

        def top16b(items):
            for (vout, iout, src, scratch) in items:
                P.op("dve", lambda e, vout=vout, src=src: e.max(out=vout[:, 0:8], in_=src), reads=[src], writes=[vout[:, 0:8]])
            for (vout, iout, src, scratch) in items:
                P.op("dve", lambda e, vout=vout, iout=iout, src=src: e.max_index(out=iout[:, 0:8], in_max=vout[:, 0:8], in_values=src),
                     reads=[src, vout[:, 0:8]], writes=[iout[:, 0:8]])
            for (vout, iout, src, scratch) in items:
                P.op("dve", lambda e, vout=vout, src=src, scratch=scratch: e.match_replace(out=scratch, in_to_replace=vout[:, 0:8], in_values=src, imm_value=-1e30),
                     reads=[src, vout[:, 0:8]], writes=[scratch])
            for (vout, iout, src, scratch) in items:
                P.op("dve", lambda e, vout=vout, scratch=scratch: e.max(out=vout[:, 8:16], in_=scratch), reads=[scratch], writes=[vout[:, 8:16]])
            for (vout, iout, src, scratch) in items:
                P.op("dve", lambda e, vout=vout, iout=iout, scratch=scratch: e.max_index(out=iout[:, 8:16], in_max=vout[:, 8:16], in_values=scratch),
                     reads=[scratch, vout[:, 8:16]], writes=[iout[:, 8:16]])

        def pstr(t_):
            return t_[:].ap[0][0]

        for i in range(16):
            tsl = slice(i * 128, (i + 1) * 128)
            for hp in range(16):
                pq = psQ[(hp // 4) % 2]
                for k in range(8):
                    P.mm(pq[:, (hp % 4) * 128:(hp % 4 + 1) * 128], wq[:, k, hp * 128:(hp + 1) * 128], u2T[:, k, tsl],
                         start=(k == 0), stop=(k == 7))
                if hp % 4 == 3:
                    P.copy("act", qT[:, hp - 3:hp + 1, :], pq[:, :].rearrange("p (h t) -> p h t", h=4))
            for hp in range(16):
                P.mm(psSc[:, hp * 128:(hp + 1) * 128], qT[:, hp, :], keysT[:, hp, :])
            P.copy("act", scs[:], psSc[:, :].rearrange("p (h n) -> p h n", h=16))
            if os.environ.get('PA_SKIP'):
                continue
            top16b([(vals[:, hp, :], idxu[:, hp, :], scs[:, hp, :], wk[:, hp, :]) for hp in range(16)])
            P.copy("act", idxf[:], idxu[:])
            vb_ = vals[:]
            P.tt("pool", cand[:].rearrange("p h (i j) -> p h i j", i=16),
                 cap(vb_, int(vb_.offset), [[pstr(vals), 128], [32, 8], [1, 16], [0, 16]]),
                 cap(vb_, int(vb_.offset) + 16, [[pstr(vals), 128], [32, 8], [0, 16], [1, 16]]), ALU.add)
            top16b([(best[:, h, :], cidx[:, h, :], cand[:, h, :], wk2[:, h, :]) for h in range(8)])
            cflat = cidx[:].rearrange("p h k -> p (h k)")
            P.ts("dve", iku[:], cflat, 4, None, op0=ALU.logical_shift_right)
            P.ts("dve", jku[:], cflat, 15, None, op0=ALU.bitwise_and)
            P.copy("act", ikf[:], iku[:])
            P.copy("act", jkf[:], jku[:])
            io = iota16[:]
            iob = cap(io, int(io.offset), [[pstr(iota16), 128], [0, 8], [0, 16], [1, 16]])
            fb_ = idxf[:]
            for (kf_, off, col, e_) in ((ikf, 0, 0, eq), (jkf, 16, 1, eq2)):
                kb_ = kf_[:]
                P.tt("dve", e_[:], cap(kb_, int(kb_.offset), [[pstr(kf_), 128], [16, 8], [1, 16], [0, 16]]), iob, ALU.is_equal)
                P.tt("pool", e_[:], e_[:],
                     cap(fb_, int(fb_.offset) + off, [[pstr(idxf), 128], [32, 8], [0, 16], [1, 16]]), ALU.mult)
                P.reduce("dve", abg[:, col, :].rearrange("p (h k) -> p h k", h=8), e_[:], ALU.add)
            bb_ = best[:]
            P.tt("pool", eg[:], best[:], cap(bb_, int(bb_.offset), [[pstr(best), 128], [16, 8], [0, 16]]), ALU.subtract)
            P.act(eg[:], eg[:], AF.Exp)
            P.reduce("dve", zz[:], eg[:], ALU.add)
            P.op("dve", lambda e: e.reciprocal(zz[:], zz[:]), reads=[zz[:]], writes=[zz[:]])
            zb_ = zz[:]
            P.tt("pool", abg[:, 2, :].rearrange("p (h k) -> p h k", h=8), eg[:],
                 cap(zb_, int(zb_.offset), [[pstr(zz), 128], [1, 8], [0, 16]]), ALU.mult)
            at = abgT[i % 2]
            for j in range(3):
                P.tr(psK[:, j * 128:(j + 1) * 128], abg[:, j, :], identf[:])
            P.copy("act", at[:], psK[:, 0:384].rearrange("p (j t) -> p j t", j=3))
            for j, nm in enumerate(("pa", "pb", "pg")):
                P.dma("pool", self.S[nm][:, tsl], at[:, j, :])


def st_peer_b(self, s, l, a2=False):
    P = self.P
    I = self.I
    ada = self.S["ada%d" % l]
    dest = self.out[s] if l == 1 else self.S["xres"][s]
    with self.stage() as sc:
        iota = self.load_const(sc, "iota128")
        g2b = self.bcast_row(sc, ada[s, 5120:6144], 1024, "g2b")
        lng = self.bcast_row(sc, I["ln_g"][l, 1], 1024, "lng")
        lnb = self.bcast_row(sc, I["ln_b"][l, 1], 1024, "lnb")
        lb = LNBufs(sc)
        GTs = [sc.sb([128, 256, 64], BF16, "GT%d" % i) for i in range(2)]
        psBig = sc.ps([128, 2048], F32, "psBig")
        psA = [sc.ps([128, 512], F32, "psA%d" % i) for i in range(2)]
        psG = [sc.ps([128, 512], F32, "psG%d" % i) for i in range(2)]
        u2s = [sc.sb([128, 8, 256], BF16, "u2_%d" % i) for i in range(2)]
        abTs = [sc.sb([128, 3, 256], BF16, "abT%d" % i) for i in range(2)]
        OAs = [sc.sb([128, 16, 128], BF16, "OA%d" % i) for i in range(3)]
        OBs = [sc.sb([128, 16, 64], BF16, "OB%d" % i) for i in range(3)]
        uts = [sc.sb([128, 8, 128], BF16, "utb%d" % i) for i in range(5)]
        vbs = [sc.sb([128, DM], BF16, "vbb%d" % i) for i in range(5)]
        gls = [sc.sb([128, 256], F32, "gl%d" % i) for i in range(4)]
        Ws = [sc.sb([128, 256], BF16, "W%d" % i) for i in range(4)]
        xts = [sc.sb([128, DM], F32, "xt%d" % i) for i in range(1)] * 2
        tms = [sc.sb([128, DM], F32, "tm%d" % i) for i in range(1)] * 2
        io = iota[:]
        pio = io.ap[0][0]
        iob = cap(io, int(io.offset), [[pio, 128], [0, 16], [1, 128]])
        st = {"ne": 0, "nb": 0}

        def load_group(g):
            t0 = g * 256
            P.dma("sp", u2s[g % 2][:], self.S["u2T"][:, :, t0:t0 + 256])
            for j, nm in enumerate(("pa", "pb", "pg")):
                P.dma("sp", abTs[g % 2][:, j, :], self.S[nm][:, t0:t0 + 256])

        def gen_sub(g, half, sub):
            ab = abTs[g % 2][:]
            pab = ab.ap[0][0]
            OA, OB = OAs[sub % 3], OBs[sub % 3]
            av = cap(ab, int(ab.offset) + sub * 16, [[pab, 128], [1, 16], [0, 128]])
            bv = cap(ab, int(ab.offset) + 256 + sub * 16, [[pab, 128], [1, 16], [0, 64]])
            gv = cap(ab, int(ab.offset) + 512 + sub * 16, [[pab, 128], [1, 16], [0, 64]])
            iobh = cap(io, int(io.offset) + half * 64, [[pio, 128], [0, 16], [1, 64]])
            P.tt("dve", OB[:], iobh, bv, ALU.is_equal)
            P.tt("pool", OB[:], OB[:], gv, ALU.mult)
            P.tt("dve", OA[:], iob, av, ALU.is_equal)

        def mm_sub(g, half, sub):
            GT = GTs[half]
            OA, OB = OAs[sub % 3], OBs[sub % 3]
            for q in range(4):
                pg = psG[st["ne"] % 2]
                for t4 in range(4):
                    tk = q * 4 + t4
                    P.mm(pg[:, t4 * 64:(t4 + 1) * 64], OA[:, tk, :], OB[:, tk, :])
                tok = sub * 16 + q * 4
                P.copy("act", GT[:, tok:tok + 4, :],
                       pg[:, 0:256].rearrange("p (t b) -> p t b", t=4))
                st["ne"] += 1

        def build_sub(g, half, sub):
            gen_sub(g, half, sub)
            mm_sub(g, half, sub)

        def a_phase(g, b):
            u2 = u2s[g % 2]
            ut, vb = uts[b % 5], vbs[b % 5]
            P.dma("sp", ut[:], self.S["ut%d" % l][b])
            P.dma("sp", vb[:], self.S["vb%d" % l][b])
            pa_ = psA[b % 2][:, 0:256]
            for k in range(8):
                P.mm(pa_, ut[:, k, :], u2[:, k, :], start=(k == 0), stop=(k == 7))
            gl, W = gls[b % 4], Ws[b % 4]
            P.act(gl[:], pa_, AF.Gelu)
            P.tt("pool", W[:], gl[:], GTs[b // 64][:, :, b % 64], ALU.mult)

        def v_phase(b):
            vb = vbs[b % 5]
            W = Ws[b % 4]
            for ti in range(2):
                for hf in range(2):
                    o0 = ti * 1024 + hf * 512
                    P.mm(psBig[:, o0:o0 + 512], W[:, ti * 128:(ti + 1) * 128], vb[:, hf * 512:(hf + 1) * 512],
                         start=(b == 0), stop=(b == 127))

        a2gen = None
        if a2:
            bf = A2Bufs(self, sc)
            trp_ab = psG[0][:, 256:512]
            trp_g = psG[1][:, 256:384]

            def a2_pair(g2):
                for ti_ in (2 * g2, 2 * g2 + 1):
                    yield from _a2_tile(self, bf, ti_, trp_ab, trp_g)
        load_group(0)
        for sub in range(16):
            build_sub(0, 0, sub)
        for g in range(8):
            t0 = g * 256
            if a2 and a2gen is not None:
                for _ in a2gen:
                    pass
                a2gen = None
            if g + 1 < 8:
                load_group(g + 1)
            if a2 and g + 2 < 8:
                a2gen = a2_pair(g + 2)
            for b in range(128):
                if a2gen is not None and b % 2 == 0:
                    try:
                        next(a2gen)
                    except StopIteration:
                        a2gen = None
                a_phase(g, b)
                if b >= 2:
                    v_phase(b - 2)
                tgt = (g, 1) if b < 64 else ((g + 1, 0) if g + 1 < 8 else None)
                if tgt is not None:
                    if b % 4 == 1:
                        sub = (b % 64) // 4
                        if sub > 1:
                            mm_sub(tgt[0], tgt[1], sub - 2)
                        gen_sub(tgt[0], tgt[1], sub)
                    elif b % 64 == 62:
                        mm_sub(tgt[0], tgt[1], 14)
                    elif b % 64 == 63:
                        mm_sub(tgt[0], tgt[1], 15)
            v_phase(126)
            v_phase(127)
            for ti in range(2):
                xt, tm = xts[ti], tms[ti]
                rows = slice(t0 + ti * 128, t0 + (ti + 1) * 128)
                P.dma("sp", xt[:], self.S["xres"][s][rows, :])
                P.tt("dve", tm[:], psBig[:, ti * 1024:(ti + 1) * 1024], g2b[:], ALU.mult)
                P.stt("dve", tm[:], xt[:], ALPHA_C, tm[:], ALU.mult, ALU.add)
                ln_tile(P, lb, tm, lng, lnb, dest[rows, :])


Builder.st_merge = st_merge


def st_uv(self, l):
    with self.stage() as sc:
        for _ in _uv_body(self, sc, l):
            pass


def st_pre(self, l):
    with self.stage() as sc:
        ga = _filter_body(self, sc, l)
        gb = _uv_body(self, sc, l)
        da = db = False
        while not (da and db):
            if not da:
                try:
                    next(ga)
                except StopIteration:
                    da = True
            for _ in range(2):
                if not db:
                    try:
                        next(gb)
                    except StopIteration:
                        db = True


Builder.st_uv = st_uv
Builder.st_pre = st_pre
Builder.st_peer_a = st_peer_a
Builder.st_peer_b = st_peer_b


A2E = "dve"


class A2Bufs:
    def __init__(self, B, sc):
        self.identf = B.load_const(sc, "identf")
        self.iota16 = B.load_const(sc, "iota16")
        self.scs = sc.sb([128, 16, 128], F32, "a2scs")
        self.wk = sc.sb([128, 16, 128], F32, "a2wk")
        self.vals = sc.sb([128, 16, 16], F32, "a2vals")
        self.idxu = sc.sb([128, 16, 16], U32, "a2idxu")
        self.idxf = sc.sb([128, 16, 16], F32, "a2idxf")
        self.best = sc.sb([128, 8, 16], F32, "a2best")
        self.cidx = sc.sb([128, 8, 16], U32, "a2cidx")
        self.iku = sc.sb([128, 128], U32, "a2iku")
        self.jku = sc.sb([128, 128], U32, "a2jku")
        self.ikf = sc.sb([128, 128], F32, "a2ikf")
        self.jkf = sc.sb([128, 128], F32, "a2jkf")
        self.eq = sc.sb([128, 8, 16, 16], F32, "a2eq")
        self.abg = sc.sb([128, 3, 128], F32, "a2abg")
        self.eg = sc.sb([128, 8, 16], F32, "a2eg")
        self.zz = sc.sb([128, 8], F32, "a2zz")
        self.abgT = [sc.sb([128, 3, 128], BF16, "a2abgT%d" % i) for i in range(2)]


def _a2_tile(B, bf, i, trp_ab, trp_g):
    P = B.P
    scs, wk, vals, idxu, idxf = bf.scs, bf.wk, bf.vals, bf.idxu, bf.idxf
    best, cidx, eq, abg, eg, zz = bf.best, bf.cidx, bf.eq, bf.abg, bf.eg, bf.zz
    cand = scs[:].rearrange("p a b -> p (a b)").rearrange("p (h n) -> p h n", h=8)
    wk2 = wk[:].rearrange("p a b -> p (a b)").rearrange("p (h n) -> p h n", h=8)
    tsl = slice(i * 128, (i + 1) * 128)
    P.dma("sp", scs[:], B.S["scs"][tsl, :].rearrange("p (h n) -> p h n", h=16))

    def pstr(t_):
        return t_[:].ap[0][0]

    def top16b(items, nsplit):
        steps = []
        for (vout, iout, src, scratch) in items:
            steps.append((lambda e, vout=vout, src=src: e.max(out=vout[:, 0:8], in_=src), [src], [vout[:, 0:8]]))
        for (vout, iout, src, scratch) in items:
            steps.append((lambda e, vout=vout, iout=iout, src=src: e.max_index(out=iout[:, 0:8], in_max=vout[:, 0:8], in_values=src),
                          [src, vout[:, 0:8]], [iout[:, 0:8]]))
        for (vout, iout, src, scratch) in items:
            steps.append((lambda e, vout=vout, src=src, scratch=scratch: e.match_replace(out=scratch, in_to_replace=vout[:, 0:8], in_values=src, imm_value=-1e30),
                          [src, vout[:, 0:8]], [scratch]))
        for (vout, iout, src, scratch) in items:
            steps.append((lambda e, vout=vout, scratch=scratch: e.max(out=vout[:, 8:16], in_=scratch), [scratch], [vout[:, 8:16]]))
        for (vout, iout, src, scratch) in items:
            steps.append((lambda e, vout=vout, iout=iout, scratch=scratch: e.max_index(out=iout[:, 8:16], in_max=vout[:, 8:16], in_values=scratch),
                          [scratch, vout[:, 8:16]], [iout[:, 8:16]]))
        for n_, (fn, rd, wr) in enumerate(steps):
            P.op("dve", fn, reads=rd, writes=wr)
            if n_ % nsplit == nsplit - 1:
                yield

    yield
    yield from top16b([(vals[:, hp, :], idxu[:, hp, :], scs[:, hp, :], wk[:, hp, :]) for hp in range(16)], 8)
    P.copy("dve", idxf[:], idxu[:])
    vb_ = vals[:]
    c4 = cand.rearrange("p h (i j) -> p h i j", i=16)
    for hq in range(4):
        P.tt(A2E, c4[:, 2 * hq:2 * hq + 2],
             cap(vb_, int(vb_.offset) + 64 * hq, [[pstr(vals), 128], [32, 2], [1, 16], [0, 16]]),
             cap(vb_, int(vb_.offset) + 64 * hq + 16, [[pstr(vals), 128], [32, 2], [0, 16], [1, 16]]), ALU.add)
        yield
    yield from top16b([(best[:, h, :], cidx[:, h, :], cand[:, h, :], wk2[:, h, :]) for h in range(8)], 8)
    cflat = cidx[:].rearrange("p h k -> p (h k)")
    P.ts("dve", bf.iku[:], cflat, 4, None, op0=ALU.logical_shift_right)
    P.ts("dve", bf.jku[:], cflat, 15, None, op0=ALU.bitwise_and)
    P.copy("dve", bf.ikf[:], bf.iku[:])
    P.copy("dve", bf.jkf[:], bf.jku[:])
    yield
    io = bf.iota16[:]
    iob = cap(io, int(io.offset), [[pstr(bf.iota16), 128], [0, 8], [0, 16], [1, 16]])
    fb_ = idxf[:]
    for (kf_, off, col) in ((bf.ikf, 0, 0), (bf.jkf, 16, 1)):
        kb_ = kf_[:]
        iob2 = cap(io, int(io.offset), [[pstr(bf.iota16), 128], [0, 2], [0, 16], [1, 16]])
        for hq in range(4):
            e_ = eq[:, 2 * hq:2 * hq + 2]
            P.tt("dve", e_, cap(kb_, int(kb_.offset) + 32 * hq, [[pstr(kf_), 128], [16, 2], [1, 16], [0, 16]]), iob2, ALU.is_equal)
            P.tt(A2E, e_, e_,
                 cap(fb_, int(fb_.offset) + off + 64 * hq, [[pstr(idxf), 128], [32, 2], [0, 16], [1, 16]]), ALU.mult)
            yield
        P.reduce("dve", abg[:, col, :].rearrange("p (h k) -> p h k", h=8), eq[:], ALU.add)
        yield
    bb_ = best[:]
    P.tt(A2E, eg[:], best[:], cap(bb_, int(bb_.offset), [[pstr(best), 128], [16, 8], [0, 16]]), ALU.subtract)
    P.act(eg[:], eg[:], AF.Exp)
    P.reduce("dve", zz[:], eg[:], ALU.add)
    P.op("dve", lambda e: e.reciprocal(zz[:], zz[:]), reads=[zz[:]], writes=[zz[:]])
    zb_ = zz[:]
    P.tt(A2E, abg[:, 2, :].rearrange("p (h k) -> p h k", h=8), eg[:],
         cap(zb_, int(zb_.offset), [[pstr(zz), 128], [1, 8], [0, 16]]), ALU.mult)
    yield
    at = bf.abgT[i % 2]
    P.tr(trp_ab[:, 0:128], abg[:, 0, :], bf.identf[:])
    P.tr(trp_ab[:, 128:256], abg[:, 1, :], bf.identf[:])
    P.copy("act", at[:, 0:2, :], trp_ab.rearrange("p (j t) -> p j t", j=2))
    P.tr(trp_g, abg[:, 2, :], bf.identf[:])
    P.copy("act", at[:, 2, :], trp_g)
    for j, nm in enumerate(("pa", "pb", "pg")):
        P.dma("act", B.S[nm][:, tsl], at[:, j, :])
    yield


def st_peer_a1(self, s, l):
    P = self.P
    I = self.I
    ada = self.S["ada%d" % l]
    with self.stage() as sc:
        identb = self.load_const(sc, "identb")
        identf = self.load_const(sc, "identf")
        shb = self.bcast_row(sc, ada[s, 3072:4096], 1024, "sh2b")
        scb = self.bcast_row(sc, ada[s, 4096:5120], 1024, "sc2b")
        u2T = sc.sb([128, 8, T], BF16, "u2T")
        pst = sc.ps([128, 1024], BF16, "pst")
        self.make_uT(sc, self.S["xres"][s], scb, shb, u2T, identb, pst)
        P.dma("act", self.S["u2T"], u2T[:])
        wq = sc.sb([128, 8, 2048], BF16, "wq")
        wst = [sc.sb([128, 2048], F32, "wqs%d" % i) for i in range(2)]
        for k in range(8):
            P.dma("sp", wst[k % 2][:], I["peer_wq"][l][k * 128:(k + 1) * 128, :])
            P.copy("pool" if k % 2 else "dve", wq[:, k, :], wst[k % 2][:])
        psK = sc.ps([128, 512], F32, "psK")
        keysT = sc.sb([128, 16, 128], BF16, "keysT")
        kst = [sc.sb([128, 128], F32, "kst%d" % i) for i in range(2)]
        for hp in range(16):
            P.dma("sp", kst[hp % 2][:], I["peer_keys"][l][hp // 2, hp % 2])
            P.tr(psK[:, 0:128], kst[hp % 2][:], identf[:])
            P.copy("act", keysT[:, hp, :], psK[:, 0:128])
        psQ = [sc.ps([128, 512], F32, "psQ%d" % i) for i in range(2)]
        psSc = sc.ps([128, 2048], F32, "psSc")
        qTs = [sc.sb([128, 16, 128], BF16, "qT%d" % i) for i in range(2)]
        scss = [sc.sb([128, 16, 128], F32, "scs%d" % i) for i in range(2)]
        bf = A2Bufs(self, sc)
        trp_ab = psK[:, 0:256]
        trp_g = psK[:, 256:384]
        a2state = {"gen": None, "next": 0}

        def a2_hook(done_tiles):
            if a2state["gen"] is None and a2state["next"] < min(4, done_tiles):
                a2state["gen"] = _a2_tile(self, bf, a2state["next"], trp_ab, trp_g)
                a2state["next"] += 1
            if a2state["gen"] is not None:
                try:
                    next(a2state["gen"])
                except StopIteration:
                    a2state["gen"] = None
        for i in range(16):
            tsl = slice(i * 128, (i + 1) * 128)
            qT, scs = qTs[i % 2], scss[i % 2]
            for hp in range(16):
                a2_hook(i)
                pq = psQ[(hp // 4) % 2]
                for k in range(8):
                    P.mm(pq[:, (hp % 4) * 128:(hp % 4 + 1) * 128], wq[:, k, hp * 128:(hp + 1) * 128], u2T[:, k, tsl],
                         start=(k == 0), stop=(k == 7))
                if hp % 4 == 3:
                    P.copy("act" if (hp // 4) % 2 else "dve", qT[:, hp - 3:hp + 1, :], pq[:, :].rearrange("p (h t) -> p h t", h=4))
            for hp in range(16):
                P.mm(psSc[:, hp * 128:(hp + 1) * 128], qT[:, hp, :], keysT[:, hp, :])
            P.copy("act", scs[:, 0:8, :], psSc[:, 0:1024].rearrange("p (h n) -> p h n", h=8))
            P.copy("dve", scs[:, 8:16, :], psSc[:, 1024:2048].rearrange("p (h n) -> p h n", h=8))
            P.dma("act", self.S["scs"][tsl, :].rearrange("p (h n) -> p h n", h=16), scs[:])
        while a2state["gen"] is not None or a2state["next"] < 4:
            a2_hook(16)


Builder.st_peer_a1 = st_peer_a1


def build_all(B):
    for l in range(2):
        B.st_pre(l)
        B.st_ada(l)
    for s in range(2):
        for l in range(2):
            B.st_inproj(s, l)
            B.st_attn_a(s, l)
            B.st_attn_c(s, l, with_hconv=True)
            B.st_hyena(s, l, 0)
            B.st_hyena(s, l, 1)
            B.st_merge(s, l)
            B.st_peer_a1(s, l)
            B.st_peer_b(s, l, a2=True)


def kernel(**inputs):
    from concourse.bass_utils import run_bass_kernel_spmd
    inp = {k: np.ascontiguousarray(np.asarray(v, dtype=np.float32)) for k, v in inputs.items()}
    B = Builder()
    build_all(B)
    B.finish()
    ncores = 8
    maps = []
    for c in range(ncores):
        m = {"x": np.ascontiguousarray(inp["x"][2 * c:2 * c + 2]),
             "c": np.ascontiguousarray(inp["c"][2 * c:2 * c + 2])}
        for k in WSHAPES:
            m[k] = inp[k]
        for k, v in B.hc.items():
            m["k_" + k] = v
        maps.append(m)
    res = run_bass_kernel_spmd(B.nc, maps, core_ids=list(range(ncores)))
    out = np.concatenate([np.asarray(r["out"], dtype=np.float32) for r in res.results], axis=0)
    return out
```

```python
import numpy as np
import concourse.bass as bass
import concourse.mybir as mybir
from contextlib import ExitStack

F32 = mybir.dt.float32
BF16 = mybir.dt.bfloat16
I32 = mybir.dt.int32
U32 = mybir.dt.uint32
ALU = mybir.AluOpType
AF = mybir.ActivationFunctionType
AX = mybir.AxisListType

ENGS = ("pe", "act", "dve", "pool", "sp")
EPOCH = 12000
NDMASEM = 48
SELF_SKIP = 1 << 30


def _region(ap):
    t = ap.tensor
    name = t.name
    pairs = ap.ap
    off = int(ap.offset)
    sp = str(ap.space)
    if "SB" in sp or "PSUM" in sp:
        pstride = pairs[0][0]
        if pstride == 0:
            pstride = 1 << 40
        p0 = off // pstride if pstride < (1 << 40) else 0
        f0 = off - p0 * pstride if pstride < (1 << 40) else off
        p1 = p0 + pairs[0][1]
        ext = 0
        for st, cn in pairs[1:]:
            ext += abs(st) * (cn - 1)
        if "PSUM" in sp:
            bank = 2048 // mybir.dt.size(ap.dtype)
            f1 = f0 + ext + 1
            return (name, 0, 128, (f0 // bank) * bank, ((f1 + bank - 1) // bank) * bank)
        return (name, p0, p1, f0, f0 + ext + 1)
    ext = 0
    for st, cn in pairs:
        ext += abs(st) * (cn - 1)
    return (name, 0, 1, off, off + ext + 1)


def _ovl(a, b):
    return a[1] < b[2] and b[1] < a[2] and a[3] < b[4] and b[3] < a[4]


def _covers(a, b):
    return a[1] <= b[1] and a[2] >= b[2] and a[3] <= b[3] and a[4] >= b[4]


class Prog:
    def __init__(self, nc):
        self.nc = nc
        self.es = ExitStack()
        self.streams = {e: [] for e in ENGS}
        self.cnt = {e: 0 for e in ENGS}
        self.cur = {}
        self.allsems = []
        for e in ENGS:
            self.cur[e] = self._newsem("pg_" + e)
        self.dsems = [self._newsem("dma%d" % i) for i in range(NDMASEM)]
        self.dcum = [0] * NDMASEM
        self.dpool = {"sp": list(range(0, 24)), "act": list(range(24, 36)), "pool": list(range(36, 48))}
        self.dnext = {"sp": 0, "act": 0, "pool": 0}
        self.waited = {e: {} for e in ENGS}
        self.hist = {}
        self.floor = {}
        self.semobj = {}
        self.ninstr = 0

    def _newsem(self, name):
        s = self.es.enter_context(self.nc.semaphore(name + "_%d" % len(self.allsems)))
        self.allsems.append(s)
        return s

    def _deps(self, reads, writes, eng=None):
        deps = []
        for ap in reads:
            r = _region(ap)
            psum = "PSUM" in str(ap.space)
            for (reg, isw, ev) in self.hist.get(r[0], ()):
                if _ovl(reg, r) and (isw or (psum and ev[0] != eng)):
                    deps.append(ev)
            deps.extend(self.floor.get(r[0], ()))
        for ap in writes:
            r = _region(ap)
            for (reg, isw, ev) in self.hist.get(r[0], ()):
                if _ovl(reg, r):
                    deps.append(ev)
            deps.extend(self.floor.get(r[0], ()))
        return deps

    def _record(self, reads, writes, ev):
        for ap in writes:
            r = _region(ap)
            h = self.hist.setdefault(r[0], [])
            h[:] = [x for x in h if not _covers(r, x[0])]
            h.append((r, True, ev))
            self._trim(r[0])
        for ap in reads:
            r = _region(ap)
            h = self.hist.setdefault(r[0], [])
            rep = False
            for i, (reg, isw, oev) in enumerate(h):
                if (not isw) and reg == r and oev[0] == ev[0] and oev[3] is False:
                    h[i] = (r, False, ev)
                    rep = True
                    break
            if not rep:
                h.append((r, False, ev))
                self._trim(r[0])

    def _trim(self, name):
        h = self.hist[name]
        if len(h) > 96:
            drop = h[:32]
            del h[:32]
            fl = self.floor.setdefault(name, [])
            fl.extend(x[2] for x in drop)
            best = {}
            for ev in fl:
                k = id(ev[1])
                if k not in best or best[k][2] < ev[2]:
                    best[k] = ev
            self.floor[name] = list(best.values())

    def _waits(self, eng, deps, skip_self_pe=True):
        need = {}
        for ev in deps:
            src, sem, val, isdma = ev
            if src == eng and not isdma and eng == "pe":
                continue
            if src == eng and not isdma and sem is self.cur[eng] and self.cnt[eng] - val >= SELF_SKIP:
                continue
            k = id(sem)
            if self.waited[eng].get(k, 0) >= val:
                continue
            if k not in need or need[k][1] < val:
                need[k] = (sem, val)
        out = []
        for k, (sem, val) in need.items():
            self.waited[eng][k] = val
            out.append((sem, val))
        return out

    def op(self, eng, fn, reads=(), writes=()):
        reads = [a for a in reads if a is not None and not isinstance(a, (int, float))]
        deps = self._deps(reads, writes, eng)
        waits = self._waits(eng, deps)
        if self.cnt[eng] >= EPOCH:
            self.cur[eng] = self._newsem("pg_" + eng)
            self.cnt[eng] = 0
        sem = self.cur[eng]
        self.cnt[eng] += 1
        ev = (eng, sem, self.cnt[eng], False)
        self._record(reads, writes, ev)
        self.streams[eng].append((waits, fn, sem, 1))
        self.ninstr += 1

    def dma(self, q, out, in_, **kw):
        deps = self._deps([in_], [out], q)
        waits = self._waits(q, deps)
        pl = self.dpool[q]
        k = pl[self.dnext[q] % len(pl)]
        self.dnext[q] += 1
        sem = self.dsems[k]
        if self.dcum[k] > 0 and self.waited[q].get(id(sem), 0) < self.dcum[k]:
            self.waited[q][id(sem)] = self.dcum[k]
            waits = waits + [(sem, self.dcum[k])]
        self.dcum[k] += 16
        ev = (q, sem, self.dcum[k], True)
        self._record([in_], [out], ev)
        self.streams[q].append((waits, lambda e, o=out, i=in_, kw=kw: e.dma_start(out=o, in_=i, **kw), sem, 16))
        self.ninstr += 1

    def barrier(self):
        evs = []
        for e in ENGS:
            if self.cnt[e] > 0:
                evs.append((e, self.cur[e], self.cnt[e], False))
        for k in range(NDMASEM):
            if self.dcum[k] > 0:
                evs.append(("dma", self.dsems[k], self.dcum[k], True))
        for e in ENGS:
            ws = []
            for (src, sem, val, isdma) in evs:
                if src == e and not isdma:
                    continue
                kk = id(sem)
                if self.waited[e].get(kk, 0) >= val:
                    continue
                self.waited[e][kk] = val
                ws.append((sem, val))
            if ws:
                self.streams[e].append((ws, None, None, 0))
        self.hist = {}
        self.floor = {}

    def emit(self):
        nc = self.nc
        streams = self.streams
        self.streams = {e: [] for e in ENGS}

        def run(engobj, lst):
            for (waits, fn, sem, inc) in lst:
                for (s, v) in waits:
                    engobj.wait_ge(s, v)
                if fn is not None:
                    ins = fn(engobj)
                    ins.then_inc(sem, inc)

        with nc.Block() as block:
            @block.tensor
            def _(e):
                run(e, streams["pe"])

            @block.scalar
            def _(e):
                run(e, streams["act"])

            @block.vector
            def _(e):
                run(e, streams["dve"])

            @block.gpsimd
            def _(e):
                run(e, streams["pool"])

            @block.sync
            def _(e):
                run(e, streams["sp"])

    def mm(self, out, lhsT, rhs, start=True, stop=True):
        self.op("pe", lambda e: e.matmul(out, lhsT, rhs, start=start, stop=stop),
                reads=[lhsT, rhs], writes=[out])

    def tr(self, out, in_, ident):
        self.op("pe", lambda e: e.transpose(out, in_, ident), reads=[in_, ident], writes=[out])

    def act(self, out, in_, func, bias=0.0, scale=1.0, accum_out=None, eng="act"):
        rd = [in_]
        if not isinstance(bias, (int, float)):
            rd.append(bias)
        if not isinstance(scale, (int, float)):
            rd.append(scale)
        wr = [out] + ([accum_out] if accum_out is not None else [])
        kw = {}
        if accum_out is not None:
            kw["accum_out"] = accum_out
        self.op("act", lambda e: e.activation(out, in_, func, bias=bias, scale=scale, **kw),
                reads=rd, writes=wr)

    def tt(self, eng, out, in0, in1, op):
        self.op(eng, lambda e: e.tensor_tensor(out, in0, in1, op), reads=[in0, in1], writes=[out])

    def ts(self, eng, out, in0, s1, s2=None, op0=ALU.mult, op1=None, accum_out=None):
        rd = [in0] + [s for s in (s1, s2) if s is not None and not isinstance(s, (int, float))]
        wr = [out] + ([accum_out] if accum_out is not None else [])
        kw = {}
        if op1 is not None:
            kw["op1"] = op1
        if accum_out is not None:
            kw["accum_out"] = accum_out
        self.op(eng, lambda e: e.tensor_scalar(out, in0, s1, s2, op0, **kw), reads=rd, writes=wr)

    def stt(self, eng, out, in0, scalar, in1, op0, op1):
        rd = [in0, in1] + ([scalar] if not isinstance(scalar, (int, float)) else [])
        self.op(eng, lambda e: e.scalar_tensor_tensor(out, in0, scalar, in1, op0, op1), reads=rd, writes=[out])

    def copy(self, eng, out, in_):
        if eng == "act":
            self.op("act", lambda e: e.copy(out, in_), reads=[in_], writes=[out])
        else:
            self.op(eng, lambda e: e.tensor_copy(out, in_), reads=[in_], writes=[out])

    def memset(self, eng, ap, val):
        self.op(eng, lambda e: e.memset(ap, val), reads=[], writes=[ap])

    def reduce(self, eng, out, in_, op, axis=AX.X):
        self.op(eng, lambda e: e.tensor_reduce(out, in_, axis, op), reads=[in_], writes=[out])

import math
import os
import ml_dtypes
from contextlib import contextmanager

NPBF = ml_dtypes.bfloat16
T = 2048
DM = 1024
ALPHA_C = 4.0 ** 0.25
LN_EPS_C = 1e-5
PI = math.pi


def host_consts():
    c = {}
    L = T
    N = 2 * T
    pos = np.arange(L, dtype=np.float32)
    inv = np.power(np.float32(10000.0), -(np.arange(0, 64, 2, dtype=np.float32) / np.float32(64))).astype(np.float32)
    ang = (pos[:, None] * inv[None, :]).astype(np.float32)
    cs, sn = np.cos(ang).astype(np.float32), np.sin(ang).astype(np.float32)
    p = np.arange(128)
    ropec = cs[:, p % 32].T.copy()
    sgn = np.where((p % 64) < 32, 1.0, -1.0).astype(np.float32)
    ropes = (sn[:, p % 32].T * sgn[:, None]).astype(np.float32)
    c["ropec"] = np.ascontiguousarray(ropec)
    c["ropes"] = np.ascontiguousarray(ropes)
    t = np.arange(L, dtype=np.int64)
    idx = (t[:, None] * t[None, :]) % N
    th = 2.0 * np.pi * idx.astype(np.float64) / N
    C = np.cos(th)
    S = -np.sin(th)
    alt = np.where(t % 2 == 0, 1.0, -1.0)
    Sf = S.copy()
    Sf[:, 0] = alt
    def blk_f(M):
        return np.ascontiguousarray(M.reshape(16, 128, 16, 128).transpose(2, 1, 0, 3)).astype(NPBF)
    c["cf"] = blk_f(C)
    c["sf"] = blk_f(Sf)
    Ci = (2.0 / N) * C
    Ci[0, :] = 1.0 / N
    Si = (2.0 / N) * S
    Si[0, :] = alt / N
    def blk_i(M):
        return np.ascontiguousarray(M.reshape(16, 128, 8, 256).transpose(2, 1, 0, 3)).astype(NPBF)
    c["ci"] = blk_i(Ci)
    c["si"] = blk_i(Si)
    idxf = np.arange(L, dtype=np.float32)
    tt_ = (idxf / np.float32(L - 1)).astype(np.float32)
    w = (np.float32(2.0 * math.pi) * idxf / np.float32(L)).astype(np.float32)
    bands = np.linspace(1e-4, 15, 16, dtype=np.float32)
    ang2 = (w[:, None] * bands[None, :]).astype(np.float32)
    z = np.concatenate([tt_[:, None], np.cos(ang2), -np.sin(ang2)], axis=-1).astype(np.float32)
    c["zembT"] = np.ascontiguousarray(z.T)
    c["negt"] = np.ascontiguousarray((-tt_).reshape(16, 128).T)
    a = np.arange(128)[:, None]
    b = np.arange(128)[None, :]
    mA = np.concatenate([(np.abs(128 * (sg - 1) + a - b) <= 64) for sg in range(3)], axis=1)
    c["mA"] = mA.astype(NPBF)
    mC = np.stack([(b <= a), np.ones((128, 128), bool), (a <= b)], axis=1)
    c["mC"] = np.ascontiguousarray(mC).astype(NPBF)
    sel = np.zeros((65, 64), np.float32)
    sel[64, :] = 1.0
    c["sel"] = sel
    c["ones"] = np.ones((128, 128), np.float32)
    c["identf"] = np.eye(128, dtype=np.float32)
    c["identb"] = np.eye(128).astype(NPBF)
    sw = np.zeros((128, 128), np.float32)
    sw[np.arange(128), np.arange(128) ^ 32] = 1.0
    c["swapm"] = sw.astype(NPBF)
    c["iota16"] = np.tile(np.arange(16, dtype=np.float32)[None, :], (128, 1))
    c["iota128"] = np.tile(np.arange(128)[None, :], (128, 1)).astype(NPBF)
    return c


CONST_DT = {"ropec": F32, "ropes": F32, "cf": BF16, "sf": BF16, "ci": BF16, "si": BF16, "zembT": F32,
            "negt": F32, "mA": BF16, "mC": BF16, "sel": F32, "ones": F32, "identf": F32, "identb": BF16,
            "iota16": F32, "iota128": BF16, "swapm": BF16}

WSHAPES = {
    "w_ada": [2, 1024, 6144], "b_ada": [2, 6144], "w_in": [2, 1024, 7680], "conv_w": [2, 3, 1536],
    "conv_b": [2, 1536], "hy_w1": [2, 33, 64], "hy_b1": [2, 64], "hy_w2": [2, 64, 64], "hy_b2": [2, 64],
    "hy_w3": [2, 64, 2048], "hy_freq": [2, 2, 64], "hy_log_decay": [2, 2048], "hy_bias": [2, 2, 512],
    "attn_sink": [2, 8], "w_branch_a": [2, 256, 1024], "w_branch_b": [2, 512, 1024],
    "w_branch_c": [2, 512, 1024], "w_out": [2, 1024, 1024], "ln_g": [2, 2, 1024], "ln_b": [2, 2, 1024],
    "peer_wq": [2, 1024, 2048], "peer_keys": [2, 8, 2, 128, 128], "peer_u": [2, 16384, 1024],
    "peer_v": [2, 16384, 1024],
}


def cap(ap, offset, pairs):
    return bass.AP(ap.tensor, offset, [list(p) for p in pairs])


class Scope:
    def __init__(self, B):
        self.B = B
        self.es = ExitStack()

    def sb(self, shape, dt, name="t"):
        self.B.uid += 1
        t = self.es.enter_context(self.B.nc.sbuf_tensor("%s_%d" % (name, self.B.uid), list(shape), dt))
        return t

    def ps(self, shape, dt=F32, name="ps"):
        self.B.uid += 1
        return self.es.enter_context(self.B.nc.psum_tensor("%s_%d" % (name, self.B.uid), list(shape), dt))


class Builder:
    def __init__(self, dbg=()):
        self.nc = nc = bass.Bass("TRN2", target_bir_lowering=False)
        self.P = Prog(nc)
        self.uid = 0
        self.dbg = set(dbg)
        self.I = {}
        self.I["x"] = nc.dram_tensor("x", [2, T, DM], F32, kind="ExternalInput").ap()
        self.I["c"] = nc.dram_tensor("c", [2, DM], F32, kind="ExternalInput").ap()
        for k, shp in WSHAPES.items():
            self.I[k] = nc.dram_tensor(k, shp, F32, kind="ExternalInput").ap()
        self.C = {}
        hc = host_consts()
        self.hc = hc
        for k, v in hc.items():
            self.C[k] = nc.dram_tensor("k_" + k, list(v.shape), CONST_DT[k], kind="ExternalInput").ap()
        self.out = nc.dram_tensor("out", [2, T, DM], F32, kind="ExternalOutput").ap()
        S = self.S = {}

        def scr(name, shape, dt):
            kind = "ExternalOutput" if name in self.dbg else "Internal"
            S[name] = nc.dram_tensor("s_" + name, list(shape), dt, kind=kind).ap()
        for l in range(2):
            scr("ada%d" % l, [2, 6144], F32)
            scr("hspec%d" % l, [3, 2048, 1024], BF16)
            scr("ut%d" % l, [128, 128, 8, 128], BF16)
            scr("vb%d" % l, [128, 128, 1024], BF16)
        scr("xres", [2, T, DM], F32)
        scr("qTa", [768, T], BF16)
        scr("kTa", [768, T], BF16)
        scr("va", [3, T, 256], BF16)
        scr("hyT", [1536, T], F32)
        scr("hcT", [1536, T], F32)
        scr("z1T", [512, T], F32)
        scr("qTc", [512, T], BF16)
        scr("kTc", [128, T], BF16)
        scr("vc", [T, 128], BF16)
        scr("gateT", [3072, T], BF16)
        scr("yaT", [256, T], BF16)
        scr("ybT", [512, T], BF16)
        scr("ycT", [512, T], BF16)
        scr("u2T", [128, 8, T], BF16)
        scr("scs", [T, 2048], F32)
        scr("pa", [128, T], BF16)
        scr("pb", [128, T], BF16)
        scr("pg", [128, T], BF16)

    @contextmanager
    def stage(self):
        sc = Scope(self)
        try:
            yield sc
        finally:
            self.P.barrier()
            self.P.emit()
            sc.es.close()

    def finish(self):
        self.P.es.close()

    def bcast_row(self, sc, src_ap_1d, n, name="bc"):
        t = sc.sb([128, n], F32, name)
        src = cap(src_ap_1d, int(src_ap_1d.offset), [[0, 128], [1, n]])
        self.P.dma("sp", t[:], src)
        return t

    def load_const(self, sc, name):
        v = self.hc[name]
        t = sc.sb(list(v.shape), CONST_DT[name], "c_" + name)
        self.P.dma("sp", t[:], self.C[name])
        return t

    def st_ada(self, l):
        P = self.P
        with self.stage() as sc:
            cT = sc.sb([128, 8, 2], F32, "cT")
            for b in range(2):
                P.dma("sp", cT[:, :, b], self.I["c"][b].rearrange("(k p) -> p k", p=128),
                      allow_slow_non_contiguous=True)
            P.act(cT[:], cT[:], AF.Silu)
            ada = sc.sb([2, 6144], F32, "ada")
            bb = sc.sb([2, 6144], F32, "bb")
            ba = self.I["b_ada"][l]
            P.dma("sp", bb[:], cap(ba, int(ba.offset), [[0, 2], [1, 6144]]))
            ws = [sc.sb([128, 8, 512], F32, "w%d" % i) for i in range(2)]
            pss = [sc.ps([128, 512], F32) for i in range(2)]
            for cb in range(12):
                w = ws[cb % 2]
                P.dma("sp", w[:], self.I["w_ada"][l][:, cb * 512:(cb + 1) * 512].rearrange("(k p) n -> p k n", p=128))
                ps = pss[cb % 2]
                for k in range(8):
                    P.mm(ps[0:2, :], cT[:, k, :], w[:, k, :], start=(k == 0), stop=(k == 7))
                P.tt("dve", ada[:, cb * 512:(cb + 1) * 512], ps[0:2, :], bb[:, cb * 512:(cb + 1) * 512], ALU.add)
            for o in (1024, 4096):
                P.ts("dve", ada[:, o:o + 1024], ada[:, o:o + 1024], 1.0, None, op0=ALU.add)
            P.dma("sp", self.S["ada%d" % l], ada[:])

    def make_uT(self, sc, xsrc, scb, shb, uT, identb, pst, ntiles=16, tok0=0, xkeep=None):
        P = self.P
        xts = [sc.sb([128, DM], F32, "xt%d" % i) for i in range(2)]
        ubs = [sc.sb([128, DM], BF16, "ub%d" % i) for i in range(2)]
        uf = sc.sb([128, DM], F32, "uf")
        for i in range(ntiles):
            xt = xts[i % 2] if xkeep is None else xkeep[i]
            ub = ubs[i % 2]
            P.dma("sp", xt[:], xsrc[tok0 + i * 128: tok0 + (i + 1) * 128, :])
            P.tt("dve", uf[:], xt[:], scb[:], ALU.mult)
            P.tt("pool", ub[:], uf[:], shb[:], ALU.add)
            for k in range(8):
                P.tr(pst[:, k * 128:(k + 1) * 128], ub[:, k * 128:(k + 1) * 128], identb[:])
            P.copy("act", uT[:, :, i * 128:(i + 1) * 128], pst[:].rearrange("p (k t) -> p k t", k=8))

    def st_inproj(self, s, l):
        P = self.P
        xsrc = self.I["x"][s] if l == 0 else self.S["xres"][s]
        ada = self.S["ada%d" % l]
        with self.stage() as sc:
            identb = self.load_const(sc, "identb")
            ropec = self.load_const(sc, "ropec")
            ropes = self.load_const(sc, "ropes")
            shb = self.bcast_row(sc, ada[s, 0:1024], 1024, "shb")
            scb = self.bcast_row(sc, ada[s, 1024:2048], 1024, "scb")
            uT = sc.sb([128, 8, T], BF16, "uT")
            pst = sc.ps([128, 1024], BF16, "pst")
            self.make_uT(sc, xsrc, scb, shb, uT, identb, pst)
            wfs = [sc.sb([128, 8, 256], F32, "wf%d" % i) for i in range(2)]
            wbs = [sc.sb([128, 8, 256], BF16, "wb%d" % i) for i in range(2)]
            pss = [sc.ps([128, 512], F32, "pf%d" % i) for i in range(4)]
            stg = [sc.sb([128, T], BF16, "stg%d" % i) for i in range(3)]
            stf = [sc.sb([128, T], F32, "stf%d" % i) for i in range(2)]
            tbs = [sc.sb([128, 512], BF16, "tbs%d" % i) for i in range(2)]
            ps2 = [sc.ps([128, 512], F32, "ps2_%d" % i) for i in range(2)]
            swapm = self.load_const(sc, "swapm")
            tA = [sc.sb([128, 512], F32, "tA%d" % i) for i in range(2)]
            tB = [sc.sb([128, 512], F32, "tB%d" % i) for i in range(2)]
            vst = [sc.sb([128, 16, 256], BF16, "vst%d" % i) for i in range(2)]
            cnt = {"ps": 0, "stg": 0, "stf": 0, "tmp": 0, "v": 0}

            def tokview(ap2, tt, D):
                if D == 1:
                    return ap2[:, tt * 512:(tt + 1) * 512]
                if D == 4:
                    return ap2.rearrange("p (j r) -> p r j", r=4)[:, tt, :]
                return ap2.rearrange("p (j r) -> p r j", r=16)[:, 4 * tt:4 * tt + 4, :]

            def shp(ap2, D):
                if D == 16:
                    return ap2.rearrange("p (r j) -> p r j", r=4)
                return ap2

            def tok128(ap2, i, D):
                if D == 1:
                    return ap2[:, i * 128:(i + 1) * 128]
                if D == 4:
                    return ap2.rearrange("p (j r) -> p r j", r=4)[:, i // 4, (i % 4) * 128:(i % 4 + 1) * 128]
                return ap2.rearrange("p (j r) -> p r j", r=16)[:, i, :]

            def fm_chunk(wb, cc, kind, D, dest):
                if kind == "hy":
                    st = stf[cnt["stf"] % 2]
                    cnt["stf"] += 1
                else:
                    st = stg[cnt["stg"] % 3]
                    cnt["stg"] += 1
                for tt in range(4):
                    ps = pss[cnt["ps"] % 4]
                    cnt["ps"] += 1
                    for k in range(8):
                        P.mm(ps[:, :], wb[:, k, cc * 128:(cc + 1) * 128], uT[:, k, tt * 512:(tt + 1) * 512],
                             start=(k == 0), stop=(k == 7))
                    o = st[:, tt * 512:(tt + 1) * 512]
                    if kind == "hy":
                        P.copy("act", o, ps[:, :])
                    elif kind == "gate":
                        P.act(o, ps[:, :], AF.Sigmoid)
                    else:
                        j = cnt["tmp"] % 2
                        cnt["tmp"] += 1
                        tb_, a_, b_, p2 = tbs[j], tA[j], tB[j], ps2[j]
                        P.copy("act", tb_[:], ps[:, :])
                        P.mm(p2[:, :], swapm[:], tb_[:])
                        ts_ = slice(tt * 512, (tt + 1) * 512)
                        P.tt("dve", a_[:], ps[:, :], ropec[:, ts_], ALU.mult)
                        P.tt("dve", b_[:], p2[:, :], ropes[:, ts_], ALU.mult)
                        if D == 1:
                            P.tt("pool" if j else "dve", o, a_[:], b_[:], ALU.subtract)
                        else:
                            n_ = 512 // D
                            ov = st[:, :].rearrange("p (r j) -> p j r", r=D)[:, tt * n_:(tt + 1) * n_, :]
                            P.tt("pool" if j else "dve", ov, a_[:].rearrange("p (j r) -> p j r", r=D),
                                 b_[:].rearrange("p (j r) -> p j r", r=D), ALU.subtract)
                P.dma("pool" if kind == "rope" else "act", dest, st[:])

            def tm_block(wb, c0, ncols, D, dest):
                st = vst[cnt["v"] % 2]
                cnt["v"] += 1
                for i in range(16):
                    ps = pss[cnt["ps"] % 4]
                    cnt["ps"] += 1
                    for k in range(8):
                        P.mm(ps[:, 0:ncols], tok128(uT[:, k, :], i, D), wb[:, k, c0:c0 + ncols],
                             start=(k == 0), stop=(k == 7))
                    P.copy("act", st[:, i, 0:ncols], ps[:, 0:ncols])
                P.dma("act", dest.rearrange("(i p) c -> p i c", p=128), st[:, :, 0:ncols])

            DG = (1, 4, 16)
            for blk in range(30):
                wf, wb = wfs[blk % 2], wbs[blk % 2]
                col = blk * 256
                P.dma("sp", wf[:], self.I["w_in"][l][:, col:col + 256].rearrange("(k p) n -> p k n", p=128))
                P.copy("pool" if blk % 2 else "dve", wb[:], wf[:])
                if col < 1536:
                    nm = "qTa" if col < 768 else "kTa"
                    base = 0 if col < 768 else 768
                    for cc in range(2):
                        j = (col - base) // 128 + cc
                        fm_chunk(wb, cc, "rope", DG[j // 2], self.S[nm][j * 128:(j + 1) * 128, :])
                elif col < 2304:
                    g = (col - 1536) // 256
                    tm_block(wb, 0, 256, DG[g], self.S["va"][g])
                elif col < 3840:
                    for cc in range(2):
                        j = (col - 2304) // 128 + cc
                        fm_chunk(wb, cc, "hy", 1, self.S["hyT"][j * 128:(j + 1) * 128, :])
                elif col < 4352:
                    for cc in range(2):
                        j = (col - 3840) // 128 + cc
                        fm_chunk(wb, cc, "rope", 1, self.S["qTc"][j * 128:(j + 1) * 128, :])
                elif col < 4608:
                    fm_chunk(wb, 0, "rope", 1, self.S["kTc"][:, :])
                    tm_block(wb, 128, 128, 1, self.S["vc"])
                else:
                    for cc in range(2):
                        j = (col - 4608) // 128 + cc
                        fm_chunk(wb, cc, "gate", 1, self.S["gateT"][j * 128:(j + 1) * 128, :])


def _attn_finalize(B, sc, numer, nheads, sel, psB, dest):
    P = B.P
    recs = [sc.sb([64, 512], F32, "rec%d" % i) for i in range(2)]
    ysts = [sc.sb([64, T], BF16, "yst%d" % i) for i in range(2)]
    n = 0
    for h in range(nheads):
        yst = ysts[h % 2]
        for tt in range(4):
            ps = psB[n % 2]
            rec = recs[n % 2]
            n += 1
            P.mm(ps[0:64, :], sel[0:65, 0:64], numer[0:65, h, tt * 512:(tt + 1) * 512])
            P.op("dve", lambda e, o=rec[:], i=ps[0:64, :]: e.reciprocal(o, i), reads=[ps[0:64, :]], writes=[rec[:]])
            P.tt("pool", yst[:, tt * 512:(tt + 1) * 512], numer[0:64, h, tt * 512:(tt + 1) * 512], rec[:], ALU.mult)
        P.dma("pool", dest[h * 64:(h + 1) * 64, :], yst[:])


def st_attn_a(self, s, l):
    P = self.P
    with self.stage() as sc:
        mA = self.load_const(sc, "mA")
        sel = self.load_const(sc, "sel")
        numer = sc.sb([65, 4, T], F32, "numer")
        q = sc.sb([64, 4, T], BF16, "q")
        k = sc.sb([64, 4, T], BF16, "k")
        va = sc.sb([128, 16, 4, 65], BF16, "va")
        psS = [sc.ps([128, 512], F32, "psS%d" % i) for i in range(3)]
        psO = [sc.ps([128, 512], F32, "psO%d" % i) for i in range(2)]
        psB = [sc.ps([128, 512], F32, "psB%d" % i) for i in range(2)]
        pT = [sc.sb([128, 384], BF16, "pT%d" % i) for i in range(2)]
        pT2 = [sc.sb([128, 384], BF16, "pTm%d" % i) for i in range(3)]
        P.memset("pool", va[:], 1.0)
        n = 0
        for g, D in enumerate((1, 4, 16)):
            Ls = T // D
            nb = Ls // 128
            for h in range(4):
                hh = g * 4 + h
                P.dma("sp", q[:, h, :], self.S["qTa"][hh * 64:(hh + 1) * 64, :])
                P.dma("sp", k[:, h, :], self.S["kTa"][hh * 64:(hh + 1) * 64, :])
                P.dma("sp", va[:, :, h, 0:64],
                      self.S["va"][g][:, h * 64:(h + 1) * 64].rearrange("(i p) d -> p i d", p=128))
            jobs = [(h, rho, i) for h in range(4) for rho in range(D) for i in range(nb)]

            def s_phase(n, job):
                h, rho, i = job
                base = rho * Ls
                js = [j for j in (i - 1, i, i + 1) if 0 <= j < nb]
                ps = psS[n % 3]
                p1 = pT[n % 2]
                p2 = pT2[n % 3]
                q0 = base + i * 128
                for j in js:
                    sg = j - i + 1
                    P.mm(ps[:, sg * 128:(sg + 1) * 128], k[0:64, h, base + j * 128: base + (j + 1) * 128],
                         q[0:64, h, q0:q0 + 128])
                c0 = (js[0] - i + 1) * 128
                c1 = (js[-1] - i + 2) * 128
                P.act(p1[:, c0:c1], ps[:, c0:c1], AF.Exp, scale=0.125)
                P.tt("dve", p2[:, c0:c1], p1[:, c0:c1], mA[:, c0:c1], ALU.mult)

            def pv_phase(n, job):
                h, rho, i = job
                js = [j for j in (i - 1, i, i + 1) if 0 <= j < nb]
                po = psO[n % 2]
                p2 = pT2[n % 3]
                for jj, j in enumerate(js):
                    sg = j - i + 1
                    P.mm(po[0:65, 0:128], va[:, rho * nb + j, h, :], p2[:, sg * 128:(sg + 1) * 128],
                         start=(jj == 0), stop=(jj == len(js) - 1))
                if D == 1:
                    nv = numer[0:65, h, i * 128:(i + 1) * 128]
                else:
                    nv = numer[0:65, h, :].rearrange("p (j r) -> p r j", r=D)[:, rho, i * 128:(i + 1) * 128]
                if g == 0:
                    P.copy("act", nv, po[0:65, 0:128])
                else:
                    P.tt("dve", nv, po[0:65, 0:128], nv, ALU.add)
            for n_, job in enumerate(jobs):
                s_phase(n + n_, job)
                if n_ > 0:
                    pv_phase(n + n_ - 1, jobs[n_ - 1])
            pv_phase(n + len(jobs) - 1, jobs[-1])
            n += len(jobs)
        _attn_finalize(self, sc, numer, 4, sel, psB, self.S["yaT"])


def st_attn_c(self, s, l, with_hconv=False):
    P = self.P
    with self.stage() as sc:
        hgen = _hconv_body(self, sc, l) if with_hconv else None
        mC = self.load_const(sc, "mC")
        sel = self.load_const(sc, "sel")
        numer = sc.sb([65, 8, T], F32, "numerc")
        q = sc.sb([64, 8, T], BF16, "qc")
        k = sc.sb([64, 2, T], BF16, "kc")
        va = sc.sb([128, 16, 2, 65], BF16, "vca")
        sk = sc.sb([65, 8], F32, "sk")
        psS = [sc.ps([128, 512], F32, "psS%d" % i) for i in range(3)]
        psO = [sc.ps([128, 512], F32, "psO%d" % i) for i in range(2)]
        psB = [sc.ps([128, 512], F32, "psB%d" % i) for i in range(2)]
        pT = [sc.sb([128, 3, 512], BF16, "pTc%d" % i) for i in range(2)]
        P.memset("pool", va[:], 1.0)
        for h in range(8):
            P.dma("sp", q[:, h, :], self.S["qTc"][h * 64:(h + 1) * 64, :])
        for h in range(2):
            P.dma("sp", k[:, h, :], self.S["kTc"][h * 64:(h + 1) * 64, :])
            P.dma("sp", va[:, :, h, 0:64], self.S["vc"][:, h * 64:(h + 1) * 64].rearrange("(i p) d -> p i d", p=128))
        asink = self.I["attn_sink"][l]
        P.dma("sp", sk[64:65, :], cap(asink, int(asink.offset), [[0, 1], [1, 8]]))
        P.act(sk[64:65, :], sk[64:65, :], AF.Exp)
        n = 0
        mcb = mC[:]
        pst = mcb.ap[0][0]
        jobs = [(kv, i) for kv in range(2) for i in range(16)]

        def s_phase(n, job):
            kv, i = job
            js = [j for j in (i - 1, i, i + 1) if 0 <= j < 16]
            p1 = pT[n % 2]
            for j in js:
                sg = j - i + 1
                ps = psS[sg]
                P.mm(ps[:, :].rearrange("p (h t) -> p h t", h=4), k[0:64, kv, j * 128:(j + 1) * 128],
                     q[0:64, 4 * kv:4 * kv + 4, i * 128:(i + 1) * 128])
                P.act(p1[:, sg, :], ps[:, :], AF.Exp, scale=0.125)
                if sg != 1:
                    mv = cap(mcb, int(mcb.offset) + sg * 128, [[pst, 128], [0, 4], [1, 128]])
                    pv = p1[:, sg, :].rearrange("p (h t) -> p h t", h=4)
                    P.tt("pool" if sg == 0 else "dve", pv, pv, mv, ALU.mult)

        def pv_phase(n, job):
            kv, i = job
            js = [j for j in (i - 1, i, i + 1) if 0 <= j < 16]
            p1 = pT[n % 2]
            po = psO[n % 2]
            for jj, j in enumerate(js):
                sg = j - i + 1
                P.mm(po[0:65, :], va[:, j, kv, :], p1[:, sg, :], start=(jj == 0), stop=(jj == len(js) - 1))
            P.copy("act", numer[0:65, 4 * kv:4 * kv + 4, i * 128:(i + 1) * 128],
                   po[0:65, :].rearrange("p (h t) -> p h t", h=4))
        for n_, job in enumerate(jobs):
            s_phase(n_, job)
            if n_ > 0:
                pv_phase(n_ - 1, jobs[n_ - 1])
            if hgen is not None:
                try:
                    next(hgen)
                except StopIteration:
                    hgen = None
        pv_phase(len(jobs) - 1, jobs[-1])
        if hgen is not None:
            for _ in hgen:
                pass
        for h in range(8):
            P.ts("dve", numer[64:65, h, :], numer[64:65, h, :], sk[64:65, h:h + 1], None, op0=ALU.add)
        _attn_finalize(self, sc, numer, 8, sel, psB, self.S["ycT"])


Builder.st_attn_a = st_attn_a
Builder.st_attn_c = st_attn_c


def _filter_body(self, sc, l):
    P = self.P
    I = self.I
    if True:
        zembT = self.load_const(sc, "zembT")
        negt = self.load_const(sc, "negt")
        ones = self.load_const(sc, "ones")
        w1 = sc.sb([33, 64], F32, "w1")
        w2 = sc.sb([64, 64], F32, "w2")
        w3 = sc.sb([64, 2048], F32, "w3")
        P.dma("sp", w1[:], I["hy_w1"][l])
        P.dma("sp", w2[:], I["hy_w2"][l])
        P.dma("sp", w3[:], I["hy_w3"][l])
        prm = sc.sb([64, 4], F32, "prm")
        for j, ap1 in enumerate((I["hy_b1"][l], I["hy_b2"][l], I["hy_freq"][l][0], I["hy_freq"][l][1])):
            P.dma("sp", prm[:, j:j + 1], cap(ap1, int(ap1.offset), [[1, 64], [1, 1]]))
        fb = sc.sb([64, 2], F32, "fb")
        P.tt("dve", fb[:, 0:1], prm[:, 0:1], prm[:, 2:3], ALU.mult)
        P.tt("dve", fb[:, 1:2], prm[:, 1:2], prm[:, 3:4], ALU.mult)
        psF = sc.ps([128, 2048], F32, "psF")
        psN = sc.ps([128, 1024], F32, "psN")
        h1T = sc.sb([64, T], F32, "h1T")
        h2T = sc.sb([64, T], F32, "h2T")
        arg = sc.sb([64, 512], F32, "arg")
        ki = sc.sb([64, 512], I32, "ki")
        kf = sc.sb([64, 512], F32, "kf")
        mk = sc.sb([64, 512], F32, "mk")

        def sin_layer(dst, lhsT, src, kk, fcol, fbcol):
            for tt in range(4):
                ps = psF[0:64, tt * 512:(tt + 1) * 512]
                P.mm(ps, lhsT, src[0:kk, tt * 512:(tt + 1) * 512])
                P.ts("dve", arg[:], ps, prm[:, fcol:fcol + 1], fb[:, fbcol:fbcol + 1], op0=ALU.mult, op1=ALU.add)
                P.ts("dve", arg[:], arg[:], 1.0 / (2.0 * PI), None, op0=ALU.mult)
                P.copy("dve", ki[:], arg[:])
                P.copy("dve", kf[:], ki[:])
                P.tt("dve", arg[:], arg[:], kf[:], ALU.subtract)
                P.ts("dve", mk[:], arg[:], 0.5, None, op0=ALU.is_gt)
                P.tt("dve", arg[:], arg[:], mk[:], ALU.subtract)
                P.ts("dve", mk[:], arg[:], -0.5, None, op0=ALU.is_lt)
                P.tt("dve", arg[:], arg[:], mk[:], ALU.add)
                P.act(dst[:, tt * 512:(tt + 1) * 512], arg[:], AF.Sin, scale=6.28318)
                yield
        yield from sin_layer(h1T, w1[0:33, :], zembT, 33, 2, 0)
        yield from sin_layer(h2T, w2[0:64, :], h1T, 64, 3, 1)
        ld = self.bcast_row(sc, I["hy_log_decay"][l], 2048, "ld")
        P.act(ld[:], ld[:], AF.Exp)
        gsum = sc.sb([128, 16, 1024], BF16, "gsum")
        gdiff = sc.sb([128, 16, 1024], BF16, "gdiff")
        dec = sc.sb([128, 2048], F32, "dec")
        filt = sc.sb([128, 2048], F32, "filt")
        sq = dec
        for mc in range(16):
            for cb in range(4):
                P.mm(psF[:, cb * 512:(cb + 1) * 512], h2T[0:64, mc * 128:(mc + 1) * 128], w3[0:64, cb * 512:(cb + 1) * 512])
            P.act(dec[:], ld[:], AF.Exp, scale=negt[:, mc:mc + 1])
            P.tt("dve", filt[:], psF[:, :], dec[:], ALU.mult)
            if mc == 0:
                P.memset("dve", filt[0:1, 1024:2048], 0.0)
            P.act(sq[:], filt[:], AF.Square)
            for half in range(2):
                for d in range(2):
                    c0 = d * 1024 + half * 512
                    P.mm(psN[:, half * 512:(half + 1) * 512], ones[:, :], sq[:, c0:c0 + 512],
                         start=(mc == 0 and d == 0), stop=(mc == 15 and d == 1))
            P.tt("pool", gsum[:, mc, :], filt[:, 0:1024], filt[:, 1024:2048], ALU.add)
            P.tt("dve", gdiff[:, mc, :], filt[:, 0:1024], filt[:, 1024:2048], ALU.subtract)
            yield
        rs = sc.sb([128, 1024], F32, "rs")
        P.ts("dve", rs[:], psN[:, :], 1e-12, None, op0=ALU.add)
        P.act(rs[:], rs[:], AF.Sqrt)
        P.op("dve", lambda e: e.reciprocal(rs[:], rs[:]), reads=[rs[:]], writes=[rs[:]])
        cfs = [sc.sb([128, 16, 128], BF16, "cf%d" % i) for i in range(2)]
        sfs = [sc.sb([128, 16, 128], BF16, "sf%d" % i) for i in range(2)]
        ho = [[sc.sb([128, 512], BF16, "ho%d_%d" % (i, j)) for j in range(3)] for i in range(2)]
        hs = self.S["hspec%d" % l]
        n = 0
        for fc in range(16):
            cf, sf = cfs[fc % 2], sfs[fc % 2]
            P.dma("sp", cf[:], self.C["cf"][fc])
            P.dma("sp", sf[:], self.C["sf"][fc])
            for half in range(2):
                cs = slice(half * 512, (half + 1) * 512)
                pre, pim, px = psF[:, 0:512], psF[:, 512:1024], psF[:, 1024:1536]
                for mc in range(16):
                    P.mm(pre, cf[:, mc, :], gsum[:, mc, cs], start=(mc == 0), stop=(mc == 15))
                for mc in range(16):
                    P.mm(pim, sf[:, mc, :], gdiff[:, mc, cs], start=(mc == 0), stop=(mc == 15))
                if fc == 0:
                    for mc in range(16):
                        P.mm(px, sf[:, mc, :], gsum[:, mc, cs], start=(mc == 0), stop=(mc == 15))
                hre, him, hrb = ho[n % 2]
                n += 1
                P.tt("dve", hre[:], pre, rs[:, cs], ALU.mult)
                P.tt("dve", him[:], pim, rs[:, cs], ALU.mult)
                P.copy("pool", hrb[:], hre[:])
                if fc == 0:
                    P.memset("pool", him[0:1, :], 0.0)
                    P.tt("dve", hrb[0:1, :], px[0:1, :], rs[0:1, cs], ALU.mult)
                for j, t_ in enumerate((hre, him, hrb)):
                    P.dma("pool", hs[j, fc * 128:(fc + 1) * 128, cs], t_[:])
                yield


def _hconv_body(self, sc, l):
    P = self.P
    cw = sc.sb([128, 12, 3], F32, "cw")
    cb = sc.sb([128, 12], F32, "cb")
    for i in range(3):
        P.dma("sp", cw[:, :, i], self.I["conv_w"][l][i].rearrange("(k p) -> p k", p=128), allow_slow_non_contiguous=True)
    P.dma("sp", cb[:], self.I["conv_b"][l].rearrange("(k p) -> p k", p=128), allow_slow_non_contiguous=True)
    xs = [sc.sb([128, T], F32, "hx%d" % i) for i in range(2)]
    os_ = [sc.sb([128, T], F32, "ho%d" % i) for i in range(2)]
    for cc in range(12):
        x, o = xs[cc % 2], os_[cc % 2]
        P.dma("sp", x[:], self.S["hyT"][cc * 128:(cc + 1) * 128, :])
        yield
        P.act(o[:], x[:], AF.Identity, bias=cb[:, cc:cc + 1], scale=cw[:, cc, 1:2])
        P.stt("dve", o[:, 1:T], x[:, 0:T - 1], cw[:, cc, 0:1], o[:, 1:T], ALU.mult, ALU.add)
        P.stt("dve", o[:, 0:T - 1], x[:, 1:T], cw[:, cc, 2:3], o[:, 0:T - 1], ALU.mult, ALU.add)
        P.dma("pool", self.S["hcT"][cc * 128:(cc + 1) * 128, :], o[:])
        yield


def st_hconv(self, s, l):
    with self.stage() as sc:
        for _ in _hconv_body(self, sc, l):
            pass


def st_hyena(self, s, l, o):
    P = self.P
    zsrc = self.S["hcT"][0:512, :] if o == 0 else self.S["z1T"]
    gsrc = self.S["hcT"][(o + 1) * 512:(o + 2) * 512, :]
    hs = self.S["hspec%d" % l]
    with self.stage() as sc:
        identb = self.load_const(sc, "identb")
        hb = sc.sb([128, 4], F32, "hb")
        P.dma("sp", hb[:], self.I["hy_bias"][l][o].rearrange("(k p) -> p k", p=128), allow_slow_non_contiguous=True)
        zb = sc.sb([128, 4, T], BF16, "zb")
        zfs = [sc.sb([128, T], F32, "zf%d" % i) for i in range(2)]
        for cc in range(4):
            zf = zfs[cc % 2]
            P.dma("sp", zf[:], zsrc[cc * 128:(cc + 1) * 128, :])
            P.copy("pool" if cc % 2 else "dve", zb[:, cc, :], zf[:])
        zTM = sc.sb([128, 16, 512], BF16, "zTM")
        psT = [sc.ps([128, 1024], BF16, "psT%d" % i) for i in range(2)]
        for tc in range(16):
            pt = psT[tc % 2]
            for cc in range(4):
                P.tr(pt[:, cc * 128:(cc + 1) * 128], zb[:, cc, tc * 128:(tc + 1) * 128], identb[:])
            P.copy("act", zTM[:, tc, :], pt[:, 0:512])
        Yre = sc.sb([128, 16, 512], BF16, "Yre")
        Yim = sc.sb([128, 16, 512], BF16, "Yim")
        cfs = [sc.sb([128, 16, 128], BF16, "cf%d" % i) for i in range(2)]
        sfs = [sc.sb([128, 16, 128], BF16, "sf%d" % i) for i in range(2)]
        hts = [[sc.sb([128, 512], BF16, "h%d_%d" % (i, j)) for j in range(3)] for i in range(2)]
        tmps = [[sc.sb([128, 512], F32, "tm%d_%d" % (i, j)) for j in range(4)] for i in range(2)]
        psR = [sc.ps([128, 512], F32, "psR%d" % i) for i in range(2)]
        psI = [sc.ps([128, 512], F32, "psI%d" % i) for i in range(2)]
        cs = slice(o * 512, (o + 1) * 512)
        for fc in range(16):
            cf, sf = cfs[fc % 2], sfs[fc % 2]
            P.dma("sp", cf[:], self.C["cf"][fc])
            P.dma("sp", sf[:], self.C["sf"][fc])
            hre, him, hrb = hts[fc % 2]
            for j, t_ in enumerate((hre, him, hrb)):
                P.dma("sp", t_[:], hs[j, fc * 128:(fc + 1) * 128, cs])
            pr, pi = psR[fc % 2], psI[fc % 2]
            for tc in range(16):
                P.mm(pr[:, :], cf[:, tc, :], zTM[:, tc, :], start=(tc == 0), stop=(tc == 15))
            for tc in range(16):
                P.mm(pi[:, :], sf[:, tc, :], zTM[:, tc, :], start=(tc == 0), stop=(tc == 15))
            t1, t2, t3, t4 = tmps[fc % 2]
            P.tt("dve", t1[:], pr[:, :], hre[:], ALU.mult)
            P.tt("dve", t2[:], pi[:, :], him[:], ALU.mult)
            P.tt("dve", t3[:], pr[:, :], him[:], ALU.mult)
            P.tt("dve", t4[:], pi[:, :], hrb[:], ALU.mult)
            P.tt("pool", Yre[:, fc, :], t1[:], t2[:], ALU.subtract)
            P.tt("pool", Yim[:, fc, :], t3[:], t4[:], ALU.add)
        cis = [sc.sb([128, 16, 256], BF16, "ci%d" % i) for i in range(2)]
        sis = [sc.sb([128, 16, 256], BF16, "si%d" % i) for i in range(2)]
        psY = [sc.ps([128, 512], F32, "psY%d" % i) for i in range(2)]
        zps = [sc.sb([128, 256], F32, "zp%d" % i) for i in range(2)]
        gps = [sc.sb([128, 256], F32, "gp%d" % i) for i in range(2)]
        tms = [sc.sb([128, 256], F32, "tq%d" % i) for i in range(2)]
        odt = F32 if o == 0 else BF16
        ors = [sc.sb([128, 256], odt, "or%d" % i) for i in range(2)]
        dest = self.S["z1T"] if o == 0 else self.S["ybT"]
        n = 0
        for tt in range(8):
            ci, si = cis[tt % 2], sis[tt % 2]
            P.dma("sp", ci[:], self.C["ci"][tt])
            P.dma("sp", si[:], self.C["si"][tt])
            ts_ = slice(tt * 256, (tt + 1) * 256)
            for cc in range(4):
                py = psY[n % 2]
                zp, gp, tm, orr = zps[n % 2], gps[n % 2], tms[n % 2], ors[n % 2]
                n += 1
                P.dma("sp", zp[:], zsrc[cc * 128:(cc + 1) * 128, ts_])
                P.dma("sp", gp[:], gsrc[cc * 128:(cc + 1) * 128, ts_])
                for fc in range(16):
                    P.mm(py[:, 0:256], Yre[:, fc, cc * 128:(cc + 1) * 128], ci[:, fc, :], start=(fc == 0), stop=False)
                for fc in range(16):
                    P.mm(py[:, 0:256], Yim[:, fc, cc * 128:(cc + 1) * 128], si[:, fc, :], start=False, stop=(fc == 15))
                P.stt("dve", tm[:], zp[:], hb[:, cc:cc + 1], py[:, 0:256], ALU.mult, ALU.add)
                P.tt("pool", orr[:], tm[:], gp[:], ALU.mult)
                P.dma("pool", dest[cc * 128:(cc + 1) * 128, ts_], orr[:])


def st_filter(self, l):
    with self.stage() as sc:
        for _ in _filter_body(self, sc, l):
            pass


Builder.st_filter = st_filter
Builder.st_hconv = st_hconv
Builder.st_hyena = st_hyena

import os


class LNBufs:
    def __init__(self, sc):
        self.s1 = sc.sb([128, 1], F32, "ln_s1")
        self.nm = sc.sb([128, 1], F32, "ln_nm")
        self.ss = sc.sb([128, 1], F32, "ln_ss")
        self.rstd = sc.sb([128, 1], F32, "ln_rstd")
        self.sq = sc.sb([128, DM], F32, "ln_sq")
        self.y = [sc.sb([128, DM], F32, "ln_y%d" % i) for i in range(2)]
        self.n = 0


def ln_tile(P, lb, r, lng, lnb, dest):
    y = lb.y[lb.n % 2]
    lb.n += 1
    P.reduce("dve", lb.s1[:], r[:], ALU.add)
    P.ts("dve", lb.nm[:], lb.s1[:], -1.0 / DM, None, op0=ALU.mult)
    P.act(lb.sq[:], r[:], AF.Square, bias=lb.nm[:, 0:1])
    P.reduce("dve", lb.ss[:], lb.sq[:], ALU.add)
    P.ts("dve", lb.rstd[:], lb.ss[:], 1.0 / DM, LN_EPS_C, op0=ALU.mult, op1=ALU.add)
    P.act(lb.rstd[:], lb.rstd[:], AF.Sqrt)
    P.op("dve", lambda e: e.reciprocal(lb.rstd[:], lb.rstd[:]), reads=[lb.rstd[:]], writes=[lb.rstd[:]])
    P.ts("dve", y[:], r[:], lb.nm[:, 0:1], lb.rstd[:, 0:1], op0=ALU.add, op1=ALU.mult)
    P.tt("pool", y[:], y[:], lng[:], ALU.mult)
    P.tt("pool", y[:], y[:], lnb[:], ALU.add)
    P.dma("pool", dest, y[:])


def st_merge(self, s, l):
    P = self.P
    I = self.I
    xsrc = I["x"][s] if l == 0 else self.S["xres"][s]
    ada = self.S["ada%d" % l]
    with self.stage() as sc:
        ys = {}
        for nm, nk in (("yaT", 2), ("ybT", 4), ("ycT", 4)):
            t_ = sc.sb([128, nk, T], BF16, nm)
            for k in range(nk):
                P.dma("sp", t_[:, k, :], self.S[nm][k * 128:(k + 1) * 128, :])
            ys[nm] = t_
        wst = [sc.sb([128, DM], F32, "wst%d" % i) for i in range(2)]
        ws = {}
        n = 0
        for nm, nk in (("w_branch_a", 2), ("w_branch_b", 4), ("w_branch_c", 4), ("w_out", 8)):
            t_ = sc.sb([128, nk, DM], BF16, nm)
            for k in range(nk):
                st = wst[n % 2]
                P.dma("sp", st[:], I[nm][l][k * 128:(k + 1) * 128, :])
                P.copy("pool" if n % 2 else "dve", t_[:, k, :], st[:])
                n += 1
            ws[nm] = t_
        mergedT = sc.sb([128, 8, T], BF16, "mergedT")
        psb = [sc.ps([128, 512], F32, "psbr%d" % i) for i in range(3)]
        gts = [[sc.sb([128, 512], BF16, "g%d_%d" % (i, j)) for j in range(3)] for i in range(2)]
        ms = [[sc.sb([128, 512], F32, "m%d_%d" % (i, j)) for j in range(3)] for i in range(2)]
        n = 0
        for fc in range(8):
            for tt in range(4):
                ts_ = slice(tt * 512, (tt + 1) * 512)
                g3 = gts[n % 2]
                m3 = ms[n % 2]
                n += 1
                for j, (yn, wn, nk) in enumerate((("yaT", "w_branch_a", 2), ("ybT", "w_branch_b", 4), ("ycT", "w_branch_c", 4))):
                    for k in range(nk):
                        P.mm(psb[j][:, :], ws[wn][:, k, fc * 128:(fc + 1) * 128], ys[yn][:, k, ts_],
                             start=(k == 0), stop=(k == nk - 1))
                    P.dma("sp", g3[j][:], self.S["gateT"][j * 1024 + fc * 128: j * 1024 + (fc + 1) * 128, ts_])
                    P.tt("dve", m3[j][:], psb[j][:, :], g3[j][:], ALU.mult)
                P.tt("dve", m3[0][:], m3[0][:], m3[1][:], ALU.add)
                P.tt("pool", mergedT[:, fc, ts_], m3[0][:], m3[2][:], ALU.add)
        g1b = self.bcast_row(sc, ada[s, 2048:3072], 1024, "g1b")
        lng = self.bcast_row(sc, I["ln_g"][l, 0], 1024, "lng")
        lnb = self.bcast_row(sc, I["ln_b"][l, 0], 1024, "lnb")
        lb = LNBufs(sc)
        psM = [sc.ps([128, 1024], F32, "psM%d" % i) for i in range(2)]
        xts = [sc.sb([128, DM], F32, "xt%d" % i) for i in range(2)]
        tms = [sc.sb([128, DM], F32, "tm%d" % i) for i in range(2)]
        for i in range(16):
            pm = psM[i % 2]
            xt = xts[i % 2]
            tm = tms[i % 2]
            P.dma("sp", xt[:], xsrc[i * 128:(i + 1) * 128, :])
            for hf in range(2):
                for k in range(8):
                    P.mm(pm[:, hf * 512:(hf + 1) * 512], mergedT[:, k, i * 128:(i + 1) * 128],
                         ws["w_out"][:, k, hf * 512:(hf + 1) * 512], start=(k == 0), stop=(k == 7))
            P.tt("dve", tm[:], pm[:, :], g1b[:], ALU.mult)
            P.stt("dve", tm[:], xt[:], ALPHA_C, tm[:], ALU.mult, ALU.add)
            ln_tile(P, lb, tm, lng, lnb, self.S["xres"][s][i * 128:(i + 1) * 128, :])


def _uv_body(self, sc, l):
    P = self.P
    if True:
        identb = self.load_const(sc, "identb")
        ufs = [sc.sb([128, DM], F32, "uf%d" % i) for i in range(2)]
        vfs = [sc.sb([128, DM], F32, "vf%d" % i) for i in range(2)]
        ubs = [sc.sb([128, DM], BF16, "ub%d" % i) for i in range(2)]
        vbs = [sc.sb([128, DM], BF16, "vb%d" % i) for i in range(2)]
        uts = [sc.sb([128, 8, 128], BF16, "ut%d" % i) for i in range(2)]
        pst = [sc.ps([128, 1024], BF16, "pst%d" % i) for i in range(2)]
        U = self.I["peer_u"][l]
        V = self.I["peer_v"][l]
        for b in range(128):
            uf, vf, ub, vb, ut, pt = ufs[b % 2], vfs[b % 2], ubs[b % 2], vbs[b % 2], uts[b % 2], pst[b % 2]
            P.dma("sp", uf[:], cap(U, int(U.offset) + b * DM, [[128 * DM, 128], [1, DM]]))
            P.dma("sp", vf[:], cap(V, int(V.offset) + b * DM, [[128 * DM, 128], [1, DM]]))
            P.copy("dve", ub[:], uf[:])
            P.copy("pool", vb[:], vf[:])
            for k in range(8):
                P.tr(pt[:, k * 128:(k + 1) * 128], ub[:, k * 128:(k + 1) * 128], identb[:])
            P.copy("act", ut[:], pt[:, :].rearrange("p (k a) -> p k a", k=8))
            P.dma("act", self.S["ut%d" % l][b], ut[:])
            P.dma("pool", self.S["vb%d" % l][b], vb[:])
            yield


def st_peer_a(self, s, l):
    P = self.P
    I = self.I
    ada = self.S["ada%d" % l]
    with self.stage() as sc:
        identb = self.load_const(sc, "identb")
        identf = self.load_const(sc, "identf")
        iota16 = self.load_const(sc, "iota16")
        shb = self.bcast_row(sc, ada[s, 3072:4096], 1024, "sh2b")
        scb = self.bcast_row(sc, ada[s, 4096:5120], 1024, "sc2b")
        u2T = sc.sb([128, 8, T], BF16, "u2T")
        pst = sc.ps([128, 1024], BF16, "pst")
        self.make_uT(sc, self.S["xres"][s], scb, shb, u2T, identb, pst)
        P.dma("pool", self.S["u2T"], u2T[:])
        wq = sc.sb([128, 8, 2048], BF16, "wq")
        wst = [sc.sb([128, 2048], F32, "wqs%d" % i) for i in range(2)]
        for k in range(8):
            P.dma("sp", wst[k % 2][:], I["peer_wq"][l][k * 128:(k + 1) * 128, :])
            P.copy("pool" if k % 2 else "dve", wq[:, k, :], wst[k % 2][:])
        psK = sc.ps([128, 512], F32, "psK")
        keysT = sc.sb([128, 16, 128], BF16, "keysT")
        kst = [sc.sb([128, 128], F32, "kst%d" % i) for i in range(2)]
        for hp in range(16):
            P.dma("sp", kst[hp % 2][:], I["peer_keys"][l][hp // 2, hp % 2])
            P.tr(psK[:, 0:128], kst[hp % 2][:], identf[:])
            P.copy("act", keysT[:, hp, :], psK[:, 0:128])
        psQ = [sc.ps([128, 512], F32, "psQ%d" % i) for i in range(2)]
        psSc = sc.ps([128, 2048], F32, "psSc")
        qT = sc.sb([128, 16, 128], BF16, "qT")
        scs = sc.sb([128, 16, 128], F32, "scs")
        vals = sc.sb([128, 16, 16], F32, "vals")
        idxu = sc.sb([128, 16, 16], U32, "idxu")
        idxf = sc.sb([128, 16, 16], F32, "idxf")
        wk = sc.sb([128, 16, 128], F32, "wk")
        cand = sc.sb([128, 8, 256], F32, "cand")
        wk2 = sc.sb([128, 8, 256], F32, "wk2")
        best = sc.sb([128, 8, 16], F32, "best")
        cidx = sc.sb([128, 8, 16], U32, "cidx")
        iku = sc.sb([128, 128], U32, "iku")
        jku = sc.sb([128, 128], U32, "jku")
        ikf = sc.sb([128, 128], F32, "ikf")
        jkf = sc.sb([128, 128], F32, "jkf")
        eq = sc.sb([128, 8, 16, 16], F32, "eq")
        eq2 = sc.sb([128, 8, 16, 16], F32, "eq2")
        abg = sc.sb([128, 3, 128], F32, "abg")
        eg = sc.sb([128, 8, 16], F32, "eg")
        zz = sc.sb([128, 8], F32, "zz")
        abgT = [sc.sb([128, 3, 128], BF16, "abgT%d" % i) for i in range(2)]

        def top16b(items):
            for (vout, iout, src, scratch) in items:
                P.op("dve", lambda e, vout=vout, src=src: e.max(out=vout[:, 0:8], in_=src), reads=[src], writes=[vout[:, 0:8]])
            for (vout, iout, src, scratch) in items:
                P.op("dve", lambda e, vout=vout, iout=iout, src=src: e.max_index(out=iout[:, 0:8], in_max=vout[:, 0:8], in_values=src),
                     reads=[src, vout[:, 0:8]], writes=[iout[:, 0:8]])
            for (vout, iout, src, scratch) in items:
                P.op("dve", lambda e, vout=vout, src=src, scratch=scratch: e.match_replace(out=scratch, in_to_replace=vout[:, 0:8], in_values=src, imm_value=-1e30),
                     reads=[src, vout[:, 0:8]], writes=[scratch])
            for (vout, iout, src, scratch) in items:
                P.op("dve", lambda e, vout=vout, scratch=scratch: e.max(out=vout[:, 8:16], in_=scratch), reads=[scratch], writes=[vout[:, 8:16]])
            for (vout, iout, src, scratch) in items:
                P.op("dve", lambda e, vout=vout, iout=iout, scratch=scratch: e.max_index(out=iout[:, 8:16], in_max=vout[:, 8:16], in_values=scratch),
                     reads=[scratch, vout[:, 8:16]], writes=[iout[:, 8:16]])

        def pstr(t_):
            return t_[:].ap[0][0]

        for i in range(16):
            tsl = slice(i * 128, (i + 1) * 128)
            for hp in range(16):
                pq = psQ[(hp // 4) % 2]
                for k in range(8):
                    P.mm(pq[:, (hp % 4) * 128:(hp % 4 + 1) * 128], wq[:, k, hp * 128:(hp + 1) * 128], u2T[:, k, tsl],
                         start=(k == 0), stop=(k == 7))
                if hp % 4 == 3:
                    P.copy("act", qT[:, hp - 3:hp + 1, :], pq[:, :].rearrange("p (h t) -> p h t", h=4))
            for hp in range(16):
                P.mm(psSc[:, hp * 128:(hp + 1) * 128], qT[:, hp, :], keysT[:, hp, :])
            P.copy("act", scs[:], psSc[:, :].rearrange("p (h n) -> p h n", h=16))
            if os.environ.get('PA_SKIP'):
                continue
            top16b([(vals[:, hp, :], idxu[:, hp, :], scs[:, hp, :], wk[:, hp, :]) for hp in range(16)])
            P.copy("act", idxf[:], idxu[:])
            vb_ = vals[:]
            P.tt("pool", cand[:].rearrange("p h (i j) -> p h i j", i=16),
                 cap(vb_, int(vb_.offset), [[pstr(vals), 128], [32, 8], [1, 16], [0, 16]]),
                 cap(vb_, int(vb_.offset) + 16, [[pstr(vals), 128], [32, 8], [0, 16], [1, 16]]), ALU.add)
            top16b([(best[:, h, :], cidx[:, h, :], cand[:, h, :], wk2[:, h, :]) for h in range(8)])
            cflat = cidx[:].rearrange("p h k -> p (h k)")
            P.ts("dve", iku[:], cflat, 4, None, op0=ALU.logical_shift_right)
            P.ts("dve", jku[:], cflat, 15, None, op0=ALU.bitwise_and)
            P.copy("act", ikf[:], iku[:])
            P.copy("act", jkf[:], jku[:])
            io = iota16[:]
            iob = cap(io, int(io.offset), [[pstr(iota16), 128], [0, 8], [0, 16], [1, 16]])
            fb_ = idxf[:]
            for (kf_, off, col, e_) in ((ikf, 0, 0, eq), (jkf, 16, 1, eq2)):
                kb_ = kf_[:]
                P.tt("dve", e_[:], cap(kb_, int(kb_.offset), [[pstr(kf_), 128], [16, 8], [1, 16], [0, 16]]), iob, ALU.is_equal)
                P.tt("pool", e_[:], e_[:],
                     cap(fb_, int(fb_.offset) + off, [[pstr(idxf), 128], [32, 8], [0, 16], [1, 16]]), ALU.mult)
                P.reduce("dve", abg[:, col, :].rearrange("p (h k) -> p h k", h=8), e_[:], ALU.add)
            bb_ = best[:]
            P.tt("pool", eg[:], best[:], cap(bb_, int(bb_.offset), [[pstr(best), 128], [16, 8], [0, 16]]), ALU.subtract)
            P.act(eg[:], eg[:], AF.Exp)
            P.reduce("dve", zz[:], eg[:], ALU.add)
            P.op("dve", lambda e: e.reciprocal(zz[:], zz[:]), reads=[zz[:]], writes=[zz[:]])
            zb_ = zz[:]
            P.tt("pool", abg[:, 2, :].rearrange("p (h k) -> p h k", h=8), eg[:],
                 cap(zb_, int(zb_.offset), [[pstr(zz), 128], [1, 8], [0, 16]]), ALU.mult)
            at = abgT[i % 2]
            for j in range(3):
                P.tr(psK[:, j * 128:(j + 1) * 128], abg[:, j, :], identf[:])
            P.copy("act", at[:], psK[:, 0:384].rearrange("p (j t) -> p j t", j=3))
            for j, nm in enumerate(("pa", "pb", "pg")):
                P.dma("pool", self.S[nm][:, tsl], at[:, j, :])


def st_peer_b(self, s, l, a2=False):
    P = self.P
    I = self.I
    ada = self.S["ada%d" % l]
    dest = self.out[s] if l == 1 else self.S["xres"][s]
    with self.stage() as sc:
        iota = self.load_const(sc, "iota128")
        g2b = self.bcast_row(sc, ada[s, 5120:6144], 1024, "g2b")
        lng = self.bcast_row(sc, I["ln_g"][l, 1], 1024, "lng")
        lnb = self.bcast_row(sc, I["ln_b"][l, 1], 1024, "lnb")
        lb = LNBufs(sc)
        GTs = [sc.sb([128, 256, 64], BF16, "GT%d" % i) for i in range(2)]
        psBig = sc.ps([128, 2048], F32, "psBig")
        psA = [sc.ps([128, 512], F32, "psA%d" % i) for i in range(2)]
        psG = [sc.ps([128, 512], F32, "psG%d" % i) for i in range(2)]
        u2s = [sc.sb([128, 8, 256], BF16, "u2_%d" % i) for i in range(2)]
        abTs = [sc.sb([128, 3, 256], BF16, "abT%d" % i) for i in range(2)]
        OAs = [sc.sb([128, 16, 128], BF16, "OA%d" % i) for i in range(3)]
        OBs = [sc.sb([128, 16, 64], BF16, "OB%d" % i) for i in range(3)]
        uts = [sc.sb([128, 8, 128], BF16, "utb%d" % i) for i in range(5)]
        vbs = [sc.sb([128, DM], BF16, "vbb%d" % i) for i in range(5)]
        gls = [sc.sb([128, 256], F32, "gl%d" % i) for i in range(4)]
        Ws = [sc.sb([128, 256], BF16, "W%d" % i) for i in range(4)]
        xts = [sc.sb([128, DM], F32, "xt%d" % i) for i in range(1)] * 2
        tms = [sc.sb([128, DM], F32, "tm%d" % i) for i in range(1)] * 2
        io = iota[:]
        pio = io.ap[0][0]
        iob = cap(io, int(io.offset), [[pio, 128], [0, 16], [1, 128]])
        st = {"ne": 0, "nb": 0}

        def load_group(g):
            t0 = g * 256
            P.dma("sp", u2s[g % 2][:], self.S["u2T"][:, :, t0:t0 + 256])
            for j, nm in enumerate(("pa", "pb", "pg")):
                P.dma("sp", abTs[g % 2][:, j, :], self.S[nm][:, t0:t0 + 256])

        def gen_sub(g, half, sub):
            ab = abTs[g % 2][:]
            pab = ab.ap[0][0]
            OA, OB = OAs[sub % 3], OBs[sub % 3]
            av = cap(ab, int(ab.offset) + sub * 16, [[pab, 128], [1, 16], [0, 128]])
            bv = cap(ab, int(ab.offset) + 256 + sub * 16, [[pab, 128], [1, 16], [0, 64]])
            gv = cap(ab, int(ab.offset) + 512 + sub * 16, [[pab, 128], [1, 16], [0, 64]])
            iobh = cap(io, int(io.offset) + half * 64, [[pio, 128], [0, 16], [1, 64]])
            P.tt("dve", OB[:], iobh, bv, ALU.is_equal)
            P.tt("pool", OB[:], OB[:], gv, ALU.mult)
            P.tt("dve", OA[:], iob, av, ALU.is_equal)

        def mm_sub(g, half, sub):
            GT = GTs[half]
            OA, OB = OAs[sub % 3], OBs[sub % 3]
            for q in range(4):
                pg = psG[st["ne"] % 2]
                for t4 in range(4):
                    tk = q * 4 + t4
                    P.mm(pg[:, t4 * 64:(t4 + 1) * 64], OA[:, tk, :], OB[:, tk, :])
                tok = sub * 16 + q * 4
                P.copy("act", GT[:, tok:tok + 4, :],
                       pg[:, 0:256].rearrange("p (t b) -> p t b", t=4))
                st["ne"] += 1

        def build_sub(g, half, sub):
            gen_sub(g, half, sub)
            mm_sub(g, half, sub)

        def a_phase(g, b):
            u2 = u2s[g % 2]
            ut, vb = uts[b % 5], vbs[b % 5]
            P.dma("sp", ut[:], self.S["ut%d" % l][b])
            P.dma("sp", vb[:], self.S["vb%d" % l][b])
            pa_ = psA[b % 2][:, 0:256]
            for k in range(8):
                P.mm(pa_, ut[:, k, :], u2[:, k, :], start=(k == 0), stop=(k == 7))
            gl, W = gls[b % 4], Ws[b % 4]
            P.act(gl[:], pa_, AF.Gelu)
            P.tt("pool", W[:], gl[:], GTs[b // 64][:, :, b % 64], ALU.mult)

        def v_phase(b):
            vb = vbs[b % 5]
            W = Ws[b % 4]
            for ti in range(2):
                for hf in range(2):
                    o0 = ti * 1024 + hf * 512
                    P.mm(psBig[:, o0:o0 + 512], W[:, ti * 128:(ti + 1) * 128], vb[:, hf * 512:(hf + 1) * 512],
                         start=(b == 0), stop=(b == 127))

        a2gen = None
        if a2:
            bf = A2Bufs(self, sc)
            trp_ab = psG[0][:, 256:512]
            trp_g = psG[1][:, 256:384]

            def a2_pair(g2):
                for ti_ in (2 * g2, 2 * g2 + 1):
                    yield from _a2_tile(self, bf, ti_, trp_ab, trp_g)
        load_group(0)
        for sub in range(16):
            build_sub(0, 0, sub)
        for g in range(8):
            t0 = g * 256
            if a2 and a2gen is not None:
                for _ in a2gen:
                    pass
                a2gen = None
            if g + 1 < 8:
                load_group(g + 1)
            if a2 and g + 2 < 8:
                a2gen = a2_pair(g + 2)
            for b in range(128):
                if a2gen is not None and b % 2 == 0:
                    try:
                        next(a2gen)
                    except StopIteration:
                        a2gen = None
                a_phase(g, b)
                if b >= 2:
                    v_phase(b - 2)
                tgt = (g, 1) if b < 64 else ((g + 1, 0) if g + 1 < 8 else None)
                if tgt is not None:
                    if b % 4 == 1:
                        sub = (b % 64) // 4
                        if sub > 1:
                            mm_sub(tgt[0], tgt[1], sub - 2)
                        gen_sub(tgt[0], tgt[1], sub)
                    elif b % 64 == 62:
                        mm_sub(tgt[0], tgt[1], 14)
                    elif b % 64 == 63:
                        mm_sub(tgt[0], tgt[1], 15)
            v_phase(126)
            v_phase(127)
            for ti in range(2):
                xt, tm = xts[ti], tms[ti]
                rows = slice(t0 + ti * 128, t0 + (ti + 1) * 128)
                P.dma("sp", xt[:], self.S["xres"][s][rows, :])
                P.tt("dve", tm[:], psBig[:, ti * 1024:(ti + 1) * 1024], g2b[:], ALU.mult)
                P.stt("dve", tm[:], xt[:], ALPHA_C, tm[:], ALU.mult, ALU.add)
                ln_tile(P, lb, tm, lng, lnb, dest[rows, :])


Builder.st_merge = st_merge


def st_uv(self, l):
    with self.stage() as sc:
        for _ in _uv_body(self, sc, l):
            pass


def st_pre(self, l):
    with self.stage() as sc:
        ga = _filter_body(self, sc, l)
        gb = _uv_body(self, sc, l)
        da = db = False
        while not (da and db):
            if not da:
                try:
                    next(ga)
                except StopIteration:
                    da = True
            for _ in range(2):
                if not db:
                    try:
                        next(gb)
                    except StopIteration:
                        db = True


Builder.st_uv = st_uv
Builder.st_pre = st_pre
Builder.st_peer_a = st_peer_a
Builder.st_peer_b = st_peer_b


A2E = "dve"


class A2Bufs:
    def __init__(self, B, sc):
        self.identf = B.load_const(sc, "identf")
        self.iota16 = B.load_const(sc, "iota16")
        self.scs = sc.sb([128, 16, 128], F32, "a2scs")
        self.wk = sc.sb([128, 16, 128], F32, "a2wk")
        self.vals = sc.sb([128, 16, 16], F32, "a2vals")
        self.idxu = sc.sb([128, 16, 16], U32, "a2idxu")
        self.idxf = sc.sb([128, 16, 16], F32, "a2idxf")
        self.best = sc.sb([128, 8, 16], F32, "a2best")
        self.cidx = sc.sb([128, 8, 16], U32, "a2cidx")
        self.iku = sc.sb([128, 128], U32, "a2iku")
        self.jku = sc.sb([128, 128], U32, "a2jku")
        self.ikf = sc.sb([128, 128], F32, "a2ikf")
        self.jkf = sc.sb([128, 128], F32, "a2jkf")
        self.eq = sc.sb([128, 8, 16, 16], F32, "a2eq")
        self.abg = sc.sb([128, 3, 128], F32, "a2abg")
        self.eg = sc.sb([128, 8, 16], F32, "a2eg")
        self.zz = sc.sb([128, 8], F32, "a2zz")
        self.abgT = [sc.sb([128, 3, 128], BF16, "a2abgT%d" % i) for i in range(2)]


def _a2_tile(B, bf, i, trp_ab, trp_g):
    P = B.P
    scs, wk, vals, idxu, idxf = bf.scs, bf.wk, bf.vals, bf.idxu, bf.idxf
    best, cidx, eq, abg, eg, zz = bf.best, bf.cidx, bf.eq, bf.abg, bf.eg, bf.zz
    cand = scs[:].rearrange("p a b -> p (a b)").rearrange("p (h n) -> p h n", h=8)
    wk2 = wk[:].rearrange("p a b -> p (a b)").rearrange("p (h n) -> p h n", h=8)
    tsl = slice(i * 128, (i + 1) * 128)
    P.dma("sp", scs[:], B.S["scs"][tsl, :].rearrange("p (h n) -> p h n", h=16))

    def pstr(t_):
        return t_[:].ap[0][0]

    def top16b(items, nsplit):
        steps = []
        for (vout, iout, src, scratch) in items:
            steps.append((lambda e, vout=vout, src=src: e.max(out=vout[:, 0:8], in_=src), [src], [vout[:, 0:8]]))
        for (vout, iout, src, scratch) in items:
            steps.append((lambda e, vout=vout, iout=iout, src=src: e.max_index(out=iout[:, 0:8], in_max=vout[:, 0:8], in_values=src),
                          [src, vout[:, 0:8]], [iout[:, 0:8]]))
        for (vout, iout, src, scratch) in items:
            steps.append((lambda e, vout=vout, src=src, scratch=scratch: e.match_replace(out=scratch, in_to_replace=vout[:, 0:8], in_values=src, imm_value=-1e30),
                          [src, vout[:, 0:8]], [scratch]))
        for (vout, iout, src, scratch) in items:
            steps.append((lambda e, vout=vout, scratch=scratch: e.max(out=vout[:, 8:16], in_=scratch), [scratch], [vout[:, 8:16]]))
        for (vout, iout, src, scratch) in items:
            steps.append((lambda e, vout=vout, iout=iout, scratch=scratch: e.max_index(out=iout[:, 8:16], in_max=vout[:, 8:16], in_values=scratch),
                          [scratch, vout[:, 8:16]], [iout[:, 8:16]]))
        for n_, (fn, rd, wr) in enumerate(steps):
            P.op("dve", fn, reads=rd, writes=wr)
            if n_ % nsplit == nsplit - 1:
                yield

    yield
    yield from top16b([(vals[:, hp, :], idxu[:, hp, :], scs[:, hp, :], wk[:, hp, :]) for hp in range(16)], 8)
    P.copy("dve", idxf[:], idxu[:])
    vb_ = vals[:]
    c4 = cand.rearrange("p h (i j) -> p h i j", i=16)
    for hq in range(4):
        P.tt(A2E, c4[:, 2 * hq:2 * hq + 2],
             cap(vb_, int(vb_.offset) + 64 * hq, [[pstr(vals), 128], [32, 2], [1, 16], [0, 16]]),
             cap(vb_, int(vb_.offset) + 64 * hq + 16, [[pstr(vals), 128], [32, 2], [0, 16], [1, 16]]), ALU.add)
        yield
    yield from top16b([(best[:, h, :], cidx[:, h, :], cand[:, h, :], wk2[:, h, :]) for h in range(8)], 8)
    cflat = cidx[:].rearrange("p h k -> p (h k)")
    P.ts("dve", bf.iku[:], cflat, 4, None, op0=ALU.logical_shift_right)
    P.ts("dve", bf.jku[:], cflat, 15, None, op0=ALU.bitwise_and)
    P.copy("dve", bf.ikf[:], bf.iku[:])
    P.copy("dve", bf.jkf[:], bf.jku[:])
    yield
    io = bf.iota16[:]
    iob = cap(io, int(io.offset), [[pstr(bf.iota16), 128], [0, 8], [0, 16], [1, 16]])
    fb_ = idxf[:]
    for (kf_, off, col) in ((bf.ikf, 0, 0), (bf.jkf, 16, 1)):
        kb_ = kf_[:]
        iob2 = cap(io, int(io.offset), [[pstr(bf.iota16), 128], [0, 2], [0, 16], [1, 16]])
        for hq in range(4):
            e_ = eq[:, 2 * hq:2 * hq + 2]
            P.tt("dve", e_, cap(kb_, int(kb_.offset) + 32 * hq, [[pstr(kf_), 128], [16, 2], [1, 16], [0, 16]]), iob2, ALU.is_equal)
            P.tt(A2E, e_, e_,
                 cap(fb_, int(fb_.offset) + off + 64 * hq, [[pstr(idxf), 128], [32, 2], [0, 16], [1, 16]]), ALU.mult)
            yield
        P.reduce("dve", abg[:, col, :].rearrange("p (h k) -> p h k", h=8), eq[:], ALU.add)
        yield
    bb_ = best[:]
    P.tt(A2E, eg[:], best[:], cap(bb_, int(bb_.offset), [[pstr(best), 128], [16, 8], [0, 16]]), ALU.subtract)
    P.act(eg[:], eg[:], AF.Exp)
    P.reduce("dve", zz[:], eg[:], ALU.add)
    P.op("dve", lambda e: e.reciprocal(zz[:], zz[:]), reads=[zz[:]], writes=[zz[:]])
    zb_ = zz[:]
    P.tt(A2E, abg[:, 2, :].rearrange("p (h k) -> p h k", h=8), eg[:],
         cap(zb_, int(zb_.offset), [[pstr(zz), 128], [1, 8], [0, 16]]), ALU.mult)
    yield
    at = bf.abgT[i % 2]
    P.tr(trp_ab[:, 0:128], abg[:, 0, :], bf.identf[:])
    P.tr(trp_ab[:, 128:256], abg[:, 1, :], bf.identf[:])
    P.copy("act", at[:, 0:2, :], trp_ab.rearrange("p (j t) -> p j t", j=2))
    P.tr(trp_g, abg[:, 2, :], bf.identf[:])
    P.copy("act", at[:, 2, :], trp_g)
    for j, nm in enumerate(("pa", "pb", "pg")):
        P.dma("act", B.S[nm][:, tsl], at[:, j, :])
    yield


def st_peer_a1(self, s, l):
    P = self.P
    I = self.I
    ada = self.S["ada%d" % l]
    with self.stage() as sc:
        identb = self.load_const(sc, "identb")
        identf = self.load_const(sc, "identf")
        shb = self.bcast_row(sc, ada[s, 3072:4096], 1024, "sh2b")
        scb = self.bcast_row(sc, ada[s, 4096:5120], 1024, "sc2b")
        u2T = sc.sb([128, 8, T], BF16, "u2T")
        pst = sc.ps([128, 1024], BF16, "pst")
        self.make_uT(sc, self.S["xres"][s], scb, shb, u2T, identb, pst)
        P.dma("act", self.S["u2T"], u2T[:])
        wq = sc.sb([128, 8, 2048], BF16, "wq")
        wst = [sc.sb([128, 2048], F32, "wqs%d" % i) for i in range(2)]
        for k in range(8):
            P.dma("sp", wst[k % 2][:], I["peer_wq"][l][k * 128:(k + 1) * 128, :])
            P.copy("pool" if k % 2 else "dve", wq[:, k, :], wst[k % 2][:])
        psK = sc.ps([128, 512], F32, "psK")
        keysT = sc.sb([128, 16, 128], BF16, "keysT")
        kst = [sc.sb([128, 128], F32, "kst%d" % i) for i in range(2)]
        for hp in range(16):
            P.dma("sp", kst[hp % 2][:], I["peer_keys"][l][hp // 2, hp % 2])
            P.tr(psK[:, 0:128], kst[hp % 2][:], identf[:])
            P.copy("act", keysT[:, hp, :], psK[:, 0:128])
        psQ = [sc.ps([128, 512], F32, "psQ%d" % i) for i in range(2)]
        psSc = sc.ps([128, 2048], F32, "psSc")
        qTs = [sc.sb([128, 16, 128], BF16, "qT%d" % i) for i in range(2)]
        scss = [sc.sb([128, 16, 128], F32, "scs%d" % i) for i in range(2)]
        bf = A2Bufs(self, sc)
        trp_ab = psK[:, 0:256]
        trp_g = psK[:, 256:384]
        a2state = {"gen": None, "next": 0}

        def a2_hook(done_tiles):
            if a2state["gen"] is None and a2state["next"] < min(4, done_tiles):
                a2state["gen"] = _a2_tile(self, bf, a2state["next"], trp_ab, trp_g)
                a2state["next"] += 1
            if a2state["gen"] is not None:
                try:
                    next(a2state["gen"])
                except StopIteration:
                    a2state["gen"] = None
        for i in range(16):
            tsl = slice(i * 128, (i + 1) * 128)
            qT, scs = qTs[i % 2], scss[i % 2]
            for hp in range(16):
                a2_hook(i)
                pq = psQ[(hp // 4) % 2]
                for k in range(8):
                    P.mm(pq[:, (hp % 4) * 128:(hp % 4 + 1) * 128], wq[:, k, hp * 128:(hp + 1) * 128], u2T[:, k, tsl],
                         start=(k == 0), stop=(k == 7))
                if hp % 4 == 3:
                    P.copy("act" if (hp // 4) % 2 else "dve", qT[:, hp - 3:hp + 1, :], pq[:, :].rearrange("p (h t) -> p h t", h=4))
            for hp in range(16):
                P.mm(psSc[:, hp * 128:(hp + 1) * 128], qT[:, hp, :], keysT[:, hp, :])
            P.copy("act", scs[:, 0:8, :], psSc[:, 0:1024].rearrange("p (h n) -> p h n", h=8))
            P.copy("dve", scs[:, 8:16, :], psSc[:, 1024:2048].rearrange("p (h n) -> p h n", h=8))
            P.dma("act", self.S["scs"][tsl, :].rearrange("p (h n) -> p h n", h=16), scs[:])
        while a2state["gen"] is not None or a2state["next"] < 4:
            a2_hook(16)


Builder.st_peer_a1 = st_peer_a1


def build_all(B):
    for l in range(2):
        B.st_pre(l)
        B.st_ada(l)
    for s in range(2):
        for l in range(2):
            B.st_inproj(s, l)
            B.st_attn_a(s, l)
            B.st_attn_c(s, l, with_hconv=True)
            B.st_hyena(s, l, 0)
            B.st_hyena(s, l, 1)
            B.st_merge(s, l)
            B.st_peer_a1(s, l)
            B.st_peer_b(s, l, a2=True)


def kernel(**inputs):
    from concourse.bass_utils import run_bass_kernel_spmd
    inp = {k: np.ascontiguousarray(np.asarray(v, dtype=np.float32)) for k, v in inputs.items()}
    B = Builder()
    build_all(B)
    B.finish()
    ncores = 8
    maps = []
    for c in range(ncores):
        m = {"x": np.ascontiguousarray(inp["x"][2 * c:2 * c + 2]),
             "c": np.ascontiguousarray(inp["c"][2 * c:2 * c + 2])}
        for k in WSHAPES:
            m[k] = inp[k]
        for k, v in B.hc.items():
            m["k_" + k] = v
        maps.append(m)
    res = run_bass_kernel_spmd(B.nc, maps, core_ids=list(range(ncores)))
    out = np.concatenate([np.asarray(r["out"], dtype=np.float32) for r in res.results], axis=0)
    return out
```
